# Optimizing a Trainium2 kernel written in Bass

```python
import math
import jax, jax.numpy as jnp
from jax import lax
import numpy as np

D_MODEL = 1024
BATCH = 8
SEQ = 4096
DEPTH = 2

N_A_LAYERS = DEPTH // 2
N_B_LAYERS = DEPTH - N_A_LAYERS
S5_GROUP = 16
S5_GROUPS = D_MODEL // S5_GROUP
S5_STATE = 64
DT_MIN = 1e-3
DT_MAX = 1e-1
N_HEADS = 16
HEAD_DIM = D_MODEL // N_HEADS
Q_BLOCK = 128
N_EXPERT_GROUPS = 4
EXPERTS_PER_GROUP = 8
N_EXPERTS = N_EXPERT_GROUPS * EXPERTS_PER_GROUP
TOP_K = 2
D_EXPERT = D_MODEL // 4
EPS = 1e-6
NEG = -1e30

kernel_name = "yoco_s5_fox_hier_moe_adaln"


def rmsnorm(x, g):
    x32 = x.astype(jnp.float32)
    y = x32 * lax.rsqrt(jnp.mean(x32 * x32, axis=-1, keepdims=True) + EPS)
    return (y * g.astype(jnp.float32)).astype(x.dtype)


def adaln(c, w, b, n):
    mod = jax.nn.silu(c) @ w + b
    return jnp.split(mod, n, axis=-1)


def modulate(h, shift, scale):
    return h * (1.0 + scale[:, None, :]) + shift[:, None, :]


def _complex_combine(e1, e2):
    ar1, ai1, br1, bi1 = e1
    ar2, ai2, br2, bi2 = e2
    ar = ar2 * ar1 - ai2 * ai1
    ai = ar2 * ai1 + ai2 * ar1
    br = ar2 * br1 - ai2 * bi1 + br2
    bi = ar2 * bi1 + ai2 * br1 + bi2
    return (ar, ai, br, bi)


def s5_mixer(h, w_in, lam_re, lam_im, log_dt, b_re, b_im, c_re, c_im, d_skip, w_out):
    bsz, seq, _ = h.shape
    u = (h @ w_in).astype(jnp.float32)
    ug = u.reshape(bsz, seq, S5_GROUPS, S5_GROUP)
    dt = jnp.exp(log_dt.astype(jnp.float32))[:, None]
    lr = lam_re.astype(jnp.float32)
    li = lam_im.astype(jnp.float32)
    mag = jnp.exp(lr * dt)
    a_re = mag * jnp.cos(li * dt)
    a_im = mag * jnp.sin(li * dt)
    den = lr * lr + li * li
    coef_re = ((a_re - 1.0) * lr + a_im * li) / den
    coef_im = (a_im * lr - (a_re - 1.0) * li) / den
    br_, bi_ = b_re.astype(jnp.float32), b_im.astype(jnp.float32)
    bbar_re = coef_re[..., None] * br_ - coef_im[..., None] * bi_
    bbar_im = coef_re[..., None] * bi_ + coef_im[..., None] * br_
    bu_re = jnp.einsum('blgc,gpc->blgp', ug, bbar_re)
    bu_im = jnp.einsum('blgc,gpc->blgp', ug, bbar_im)
    shape_a = (1, seq, S5_GROUPS, S5_STATE)
    a_re_l = jnp.broadcast_to(a_re[None, None], shape_a)
    a_im_l = jnp.broadcast_to(a_im[None, None], shape_a)
    _, _, s_re, s_im = lax.associative_scan(_complex_combine, (a_re_l, a_im_l, bu_re, bu_im), axis=1)
    y = (jnp.einsum('blgp,gcp->blgc', s_re, c_re.astype(jnp.float32))
         - jnp.einsum('blgp,gcp->blgc', s_im, c_im.astype(jnp.float32)))
    y = y.reshape(bsz, seq, D_MODEL) + d_skip.astype(jnp.float32) * u
    y = jax.nn.gelu(y).astype(h.dtype)
    val, gate = jnp.split(y @ w_out, 2, axis=-1)
    return val * jax.nn.sigmoid(gate)


def shared_kv(x, c, kv_g, kv_ada_w, kv_ada_b, kv_w, kv_fb, k_norm_g):
    bsz, seq, _ = x.shape
    shift, scale = adaln(c, kv_ada_w, kv_ada_b, 2)
    h = modulate(rmsnorm(x, kv_g), shift, scale)
    k, v, fz = jnp.split(h @ kv_w, [D_MODEL, 2 * D_MODEL], axis=-1)
    k = rmsnorm(k.reshape(bsz, seq, N_HEADS, HEAD_DIM), k_norm_g).transpose(0, 2, 1, 3)
    v = v.reshape(bsz, seq, N_HEADS, HEAD_DIM).transpose(0, 2, 1, 3)
    logf = jax.nn.log_sigmoid(fz.astype(jnp.float32) + kv_fb.astype(jnp.float32))
    fcum = jnp.cumsum(logf, axis=1).transpose(0, 2, 1)
    return k, v, fcum


def fox_mixer(h, w_qg, q_norm_g, w_o, k, v, fcum):
    bsz, seq, _ = h.shape
    q, og = jnp.split(h @ w_qg, 2, axis=-1)
    q = rmsnorm(q.reshape(bsz, seq, N_HEADS, HEAD_DIM), q_norm_g).transpose(0, 2, 1, 3)
    scale = HEAD_DIM ** -0.5
    outs = []
    for i in range(seq // Q_BLOCK):
        s0 = i * Q_BLOCK
        s1 = s0 + Q_BLOCK
        qb = q[:, :, s0:s1]
        kb = k[:, :, :s1]
        vb = v[:, :, :s1]
        logits = (jnp.einsum('bhqd,bhkd->bhqk', qb, kb).astype(jnp.float32) * scale
                  + fcum[:, :, s0:s1, None] - fcum[:, :, None, :s1])
        mask = (s0 + jnp.arange(Q_BLOCK))[:, None] >= jnp.arange(s1)[None, :]
        p = jax.nn.softmax(jnp.where(mask, logits, NEG), axis=-1)
        outs.append(jnp.einsum('bhqk,bhkd->bhqd', p.astype(vb.dtype), vb))
    o = jnp.concatenate(outs, axis=2).transpose(0, 2, 1, 3).reshape(bsz, seq, D_MODEL)
    return (o * jax.nn.sigmoid(og)) @ w_o


def hier_moe(h, wg, bg, we, be, w1, w3, w2):
    bsz, seq, d = h.shape
    t = h.reshape(-1, d)
    g_logits = (t @ wg + bg).astype(jnp.float32)
    g_prob = jax.nn.softmax(g_logits, axis=-1)
    g_top_v, g_idx = lax.top_k(g_logits, 1)
    p_g = jnp.take_along_axis(g_prob, g_idx, axis=1)
    e_logits = (t @ we + be).astype(jnp.float32).reshape(-1, N_EXPERT_GROUPS, EXPERTS_PER_GROUP)
    e_sel = jnp.take_along_axis(e_logits, g_idx[:, :, None], axis=1)[:, 0]
    top_v, top_i = lax.top_k(e_sel, TOP_K)
    w = jax.nn.softmax(top_v, axis=-1) * p_g
    eid = g_idx * EXPERTS_PER_GROUP + top_i
    gates = jnp.sum(jax.nn.one_hot(eid, N_EXPERTS, dtype=jnp.float32) * w[..., None], axis=1)
    out = jnp.zeros(t.shape, jnp.float32)
    for e in range(N_EXPERTS):
        y = (jax.nn.silu(t @ w1[e]) * (t @ w3[e])) @ w2[e]
        out = out + gates[:, e:e + 1] * y.astype(jnp.float32)
    return out.astype(h.dtype).reshape(bsz, seq, d)


def setup_inputs(seed: int = 0) -> dict:
    key = jax.random.key(seed)
    ks = iter(jax.random.split(key, 40))
    f32 = jnp.float32

    def nrm(shape, std):
        return std * jax.random.normal(next(ks), shape, f32)

    D, G, P, GC = D_MODEL, S5_GROUPS, S5_STATE, S5_GROUP
    H, HD, E, F, NG = N_HEADS, HEAD_DIM, N_EXPERTS, D_EXPERT, N_EXPERT_GROUPS
    NA, NB = N_A_LAYERS, N_B_LAYERS
    x = nrm((BATCH, SEQ, D), 1.0)
    c = nrm((BATCH, D), 1.0)
    ln_g = 1.0 + nrm((DEPTH, 2, D), 0.02)
    ada_w = nrm((DEPTH, 2, D, 3 * D), 0.5 * D ** -0.5)
    ada_b = nrm((DEPTH, 2, 3 * D), 0.02)
    s5_w_in = nrm((NA, D, D), D ** -0.5)
    n_idx = jnp.arange(P, dtype=f32)
    s5_lambda_re = -0.5 * jnp.exp(nrm((NA, G, P), 0.05))
    s5_lambda_im = math.pi * n_idx + nrm((NA, G, P), 0.01)
    s5_log_dt = jax.random.uniform(next(ks), (NA, G), f32, math.log(DT_MIN), math.log(DT_MAX))
    s5_b_re = nrm((NA, G, P, GC), (2 * GC) ** -0.5)
    s5_b_im = nrm((NA, G, P, GC), (2 * GC) ** -0.5)
    s5_c_re = nrm((NA, G, GC, P), P ** -0.5)
    s5_c_im = nrm((NA, G, GC, P), P ** -0.5)
    s5_d = nrm((NA, D), 0.5)
    s5_w_out = nrm((NA, D, 2 * D), D ** -0.5)
    kv_g = 1.0 + nrm((D,), 0.02)
    kv_ada_w = nrm((D, 2 * D), 0.5 * D ** -0.5)
    kv_ada_b = nrm((2 * D,), 0.02)
    kv_w = nrm((D, 2 * D + H), D ** -0.5)
    kv_fb = jax.random.uniform(next(ks), (H,), f32, 1.0, 6.0)
    k_norm_g = 1.0 + nrm((HD,), 0.02)
    fox_w_qg = nrm((NB, D, 2 * D), D ** -0.5)
    fox_q_norm_g = 1.0 + nrm((NB, HD), 0.02)
    fox_w_o = nrm((NB, D, D), D ** -0.5)
    moe_wg = nrm((DEPTH, D, NG), D ** -0.5)
    moe_bg = nrm((DEPTH, NG), 0.01)
    moe_we = nrm((DEPTH, D, E), D ** -0.5)
    moe_be = nrm((DEPTH, E), 0.01)
    moe_w1 = nrm((DEPTH, E, D, F), D ** -0.5)
    moe_w3 = nrm((DEPTH, E, D, F), D ** -0.5)
    moe_w2 = nrm((DEPTH, E, F, D), F ** -0.5)
    return {"x": x, "c": c, "ln_g": ln_g, "ada_w": ada_w, "ada_b": ada_b,
            "s5_w_in": s5_w_in, "s5_lambda_re": s5_lambda_re, "s5_lambda_im": s5_lambda_im,
            "s5_log_dt": s5_log_dt, "s5_b_re": s5_b_re, "s5_b_im": s5_b_im,
            "s5_c_re": s5_c_re, "s5_c_im": s5_c_im, "s5_d": s5_d, "s5_w_out": s5_w_out,
            "kv_g": kv_g, "kv_ada_w": kv_ada_w, "kv_ada_b": kv_ada_b, "kv_w": kv_w,
            "kv_fb": kv_fb, "k_norm_g": k_norm_g,
            "fox_w_qg": fox_w_qg, "fox_q_norm_g": fox_q_norm_g, "fox_w_o": fox_w_o,
            "moe_wg": moe_wg, "moe_bg": moe_bg, "moe_we": moe_we, "moe_be": moe_be,
            "moe_w1": moe_w1, "moe_w3": moe_w3, "moe_w2": moe_w2}


def reference(x, c, ln_g, ada_w, ada_b,
              s5_w_in, s5_lambda_re, s5_lambda_im, s5_log_dt, s5_b_re, s5_b_im,
              s5_c_re, s5_c_im, s5_d, s5_w_out,
              kv_g, kv_ada_w, kv_ada_b, kv_w, kv_fb, k_norm_g,
              fox_w_qg, fox_q_norm_g, fox_w_o,
              moe_wg, moe_bg, moe_we, moe_be, moe_w1, moe_w3, moe_w2):
    h = x
    k = v = fcum = None
    for l in range(DEPTH):
        shift, scale, gate = adaln(c, ada_w[l, 0], ada_b[l, 0], 3)
        hn = modulate(rmsnorm(h, ln_g[l, 0]), shift, scale)
        if l < N_A_LAYERS:
            mix = s5_mixer(hn, s5_w_in[l], s5_lambda_re[l], s5_lambda_im[l], s5_log_dt[l],
                           s5_b_re[l], s5_b_im[l], s5_c_re[l], s5_c_im[l], s5_d[l], s5_w_out[l])
        else:
            j = l - N_A_LAYERS
            mix = fox_mixer(hn, fox_w_qg[j], fox_q_norm_g[j], fox_w_o[j], k, v, fcum)
        h = h + gate[:, None, :] * mix
        shift, scale, gate = adaln(c, ada_w[l, 1], ada_b[l, 1], 3)
        hn = modulate(rmsnorm(h, ln_g[l, 1]), shift, scale)
        h = h + gate[:, None, :] * hier_moe(hn, moe_wg[l], moe_bg[l], moe_we[l], moe_be[l],
                                            moe_w1[l], moe_w3[l], moe_w2[l])
        if l == N_A_LAYERS - 1:
            k, v, fcum = shared_kv(h, c, kv_g, kv_ada_w, kv_ada_b, kv_w, kv_fb, k_norm_g)
    return h
```

```python
import math
from contextlib import ExitStack
import numpy as np
import concourse.bass as bass
import concourse.mybir as mybir
from concourse.bass_utils import run_bass_kernel_spmd

F32 = mybir.dt.float32
BF16 = mybir.dt.bfloat16
AF = mybir.ActivationFunctionType
ALU = mybir.AluOpType
AX = mybir.AxisListType

D = 1024
L = 4096
NCH = 8
TT = 512
NTT = L // TT
NG = 4
NE = 32
FE = 256
H = 16
HD = 64
EPS = 1e-6
MAGIC = 12582912.0
S2PI = 6.283180
HALFPI = 1.570795
GELU_C = 2.0 * math.sqrt(2.0 / math.pi)


class Trk:
    __slots__ = ("w", "r")

    def __init__(self):
        self.w = {}
        self.r = {}


class Ctx:
    def __init__(self, nc, es):
        self.nc = nc
        self.es = es
        self.engs = {"pe": nc.tensor, "act": nc.scalar, "dve": nc.vector, "pool": nc.gpsimd, "sp": nc.sync}
        self.sems = {}
        self.cnt = {}
        for k in ["pe", "act", "dve", "pool"]:
            self.sems[k] = es.enter_context(nc.semaphore("s_" + k))
            self.cnt[k] = 0
        self.seen = {k: {} for k in self.engs}
        self.dpool = {}
        self.dnext = {}
        for q, n in [("sp", 24), ("pool", 16), ("act", 6)]:
            keys = []
            for i in range(n):
                key = "d_%s%d" % (q, i)
                self.sems[key] = es.enter_context(nc.semaphore(key))
                self.cnt[key] = 0
                keys.append(key)
            self.dpool[q] = keys
            self.dnext[q] = 0

    def _wait(self, eng, deps):
        seen = self.seen[eng]
        for key, val in deps.items():
            if eng == "pe" and key == "pe":
                continue
            if seen.get(key, 0) < val:
                self.engs[eng].wait_ge(self.sems[key], val)
                seen[key] = val

    @staticmethod
    def _merge(dst, src):
        for k, v in src.items():
            if dst.get(k, 0) < v:
                dst[k] = v

    def _deps(self, reads, writes):
        deps = {}
        for t in reads:
            self._merge(deps, t.w)
        for t in writes:
            self._merge(deps, t.w)
            self._merge(deps, t.r)
        return deps

    def _record(self, ev, reads, writes):
        k, v = ev
        for t in reads:
            if t.r.get(k, 0) < v:
                t.r[k] = v
        for t in writes:
            t.w = {k: v}
            t.r = {}

    def op(self, eng, fn, reads=(), writes=()):
        self._wait(eng, self._deps(reads, writes))
        ins = fn(self.engs[eng])
        self.cnt[eng] += 1
        ins.then_inc(self.sems[eng], 1)
        self._record((eng, self.cnt[eng]), reads, writes)

    def dma(self, q, out, in_, reads=(), writes=(), **kw):
        keys = self.dpool[q]
        key = keys[self.dnext[q] % len(keys)]
        self.dnext[q] += 1
        deps = self._deps(reads, writes)
        if self.cnt[key] > 0:
            deps[key] = max(deps.get(key, 0), self.cnt[key])
        self._wait(q, deps)
        ins = self.engs[q].dma_start(out=out, in_=in_, **kw)
        self.cnt[key] += 16
        ins.then_inc(self.sems[key], 16)
        self._record((key, self.cnt[key]), reads, writes)

    def barrier(self, engines=("pe", "act", "dve", "pool", "sp")):
        allev = {k: v for k, v in self.cnt.items() if v > 0}
        for e in engines:
            self._wait(e, allev)


class _Stop(Exception):
    pass


def build(debug=False, stop=None):
    try:
        return _build(debug, stop)
    except _Stop as e:
        return e.args[0]


def _build(debug, stop):
    nc = bass.Bass("TRN2", target_bir_lowering=False)
    okind = "ExternalOutput" if debug else "Internal"

    def din(name, shape, dt=F32):
        return nc.dram_tensor(name, list(shape), dt, kind="ExternalInput").ap()

    def dscr(name, shape, dt=F32, out=False):
        return nc.dram_tensor(name, list(shape), dt, kind=("ExternalOutput" if out else okind)).ap()

    xT = din("xT", [NCH, 128, L])
    c_col = din("c_col", [128, NCH])
    ada_w = din("ada_w", [2, 2, D, 3 * D])
    ada_b = din("ada_b", [4, 128, 24])
    ln_g = din("ln_g", [4, 128, NCH])
    kv_ada_w = din("kv_ada_w", [D, 2 * D])
    kv_ada_b = din("kv_ada_b", [128, 16])
    kv_g = din("kv_g", [128, NCH])
    s5_w_in = din("s5_w_in", [D, D])
    s5_w_out = din("s5_w_out", [D, 2 * D])
    s5_par = din("s5_par", [3, 128, 32])
    s5_bpad = din("s5_bpad", [2, 128, 32, 128])
    s5_cpad = din("s5_cpad", [2, 128, 32, 128])
    s5_d = din("s5_d", [128, NCH])
    iota_t = din("iota_t", [128, L])
    ident = din("ident", [128, 128])
    hout = dscr("hout", [NCH, 128, L], out=True)
    moe_w1 = din("moe_w1", [2, NE, D, FE])
    moe_w3 = din("moe_w3", [2, NE, D, FE])
    moe_w2 = din("moe_w2", [2, NE, FE, D])
    moe_wr = din("moe_wr", [2, D, 36])
    moe_br = din("moe_br", [2, 128, 36])
    sel_c = din("sel_c", [32, NE, 128])
    h2 = dscr("h2", [NCH, 128, L])
    kv_w = din("kv_w", [D, 2 * D + H])
    gk_col = din("gk_col", [128, 1])
    gq_col = din("gq_col", [128, 1])
    fb_col = din("fb_col", [H, 1])
    blk_c = din("blk_c", [128, 128])
    tri_c = din("tri_c", [128, 128])
    fox_w_qg = din("fox_w_qg", [D, 2 * D])
    fox_w_o = din("fox_w_o", [D, D])
    Kd = dscr("Kd", [NCH, 128, L], BF16)
    Vd = dscr("Vd", [L, D], BF16)
    Fq = dscr("Fq", [H, 3, L], BF16)
    Fk = dscr("Fk", [H, 3, L], BF16)
    Qd = dscr("Qd", [NCH, 128, L], BF16)
    SGd = dscr("SGd", [NCH, 128, L], BF16)
    Od = dscr("Od", [NCH, 128, L], BF16)
    h3 = dscr("h3", [NCH, 128, L])

    h1 = dscr("h1", [NCH, 128, L])
    Gd = dscr("Gd", [NCH, 128, L], BF16)
    moddbg = dscr("moddbg", [128, 24 * 4 + 16])
    udbg = dscr("udbg", [NCH, 128, L], BF16) if debug else None

    es = ExitStack()
    cx = Ctx(nc, es)

    def chk(name):
        if stop == name:
            cx.barrier()
            raise _Stop(nc, es)

    _uid = [0]

    def sb(st, name, shape, dt=F32):
        _uid[0] += 1
        return st.enter_context(nc.sbuf_tensor("%s_%d" % (name, _uid[0]), list(shape), dt))

    ps = [es.enter_context(nc.psum_tensor("ps%d" % i, [128, 512], F32)) for i in range(8)]
    pst = [Trk() for _ in range(8)]

    mods = sb(es, "mods", [128, 24 * 4 + 16])
    modA = sb(es, "modA", [128, 5, NCH])
    t_mods = Trk()
    t_modA = Trk()
    identf = sb(es, "identf", [128, 128])
    onesf = sb(es, "onesf", [128, 128])
    t_const = Trk()
    cx.dma("sp", identf[:], ident[:, :], writes=[t_const])
    cx.op("dve", lambda e: e.memset(onesf[:], 1.0), writes=[t_const])

    with ExitStack() as st:
        ccol = sb(st, "ccol", [128, NCH])
        sc = sb(st, "sc", [128, NCH])
        bias_all = sb(st, "bias_all", [128, 24 * 4 + 16])
        g_all = sb(st, "g_all", [128, 5, NCH])
        wbuf = [sb(st, "wbuf%d" % i, [128, NCH, 1536]) for i in range(2)]
        t_w = [Trk(), Trk()]
        t_c = Trk()
        t_b = Trk()
        cx.dma("sp", ccol[:], c_col[:, :], writes=[t_c])
        for i in range(4):
            cx.dma("sp", bias_all[:, 24 * i:24 * (i + 1)], ada_b[i], writes=[t_b])
            cx.dma("sp", g_all[:, i, :], ln_g[i], writes=[t_b])
        cx.dma("sp", bias_all[:, 96:112], kv_ada_b[:, :], writes=[t_b])
        cx.dma("sp", g_all[:, 4, :], kv_g[:, :], writes=[t_b])
        cx.op("act", lambda e: e.activation(out=sc[:], in_=ccol[:], func=AF.Silu), reads=[t_c], writes=[t_c])
        units = []
        for i in range(4):
            for hf in range(2):
                units.append((ada_w[i // 2, i % 2], hf * 1536, 1536, 24 * i + 12 * hf))
        units.append((kv_ada_w, 0, 1024, 96))
        units.append((kv_ada_w, 1024, 1024, 104))
        for ui, (wap, c0, ncol, mcol) in enumerate(units):
            wb = wbuf[ui % 2]
            tw = t_w[ui % 2]
            src = wap.rearrange("(c p) n -> p c n", p=128)
            for kc in range(NCH):
                cx.dma("sp" if kc % 2 == 0 else "pool", wb[:, kc, 0:ncol], src[:, kc, c0:c0 + ncol], writes=[tw])
            pidx = ui % 2
            for n in range(ncol // 128):
                for kc in range(NCH):
                    cx.op("pe", lambda e, wb=wb, n=n, kc=kc, pidx=pidx: e.matmul(
                        ps[pidx][:, n:n + 1], lhsT=wb[:, kc, n * 128:(n + 1) * 128], rhs=sc[:, kc:kc + 1],
                        start=(kc == 0), stop=(kc == NCH - 1)), reads=[tw, t_c], writes=[pst[pidx]])
            nn = ncol // 128
            cx.op("dve", lambda e, pidx=pidx, nn=nn, mcol=mcol: e.tensor_tensor(
                out=mods[:, mcol:mcol + nn], in0=ps[pidx][:, 0:nn], in1=bias_all[:, mcol:mcol + nn], op=ALU.add),
                reads=[pst[pidx], t_b], writes=[t_mods])
        for i in range(5):
            sc0 = 24 * i + 8
            cx.op("dve", lambda e, i=i, sc0=sc0: e.scalar_tensor_tensor(
                out=modA[:, i, :], in0=mods[:, sc0:sc0 + 8], scalar=1.0, in1=g_all[:, i, :],
                op0=ALU.add, op1=ALU.mult), reads=[t_mods, t_b], writes=[t_modA])
        if debug:
            cx.dma("sp", moddbg[:, :], mods[:], reads=[t_mods])
        cx.barrier()

    chk("s0")

    def shiftc(i, c):
        return mods[:, 24 * i + c:24 * i + c + 1]

    def gatec(i, c):
        return mods[:, 24 * i + 16 + c:24 * i + 16 + c + 1]

    def scaleA(i, c):
        return modA[:, i, c:c + 1]

    def norm_mod(i, htile, t_h, sq, t_sq, rinv, t_rinv, tmp, t_tmp, hn_bf, t_hn, psi, hn_f=None, t_hnf=None):
        cx.op("act", lambda e: e.activation(out=sq[:], in_=htile[:], func=AF.Square), reads=[t_h], writes=[t_sq])
        for c in range(NCH):
            cx.op("pe", lambda e, c=c: e.matmul(ps[psi][:, :], lhsT=onesf[:], rhs=sq[:, c, :],
                                                start=(c == 0), stop=(c == NCH - 1)),
                  reads=[t_sq, t_const], writes=[pst[psi]])
        cx.op("act", lambda e: e.activation(out=rinv[:], in_=ps[psi][:, :], func=AF.Sqrt, bias=epsc[:, 0:1],
                                            scale=1.0 / D), reads=[pst[psi], t_const], writes=[t_rinv])
        cx.op("dve", lambda e: e.reciprocal(out=rinv[:], in_=rinv[:]), reads=[t_rinv], writes=[t_rinv])
        for c in range(NCH):
            cx.op("dve", lambda e, c=c: e.scalar_tensor_tensor(
                out=tmp[:, c, :], in0=htile[:, c, :], scalar=scaleA(i, c), in1=rinv[:],
                op0=ALU.mult, op1=ALU.mult), reads=[t_h, t_rinv, t_modA], writes=[t_tmp])
            if hn_f is not None:
                cx.op("act", lambda e, c=c: e.activation(out=hn_f[:, c, :], in_=tmp[:, c, :], func=AF.Identity,
                                                        bias=shiftc(i, c), scale=1.0),
                      reads=[t_tmp, t_mods], writes=[t_hnf])
                cx.op("pool", lambda e, c=c: e.tensor_copy(out=hn_bf[:, c, :], in_=hn_f[:, c, :]),
                      reads=[t_hnf], writes=[t_hn])
            else:
                cx.op("act", lambda e, c=c: e.activation(out=hn_bf[:, c, :], in_=tmp[:, c, :], func=AF.Identity,
                                                        bias=shiftc(i, c), scale=1.0),
                      reads=[t_tmp, t_mods], writes=[t_hn])

    epsc = sb(es, "epsc", [128, 4])
    cx.op("dve", lambda e: e.memset(epsc[:, 0:1], EPS), writes=[t_const])
    cx.op("dve", lambda e: e.memset(epsc[:, 1:2], HALFPI), writes=[t_const])
    cx.op("dve", lambda e: e.memset(epsc[:, 2:3], 0.0), writes=[t_const])
    cx.op("dve", lambda e: e.memset(epsc[:, 3:4], -MAGIC), writes=[t_const])

    with ExitStack() as st:
        u_bf = sb(st, "u_bf", [128, NCH, L], BF16)
        t_u = [Trk() for _ in range(NTT)]
        par = sb(st, "par", [128, 3, 32])
        t_par = Trk()
        for i in range(3):
            cx.dma("sp", par[:, i, :], s5_par[i], writes=[t_par])
        sp_ = sb(st, "s5small", [128, 22, 32])
        t_sp = Trk()

        def S(i):
            return sp_[:, i, :]
        lr, li, ldt = par[:, 0, :], par[:, 1, :], par[:, 2, :]
        DT, MAG, TH, THT, V, K_, FR, SIN, COS, ARE, AIM, DEN, CRE, CIM, T0, T1, C5, S5, U0, U1, U2, U3 = range(22)

        def dv(fn, eng="dve"):
            cx.op(eng, fn, reads=[t_par, t_sp, t_const], writes=[t_sp])
        dv(lambda e: e.activation(out=S(DT), in_=ldt, func=AF.Exp), "act")
        dv(lambda e: e.tensor_tensor(out=S(T0), in0=lr, in1=S(DT), op=ALU.mult))
        dv(lambda e: e.activation(out=S(MAG), in_=S(T0), func=AF.Exp), "act")
        dv(lambda e: e.tensor_tensor(out=S(TH), in0=li, in1=S(DT), op=ALU.mult))
        dv(lambda e: e.tensor_scalar(out=S(THT), in0=S(TH), scalar1=1.0 / (2 * math.pi), scalar2=None, op0=ALU.mult))
        dv(lambda e: e.tensor_scalar(out=S(V), in0=S(THT), scalar1=MAGIC, scalar2=None, op0=ALU.add))
        dv(lambda e: e.tensor_scalar(out=S(K_), in0=S(V), scalar1=-MAGIC, scalar2=None, op0=ALU.add))
        dv(lambda e: e.tensor_tensor(out=S(FR), in0=S(THT), in1=S(K_), op=ALU.subtract))
        dv(lambda e: e.activation(out=S(SIN), in_=S(FR), func=AF.Sin, scale=S2PI), "act")
        dv(lambda e: e.tensor_scalar(out=S(T0), in0=S(FR), scalar1=0.25, scalar2=-1.0, op0=ALU.is_gt, op1=ALU.mult))
        dv(lambda e: e.tensor_tensor(out=S(T0), in0=S(T0), in1=S(FR), op=ALU.add))
        dv(lambda e: e.activation(out=S(COS), in_=S(T0), func=AF.Sin, scale=S2PI, bias=epsc[:, 1:2]), "act")
        dv(lambda e: e.tensor_tensor(out=S(ARE), in0=S(MAG), in1=S(COS), op=ALU.mult))
        dv(lambda e: e.tensor_tensor(out=S(AIM), in0=S(MAG), in1=S(SIN), op=ALU.mult))
        dv(lambda e: e.tensor_tensor(out=S(T0), in0=lr, in1=lr, op=ALU.mult))
        dv(lambda e: e.tensor_tensor(out=S(T1), in0=li, in1=li, op=ALU.mult))
        dv(lambda e: e.tensor_tensor(out=S(DEN), in0=S(T0), in1=S(T1), op=ALU.add))
        dv(lambda e: e.reciprocal(out=S(DEN), in_=S(DEN)))
        dv(lambda e: e.tensor_scalar(out=S(T0), in0=S(ARE), scalar1=-1.0, scalar2=None, op0=ALU.add))
        dv(lambda e: e.tensor_tensor(out=S(CRE), in0=S(T0), in1=lr, op=ALU.mult))
        dv(lambda e: e.tensor_tensor(out=S(T1), in0=S(AIM), in1=li, op=ALU.mult))
        dv(lambda e: e.tensor_tensor(out=S(CRE), in0=S(CRE), in1=S(T1), op=ALU.add))
        dv(lambda e: e.tensor_tensor(out=S(CRE), in0=S(CRE), in1=S(DEN), op=ALU.mult))
        dv(lambda e: e.tensor_tensor(out=S(CIM), in0=S(AIM), in1=lr, op=ALU.mult))
        dv(lambda e: e.tensor_tensor(out=S(T1), in0=S(T0), in1=li, op=ALU.mult))
        dv(lambda e: e.tensor_tensor(out=S(CIM), in0=S(CIM), in1=S(T1), op=ALU.subtract))
        dv(lambda e: e.tensor_tensor(out=S(CIM), in0=S(CIM), in1=S(DEN), op=ALU.mult))
        dv(lambda e: e.tensor_copy(out=S(C5), in_=S(COS)))
        dv(lambda e: e.tensor_copy(out=S(S5), in_=S(SIN)))
        for _sq in range(9):
            dv(lambda e: e.tensor_tensor(out=S(U0), in0=S(C5), in1=S(C5), op=ALU.mult))
            dv(lambda e: e.tensor_tensor(out=S(U1), in0=S(S5), in1=S(S5), op=ALU.mult))
            dv(lambda e: e.scalar_tensor_tensor(out=S(U2), in0=S(C5), scalar=2.0, in1=S(S5), op0=ALU.mult, op1=ALU.mult))
            dv(lambda e: e.tensor_tensor(out=S(C5), in0=S(U0), in1=S(U1), op=ALU.subtract))
            dv(lambda e: e.tensor_copy(out=S(S5), in_=S(U2)))
        dv(lambda e: e.tensor_scalar(out=S(T1), in0=S(CIM), scalar1=-1.0, scalar2=None, op0=ALU.mult))

        Lre = sb(st, "Lre", [128, 32, 128], BF16)
        Lim = sb(st, "Lim", [128, 32, 128], BF16)
        Cre = sb(st, "Cre", [128, 32, 128], BF16)
        nCre = sb(st, "nCre", [128, 32, 128], BF16)
        nCim = sb(st, "nCim", [128, 32, 128], BF16)
        t_L = Trk()
        t_C = Trk()
        with ExitStack() as st2:
            bre = sb(st2, "bre", [128, 32, 128])
            bim = sb(st2, "bim", [128, 32, 128])
            t_bp = Trk()
            cx.dma("sp", bre[:], s5_bpad[0], writes=[t_bp])
            cx.dma("sp", bim[:], s5_bpad[1], writes=[t_bp])
            xa = [sb(st2, "xa%d" % i, [128, 128]) for i in range(2)]
            xb = [sb(st2, "xb%d" % i, [128, 128]) for i in range(2)]
            t_xa = [Trk(), Trk()]
            t_xb = [Trk(), Trk()]
            for j in range(32):
                b = j % 2
                cx.op("dve", lambda e, j=j, b=b: e.tensor_scalar(out=xa[b][:], in0=bim[:, j, :], scalar1=sp_[:, T1, j:j + 1],
                                                                 scalar2=None, op0=ALU.mult),
                      reads=[t_bp, t_sp], writes=[t_xa[b]])
                cx.op("dve", lambda e, j=j, b=b: e.scalar_tensor_tensor(out=xa[b][:], in0=bre[:, j, :], scalar=sp_[:, CRE, j:j + 1],
                                                                        in1=xa[b][:], op0=ALU.mult, op1=ALU.add),
                      reads=[t_bp, t_sp], writes=[t_xa[b]])
                cx.op("dve", lambda e, j=j, b=b: e.tensor_scalar(out=xb[b][:], in0=bre[:, j, :], scalar1=sp_[:, CIM, j:j + 1],
                                                                 scalar2=None, op0=ALU.mult),
                      reads=[t_bp, t_sp], writes=[t_xb[b]])
                cx.op("dve", lambda e, j=j, b=b: e.scalar_tensor_tensor(out=xb[b][:], in0=bim[:, j, :], scalar=sp_[:, CRE, j:j + 1],
                                                                        in1=xb[b][:], op0=ALU.mult, op1=ALU.add),
                      reads=[t_bp, t_sp], writes=[t_xb[b]])
                cx.op("pe", lambda e, b=b: e.transpose(out=ps[b][:, 0:128], in_=xa[b][:], identity=identf[:]),
                      reads=[t_xa[b], t_const], writes=[pst[b]])
                cx.op("pe", lambda e, b=b: e.transpose(out=ps[b][:, 128:256], in_=xb[b][:], identity=identf[:]),
                      reads=[t_xb[b], t_const], writes=[pst[b]])
                cx.op("act", lambda e, j=j, b=b: e.copy(out=Lre[:, j, :], in_=ps[b][:, 0:128]), reads=[pst[b]], writes=[t_L])
                cx.op("act", lambda e, j=j, b=b: e.copy(out=Lim[:, j, :], in_=ps[b][:, 128:256]), reads=[pst[b]], writes=[t_L])
            cx.dma("sp", bre[:], s5_cpad[0], reads=[], writes=[t_bp])
            cx.dma("sp", bim[:], s5_cpad[1], reads=[], writes=[t_bp])
            for q4 in range(4):
                sl = slice(q4 * 8, (q4 + 1) * 8)
                cx.op("act", lambda e, sl=sl: e.copy(out=Cre[:, sl, :], in_=bre[:, sl, :]), reads=[t_bp], writes=[t_C])
                cx.op("act", lambda e, sl=sl: e.mul(out=nCre[:, sl, :], in_=bre[:, sl, :], mul=-1.0), reads=[t_bp], writes=[t_C])
                cx.op("act", lambda e, sl=sl: e.mul(out=nCim[:, sl, :], in_=bim[:, sl, :], mul=-1.0), reads=[t_bp], writes=[t_C])
            cx.barrier()

        chk("a0")
        with ExitStack() as st2:
            win = sb(st2, "win", [128, NCH, D], BF16)
            t_win = Trk()
            wsrc = s5_w_in.rearrange("(c p) n -> p c n", p=128)
            for kc in range(NCH):
                cx.dma("pool", win[:, kc, :], wsrc[:, kc, :], writes=[t_win])
            hts = [sb(st2, "ht%d" % i, [128, NCH, TT]) for i in range(2)]
            t_ht = [Trk(), Trk()]
            sq = sb(st2, "sq", [128, NCH, TT])
            t_sq = Trk()
            rinv = sb(st2, "rinv", [128, TT])
            t_rinv = Trk()
            tmp = sb(st2, "tmpn", [128, NCH, TT])
            t_tmp = Trk()
            hnb = [sb(st2, "hnb%d" % i, [128, NCH, TT], BF16) for i in range(2)]
            t_hn = [Trk(), Trk()]
            for tt in range(NTT):
                b = tt % 2
                t0 = tt * TT
                cx.dma("sp", hts[b][:], xT[:, :, t0:t0 + TT].rearrange("c p t -> p c t"), writes=[t_ht[b]])
                norm_mod(0, hts[b], t_ht[b], sq, t_sq, rinv, t_rinv, tmp, t_tmp, hnb[b], t_hn[b], 0)
                for n in range(NCH):
                    pi = 1 + (n % 4)
                    for k in range(NCH):
                        cx.op("pe", lambda e, n=n, k=k, pi=pi, b=b: e.matmul(
                            ps[pi][:, :], lhsT=win[:, k, n * 128:(n + 1) * 128], rhs=hnb[b][:, k, :],
                            start=(k == 0), stop=(k == NCH - 1)), reads=[t_win, t_hn[b]], writes=[pst[pi]])
                    eng = "act" if n % 2 == 0 else "dve"
                    if eng == "act":
                        cx.op("act", lambda e, n=n, pi=pi, t0=t0: e.copy(out=u_bf[:, n, t0:t0 + TT], in_=ps[pi][:, :]),
                              reads=[pst[pi]], writes=[t_u[tt]])
                    else:
                        cx.op("dve", lambda e, n=n, pi=pi, t0=t0: e.tensor_copy(out=u_bf[:, n, t0:t0 + TT], in_=ps[pi][:, :]),
                              reads=[pst[pi]], writes=[t_u[tt]])
            cx.barrier()

        if debug:
            for n in range(NCH):
                cx.dma("sp", udbg[n], u_bf[:, n, :], reads=t_u)
        chk("a2")
        with ExitStack() as st2:
            iot = sb(st2, "iot", [128, TT])
            t_iot = Trk()
            cx.dma("sp", iot[:], iota_t[:, 0:TT], writes=[t_iot])
            dsk = sb(st2, "dsk", [128, NCH])
            cx.dma("sp", dsk[:], s5_d[:, :], writes=[t_iot])
            NB = 2

            def mk(name, dt=F32):
                return [sb(st2, "%s%d" % (name, i), [128, TT], dt) for i in range(NB)], [Trk() for _ in range(NB)]
            SNt, tSN = mk("SNt")
            CRt, tCR = mk("CRt")
            T1_, tT1 = mk("T1_")
            T2_, tT2 = mk("T2_")
            T3_, tT3 = mk("T3_")
            T4_, tT4 = mk("T4_")
            Vt, tVt = mk("Vt")
            Ft_, tFt = mk("Ftb")
            ini = [sb(st2, "ini%d" % i, [128, 4]) for i in range(NB)]
            t_ini = [Trk() for _ in range(NB)]
            XR, tXR = mk("XR")
            XI, tXI = mk("XI")
            SR, tSR = mk("SR")
            SI, tSI = mk("SI")
            P1, tP1 = mk("P1", BF16)
            P2, tP2 = mk("P2", BF16)
            P3, tP3 = mk("P3", BF16)
            P4, tP4 = mk("P4", BF16)
            ytmp = sb(st2, "ytmp", [128, L])
            t_y = [Trk() for _ in range(NTT)]
            gb = sb(st2, "gb", [128, L], BF16)
            t_gb = [Trk() for _ in range(NTT)]
            g1 = sb(st2, "g1", [128, TT])
            g2 = sb(st2, "g2", [128, TT])
            t_g1 = Trk()
            t_g2 = Trk()
            zero_init = epsc[:, 2:3]
            def gen_tables(j):
                thj = sp_[:, THT, j:j + 1]
                tb = j % 2
                cx.op("dve", lambda e: e.tensor_scalar(
                    out=Vt[tb][:], in0=iot[:, 0:TT], scalar1=thj, scalar2=MAGIC, op0=ALU.mult, op1=ALU.add),
                    reads=[t_iot, t_sp], writes=[tVt[tb]])
                cx.op("act", lambda e: e.activation(out=Vt[tb][:], in_=Vt[tb][:], func=AF.Identity,
                                                    bias=epsc[:, 3:4], scale=1.0),
                      reads=[tVt[tb], t_const], writes=[tVt[tb]])
                cx.op("dve", lambda e: e.scalar_tensor_tensor(
                    out=Ft_[tb][:], in0=iot[:, 0:TT], scalar=thj, in1=Vt[tb][:], op0=ALU.mult, op1=ALU.subtract),
                    reads=[t_iot, t_sp, tVt[tb]], writes=[tFt[tb]])
                cx.op("act", lambda e: e.activation(out=SNt[tb][:], in_=Ft_[tb][:], func=AF.Sin, scale=S2PI),
                      reads=[tFt[tb]], writes=[tSN[tb]])
                cx.op("dve", lambda e: e.tensor_scalar(
                    out=Vt[tb][:], in0=Ft_[tb][:], scalar1=0.25, scalar2=-1.0, op0=ALU.is_gt, op1=ALU.mult),
                    reads=[tFt[tb], tVt[tb]], writes=[tVt[tb]])
                cx.op("dve", lambda e: e.tensor_tensor(out=Vt[tb][:], in0=Vt[tb][:], in1=Ft_[tb][:], op=ALU.add),
                      reads=[tFt[tb], tVt[tb]], writes=[tVt[tb]])
                cx.op("act", lambda e: e.activation(out=CRt[tb][:], in_=Vt[tb][:], func=AF.Sin, scale=S2PI,
                                                    bias=epsc[:, 1:2]),
                      reads=[tVt[tb], t_const], writes=[tCR[tb]])

            class P_:
                pass
            pieces = []
            for c in range(NCH):
                for jj in range(4):
                    for tt in range(NTT):
                        p = P_()
                        p.c, p.jj, p.j, p.o, p.tt, p.s = c, jj, 4 * c + jj, jj * 32, tt, len(pieces)
                        p.b = p.s % NB
                        p.pb = 4 * (p.s % 2)
                        p.tb = p.j % 2
                        p.tsl = slice(tt * TT, (tt + 1) * TT)
                        pieces.append(p)

            def stg1(p):
                if p.tt == 0:
                    gen_tables(p.j)
                b, pb, tb, j, c, tsl, tt = p.b, p.pb, p.tb, p.j, p.c, p.tsl, p.tt
                cx.op("pe", lambda e: e.matmul(ps[pb][:, :], lhsT=Lre[:, j, :], rhs=u_bf[:, c, tsl], start=True, stop=True),
                      reads=[t_L, t_u[tt]], writes=[pst[pb]])
                cx.op("pe", lambda e: e.matmul(ps[pb + 1][:, :], lhsT=Lim[:, j, :], rhs=u_bf[:, c, tsl], start=True, stop=True),
                      reads=[t_L, t_u[tt]], writes=[pst[pb + 1]])
                cx.op("dve", lambda e: e.tensor_tensor(out=T1_[b][:], in0=CRt[tb][:], in1=ps[pb][:, :], op=ALU.mult),
                      reads=[tCR[tb], pst[pb]], writes=[tT1[b]])
                cx.op("dve", lambda e: e.tensor_tensor(out=T2_[b][:], in0=SNt[tb][:], in1=ps[pb + 1][:, :], op=ALU.mult),
                      reads=[tSN[tb], pst[pb + 1]], writes=[tT2[b]])
                cx.op("dve", lambda e: e.tensor_tensor(out=T3_[b][:], in0=CRt[tb][:], in1=ps[pb + 1][:, :], op=ALU.mult),
                      reads=[tCR[tb], pst[pb + 1]], writes=[tT3[b]])
                cx.op("dve", lambda e: e.tensor_tensor(out=T4_[b][:], in0=SNt[tb][:], in1=ps[pb][:, :], op=ALU.mult),
                      reads=[tSN[tb], pst[pb]], writes=[tT4[b]])

            def stg2(p):
                b = p.b
                cx.op("pool", lambda e: e.tensor_tensor(out=XR[b][:], in0=T1_[b][:], in1=T2_[b][:], op=ALU.add),
                      reads=[tT1[b], tT2[b]], writes=[tXR[b]])
                cx.op("pool", lambda e: e.tensor_tensor(out=XI[b][:], in0=T3_[b][:], in1=T4_[b][:], op=ALU.subtract),
                      reads=[tT3[b], tT4[b]], writes=[tXI[b]])

            def stg3(p):
                b, j, tt = p.b, p.j, p.tt
                pbuf = (b - 1) % NB
                magb = sp_[:, MAG, j:j + 1].to_broadcast([128, TT])
                if tt == 0:
                    ini_r, ini_i, rd = zero_init, zero_init, [t_const]
                else:
                    sre, sie = SR[pbuf][:, TT - 1:TT], SI[pbuf][:, TT - 1:TT]
                    c5, s5 = sp_[:, C5, j:j + 1], sp_[:, S5, j:j + 1]
                    rdp = [tSR[pbuf], tSI[pbuf], t_sp]
                    cx.op("dve", lambda e: e.tensor_tensor(out=ini[b][:, 2:3], in0=sie, in1=s5, op=ALU.mult),
                          reads=rdp, writes=[t_ini[b]])
                    cx.op("dve", lambda e: e.scalar_tensor_tensor(
                        out=ini[b][:, 0:1], in0=sre, scalar=c5, in1=ini[b][:, 2:3], op0=ALU.mult, op1=ALU.subtract),
                        reads=rdp + [t_ini[b]], writes=[t_ini[b]])
                    cx.op("dve", lambda e: e.tensor_tensor(out=ini[b][:, 3:4], in0=sie, in1=c5, op=ALU.mult),
                          reads=rdp + [t_ini[b]], writes=[t_ini[b]])
                    cx.op("dve", lambda e: e.scalar_tensor_tensor(
                        out=ini[b][:, 1:2], in0=sre, scalar=s5, in1=ini[b][:, 3:4], op0=ALU.mult, op1=ALU.add),
                        reads=rdp + [t_ini[b]], writes=[t_ini[b]])
                    ini_r, ini_i, rd = ini[b][:, 0:1], ini[b][:, 1:2], [t_ini[b]]
                cx.op("dve", lambda e: e.tensor_tensor_scan(
                    out=SR[b][:], data0=magb, data1=XR[b][:], initial=ini_r, op0=ALU.mult, op1=ALU.add),
                    reads=[tXR[b], t_sp] + rd, writes=[tSR[b]])
                cx.op("dve", lambda e: e.tensor_tensor_scan(
                    out=SI[b][:], data0=magb, data1=XI[b][:], initial=ini_i, op0=ALU.mult, op1=ALU.add),
                    reads=[tXI[b], t_sp] + rd, writes=[tSI[b]])

            def stg4(p):
                b, tb = p.b, p.tb
                cx.op("pool", lambda e: e.tensor_tensor(out=P1[b][:], in0=CRt[tb][:], in1=SR[b][:], op=ALU.mult),
                      reads=[tCR[tb], tSR[b]], writes=[tP1[b]])
                cx.op("pool", lambda e: e.tensor_tensor(out=P2[b][:], in0=SNt[tb][:], in1=SI[b][:], op=ALU.mult),
                      reads=[tSN[tb], tSI[b]], writes=[tP2[b]])
                cx.op("pool", lambda e: e.tensor_tensor(out=P3[b][:], in0=SNt[tb][:], in1=SR[b][:], op=ALU.mult),
                      reads=[tSN[tb], tSR[b]], writes=[tP3[b]])
                cx.op("pool", lambda e: e.tensor_tensor(out=P4[b][:], in0=CRt[tb][:], in1=SI[b][:], op=ALU.mult),
                      reads=[tCR[tb], tSI[b]], writes=[tP4[b]])

            def stg5(p):
                b, pb, j, c, o, tsl, tt = p.b, p.pb, p.j, p.c, p.o, p.tsl, p.tt
                py = pb + 2
                for idx, (cm, pp, tp) in enumerate([(Cre, P1, tP1), (nCre, P2, tP2), (nCim, P3, tP3), (nCim, P4, tP4)]):
                    cx.op("pe", lambda e, cm=cm, pp=pp, idx=idx: e.matmul(
                        ps[py][:, :], lhsT=cm[:, j, :], rhs=pp[b][:], start=(idx == 0), stop=(idx == 3)),
                        reads=[t_C, tp[b]], writes=[pst[py]])
                cx.op("dve", lambda e: e.scalar_tensor_tensor(
                    out=ytmp[o:o + 32, tsl], in0=u_bf[o:o + 32, c, tsl], scalar=dsk[o:o + 32, c:c + 1],
                    in1=ps[py][o:o + 32, :], op0=ALU.mult, op1=ALU.add),
                    reads=[t_u[tt], t_iot, pst[py]], writes=[t_y[tt]])
                if p.jj == 3 and tt == NTT - 1:
                    for t2 in range(NTT):
                        ts2 = slice(t2 * TT, (t2 + 1) * TT)
                        cx.op("act", lambda e, ts2=ts2: e.activation(out=g1[:], in_=ytmp[:, ts2], func=AF.Square),
                              reads=[t_y[t2]], writes=[t_g1])
                        cx.op("dve", lambda e: e.tensor_scalar(out=g1[:], in0=g1[:], scalar1=0.044715, scalar2=1.0,
                                                               op0=ALU.mult, op1=ALU.add), reads=[t_g1], writes=[t_g1])
                        cx.op("dve", lambda e, ts2=ts2: e.tensor_tensor(out=g2[:], in0=g1[:], in1=ytmp[:, ts2], op=ALU.mult),
                              reads=[t_g1, t_y[t2]], writes=[t_g2])
                        cx.op("act", lambda e: e.activation(out=g2[:], in_=g2[:], func=AF.Sigmoid, scale=GELU_C),
                              reads=[t_g2], writes=[t_g2])
                        cx.op("dve", lambda e, ts2=ts2: e.tensor_tensor(out=gb[:, ts2], in0=g2[:], in1=ytmp[:, ts2], op=ALU.mult),
                              reads=[t_g2, t_y[t2]], writes=[t_gb[t2]])
                    cx.dma("sp", Gd[c], gb[:], reads=t_gb, writes=[t_gd])
                    chk("a3g%d" % c)

            t_gd = Trk()
            npc = len(pieces)
            for s_ in range(npc + 2):
                if s_ < npc:
                    stg1(pieces[s_])
                    stg2(pieces[s_])
                if 1 <= s_ <= npc:
                    stg3(pieces[s_ - 1])
                    stg4(pieces[s_ - 1])
                if s_ >= 2:
                    stg5(pieces[s_ - 2])
            cx.barrier()
    cx.barrier()

    with ExitStack() as st:
        wout = sb(st, "wout", [128, NCH, 2 * D], BF16)
        t_wout = Trk()
        wsrc = s5_w_out.rearrange("(c p) n -> p c n", p=128)
        for kc in range(NCH):
            cx.dma("pool", wout[:, kc, :], wsrc[:, kc, :], writes=[t_wout])
        gt = [sb(st, "gt%d" % i, [128, NCH, TT], BF16) for i in range(2)]
        t_gt = [Trk(), Trk()]
        hts = [sb(st, "hto%d" % i, [128, NCH, TT]) for i in range(2)]
        t_ht = [Trk(), Trk()]
        ho = [sb(st, "ho%d" % i, [128, NCH, TT]) for i in range(2)]
        t_ho = [Trk(), Trk()]
        sg = [sb(st, "sg%d" % i, [128, TT]) for i in range(2)]
        t_sg = [Trk(), Trk()]
        mx = [sb(st, "mx%d" % i, [128, TT]) for i in range(2)]
        t_mx = [Trk(), Trk()]
        it = 0
        for tt in range(NTT):
            b = tt % 2
            t0 = tt * TT
            cx.dma("sp", gt[b][:], Gd[:, :, t0:t0 + TT].rearrange("c p t -> p c t"), writes=[t_gt[b]])
            cx.dma("sp", hts[b][:], xT[:, :, t0:t0 + TT].rearrange("c p t -> p c t"), writes=[t_ht[b]])
            for n in range(NCH):
                bb = it % 2
                pv = 2 * (it % 4)
                pg = pv + 1
                it += 1
                for k in range(NCH):
                    cx.op("pe", lambda e, n=n, k=k, pv=pv, b=b: e.matmul(
                        ps[pv][:, :], lhsT=wout[:, k, n * 128:(n + 1) * 128], rhs=gt[b][:, k, :],
                        start=(k == 0), stop=(k == NCH - 1)), reads=[t_wout, t_gt[b]], writes=[pst[pv]])
                for k in range(NCH):
                    cx.op("pe", lambda e, n=n, k=k, pg=pg, b=b: e.matmul(
                        ps[pg][:, :], lhsT=wout[:, k, D + n * 128:D + (n + 1) * 128], rhs=gt[b][:, k, :],
                        start=(k == 0), stop=(k == NCH - 1)), reads=[t_wout, t_gt[b]], writes=[pst[pg]])
                cx.op("act", lambda e, bb=bb, pg=pg: e.activation(out=sg[bb][:], in_=ps[pg][:, :], func=AF.Sigmoid),
                      reads=[pst[pg]], writes=[t_sg[bb]])
                cx.op("dve", lambda e, bb=bb, pv=pv: e.tensor_tensor(out=mx[bb][:], in0=sg[bb][:], in1=ps[pv][:, :], op=ALU.mult),
                      reads=[t_sg[bb], pst[pv]], writes=[t_mx[bb]])
                cx.op("dve", lambda e, bb=bb, n=n, b=b: e.scalar_tensor_tensor(
                    out=ho[b][:, n, :], in0=mx[bb][:], scalar=gatec(0, n), in1=hts[b][:, n, :], op0=ALU.mult, op1=ALU.add),
                    reads=[t_mx[bb], t_mods, t_ht[b]], writes=[t_ho[b]])
            cx.dma("sp", h1[:, :, t0:t0 + TT].rearrange("c p t -> p c t"), ho[b][:], reads=[t_ho[b]])
        cx.barrier()

    chk("a5")

    BIG = 1.0e30
    HT = 2048

    def moe_stage(l, mi, hin, hdst):
        with ExitStack() as st:
            acc = sb(st, "acc", [128, NCH, HT])
            t_acc = [[Trk() for _ in range(NCH)] for _ in range(4)]
            hn_all = sb(st, "hn_all", [128, NCH, HT], BF16)
            t_hna = [Trk() for _ in range(4)]
            gatesT = sb(st, "gatesT", [32, HT])
            t_gT = [Trk() for _ in range(4)]
            selt = sb(st, "selt", [32, NE, 128])
            t_sel = Trk()
            cx.dma("sp", selt[:], sel_c[:, :, :], writes=[t_sel])
            wr = sb(st, "wr", [128, NCH, 36])
            brt = sb(st, "brt", [128, 36])
            t_wr = Trk()
            cx.dma("sp", wr[:], moe_wr[l].rearrange("(c p) n -> p c n", p=128), writes=[t_wr])
            cx.dma("sp", brt[:], moe_br[l], writes=[t_wr])
            for half in range(L // HT):
                hbase = half * HT
                with ExitStack() as st2:
                    ht = sb(st2, "m_ht", [128, NCH, TT])
                    t_ht = Trk()
                    sq = sb(st2, "m_sq", [128, NCH, TT])
                    t_sq = Trk()
                    rinv = sb(st2, "m_rinv", [128, TT])
                    t_rinv = Trk()
                    tmp = sb(st2, "m_tmp", [128, NCH, TT])
                    t_tmp = Trk()
                    hnf = sb(st2, "m_hnf", [128, NCH, TT])
                    t_hn = Trk()
                    rt = sb(st2, "m_rt", [128, 256])
                    t_rt = Trk()
                    LG, GM, GMASK, GE, GS, PEN, MK, M1, MASK1, MK2, M2, MASK2, ED, W1, W2, GATES = (
                        slice(0, 36), slice(36, 37), slice(40, 44), slice(44, 48), slice(48, 49), slice(52, 56),
                        slice(56, 88), slice(88, 89), slice(96, 128), slice(128, 160), slice(160, 161),
                        slice(168, 200), slice(200, 201), slice(201, 202), slice(202, 203), slice(208, 240))
                    for tl in range(HT // TT):
                        t0 = hbase + tl * TT
                        cx.dma("sp", ht[:], hin[:, :, t0:t0 + TT].rearrange("c p t -> p c t"), writes=[t_ht])
                        norm_mod(mi, ht, t_ht, sq, t_sq, rinv, t_rinv, tmp, t_tmp,
                                 hn_all[:, :, tl * TT:(tl + 1) * TT], t_hna[tl], 7, hn_f=hnf, t_hnf=t_hn)
                        for sc_ in range(4):
                            for k in range(NCH):
                                cx.op("pe", lambda e, k=k, sc_=sc_: e.matmul(
                                    ps[6][:, 0:36], lhsT=hnf[:, k, sc_ * 128:(sc_ + 1) * 128], rhs=wr[:, k, :],
                                    start=(k == 0), stop=(k == NCH - 1)), reads=[t_hn, t_wr], writes=[pst[6]])

                            def R(sl):
                                return rt[:, sl]

                            def dv(fn, eng="dve", extra=()):
                                cx.op(eng, fn, reads=[t_rt] + list(extra), writes=[t_rt])
                            dv(lambda e: e.tensor_tensor(out=R(LG), in0=ps[6][:, 0:36], in1=brt[:], op=ALU.add), extra=[pst[6], t_wr])
                            dv(lambda e: e.reduce_max(out=R(GM), in_=rt[:, 0:4], axis=AX.X))
                            dv(lambda e: e.tensor_scalar(out=R(GMASK), in0=rt[:, 0:4], scalar1=rt[:, 36:37], scalar2=None, op0=ALU.is_ge))
                            dv(lambda e: e.tensor_scalar(out=R(M1), in0=R(GM), scalar1=-1.0, scalar2=None, op0=ALU.mult))
                            dv(lambda e: e.activation(out=R(GE), in_=rt[:, 0:4], func=AF.Exp, bias=rt[:, 88:89], scale=1.0), "act")
                            dv(lambda e: e.reduce_sum(out=R(GS), in_=R(GE), axis=AX.X))
                            dv(lambda e: e.reciprocal(out=R(GS), in_=R(GS)))
                            dv(lambda e: e.tensor_scalar(out=R(PEN), in0=R(GMASK), scalar1=-1.0, scalar2=BIG, op0=ALU.add, op1=ALU.mult))
                            dv(lambda e: e.tensor_tensor(
                                out=rt[:, 56:88].rearrange("p (g e) -> p g e", g=4),
                                in0=rt[:, 4:36].rearrange("p (g e) -> p g e", g=4),
                                in1=rt[:, 52:56].unsqueeze(2).to_broadcast([128, 4, 8]), op=ALU.add))
                            dv(lambda e: e.reduce_max(out=R(M1), in_=R(MK), axis=AX.X))
                            dv(lambda e: e.tensor_scalar(out=R(MASK1), in0=R(MK), scalar1=rt[:, 88:89], scalar2=None, op0=ALU.is_ge))
                            dv(lambda e: e.scalar_tensor_tensor(out=R(MK2), in0=R(MASK1), scalar=-BIG, in1=R(MK), op0=ALU.mult, op1=ALU.add))
                            dv(lambda e: e.reduce_max(out=R(M2), in_=R(MK2), axis=AX.X))
                            dv(lambda e: e.tensor_scalar(out=R(MASK2), in0=R(MK2), scalar1=rt[:, 160:161], scalar2=None, op0=ALU.is_ge))
                            dv(lambda e: e.tensor_tensor(out=R(ED), in0=R(M2), in1=R(M1), op=ALU.subtract))
                            dv(lambda e: e.activation(out=R(ED), in_=R(ED), func=AF.Exp), "act")
                            dv(lambda e: e.tensor_scalar(out=R(W1), in0=R(ED), scalar1=1.0, scalar2=None, op0=ALU.add))
                            dv(lambda e: e.reciprocal(out=R(W1), in_=R(W1)))
                            dv(lambda e: e.tensor_tensor(out=R(W1), in0=R(W1), in1=R(GS), op=ALU.mult))
                            dv(lambda e: e.tensor_tensor(out=R(W2), in0=R(W1), in1=R(ED), op=ALU.mult))
                            dv(lambda e: e.tensor_scalar(out=R(GATES), in0=R(MASK1), scalar1=rt[:, 201:202], scalar2=None, op0=ALU.mult))
                            dv(lambda e: e.scalar_tensor_tensor(out=R(GATES), in0=R(MASK2), scalar=rt[:, 202:203], in1=R(GATES), op0=ALU.mult, op1=ALU.add))
                            cx.op("pe", lambda e: e.transpose(out=ps[5][0:32, 0:128], in_=rt[:, 208:240], identity=identf[:]),
                                  reads=[t_rt, t_const], writes=[pst[5]])
                            c0 = tl * TT + sc_ * 128
                            cx.op("act", lambda e, c0=c0: e.copy(out=gatesT[:, c0:c0 + 128], in_=ps[5][0:32, 0:128]),
                                  reads=[pst[5]], writes=[t_gT[tl]])
                    cx.barrier()
                with ExitStack() as st2:
                    w1b = [sb(st2, "w1b%d" % i, [128, NCH, FE], BF16) for i in range(2)]
                    w3b = [sb(st2, "w3b%d" % i, [128, NCH, FE], BF16) for i in range(2)]
                    w2b = [sb(st2, "w2b%d" % i, [128, 2, D], BF16) for i in range(2)]
                    t_wb = [Trk(), Trk()]
                    sl_ = [sb(st2, "sl%d" % i, [128, 2, TT], BF16) for i in range(2)]
                    t_sl = [[Trk(), Trk()], [Trk(), Trk()]]
                    tl_ = [sb(st2, "tl%d" % i, [128, 2, TT], BF16) for i in range(2)]
                    t_tl = [[Trk(), Trk()], [Trk(), Trk()]]
                    gs_ = [sb(st2, "gs%d" % i, [128, TT], BF16) for i in range(2)]
                    t_gs = [Trk(), Trk()]
                    ab = [sb(st2, "ab%d" % i, [128, 2, TT], BF16) for i in range(2)]
                    t_ab = [Trk(), Trk()]
                    oi = [0]
                    munits = [(e_, tl) for e_ in range(NE) for tl in range(HT // TT)]

                    def emit_h(e_, tl, idx):
                        wb = e_ % 2
                        b = idx % 2
                        tsl = slice(tl * TT, (tl + 1) * TT)
                        for hc in range(2):
                            for k in range(NCH):
                                cx.op("pe", lambda e, k=k, hc=hc: e.matmul(
                                    ps[hc][:, :], lhsT=w1b[wb][:, k, hc * 128:(hc + 1) * 128], rhs=hn_all[:, k, tsl],
                                    start=(k == 0), stop=(k == NCH - 1)), reads=[t_wb[wb], t_hna[tl]], writes=[pst[hc]])
                        for hc in range(2):
                            for k in range(NCH):
                                cx.op("pe", lambda e, k=k, hc=hc: e.matmul(
                                    ps[2 + hc][:, :], lhsT=w3b[wb][:, k, hc * 128:(hc + 1) * 128], rhs=hn_all[:, k, tsl],
                                    start=(k == 0), stop=(k == NCH - 1)), reads=[t_wb[wb], t_hna[tl]], writes=[pst[2 + hc]])
                        cx.op("pe", lambda e: e.matmul(
                            ps[4][:, :], lhsT=selt[:, e_, :], rhs=gatesT[:, tsl], start=True, stop=True),
                            reads=[t_sel, t_gT[tl]], writes=[pst[4]])
                        cx.op("act", lambda e: e.copy(out=gs_[b][:], in_=ps[4][:, :]), reads=[pst[4]], writes=[t_gs[b]])
                        for hc in range(2):
                            cx.op("act", lambda e, hc=hc: e.activation(out=sl_[b][:, hc, :], in_=ps[hc][:, :], func=AF.Silu),
                                  reads=[pst[hc]], writes=[t_sl[b][hc]])
                            cx.op("act", lambda e, hc=hc: e.copy(out=tl_[b][:, hc, :], in_=ps[2 + hc][:, :]),
                                  reads=[pst[2 + hc]], writes=[t_tl[b][hc]])
                        for hc in range(2):
                            cx.op("pool", lambda e, hc=hc: e.tensor_tensor(out=sl_[b][:, hc, :], in0=sl_[b][:, hc, :],
                                                                        in1=tl_[b][:, hc, :], op=ALU.mult),
                                  reads=[t_tl[b][hc]], writes=[t_sl[b][hc]])
                            cx.op("pool", lambda e, hc=hc: e.tensor_tensor(out=ab[b][:, hc, :], in0=sl_[b][:, hc, :],
                                                                        in1=gs_[b][:], op=ALU.mult),
                                  reads=[t_sl[b][hc], t_gs[b]], writes=[t_ab[b]])

                    def emit_o(e_, tl, idx):
                        wb = e_ % 2
                        b = idx % 2
                        tsl = slice(tl * TT, (tl + 1) * TT)
                        for n in range(NCH):
                            po = 5 + (oi[0] % 3)
                            oi[0] += 1
                            for hc in range(2):
                                cx.op("pe", lambda e, n=n, hc=hc, po=po: e.matmul(
                                    ps[po][:, :], lhsT=w2b[wb][:, hc, n * 128:(n + 1) * 128], rhs=ab[b][:, hc, :],
                                    start=(hc == 0), stop=(hc == 1)), reads=[t_wb[wb], t_ab[b]], writes=[pst[po]])
                            if e_ == 0:
                                cx.op("dve", lambda e, n=n, po=po: e.tensor_copy(out=acc[:, n, tsl], in_=ps[po][:, :]),
                                      reads=[pst[po]], writes=[t_acc[tl][n]])
                            else:
                                cx.op("dve", lambda e, n=n, po=po: e.tensor_tensor(
                                    out=acc[:, n, tsl], in0=acc[:, n, tsl], in1=ps[po][:, :], op=ALU.add),
                                    reads=[pst[po], t_acc[tl][n]], writes=[t_acc[tl][n]])

                    def load_w(e_):
                        wb = e_ % 2
                        cx.dma("pool", w1b[wb][:], moe_w1[l, e_].rearrange("(c p) f -> p c f", p=128), writes=[t_wb[wb]])
                        cx.dma("pool", w3b[wb][:], moe_w3[l, e_].rearrange("(c p) f -> p c f", p=128), writes=[t_wb[wb]])
                        cx.dma("pool", w2b[wb][:], moe_w2[l, e_].rearrange("(c p) f -> p c f", p=128), writes=[t_wb[wb]])

                    load_w(0)
                    for idx in range(len(munits) + 1):
                        if idx < len(munits):
                            emit_h(munits[idx][0], munits[idx][1], idx)
                        if idx >= 1:
                            emit_o(munits[idx - 1][0], munits[idx - 1][1], idx - 1)
                        if idx < len(munits) and munits[idx][1] == 0 and munits[idx][0] + 1 < NE:
                            load_w(munits[idx][0] + 1)
                    cx.barrier()
                with ExitStack() as st2:
                    hts = [sb(st2, "m3h%d" % i, [128, NCH, TT]) for i in range(2)]
                    t_h3 = [Trk(), Trk()]
                    ho = [sb(st2, "m3o%d" % i, [128, NCH, TT]) for i in range(2)]
                    t_o3 = [Trk(), Trk()]
                    for tl in range(HT // TT):
                        b = tl % 2
                        t0 = hbase + tl * TT
                        tsl = slice(tl * TT, (tl + 1) * TT)
                        cx.dma("sp", hts[b][:], hin[:, :, t0:t0 + TT].rearrange("c p t -> p c t"), writes=[t_h3[b]])
                        for n in range(NCH):
                            cx.op("dve", lambda e, n=n, b=b, tsl=tsl: e.scalar_tensor_tensor(
                                out=ho[b][:, n, :], in0=acc[:, n, tsl], scalar=gatec(mi, n), in1=hts[b][:, n, :],
                                op0=ALU.mult, op1=ALU.add), reads=[t_acc[tl][n], t_mods, t_h3[b]], writes=[t_o3[b]])
                        cx.dma("sp", hdst[:, :, t0:t0 + TT].rearrange("c p t -> p c t"), ho[b][:], reads=[t_o3[b]])
                    cx.barrier()
            cx.barrier()

    moe_stage(0, 1, h1, h2)
    chk("m0")

    blkf = sb(es, "blkf", [128, 128])
    cx.dma("sp", blkf[:], blk_c[:, :], writes=[t_const])

    def head_norm(psi, ps2i, gcol, out_ap, sqk, t_sqk, rk, t_rk):
        cx.op("act", lambda e: e.activation(out=sqk[:], in_=ps[psi][:, :], func=AF.Square), reads=[pst[psi]], writes=[t_sqk])
        cx.op("pe", lambda e: e.matmul(ps[ps2i][:, :], lhsT=blkf[:], rhs=sqk[:], start=True, stop=True),
              reads=[t_sqk, t_const], writes=[pst[ps2i]])
        cx.op("act", lambda e: e.activation(out=rk[:], in_=ps[ps2i][:, :], func=AF.Sqrt, bias=epsc[:, 0:1], scale=1.0 / HD),
              reads=[pst[ps2i], t_const], writes=[t_rk])
        cx.op("dve", lambda e: e.reciprocal(out=rk[:], in_=rk[:]), reads=[t_rk], writes=[t_rk])
        return lambda wr_t: cx.op("dve", lambda e: e.scalar_tensor_tensor(
            out=out_ap, in0=ps[psi][:, :], scalar=gcol, in1=rk[:], op0=ALU.mult, op1=ALU.mult),
            reads=[pst[psi], t_rk, t_const], writes=[wr_t])

    def kv_stage():
        with ExitStack() as st:
            kvw = sb(st, "kvw", [128, NCH, 2 * D], BF16)
            t_kvw = Trk()
            ksrc = kv_w.rearrange("(c p) n -> p c n", p=128)
            for kc in range(NCH):
                cx.dma("pool", kvw[:, kc, :], ksrc[:, kc, 0:2 * D], writes=[t_kvw])
            fw = sb(st, "fw", [128, NCH, H])
            for kc in range(NCH):
                cx.dma("sp", fw[:, kc, :], ksrc[:, kc, 2 * D:2 * D + H], writes=[t_kvw])
            gk = sb(st, "gk", [128, 1])
            nfb = sb(st, "nfb", [H, 1])
            ones512 = sb(st, "ones512", [H, TT])
            cx.dma("sp", gk[:], gk_col[:, :], writes=[t_const])
            cx.dma("sp", nfb[:], fb_col[:, :], writes=[t_const])
            cx.op("dve", lambda e: e.tensor_scalar(out=nfb[:], in0=nfb[:], scalar1=-1.0, scalar2=None, op0=ALU.mult),
                  reads=[t_const], writes=[t_const])
            cx.op("dve", lambda e: e.memset(ones512[:], 1.0), writes=[t_const])
            Ft = sb(st, "Ft", [H, L])
            t_F = [Trk() for _ in range(NTT)]
            with ExitStack() as st2:
                hts = [sb(st2, "k_ht%d" % i, [128, NCH, TT]) for i in range(2)]
                t_ht = [Trk(), Trk()]
                sq = sb(st2, "k_sq", [128, NCH, TT])
                t_sq = Trk()
                rinv = sb(st2, "k_rinv", [128, TT])
                t_rinv = Trk()
                tmp = sb(st2, "k_tmp", [128, NCH, TT])
                t_tmp = Trk()
                hnf = sb(st2, "k_hnf", [128, NCH, TT])
                t_hnf = Trk()
                hnb = sb(st2, "k_hnb", [128, NCH, TT], BF16)
                t_hnb = Trk()
                kt = [sb(st2, "k_kt%d" % i, [128, NCH, TT], BF16) for i in range(2)]
                t_kt = [Trk(), Trk()]
                vt = [sb(st2, "k_vt%d" % i, [128, 4, D], BF16) for i in range(2)]
                t_vt = [Trk(), Trk()]
                sqk = sb(st2, "k_sqk", [128, TT])
                t_sqk = Trk()
                rk = sb(st2, "k_rk", [128, TT])
                t_rk = Trk()
                ef = sb(st2, "k_ef", [H, TT])
                t_ef = Trk()
                for tt in range(NTT):
                    b = tt % 2
                    t0 = tt * TT
                    cx.dma("sp", hts[b][:], h2[:, :, t0:t0 + TT].rearrange("c p t -> p c t"), writes=[t_ht[b]])
                    norm_mod(4, hts[b], t_ht[b], sq, t_sq, rinv, t_rinv, tmp, t_tmp, hnb, t_hnb, 0, hn_f=hnf, t_hnf=t_hnf)
                    for n in range(NCH):
                        pi = 1 + (n % 2)
                        for k in range(NCH):
                            cx.op("pe", lambda e, n=n, k=k, pi=pi: e.matmul(
                                ps[pi][:, :], lhsT=kvw[:, k, n * 128:(n + 1) * 128], rhs=hnb[:, k, :],
                                start=(k == 0), stop=(k == NCH - 1)), reads=[t_kvw, t_hnb], writes=[pst[pi]])
                        fin = head_norm(pi, 3, gk[:, 0:1], kt[b][:, n, :], sqk, t_sqk, rk, t_rk)
                        fin(t_kt[b])
                    cx.dma("sp", Kd[:, :, t0:t0 + TT].rearrange("c p t -> p c t"), kt[b][:], reads=[t_kt[b]])
                    for s_ in range(4):
                        for hf in range(2):
                            pi = 4 + ((s_ * 2 + hf) % 2)
                            for k in range(NCH):
                                cx.op("pe", lambda e, s_=s_, hf=hf, k=k, pi=pi: e.matmul(
                                    ps[pi][:, :], lhsT=hnb[:, k, s_ * 128:(s_ + 1) * 128],
                                    rhs=kvw[:, k, D + hf * 512:D + (hf + 1) * 512],
                                    start=(k == 0), stop=(k == NCH - 1)), reads=[t_kvw, t_hnb], writes=[pst[pi]])
                            if hf == 0:
                                cx.op("act", lambda e, s_=s_, hf=hf, pi=pi, b=b: e.copy(out=vt[b][:, s_, hf * 512:(hf + 1) * 512], in_=ps[pi][:, :]),
                                      reads=[pst[pi]], writes=[t_vt[b]])
                            else:
                                cx.op("dve", lambda e, s_=s_, hf=hf, pi=pi, b=b: e.tensor_copy(out=vt[b][:, s_, hf * 512:(hf + 1) * 512], in_=ps[pi][:, :]),
                                      reads=[pst[pi]], writes=[t_vt[b]])
                    cx.dma("sp", Vd[t0:t0 + TT, :].rearrange("(s p) d -> p s d", p=128), vt[b][:], reads=[t_vt[b]])
                    for k in range(NCH):
                        cx.op("pe", lambda e, k=k: e.matmul(ps[6][0:H, :], lhsT=fw[:, k, :], rhs=hnf[:, k, :],
                                                            start=(k == 0), stop=(k == NCH - 1)),
                              reads=[t_kvw, t_hnf], writes=[pst[6]])
                    cx.op("act", lambda e: e.activation(out=ef[:], in_=ps[6][0:H, :], func=AF.Exp, bias=nfb[:, 0:1], scale=-1.0),
                          reads=[pst[6], t_const], writes=[t_ef])
                    cx.op("act", lambda e: e.activation(out=ef[:], in_=ef[:], func=AF.Ln, bias=ones512[:, 0:1], scale=1.0),
                          reads=[t_ef, t_const], writes=[t_ef])
                    if tt == 0:
                        ini, rd = epsc[0:H, 2:3], [t_const]
                    else:
                        ini, rd = Ft[:, t0 - 1:t0], [t_F[tt - 1]]
                    cx.op("dve", lambda e, t0=t0, ini=ini: e.tensor_tensor_scan(
                        out=Ft[:, t0:t0 + TT], data0=ones512[:], data1=ef[:], initial=ini, op0=ALU.mult, op1=ALU.subtract),
                        reads=[t_ef, t_const] + rd, writes=[t_F[tt]])
                cx.barrier()
            with ExitStack() as st2:
                X = sb(st2, "f_X", [H, L])
                q3 = sb(st2, "f_q3", [H, 3, L], BF16)
                k3 = sb(st2, "f_k3", [H, 3, L], BF16)
                t_x = Trk()
                cx.op("dve", lambda e: e.tensor_scalar(out=X[:], in0=Ft[:], scalar1=8.0, scalar2=None, op0=ALU.mult),
                      reads=t_F, writes=[t_x])
                for i in range(3):
                    cx.op("dve", lambda e, i=i: e.tensor_copy(out=q3[:, i, :], in_=X[:]), reads=[t_x], writes=[t_x])
                    cx.op("dve", lambda e, i=i: e.tensor_scalar(out=k3[:, i, :], in0=q3[:, i, :], scalar1=-1.0, scalar2=None, op0=ALU.mult),
                          reads=[t_x], writes=[t_x])
                    if i < 2:
                        cx.op("dve", lambda e, i=i: e.tensor_tensor(out=X[:], in0=X[:], in1=q3[:, i, :], op=ALU.subtract),
                              reads=[t_x], writes=[t_x])
                cx.dma("sp", Fq[:, :, :], q3[:], reads=[t_x])
                cx.dma("sp", Fk[:, :, :], k3[:], reads=[t_x])
                cx.barrier()
            cx.barrier()

    def attn_stage():
        mi = 2
        with ExitStack() as st:
            wqg = sb(st, "wqg", [128, NCH, 2 * D], BF16)
            t_w = Trk()
            wsrc = fox_w_qg.rearrange("(c p) n -> p c n", p=128)
            for kc in range(NCH):
                cx.dma("pool", wqg[:, kc, :], wsrc[:, kc, :], writes=[t_w])
            gq = sb(st, "gq", [128, 1])
            cx.dma("sp", gq[:], gq_col[:, :], writes=[t_const])
            hts = [sb(st, "q_ht%d" % i, [128, NCH, TT]) for i in range(2)]
            t_ht = [Trk(), Trk()]
            sq = sb(st, "q_sq", [128, NCH, TT])
            t_sq = Trk()
            rinv = sb(st, "q_rinv", [128, TT])
            t_rinv = Trk()
            tmp = sb(st, "q_tmp", [128, NCH, TT])
            t_tmp = Trk()
            hnb = sb(st, "q_hnb", [128, NCH, TT], BF16)
            t_hnb = Trk()
            qt = [sb(st, "q_qt%d" % i, [128, NCH, TT], BF16) for i in range(2)]
            t_qt = [Trk(), Trk()]
            sgt = [sb(st, "q_sg%d" % i, [128, NCH, TT], BF16) for i in range(2)]
            t_sgt = [Trk(), Trk()]
            sqk = sb(st, "q_sqk", [128, TT])
            t_sqk = Trk()
            rk = sb(st, "q_rk", [128, TT])
            t_rk = Trk()
            for tt in range(NTT):
                b = tt % 2
                t0 = tt * TT
                cx.dma("sp", hts[b][:], h2[:, :, t0:t0 + TT].rearrange("c p t -> p c t"), writes=[t_ht[b]])
                norm_mod(mi, hts[b], t_ht[b], sq, t_sq, rinv, t_rinv, tmp, t_tmp, hnb, t_hnb, 0)
                for n in range(NCH):
                    pi = 1 + (n % 2)
                    for k in range(NCH):
                        cx.op("pe", lambda e, n=n, k=k, pi=pi: e.matmul(
                            ps[pi][:, :], lhsT=wqg[:, k, n * 128:(n + 1) * 128], rhs=hnb[:, k, :],
                            start=(k == 0), stop=(k == NCH - 1)), reads=[t_w, t_hnb], writes=[pst[pi]])
                    fin = head_norm(pi, 3, gq[:, 0:1], qt[b][:, n, :], sqk, t_sqk, rk, t_rk)
                    fin(t_qt[b])
                    pg = 4 + (n % 2)
                    for k in range(NCH):
                        cx.op("pe", lambda e, n=n, k=k, pg=pg: e.matmul(
                            ps[pg][:, :], lhsT=wqg[:, k, D + n * 128:D + (n + 1) * 128], rhs=hnb[:, k, :],
                            start=(k == 0), stop=(k == NCH - 1)), reads=[t_w, t_hnb], writes=[pst[pg]])
                    cx.op("act", lambda e, n=n, pg=pg, b=b: e.activation(out=sgt[b][:, n, :], in_=ps[pg][:, :], func=AF.Sigmoid),
                          reads=[pst[pg]], writes=[t_sgt[b]])
                cx.dma("sp", Qd[:, :, t0:t0 + TT].rearrange("c p t -> p c t"), qt[b][:], reads=[t_qt[b]])
                cx.dma("sp", SGd[:, :, t0:t0 + TT].rearrange("c p t -> p c t"), sgt[b][:], reads=[t_sgt[b]])
            cx.barrier()
        with ExitStack() as st:
            tri = sb(st, "tri", [128, 128], BF16)
            onesb = sb(st, "onesb", [128, 64], BF16)
            t_tri = Trk()
            cx.dma("pool", tri[:], tri_c[:, :], writes=[t_tri])
            identb = sb(st, "identb", [128, 128], BF16)
            cx.dma("pool", identb[:], ident[:, :], writes=[t_tri])
            cx.op("dve", lambda e: e.memset(onesb[:], 1.0), writes=[t_tri])
            Ka = [sb(st, "Ka%d" % i, [128, L], BF16) for i in range(2)]
            Qa = [sb(st, "Qa%d" % i, [128, L], BF16) for i in range(2)]
            Vh = [sb(st, "Vh%d" % i, [128, L // 128, HD + 1], BF16) for i in range(2)]
            SGh = [sb(st, "SGh%d" % i, [64, L], BF16) for i in range(2)]
            t_hd = [Trk(), Trk()]
            for i in range(2):
                cx.op("dve", lambda e, i=i: e.memset(Ka[i][64:128, :], 1.0), writes=[t_hd[i]])
                cx.op("dve", lambda e, i=i: e.memset(Qa[i][64:128, :], 1.0), writes=[t_hd[i]])
                cx.op("dve", lambda e, i=i: e.memset(Vh[i][:, :, HD:HD + 1], 1.0), writes=[t_hd[i]])
            NP = 3
            pt = [sb(st, "pt%d" % i, [128, TT], BF16) for i in range(NP)]
            t_pt = [Trk() for _ in range(NP)]
            rr = sb(st, "rr", [128, TT])
            t_rr = Trk()
            rb = sb(st, "rb", [64, TT])
            t_rb = Trk()
            ot = sb(st, "ot", [64, TT])
            t_ot = Trk()
            ob = [sb(st, "ob%d" % i, [64, TT], BF16) for i in range(2)]
            t_ob = [Trk(), Trk()]

            def load_head(h):
                hb = h % 2
                c, ro = h // 2, (h % 2) * 64
                cx.dma("sp", Ka[hb][0:64, :], Kd[c, ro:ro + 64, :], writes=[t_hd[hb]])
                cx.dma("sp", Ka[hb][67:70, :], Fk[h], writes=[t_hd[hb]])
                cx.dma("sp", Qa[hb][0:64, :], Qd[c, ro:ro + 64, :], writes=[t_hd[hb]])
                cx.dma("sp", Qa[hb][64:67, :], Fq[h], writes=[t_hd[hb]])
                cx.dma("sp", Vh[hb][:, :, 0:HD], Vd[:, h * HD:(h + 1) * HD].rearrange("(b p) d -> p b d", p=128),
                       writes=[t_hd[hb]])
                cx.dma("sp", SGh[hb][:], SGd[c, ro:ro + 64, :], writes=[t_hd[hb]])

            units = []
            ui = 0
            for h in range(H):
                for qc in range(NTT):
                    for kb in range(4 * qc + 4):
                        units.append((h, qc, kb, ui))
                    ui += 1

            def emit_s(u, idx):
                h, qc, kb, ui = u
                hb = h % 2
                i = kb - 4 * qc
                cs = max(0, i) * 128
                sbank = idx % 2
                pb = idx % NP
                cx.op("pe", lambda e: e.matmul(
                    ps[sbank][:, cs:TT], lhsT=Ka[hb][0:70, kb * 128:(kb + 1) * 128],
                    rhs=Qa[hb][0:70, qc * TT + cs:(qc + 1) * TT], start=True, stop=(i < 0)),
                    reads=[t_hd[hb]], writes=[pst[sbank]])
                if i >= 0:
                    cx.op("pe", lambda e: e.matmul(
                        ps[sbank][:, cs:cs + 128], lhsT=identb[:], rhs=tri[:], start=False, stop=True),
                        reads=[t_tri], writes=[pst[sbank]])
                cx.op("act", lambda e: e.activation(
                    out=pt[pb][:, cs:TT], in_=ps[sbank][:, cs:TT], func=AF.Exp, scale=0.125),
                    reads=[pst[sbank]], writes=[t_pt[pb]])

            def emit_pv(u, idx):
                h, qc, kb, ui = u
                hb = h % 2
                c, ro = h // 2, (h % 2) * 64
                i = kb - 4 * qc
                cs = max(0, i) * 128
                pb = idx % NP
                po = 2 + (ui % 2)
                pbb = 4 + (ui % 2)
                ub = ui % 2
                nkb = 4 * qc + 4
                cx.op("pe", lambda e: e.matmul(
                    ps[po][0:HD + 1, cs:TT], lhsT=Vh[hb][:, kb, :], rhs=pt[pb][:, cs:TT],
                    start=(kb == 0), stop=(kb == nkb - 1)), reads=[t_hd[hb], t_pt[pb]], writes=[pst[po]])
                if kb == nkb - 1:
                    cx.op("dve", lambda e: e.reciprocal(out=rr[64:65, :], in_=ps[po][64:65, :]), reads=[pst[po]], writes=[t_rr])
                    cx.op("pe", lambda e: e.matmul(ps[pbb][0:64, :], lhsT=onesf[64:65, 0:64], rhs=rr[64:65, :], start=True, stop=True),
                          reads=[t_rr, t_const], writes=[pst[pbb]])
                    cx.op("act", lambda e: e.copy(out=rb[:], in_=ps[pbb][0:64, :]), reads=[pst[pbb]], writes=[t_rb])
                    cx.op("dve", lambda e: e.tensor_tensor(out=ot[:], in0=rb[:], in1=ps[po][0:64, :], op=ALU.mult),
                          reads=[t_rb, pst[po]], writes=[t_ot])
                    cx.op("dve", lambda e: e.tensor_tensor(
                        out=ob[ub][:], in0=ot[:], in1=SGh[hb][:, qc * TT:(qc + 1) * TT], op=ALU.mult),
                        reads=[t_ot, t_hd[hb]], writes=[t_ob[ub]])
                    cx.dma("sp", Od[c, ro:ro + 64, qc * TT:(qc + 1) * TT], ob[ub][:], reads=[t_ob[ub]])

            load_head(0)
            for idx in range(len(units) + 1):
                if idx < len(units):
                    u = units[idx]
                    if u[1] == 0 and u[2] == 0 and u[0] + 1 < H and idx > 0:
                        pass
                    emit_s(u, idx)
                if idx >= 1:
                    emit_pv(units[idx - 1], idx - 1)
                    pu = units[idx - 1]
                    if idx < len(units) and units[idx][0] != pu[0]:
                        pass
                if idx < len(units):
                    u = units[idx]
                    if u[1] == 0 and u[2] == 1 - 1 and u[0] + 1 < H:
                        load_head(u[0] + 1)
            cx.barrier()
        with ExitStack() as st:
            wo = sb(st, "wo", [128, NCH, D], BF16)
            t_w = Trk()
            wsrc = fox_w_o.rearrange("(c p) n -> p c n", p=128)
            for kc in range(NCH):
                cx.dma("pool", wo[:, kc, :], wsrc[:, kc, :], writes=[t_w])
            otl = [sb(st, "o_ot%d" % i, [128, NCH, TT], BF16) for i in range(2)]
            t_otl = [Trk(), Trk()]
            hts = [sb(st, "o_ht%d" % i, [128, NCH, TT]) for i in range(2)]
            t_ht = [Trk(), Trk()]
            ho = [sb(st, "o_ho%d" % i, [128, NCH, TT]) for i in range(2)]
            t_ho = [Trk(), Trk()]
            it = 0
            for tt in range(NTT):
                b = tt % 2
                t0 = tt * TT
                cx.dma("sp", otl[b][:], Od[:, :, t0:t0 + TT].rearrange("c p t -> p c t"), writes=[t_otl[b]])
                cx.dma("sp", hts[b][:], h2[:, :, t0:t0 + TT].rearrange("c p t -> p c t"), writes=[t_ht[b]])
                for n in range(NCH):
                    pv = it % 4
                    it += 1
                    for k in range(NCH):
                        cx.op("pe", lambda e, n=n, k=k, pv=pv, b=b: e.matmul(
                            ps[pv][:, :], lhsT=wo[:, k, n * 128:(n + 1) * 128], rhs=otl[b][:, k, :],
                            start=(k == 0), stop=(k == NCH - 1)), reads=[t_w, t_otl[b]], writes=[pst[pv]])
                    cx.op("dve", lambda e, n=n, pv=pv, b=b: e.scalar_tensor_tensor(
                        out=ho[b][:, n, :], in0=ps[pv][:, :], scalar=gatec(mi, n), in1=hts[b][:, n, :],
                        op0=ALU.mult, op1=ALU.add), reads=[pst[pv], t_mods, t_ht[b]], writes=[t_ho[b]])
                cx.dma("sp", h3[:, :, t0:t0 + TT].rearrange("c p t -> p c t"), ho[b][:], reads=[t_ho[b]])
            cx.barrier()

    kv_stage()
    chk("kv")
    attn_stage()
    chk("at")
    moe_stage(1, 3, h3, hout)
    cx.barrier()
    return nc


def _state_layout(a):
    return np.ascontiguousarray(a.reshape(32, 2, 64).transpose(1, 2, 0).reshape(128, 32))


def _col_layout(v):
    return np.ascontiguousarray(v.reshape(-1, 128).T)


def make_inputs(inputs, b):
    f = np.float32
    m = {}
    m["xT"] = np.ascontiguousarray(inputs["x"][b].T).reshape(NCH, 128, L)
    m["c_col"] = _col_layout(inputs["c"][b])
    return m


def make_shared(inputs):
    f = np.float32
    m = {}
    m["ada_w"] = np.ascontiguousarray(inputs["ada_w"], dtype=f)
    m["ada_b"] = np.stack([_col_layout(inputs["ada_b"][i // 2, i % 2]) for i in range(4)])
    m["ln_g"] = np.stack([_col_layout(inputs["ln_g"][i // 2, i % 2]) for i in range(4)])
    m["kv_ada_w"] = np.ascontiguousarray(inputs["kv_ada_w"], dtype=f)
    m["kv_ada_b"] = _col_layout(inputs["kv_ada_b"])
    m["kv_g"] = _col_layout(inputs["kv_g"])
    m["s5_w_in"] = np.ascontiguousarray(inputs["s5_w_in"][0])
    m["s5_w_out"] = np.ascontiguousarray(inputs["s5_w_out"][0])
    ldt = np.repeat(inputs["s5_log_dt"][0][:, None], 64, axis=1)
    m["s5_par"] = np.stack([_state_layout(inputs["s5_lambda_re"][0]), _state_layout(inputs["s5_lambda_im"][0]),
                            _state_layout(ldt)])
    bp = np.zeros((2, 128, 32, 128), f)
    cp = np.zeros((2, 128, 32, 128), f)
    for k, (bn, cn) in enumerate([("s5_b_re", "s5_c_re"), ("s5_b_im", "s5_c_im")]):
        B_ = inputs[bn][0]
        C_ = inputs[cn][0]
        for j in range(32):
            o = (j % 4) * 32
            for gl in range(2):
                g = 2 * j + gl
                bp[k, gl * 64:(gl + 1) * 64, j, o + gl * 16:o + gl * 16 + 16] = B_[g]
                cp[k, gl * 64:(gl + 1) * 64, j, o + gl * 16:o + gl * 16 + 16] = C_[g].T
    m["s5_bpad"] = bp
    m["s5_cpad"] = cp
    m["s5_d"] = _col_layout(inputs["s5_d"][0])
    m["iota_t"] = np.ascontiguousarray(np.broadcast_to(np.arange(L, dtype=f)[None, :], (128, L)))
    m["ident"] = np.eye(128, dtype=f)
    m["moe_w1"] = np.ascontiguousarray(inputs["moe_w1"], dtype=f)
    m["moe_w3"] = np.ascontiguousarray(inputs["moe_w3"], dtype=f)
    m["moe_w2"] = np.ascontiguousarray(inputs["moe_w2"], dtype=f)
    m["moe_wr"] = np.ascontiguousarray(np.concatenate([inputs["moe_wg"], inputs["moe_we"]], axis=2), dtype=f)
    br = np.concatenate([inputs["moe_bg"], inputs["moe_be"]], axis=1).astype(f)
    m["moe_br"] = np.ascontiguousarray(np.broadcast_to(br[:, None, :], (2, 128, 36)))
    m["kv_w"] = np.ascontiguousarray(inputs["kv_w"], dtype=f)
    m["gk_col"] = np.ascontiguousarray(np.tile(inputs["k_norm_g"], 2)[:, None], dtype=f)
    m["gq_col"] = np.ascontiguousarray(np.tile(inputs["fox_q_norm_g"][0], 2)[:, None], dtype=f)
    m["fb_col"] = np.ascontiguousarray(inputs["kv_fb"][:, None], dtype=f)
    blk = np.zeros((128, 128), f)
    blk[0:64, 0:64] = 1.0
    blk[64:128, 64:128] = 1.0
    m["blk_c"] = blk
    m["tri_c"] = np.ascontiguousarray(np.tril(np.full((128, 128), -1.0e8, f), -1))
    m["fox_w_qg"] = np.ascontiguousarray(inputs["fox_w_qg"][0], dtype=f)
    m["fox_w_o"] = np.ascontiguousarray(inputs["fox_w_o"][0], dtype=f)
    sel = np.zeros((32, NE, 128), f)
    for e_ in range(NE):
        sel[e_, e_, :] = 1.0
    m["sel_c"] = sel
    return m


_NC_CACHE = {}


def kernel(**inputs):
    inputs = {k: np.asarray(v) for k, v in inputs.items()}
    if "nc" not in _NC_CACHE:
        _NC_CACHE["nc"] = build()
    nc = _NC_CACHE["nc"]
    shared = make_shared(inputs)
    in_maps = []
    for b in range(8):
        m = dict(shared)
        m.update(make_inputs(inputs, b))
        in_maps.append(m)
    res = run_bass_kernel_spmd(nc, in_maps, core_ids=list(range(8)))
    out = np.stack([np.ascontiguousarray(r["hout"].reshape(D, L).T) for r in res.results])
    return out.astype(np.float32)
```

```python
import math
from contextlib import ExitStack
import numpy as np
import concourse.bass as bass
import concourse.mybir as mybir
from concourse.bass_utils import run_bass_kernel_spmd

F32 = mybir.dt.float32
BF16 = mybir.dt.bfloat16
AF = mybir.ActivationFunctionType
ALU = mybir.AluOpType
AX = mybir.AxisListType

D = 1024
L = 4096
NCH = 8
TT = 512
NTT = L // TT
NG = 4
NE = 32
FE = 256
H = 16
HD = 64
EPS = 1e-6
MAGIC = 12582912.0
S2PI = 6.283180
HALFPI = 1.570795
GELU_C = 2.0 * math.sqrt(2.0 / math.pi)


class Trk:
    __slots__ = ("w", "r")

    def __init__(self):
        self.w = {}
        self.r = {}


class Ctx:
    def __init__(self, nc, es):
        self.nc = nc
        self.es = es
        self.engs = {"pe": nc.tensor, "act": nc.scalar, "dve": nc.vector, "pool": nc.gpsimd, "sp": nc.sync}
        self.sems = {}
        self.cnt = {}
        for k in ["pe", "act", "dve", "pool"]:
            self.sems[k] = es.enter_context(nc.semaphore("s_" + k))
            self.cnt[k] = 0
        self.seen = {k: {} for k in self.engs}
        self.dpool = {}
        self.dnext = {}
        for q, n in [("sp", 24), ("pool", 16), ("act", 6)]:
            keys = []
            for i in range(n):
                key = "d_%s%d" % (q, i)
                self.sems[key] = es.enter_context(nc.semaphore(key))
                self.cnt[key] = 0
                keys.append(key)
            self.dpool[q] = keys
            self.dnext[q] = 0

    def _wait(self, eng, deps):
        seen = self.seen[eng]
        for key, val in deps.items():
            if eng == "pe" and key == "pe":
                continue
            if seen.get(key, 0) < val:
                self.engs[eng].wait_ge(self.sems[key], val)
                seen[key] = val

    @staticmethod
    def _merge(dst, src):
        for k, v in src.items():
            if dst.get(k, 0) < v:
                dst[k] = v

    def _deps(self, reads, writes):
        deps = {}
        for t in reads:
            self._merge(deps, t.w)
        for t in writes:
            self._merge(deps, t.w)
            self._merge(deps, t.r)
        return deps

    def _record(self, ev, reads, writes):
        k, v = ev
        for t in reads:
            if t.r.get(k, 0) < v:
                t.r[k] = v
        for t in writes:
            t.w = {k: v}
            t.r = {}

    def op(self, eng, fn, reads=(), writes=()):
        self._wait(eng, self._deps(reads, writes))
        ins = fn(self.engs[eng])
        self.cnt[eng] += 1
        ins.then_inc(self.sems[eng], 1)
        self._record((eng, self.cnt[eng]), reads, writes)

    def dma(self, q, out, in_, reads=(), writes=(), **kw):
        keys = self.dpool[q]
        key = keys[self.dnext[q] % len(keys)]
        self.dnext[q] += 1
        deps = self._deps(reads, writes)
        if self.cnt[key] > 0:
            deps[key] = max(deps.get(key, 0), self.cnt[key])
        self._wait(q, deps)
        ins = self.engs[q].dma_start(out=out, in_=in_, **kw)
        self.cnt[key] += 16
        ins.then_inc(self.sems[key], 16)
        self._record((key, self.cnt[key]), reads, writes)

    def barrier(self, engines=("pe", "act", "dve", "pool", "sp")):
        allev = {k: v for k, v in self.cnt.items() if v > 0}
        for e in engines:
            self._wait(e, allev)


class _Stop(Exception):
    pass


def build(debug=False, stop=None):
    try:
        return _build(debug, stop)
    except _Stop as e:
        return e.args[0]


def _build(debug, stop):
    nc = bass.Bass("TRN2", target_bir_lowering=False)
    okind = "ExternalOutput" if debug else "Internal"

    def din(name, shape, dt=F32):
        return nc.dram_tensor(name, list(shape), dt, kind="ExternalInput").ap()

    def dscr(name, shape, dt=F32, out=False):
        return nc.dram_tensor(name, list(shape), dt, kind=("ExternalOutput" if out else okind)).ap()

    xT = din("xT", [NCH, 128, L])
    c_col = din("c_col", [128, NCH])
    ada_w = din("ada_w", [2, 2, D, 3 * D])
    ada_b = din("ada_b", [4, 128, 24])
    ln_g = din("ln_g", [4, 128, NCH])
    kv_ada_w = din("kv_ada_w", [D, 2 * D])
    kv_ada_b = din("kv_ada_b", [128, 16])
    kv_g = din("kv_g", [128, NCH])
    s5_w_in = din("s5_w_in", [D, D])
    s5_w_out = din("s5_w_out", [D, 2 * D])
    s5_par = din("s5_par", [3, 128, 32])
    s5_bpad = din("s5_bpad", [2, 128, 32, 128])
    s5_cpad = din("s5_cpad", [2, 128, 32, 128])
    s5_d = din("s5_d", [128, NCH])
    iota_t = din("iota_t", [128, L])
    ident = din("ident", [128, 128])
    hout = dscr("hout", [NCH, 128, L], out=True)
    moe_w1 = din("moe_w1", [2, NE, D, FE])
    moe_w3 = din("moe_w3", [2, NE, D, FE])
    moe_w2 = din("moe_w2", [2, NE, FE, D])
    moe_wr = din("moe_wr", [2, D, 36])
    moe_br = din("moe_br", [2, 128, 36])
    sel_c = din("sel_c", [32, NE, 128])
    h2 = dscr("h2", [NCH, 128, L])
    kv_w = din("kv_w", [D, 2 * D + H])
    gk_col = din("gk_col", [128, 1])
    gq_col = din("gq_col", [128, 1])
    fb_col = din("fb_col", [H, 1])
    blk_c = din("blk_c", [128, 128])
    tri_c = din("tri_c", [128, 128])
    fox_w_qg = din("fox_w_qg", [D, 2 * D])
    fox_w_o = din("fox_w_o", [D, D])
    Kd = dscr("Kd", [NCH, 128, L], BF16)
    Vd = dscr("Vd", [L, D], BF16)
    Fq = dscr("Fq", [H, 3, L], BF16)
    Fk = dscr("Fk", [H, 3, L], BF16)
    Qd = dscr("Qd", [NCH, 128, L], BF16)
    SGd = dscr("SGd", [NCH, 128, L], BF16)
    Od = dscr("Od", [NCH, 128, L], BF16)
    h3 = dscr("h3", [NCH, 128, L])

    h1 = dscr("h1", [NCH, 128, L])
    Gd = dscr("Gd", [NCH, 128, L], BF16)
    moddbg = dscr("moddbg", [128, 24 * 4 + 16])
    udbg = dscr("udbg", [NCH, 128, L], BF16) if debug else None

    es = ExitStack()
    cx = Ctx(nc, es)

    def chk(name):
        if stop == name:
            cx.barrier()
            raise _Stop(nc, es)

    _uid = [0]

    def sb(st, name, shape, dt=F32):
        _uid[0] += 1
        return st.enter_context(nc.sbuf_tensor("%s_%d" % (name, _uid[0]), list(shape), dt))

    ps = [es.enter_context(nc.psum_tensor("ps%d" % i, [128, 512], F32)) for i in range(8)]
    pst = [Trk() for _ in range(8)]

    mods = sb(es, "mods", [128, 24 * 4 + 16])
    modA = sb(es, "modA", [128, 5, NCH])
    t_mods = Trk()
    t_modA = Trk()
    identf = sb(es, "identf", [128, 128])
    onesf = sb(es, "onesf", [128, 128])
    t_const = Trk()
    cx.dma("sp", identf[:], ident[:, :], writes=[t_const])
    cx.op("dve", lambda e: e.memset(onesf[:], 1.0), writes=[t_const])

    with ExitStack() as st:
        ccol = sb(st, "ccol", [128, NCH])
        sc = sb(st, "sc", [128, NCH])
        bias_all = sb(st, "bias_all", [128, 24 * 4 + 16])
        g_all = sb(st, "g_all", [128, 5, NCH])
        wbuf = [sb(st, "wbuf%d" % i, [128, NCH, 1536]) for i in range(2)]
        t_w = [Trk(), Trk()]
        t_c = Trk()
        t_b = Trk()
        cx.dma("sp", ccol[:], c_col[:, :], writes=[t_c])
        for i in range(4):
            cx.dma("sp", bias_all[:, 24 * i:24 * (i + 1)], ada_b[i], writes=[t_b])
            cx.dma("sp", g_all[:, i, :], ln_g[i], writes=[t_b])
        cx.dma("sp", bias_all[:, 96:112], kv_ada_b[:, :], writes=[t_b])
        cx.dma("sp", g_all[:, 4, :], kv_g[:, :], writes=[t_b])
        cx.op("act", lambda e: e.activation(out=sc[:], in_=ccol[:], func=AF.Silu), reads=[t_c], writes=[t_c])
        units = []
        for i in range(4):
            for hf in range(2):
                units.append((ada_w[i // 2, i % 2], hf * 1536, 1536, 24 * i + 12 * hf))
        units.append((kv_ada_w, 0, 1024, 96))
        units.append((kv_ada_w, 1024, 1024, 104))
        for ui, (wap, c0, ncol, mcol) in enumerate(units):
            wb = wbuf[ui % 2]
            tw = t_w[ui % 2]
            src = wap.rearrange("(c p) n -> p c n", p=128)
            for kc in range(NCH):
                cx.dma("sp" if kc % 2 == 0 else "pool", wb[:, kc, 0:ncol], src[:, kc, c0:c0 + ncol], writes=[tw])
            pidx = ui % 2
            for n in range(ncol // 128):
                for kc in range(NCH):
                    cx.op("pe", lambda e, wb=wb, n=n, kc=kc, pidx=pidx: e.matmul(
                        ps[pidx][:, n:n + 1], lhsT=wb[:, kc, n * 128:(n + 1) * 128], rhs=sc[:, kc:kc + 1],
                        start=(kc == 0), stop=(kc == NCH - 1)), reads=[tw, t_c], writes=[pst[pidx]])
            nn = ncol // 128
            cx.op("dve", lambda e, pidx=pidx, nn=nn, mcol=mcol: e.tensor_tensor(
                out=mods[:, mcol:mcol + nn], in0=ps[pidx][:, 0:nn], in1=bias_all[:, mcol:mcol + nn], op=ALU.add),
                reads=[pst[pidx], t_b], writes=[t_mods])
        for i in range(5):
            sc0 = 24 * i + 8
            cx.op("dve", lambda e, i=i, sc0=sc0: e.scalar_tensor_tensor(
                out=modA[:, i, :], in0=mods[:, sc0:sc0 + 8], scalar=1.0, in1=g_all[:, i, :],
                op0=ALU.add, op1=ALU.mult), reads=[t_mods, t_b], writes=[t_modA])
        if debug:
            cx.dma("sp", moddbg[:, :], mods[:], reads=[t_mods])
        cx.barrier()

    chk("s0")

    def shiftc(i, c):
        return mods[:, 24 * i + c:24 * i + c + 1]

    def gatec(i, c):
        return mods[:, 24 * i + 16 + c:24 * i + 16 + c + 1]

    def scaleA(i, c):
        return modA[:, i, c:c + 1]

    def norm_mod(i, htile, t_h, sq, t_sq, rinv, t_rinv, tmp, t_tmp, hn_bf, t_hn, psi, hn_f=None, t_hnf=None):
        cx.op("act", lambda e: e.activation(out=sq[:], in_=htile[:], func=AF.Square), reads=[t_h], writes=[t_sq])
        for c in range(NCH):
            cx.op("pe", lambda e, c=c: e.matmul(ps[psi][:, :], lhsT=onesf[:], rhs=sq[:, c, :],
                                                start=(c == 0), stop=(c == NCH - 1)),
                  reads=[t_sq, t_const], writes=[pst[psi]])
        cx.op("act", lambda e: e.activation(out=rinv[:], in_=ps[psi][:, :], func=AF.Sqrt, bias=epsc[:, 0:1],
                                            scale=1.0 / D), reads=[pst[psi], t_const], writes=[t_rinv])
        cx.op("dve", lambda e: e.reciprocal(out=rinv[:], in_=rinv[:]), reads=[t_rinv], writes=[t_rinv])
        for c in range(NCH):
            cx.op("dve", lambda e, c=c: e.scalar_tensor_tensor(
                out=tmp[:, c, :], in0=htile[:, c, :], scalar=scaleA(i, c), in1=rinv[:],
                op0=ALU.mult, op1=ALU.mult), reads=[t_h, t_rinv, t_modA], writes=[t_tmp])
            if hn_f is not None:
                cx.op("act", lambda e, c=c: e.activation(out=hn_f[:, c, :], in_=tmp[:, c, :], func=AF.Identity,
                                                        bias=shiftc(i, c), scale=1.0),
                      reads=[t_tmp, t_mods], writes=[t_hnf])
                cx.op("pool", lambda e, c=c: e.tensor_copy(out=hn_bf[:, c, :], in_=hn_f[:, c, :]),
                      reads=[t_hnf], writes=[t_hn])
            else:
                cx.op("act", lambda e, c=c: e.activation(out=hn_bf[:, c, :], in_=tmp[:, c, :], func=AF.Identity,
                                                        bias=shiftc(i, c), scale=1.0),
                      reads=[t_tmp, t_mods], writes=[t_hn])

    epsc = sb(es, "epsc", [128, 4])
    cx.op("dve", lambda e: e.memset(epsc[:, 0:1], EPS), writes=[t_const])
    cx.op("dve", lambda e: e.memset(epsc[:, 1:2], HALFPI), writes=[t_const])
    cx.op("dve", lambda e: e.memset(epsc[:, 2:3], 0.0), writes=[t_const])
    cx.op("dve", lambda e: e.memset(epsc[:, 3:4], -MAGIC), writes=[t_const])

    with ExitStack() as st:
        u_bf = sb(st, "u_bf", [128, NCH, L], BF16)
        t_u = [Trk() for _ in range(NTT)]
        par = sb(st, "par", [128, 3, 32])
        t_par = Trk()
        for i in range(3):
            cx.dma("sp", par[:, i, :], s5_par[i], writes=[t_par])
        sp_ = sb(st, "s5small", [128, 22, 32])
        t_sp = Trk()

        def S(i):
            return sp_[:, i, :]
        lr, li, ldt = par[:, 0, :], par[:, 1, :], par[:, 2, :]
        DT, MAG, TH, THT, V, K_, FR, SIN, COS, ARE, AIM, DEN, CRE, CIM, T0, T1, C5, S5, U0, U1, U2, U3 = range(22)

        def dv(fn, eng="dve"):
            cx.op(eng, fn, reads=[t_par, t_sp, t_const], writes=[t_sp])
        dv(lambda e: e.activation(out=S(DT), in_=ldt, func=AF.Exp), "act")
        dv(lambda e: e.tensor_tensor(out=S(T0), in0=lr, in1=S(DT), op=ALU.mult))
        dv(lambda e: e.activation(out=S(MAG), in_=S(T0), func=AF.Exp), "act")
        dv(lambda e: e.tensor_tensor(out=S(TH), in0=li, in1=S(DT), op=ALU.mult))
        dv(lambda e: e.tensor_scalar(out=S(THT), in0=S(TH), scalar1=1.0 / (2 * math.pi), scalar2=None, op0=ALU.mult))
        dv(lambda e: e.tensor_scalar(out=S(V), in0=S(THT), scalar1=MAGIC, scalar2=None, op0=ALU.add))
        dv(lambda e: e.tensor_scalar(out=S(K_), in0=S(V), scalar1=-MAGIC, scalar2=None, op0=ALU.add))
        dv(lambda e: e.tensor_tensor(out=S(FR), in0=S(THT), in1=S(K_), op=ALU.subtract))
        dv(lambda e: e.activation(out=S(SIN), in_=S(FR), func=AF.Sin, scale=S2PI), "act")
        dv(lambda e: e.tensor_scalar(out=S(T0), in0=S(FR), scalar1=0.25, scalar2=-1.0, op0=ALU.is_gt, op1=ALU.mult))
        dv(lambda e: e.tensor_tensor(out=S(T0), in0=S(T0), in1=S(FR), op=ALU.add))
        dv(lambda e: e.activation(out=S(COS), in_=S(T0), func=AF.Sin, scale=S2PI, bias=epsc[:, 1:2]), "act")
        dv(lambda e: e.tensor_tensor(out=S(ARE), in0=S(MAG), in1=S(COS), op=ALU.mult))
        dv(lambda e: e.tensor_tensor(out=S(AIM), in0=S(MAG), in1=S(SIN), op=ALU.mult))
        dv(lambda e: e.tensor_tensor(out=S(T0), in0=lr, in1=lr, op=ALU.mult))
        dv(lambda e: e.tensor_tensor(out=S(T1), in0=li, in1=li, op=ALU.mult))
        dv(lambda e: e.tensor_tensor(out=S(DEN), in0=S(T0), in1=S(T1), op=ALU.add))
        dv(lambda e: e.reciprocal(out=S(DEN), in_=S(DEN)))
        dv(lambda e: e.tensor_scalar(out=S(T0), in0=S(ARE), scalar1=-1.0, scalar2=None, op0=ALU.add))
        dv(lambda e: e.tensor_tensor(out=S(CRE), in0=S(T0), in1=lr, op=ALU.mult))
        dv(lambda e: e.tensor_tensor(out=S(T1), in0=S(AIM), in1=li, op=ALU.mult))
        dv(lambda e: e.tensor_tensor(out=S(CRE), in0=S(CRE), in1=S(T1), op=ALU.add))
        dv(lambda e: e.tensor_tensor(out=S(CRE), in0=S(CRE), in1=S(DEN), op=ALU.mult))
        dv(lambda e: e.tensor_tensor(out=S(CIM), in0=S(AIM), in1=lr, op=ALU.mult))
        dv(lambda e: e.tensor_tensor(out=S(T1), in0=S(T0), in1=li, op=ALU.mult))
        dv(lambda e: e.tensor_tensor(out=S(CIM), in0=S(CIM), in1=S(T1), op=ALU.subtract))
        dv(lambda e: e.tensor_tensor(out=S(CIM), in0=S(CIM), in1=S(DEN), op=ALU.mult))
        dv(lambda e: e.tensor_copy(out=S(C5), in_=S(COS)))
        dv(lambda e: e.tensor_copy(out=S(S5), in_=S(SIN)))
        for _sq in range(9):
            dv(lambda e: e.tensor_tensor(out=S(U0), in0=S(C5), in1=S(C5), op=ALU.mult))
            dv(lambda e: e.tensor_tensor(out=S(U1), in0=S(S5), in1=S(S5), op=ALU.mult))
            dv(lambda e: e.scalar_tensor_tensor(out=S(U2), in0=S(C5), scalar=2.0, in1=S(S5), op0=ALU.mult, op1=ALU.mult))
            dv(lambda e: e.tensor_tensor(out=S(C5), in0=S(U0), in1=S(U1), op=ALU.subtract))
            dv(lambda e: e.tensor_copy(out=S(S5), in_=S(U2)))
        dv(lambda e: e.tensor_scalar(out=S(T1), in0=S(CIM), scalar1=-1.0, scalar2=None, op0=ALU.mult))

        Lre = sb(st, "Lre", [128, 32, 128], BF16)
        Lim = sb(st, "Lim", [128, 32, 128], BF16)
        Cre = sb(st, "Cre", [128, 32, 128], BF16)
        nCre = sb(st, "nCre", [128, 32, 128], BF16)
        nCim = sb(st, "nCim", [128, 32, 128], BF16)
        t_L = Trk()
        t_C = Trk()
        with ExitStack() as st2:
            bre = sb(st2, "bre", [128, 32, 128])
            bim = sb(st2, "bim", [128, 32, 128])
            t_bp = Trk()
            cx.dma("sp", bre[:], s5_bpad[0], writes=[t_bp])
            cx.dma("sp", bim[:], s5_bpad[1], writes=[t_bp])
            xa = [sb(st2, "xa%d" % i, [128, 128]) for i in range(2)]
            xb = [sb(st2, "xb%d" % i, [128, 128]) for i in range(2)]
            t_xa = [Trk(), Trk()]
            t_xb = [Trk(), Trk()]
            for j in range(32):
                b = j % 2
                cx.op("dve", lambda e, j=j, b=b: e.tensor_scalar(out=xa[b][:], in0=bim[:, j, :], scalar1=sp_[:, T1, j:j + 1],
                                                                 scalar2=None, op0=ALU.mult),
                      reads=[t_bp, t_sp], writes=[t_xa[b]])
                cx.op("dve", lambda e, j=j, b=b: e.scalar_tensor_tensor(out=xa[b][:], in0=bre[:, j, :], scalar=sp_[:, CRE, j:j + 1],
                                                                        in1=xa[b][:], op0=ALU.mult, op1=ALU.add),
                      reads=[t_bp, t_sp], writes=[t_xa[b]])
                cx.op("dve", lambda e, j=j, b=b: e.tensor_scalar(out=xb[b][:], in0=bre[:, j, :], scalar1=sp_[:, CIM, j:j + 1],
                                                                 scalar2=None, op0=ALU.mult),
                      reads=[t_bp, t_sp], writes=[t_xb[b]])
                cx.op("dve", lambda e, j=j, b=b: e.scalar_tensor_tensor(out=xb[b][:], in0=bim[:, j, :], scalar=sp_[:, CRE, j:j + 1],
                                                                        in1=xb[b][:], op0=ALU.mult, op1=ALU.add),
                      reads=[t_bp, t_sp], writes=[t_xb[b]])
                cx.op("pe", lambda e, b=b: e.transpose(out=ps[b][:, 0:128], in_=xa[b][:], identity=identf[:]),
                      reads=[t_xa[b], t_const], writes=[pst[b]])
                cx.op("pe", lambda e, b=b: e.transpose(out=ps[b][:, 128:256], in_=xb[b][:], identity=identf[:]),
                      reads=[t_xb[b], t_const], writes=[pst[b]])
                cx.op("act", lambda e, j=j, b=b: e.copy(out=Lre[:, j, :], in_=ps[b][:, 0:128]), reads=[pst[b]], writes=[t_L])
                cx.op("act", lambda e, j=j, b=b: e.copy(out=Lim[:, j, :], in_=ps[b][:, 128:256]), reads=[pst[b]], writes=[t_L])
            cx.dma("sp", bre[:], s5_cpad[0], reads=[], writes=[t_bp])
            cx.dma("sp", bim[:], s5_cpad[1], reads=[], writes=[t_bp])
            for q4 in range(4):
                sl = slice(q4 * 8, (q4 + 1) * 8)
                cx.op("act", lambda e, sl=sl: e.copy(out=Cre[:, sl, :], in_=bre[:, sl, :]), reads=[t_bp], writes=[t_C])
                cx.op("act", lambda e, sl=sl: e.mul(out=nCre[:, sl, :], in_=bre[:, sl, :], mul=-1.0), reads=[t_bp], writes=[t_C])
                cx.op("act", lambda e, sl=sl: e.mul(out=nCim[:, sl, :], in_=bim[:, sl, :], mul=-1.0), reads=[t_bp], writes=[t_C])
            cx.barrier()

        chk("a0")
        with ExitStack() as st2:
            win = sb(st2, "win", [128, NCH, D], BF16)
            t_win = Trk()
            wsrc = s5_w_in.rearrange("(c p) n -> p c n", p=128)
            for kc in range(NCH):
                cx.dma("pool", win[:, kc, :], wsrc[:, kc, :], writes=[t_win])
            hts = [sb(st2, "ht%d" % i, [128, NCH, TT]) for i in range(2)]
            t_ht = [Trk(), Trk()]
            sq = sb(st2, "sq", [128, NCH, TT])
            t_sq = Trk()
            rinv = sb(st2, "rinv", [128, TT])
            t_rinv = Trk()
            tmp = sb(st2, "tmpn", [128, NCH, TT])
            t_tmp = Trk()
            hnb = [sb(st2, "hnb%d" % i, [128, NCH, TT], BF16) for i in range(2)]
            t_hn = [Trk(), Trk()]
            for tt in range(NTT):
                b = tt % 2
                t0 = tt * TT
                cx.dma("sp", hts[b][:], xT[:, :, t0:t0 + TT].rearrange("c p t -> p c t"), writes=[t_ht[b]])
                norm_mod(0, hts[b], t_ht[b], sq, t_sq, rinv, t_rinv, tmp, t_tmp, hnb[b], t_hn[b], 0)
                for n in range(NCH):
                    pi = 1 + (n % 4)
                    for k in range(NCH):
                        cx.op("pe", lambda e, n=n, k=k, pi=pi, b=b: e.matmul(
                            ps[pi][:, :], lhsT=win[:, k, n * 128:(n + 1) * 128], rhs=hnb[b][:, k, :],
                            start=(k == 0), stop=(k == NCH - 1)), reads=[t_win, t_hn[b]], writes=[pst[pi]])
                    eng = "act" if n % 2 == 0 else "dve"
                    if eng == "act":
                        cx.op("act", lambda e, n=n, pi=pi, t0=t0: e.copy(out=u_bf[:, n, t0:t0 + TT], in_=ps[pi][:, :]),
                              reads=[pst[pi]], writes=[t_u[tt]])
                    else:
                        cx.op("dve", lambda e, n=n, pi=pi, t0=t0: e.tensor_copy(out=u_bf[:, n, t0:t0 + TT], in_=ps[pi][:, :]),
                              reads=[pst[pi]], writes=[t_u[tt]])
            cx.barrier()

        if debug:
            for n in range(NCH):
                cx.dma("sp", udbg[n], u_bf[:, n, :], reads=t_u)
        chk("a2")
        with ExitStack() as st2:
            iot = sb(st2, "iot", [128, TT])
            t_iot = Trk()
            cx.dma("sp", iot[:], iota_t[:, 0:TT], writes=[t_iot])
            dsk = sb(st2, "dsk", [128, NCH])
            cx.dma("sp", dsk[:], s5_d[:, :], writes=[t_iot])
            NB = 2

            def mk(name, dt=F32):
                return [sb(st2, "%s%d" % (name, i), [128, TT], dt) for i in range(NB)], [Trk() for _ in range(NB)]
            SNt, tSN = mk("SNt")
            CRt, tCR = mk("CRt")
            T1_, tT1 = mk("T1_")
            T2_, tT2 = mk("T2_")
            T3_, tT3 = mk("T3_")
            T4_, tT4 = mk("T4_")
            Vt, tVt = mk("Vt")
            Ft_, tFt = mk("Ftb")
            ini = [sb(st2, "ini%d" % i, [128, 4]) for i in range(NB)]
            t_ini = [Trk() for _ in range(NB)]
            XR, tXR = mk("XR")
            XI, tXI = mk("XI")
            SR, tSR = mk("SR")
            SI, tSI = mk("SI")
            P1, tP1 = mk("P1", BF16)
            P2, tP2 = mk("P2", BF16)
            P3, tP3 = mk("P3", BF16)
            P4, tP4 = mk("P4", BF16)
            ytmp = sb(st2, "ytmp", [128, L])
            t_y = [Trk() for _ in range(NTT)]
            gb = sb(st2, "gb", [128, L], BF16)
            t_gb = [Trk() for _ in range(NTT)]
            g1 = sb(st2, "g1", [128, TT])
            g2 = sb(st2, "g2", [128, TT])
            t_g1 = Trk()
            t_g2 = Trk()
            zero_init = epsc[:, 2:3]
            def gen_tables(j):
                thj = sp_[:, THT, j:j + 1]
                tb = j % 2
                cx.op("dve", lambda e: e.tensor_scalar(
                    out=Vt[tb][:], in0=iot[:, 0:TT], scalar1=thj, scalar2=MAGIC, op0=ALU.mult, op1=ALU.add),
                    reads=[t_iot, t_sp], writes=[tVt[tb]])
                cx.op("act", lambda e: e.activation(out=Vt[tb][:], in_=Vt[tb][:], func=AF.Identity,
                                                    bias=epsc[:, 3:4], scale=1.0),
                      reads=[tVt[tb], t_const], writes=[tVt[tb]])
                cx.op("dve", lambda e: e.scalar_tensor_tensor(
                    out=Ft_[tb][:], in0=iot[:, 0:TT], scalar=thj, in1=Vt[tb][:], op0=ALU.mult, op1=ALU.subtract),
                    reads=[t_iot, t_sp, tVt[tb]], writes=[tFt[tb]])
                cx.op("act", lambda e: e.activation(out=SNt[tb][:], in_=Ft_[tb][:], func=AF.Sin, scale=S2PI),
                      reads=[tFt[tb]], writes=[tSN[tb]])
                cx.op("dve", lambda e: e.tensor_scalar(
                    out=Vt[tb][:], in0=Ft_[tb][:], scalar1=0.25, scalar2=-1.0, op0=ALU.is_gt, op1=ALU.mult),
                    reads=[tFt[tb], tVt[tb]], writes=[tVt[tb]])
                cx.op("dve", lambda e: e.tensor_tensor(out=Vt[tb][:], in0=Vt[tb][:], in1=Ft_[tb][:], op=ALU.add),
                      reads=[tFt[tb], tVt[tb]], writes=[tVt[tb]])
                cx.op("act", lambda e: e.activation(out=CRt[tb][:], in_=Vt[tb][:], func=AF.Sin, scale=S2PI,
                                                    bias=epsc[:, 1:2]),
                      reads=[tVt[tb], t_const], writes=[tCR[tb]])

            class P_:
                pass
            pieces = []
            for c in range(NCH):
                for jj in range(4):
                    for tt in range(NTT):
                        p = P_()
                        p.c, p.jj, p.j, p.o, p.tt, p.s = c, jj, 4 * c + jj, jj * 32, tt, len(pieces)
                        p.b = p.s % NB
                        p.pb = 4 * (p.s % 2)
                        p.tb = p.j % 2
                        p.tsl = slice(tt * TT, (tt + 1) * TT)
                        pieces.append(p)

            def stg1(p):
                if p.tt == 0:
                    gen_tables(p.j)
                b, pb, tb, j, c, tsl, tt = p.b, p.pb, p.tb, p.j, p.c, p.tsl, p.tt
                cx.op("pe", lambda e: e.matmul(ps[pb][:, :], lhsT=Lre[:, j, :], rhs=u_bf[:, c, tsl], start=True, stop=True),
                      reads=[t_L, t_u[tt]], writes=[pst[pb]])
                cx.op("pe", lambda e: e.matmul(ps[pb + 1][:, :], lhsT=Lim[:, j, :], rhs=u_bf[:, c, tsl], start=True, stop=True),
                      reads=[t_L, t_u[tt]], writes=[pst[pb + 1]])
                cx.op("dve", lambda e: e.tensor_tensor(out=T1_[b][:], in0=CRt[tb][:], in1=ps[pb][:, :], op=ALU.mult),
                      reads=[tCR[tb], pst[pb]], writes=[tT1[b]])
                cx.op("dve", lambda e: e.tensor_tensor(out=T2_[b][:], in0=SNt[tb][:], in1=ps[pb + 1][:, :], op=ALU.mult),
                      reads=[tSN[tb], pst[pb + 1]], writes=[tT2[b]])
                cx.op("dve", lambda e: e.tensor_tensor(out=T3_[b][:], in0=CRt[tb][:], in1=ps[pb + 1][:, :], op=ALU.mult),
                      reads=[tCR[tb], pst[pb + 1]], writes=[tT3[b]])
                cx.op("dve", lambda e: e.tensor_tensor(out=T4_[b][:], in0=SNt[tb][:], in1=ps[pb][:, :], op=ALU.mult),
                      reads=[tSN[tb], pst[pb]], writes=[tT4[b]])

            def stg2(p):
                b = p.b
                cx.op("pool", lambda e: e.tensor_tensor(out=XR[b][:], in0=T1_[b][:], in1=T2_[b][:], op=ALU.add),
                      reads=[tT1[b], tT2[b]], writes=[tXR[b]])
                cx.op("pool", lambda e: e.tensor_tensor(out=XI[b][:], in0=T3_[b][:], in1=T4_[b][:], op=ALU.subtract),
                      reads=[tT3[b], tT4[b]], writes=[tXI[b]])

            def stg3(p):
                b, j, tt = p.b, p.j, p.tt
                pbuf = (b - 1) % NB
                magb = sp_[:, MAG, j:j + 1].to_broadcast([128, TT])
                if tt == 0:
                    ini_r, ini_i, rd = zero_init, zero_init, [t_const]
                else:
                    sre, sie = SR[pbuf][:, TT - 1:TT], SI[pbuf][:, TT - 1:TT]
                    c5, s5 = sp_[:, C5, j:j + 1], sp_[:, S5, j:j + 1]
                    rdp = [tSR[pbuf], tSI[pbuf], t_sp]
                    cx.op("dve", lambda e: e.tensor_tensor(out=ini[b][:, 2:3], in0=sie, in1=s5, op=ALU.mult),
                          reads=rdp, writes=[t_ini[b]])
                    cx.op("dve", lambda e: e.scalar_tensor_tensor(
                        out=ini[b][:, 0:1], in0=sre, scalar=c5, in1=ini[b][:, 2:3], op0=ALU.mult, op1=ALU.subtract),
                        reads=rdp + [t_ini[b]], writes=[t_ini[b]])
                    cx.op("dve", lambda e: e.tensor_tensor(out=ini[b][:, 3:4], in0=sie, in1=c5, op=ALU.mult),
                          reads=rdp + [t_ini[b]], writes=[t_ini[b]])
                    cx.op("dve", lambda e: e.scalar_tensor_tensor(
                        out=ini[b][:, 1:2], in0=sre, scalar=s5, in1=ini[b][:, 3:4], op0=ALU.mult, op1=ALU.add),
                        reads=rdp + [t_ini[b]], writes=[t_ini[b]])
                    ini_r, ini_i, rd = ini[b][:, 0:1], ini[b][:, 1:2], [t_ini[b]]
                cx.op("dve", lambda e: e.tensor_tensor_scan(
                    out=SR[b][:], data0=magb, data1=XR[b][:], initial=ini_r, op0=ALU.mult, op1=ALU.add),
                    reads=[tXR[b], t_sp] + rd, writes=[tSR[b]])
                cx.op("dve", lambda e: e.tensor_tensor_scan(
                    out=SI[b][:], data0=magb, data1=XI[b][:], initial=ini_i, op0=ALU.mult, op1=ALU.add),
                    reads=[tXI[b], t_sp] + rd, writes=[tSI[b]])

            def stg4(p):
                b, tb = p.b, p.tb
                cx.op("pool", lambda e: e.tensor_tensor(out=P1[b][:], in0=CRt[tb][:], in1=SR[b][:], op=ALU.mult),
                      reads=[tCR[tb], tSR[b]], writes=[tP1[b]])
                cx.op("pool", lambda e: e.tensor_tensor(out=P2[b][:], in0=SNt[tb][:], in1=SI[b][:], op=ALU.mult),
                      reads=[tSN[tb], tSI[b]], writes=[tP2[b]])
                cx.op("pool", lambda e: e.tensor_tensor(out=P3[b][:], in0=SNt[tb][:], in1=SR[b][:], op=ALU.mult),
                      reads=[tSN[tb], tSR[b]], writes=[tP3[b]])
                cx.op("pool", lambda e: e.tensor_tensor(out=P4[b][:], in0=CRt[tb][:], in1=SI[b][:], op=ALU.mult),
                      reads=[tCR[tb], tSI[b]], writes=[tP4[b]])

            def stg5(p):
                b, pb, j, c, o, tsl, tt = p.b, p.pb, p.j, p.c, p.o, p.tsl, p.tt
                py = pb + 2
                for idx, (cm, pp, tp) in enumerate([(Cre, P1, tP1), (nCre, P2, tP2), (nCim, P3, tP3), (nCim, P4, tP4)]):
                    cx.op("pe", lambda e, cm=cm, pp=pp, idx=idx: e.matmul(
                        ps[py][:, :], lhsT=cm[:, j, :], rhs=pp[b][:], start=(idx == 0), stop=(idx == 3)),
                        reads=[t_C, tp[b]], writes=[pst[py]])
                cx.op("dve", lambda e: e.scalar_tensor_tensor(
                    out=ytmp[o:o + 32, tsl], in0=u_bf[o:o + 32, c, tsl], scalar=dsk[o:o + 32, c:c + 1],
                    in1=ps[py][o:o + 32, :], op0=ALU.mult, op1=ALU.add),
                    reads=[t_u[tt], t_iot, pst[py]], writes=[t_y[tt]])
                if p.jj == 3 and tt == NTT - 1:
                    for t2 in range(NTT):
                        ts2 = slice(t2 * TT, (t2 + 1) * TT)
                        cx.op("act", lambda e, ts2=ts2: e.activation(out=g1[:], in_=ytmp[:, ts2], func=AF.Square),
                              reads=[t_y[t2]], writes=[t_g1])
                        cx.op("dve", lambda e: e.tensor_scalar(out=g1[:], in0=g1[:], scalar1=0.044715, scalar2=1.0,
                                                               op0=ALU.mult, op1=ALU.add), reads=[t_g1], writes=[t_g1])
                        cx.op("dve", lambda e, ts2=ts2: e.tensor_tensor(out=g2[:], in0=g1[:], in1=ytmp[:, ts2], op=ALU.mult),
                              reads=[t_g1, t_y[t2]], writes=[t_g2])
                        cx.op("act", lambda e: e.activation(out=g2[:], in_=g2[:], func=AF.Sigmoid, scale=GELU_C),
                              reads=[t_g2], writes=[t_g2])
                        cx.op("dve", lambda e, ts2=ts2: e.tensor_tensor(out=gb[:, ts2], in0=g2[:], in1=ytmp[:, ts2], op=ALU.mult),
                              reads=[t_g2, t_y[t2]], writes=[t_gb[t2]])
                    cx.dma("sp", Gd[c], gb[:], reads=t_gb, writes=[t_gd])
                    chk("a3g%d" % c)

            t_gd = Trk()
            npc = len(pieces)
            for s_ in range(npc + 2):
                if s_ < npc:
                    stg1(pieces[s_])
                    stg2(pieces[s_])
                if 1 <= s_ <= npc:
                    stg3(pieces[s_ - 1])
                    stg4(pieces[s_ - 1])
                if s_ >= 2:
                    stg5(pieces[s_ - 2])
            cx.barrier()
    cx.barrier()

    with ExitStack() as st:
        wout = sb(st, "wout", [128, NCH, 2 * D], BF16)
        t_wout = Trk()
        wsrc = s5_w_out.rearrange("(c p) n -> p c n", p=128)
        for kc in range(NCH):
            cx.dma("pool", wout[:, kc, :], wsrc[:, kc, :], writes=[t_wout])
        gt = [sb(st, "gt%d" % i, [128, NCH, TT], BF16) for i in range(2)]
        t_gt = [Trk(), Trk()]
        hts = [sb(st, "hto%d" % i, [128, NCH, TT]) for i in range(2)]
        t_ht = [Trk(), Trk()]
        ho = [sb(st, "ho%d" % i, [128, NCH, TT]) for i in range(2)]
        t_ho = [Trk(), Trk()]
        sg = [sb(st, "sg%d" % i, [128, TT]) for i in range(2)]
        t_sg = [Trk(), Trk()]
        mx = [sb(st, "mx%d" % i, [128, TT]) for i in range(2)]
        t_mx = [Trk(), Trk()]
        it = 0
        for tt in range(NTT):
            b = tt % 2
            t0 = tt * TT
            cx.dma("sp", gt[b][:], Gd[:, :, t0:t0 + TT].rearrange("c p t -> p c t"), writes=[t_gt[b]])
            cx.dma("sp", hts[b][:], xT[:, :, t0:t0 + TT].rearrange("c p t -> p c t"), writes=[t_ht[b]])
            for n in range(NCH):
                bb = it % 2
                pv = 2 * (it % 4)
                pg = pv + 1
                it += 1
                for k in range(NCH):
                    cx.op("pe", lambda e, n=n, k=k, pv=pv, b=b: e.matmul(
                        ps[pv][:, :], lhsT=wout[:, k, n * 128:(n + 1) * 128], rhs=gt[b][:, k, :],
                        start=(k == 0), stop=(k == NCH - 1)), reads=[t_wout, t_gt[b]], writes=[pst[pv]])
                for k in range(NCH):
                    cx.op("pe", lambda e, n=n, k=k, pg=pg, b=b: e.matmul(
                        ps[pg][:, :], lhsT=wout[:, k, D + n * 128:D + (n + 1) * 128], rhs=gt[b][:, k, :],
                        start=(k == 0), stop=(k == NCH - 1)), reads=[t_wout, t_gt[b]], writes=[pst[pg]])
                cx.op("act", lambda e, bb=bb, pg=pg: e.activation(out=sg[bb][:], in_=ps[pg][:, :], func=AF.Sigmoid),
                      reads=[pst[pg]], writes=[t_sg[bb]])
                cx.op("dve", lambda e, bb=bb, pv=pv: e.tensor_tensor(out=mx[bb][:], in0=sg[bb][:], in1=ps[pv][:, :], op=ALU.mult),
                      reads=[t_sg[bb], pst[pv]], writes=[t_mx[bb]])
                cx.op("dve", lambda e, bb=bb, n=n, b=b: e.scalar_tensor_tensor(
                    out=ho[b][:, n, :], in0=mx[bb][:], scalar=gatec(0, n), in1=hts[b][:, n, :], op0=ALU.mult, op1=ALU.add),
                    reads=[t_mx[bb], t_mods, t_ht[b]], writes=[t_ho[b]])
            cx.dma("sp", h1[:, :, t0:t0 + TT].rearrange("c p t -> p c t"), ho[b][:], reads=[t_ho[b]])
        cx.barrier()

    chk("a5")

    BIG = 1.0e30
    HT = 2048

    def moe_stage(l, mi, hin, hdst):
        with ExitStack() as st:
            acc = sb(st, "acc", [128, NCH, HT])
            t_acc = [[Trk() for _ in range(NCH)] for _ in range(4)]
            hn_all = sb(st, "hn_all", [128, NCH, HT], BF16)
            t_hna = [Trk() for _ in range(4)]
            gatesT = sb(st, "gatesT", [32, HT])
            t_gT = [Trk() for _ in range(4)]
            selt = sb(st, "selt", [32, NE, 128])
            t_sel = Trk()
            cx.dma("sp", selt[:], sel_c[:, :, :], writes=[t_sel])
            wr = sb(st, "wr", [128, NCH, 36])
            brt = sb(st, "brt", [128, 36])
            t_wr = Trk()
            cx.dma("sp", wr[:], moe_wr[l].rearrange("(c p) n -> p c n", p=128), writes=[t_wr])
            cx.dma("sp", brt[:], moe_br[l], writes=[t_wr])
            for half in range(L // HT):
                hbase = half * HT
                with ExitStack() as st2:
                    ht = sb(st2, "m_ht", [128, NCH, TT])
                    t_ht = Trk()
                    sq = sb(st2, "m_sq", [128, NCH, TT])
                    t_sq = Trk()
                    rinv = sb(st2, "m_rinv", [128, TT])
                    t_rinv = Trk()
                    tmp = sb(st2, "m_tmp", [128, NCH, TT])
                    t_tmp = Trk()
                    hnf = sb(st2, "m_hnf", [128, NCH, TT])
                    t_hn = Trk()
                    rt = sb(st2, "m_rt", [128, 8])
                    rb_lg = sb(st2, "rb_lg", [128, 4, 36])
                    rb_s = sb(st2, "rb_s", [128, 8, 4])
                    rb_g = sb(st2, "rb_g", [128, 3, 4, 4])
                    rb_m = sb(st2, "rb_m", [128, 4, 4, 32])
                    t_rt = Trk()
                    LG, GM, GMASK, GE, GS, PEN, MK, M1, MASK1, MK2, M2, MASK2, ED, W1, W2, GATES = (
                        slice(0, 36), slice(36, 37), slice(40, 44), slice(44, 48), slice(48, 49), slice(52, 56),
                        slice(56, 88), slice(88, 89), slice(96, 128), slice(128, 160), slice(160, 161),
                        slice(168, 200), slice(200, 201), slice(201, 202), slice(202, 203), slice(208, 240))
                    for tl in range(HT // TT):
                        t0 = hbase + tl * TT
                        cx.dma("sp", ht[:], hin[:, :, t0:t0 + TT].rearrange("c p t -> p c t"), writes=[t_ht])
                        norm_mod(mi, ht, t_ht, sq, t_sq, rinv, t_rinv, tmp, t_tmp,
                                 hn_all[:, :, tl * TT:(tl + 1) * TT], t_hna[tl], 7, hn_f=hnf, t_hnf=t_hn)
                        for sc_ in range(4):
                            for k in range(NCH):
                                cx.op("pe", lambda e, k=k, sc_=sc_: e.matmul(
                                    ps[6][:, sc_ * 36:(sc_ + 1) * 36], lhsT=hnf[:, k, sc_ * 128:(sc_ + 1) * 128], rhs=wr[:, k, :],
                                    start=(k == 0), stop=(k == NCH - 1)), reads=[t_hn, t_wr], writes=[pst[6]])

                        def dv(fn, eng="dve", extra=()):
                            cx.op(eng, fn, reads=[t_rt] + list(extra), writes=[t_rt])

                        def bc(ap2, n):
                            return ap2.unsqueeze(2).to_broadcast([128, 4, n])
                        LGv = rb_lg[:]
                        LGg = rb_lg[:, :, 0:4]
                        LGe = rb_lg[:, :, 4:36]
                        dv(lambda e: e.tensor_tensor(out=LGv, in0=ps[6][:, 0:144].rearrange("p (s n) -> p s n", s=4),
                                                     in1=brt[:].unsqueeze(1).to_broadcast([128, 4, 36]), op=ALU.add),
                           extra=[pst[6], t_wr])
                        dv(lambda e: e.reduce_max(out=rb_s[:, 0, :], in_=LGg, axis=AX.X))
                        dv(lambda e: e.tensor_tensor(out=rb_g[:, 0, :, :], in0=LGg, in1=bc(rb_s[:, 0, :], 4), op=ALU.is_ge))
                        dv(lambda e: e.tensor_tensor(out=rb_g[:, 1, :, :], in0=LGg, in1=bc(rb_s[:, 0, :], 4), op=ALU.subtract))
                        dv(lambda e: e.activation(out=rb_g[:, 1, :, :], in_=rb_g[:, 1, :, :], func=AF.Exp), "act")
                        dv(lambda e: e.reduce_sum(out=rb_s[:, 1, :], in_=rb_g[:, 1, :, :], axis=AX.X))
                        dv(lambda e: e.reciprocal(out=rb_s[:, 1, :], in_=rb_s[:, 1, :]))
                        dv(lambda e: e.tensor_scalar(out=rb_g[:, 2, :, :], in0=rb_g[:, 0, :, :], scalar1=-1.0, scalar2=BIG,
                                                     op0=ALU.add, op1=ALU.mult))
                        for s4 in range(4):
                            dv(lambda e, s4=s4: e.tensor_tensor(
                                out=rb_m[:, 0, s4, :].rearrange("p (g e) -> p g e", g=4),
                                in0=rb_lg[:, s4, 4:36].rearrange("p (g e) -> p g e", g=4),
                                in1=rb_g[:, 2, s4, :].unsqueeze(2).to_broadcast([128, 4, 8]), op=ALU.add))
                        dv(lambda e: e.reduce_max(out=rb_s[:, 2, :], in_=rb_m[:, 0, :, :], axis=AX.X))
                        dv(lambda e: e.tensor_tensor(out=rb_m[:, 1, :, :], in0=rb_m[:, 0, :, :], in1=bc(rb_s[:, 2, :], 32), op=ALU.is_ge))
                        dv(lambda e: e.scalar_tensor_tensor(out=rb_m[:, 2, :, :], in0=rb_m[:, 1, :, :], scalar=-BIG, in1=rb_m[:, 0, :, :],
                                                            op0=ALU.mult, op1=ALU.add))
                        dv(lambda e: e.reduce_max(out=rb_s[:, 3, :], in_=rb_m[:, 2, :, :], axis=AX.X))
                        dv(lambda e: e.tensor_tensor(out=rb_m[:, 3, :, :], in0=rb_m[:, 2, :, :], in1=bc(rb_s[:, 3, :], 32), op=ALU.is_ge))
                        dv(lambda e: e.tensor_tensor(out=rb_s[:, 4, :], in0=rb_s[:, 3, :], in1=rb_s[:, 2, :], op=ALU.subtract))
                        dv(lambda e: e.activation(out=rb_s[:, 4, :], in_=rb_s[:, 4, :], func=AF.Exp), "act")
                        dv(lambda e: e.tensor_scalar(out=rb_s[:, 5, :], in0=rb_s[:, 4, :], scalar1=1.0, scalar2=None, op0=ALU.add))
                        dv(lambda e: e.reciprocal(out=rb_s[:, 5, :], in_=rb_s[:, 5, :]))
                        dv(lambda e: e.tensor_tensor(out=rb_s[:, 5, :], in0=rb_s[:, 5, :], in1=rb_s[:, 1, :], op=ALU.mult))
                        dv(lambda e: e.tensor_tensor(out=rb_s[:, 6, :], in0=rb_s[:, 5, :], in1=rb_s[:, 4, :], op=ALU.mult))
                        dv(lambda e: e.tensor_tensor(out=rb_m[:, 1, :, :], in0=rb_m[:, 1, :, :], in1=bc(rb_s[:, 5, :], 32), op=ALU.mult))
                        dv(lambda e: e.tensor_tensor(out=rb_m[:, 3, :, :], in0=rb_m[:, 3, :, :], in1=bc(rb_s[:, 6, :], 32), op=ALU.mult))
                        dv(lambda e: e.tensor_tensor(out=rb_m[:, 0, :, :], in0=rb_m[:, 1, :, :], in1=rb_m[:, 3, :, :], op=ALU.add))
                        for s4 in range(4):
                            cx.op("pe", lambda e, s4=s4: e.transpose(out=ps[5][0:32, s4 * 128:(s4 + 1) * 128], in_=rb_m[:, 0, s4, :],
                                                                     identity=identf[:]),
                                  reads=[t_rt, t_const], writes=[pst[5]])
                        c0 = tl * TT
                        cx.op("act", lambda e, c0=c0: e.copy(out=gatesT[:, c0:c0 + TT], in_=ps[5][0:32, :]),
                              reads=[pst[5]], writes=[t_gT[tl]])
                    cx.barrier()
                with ExitStack() as st2:
                    w1b = [sb(st2, "w1b%d" % i, [128, NCH, FE], BF16) for i in range(2)]
                    w3b = [sb(st2, "w3b%d" % i, [128, NCH, FE], BF16) for i in range(2)]
                    w2b = [sb(st2, "w2b%d" % i, [128, 2, D], BF16) for i in range(2)]
                    t_wb = [Trk(), Trk()]
                    sl_ = [sb(st2, "sl%d" % i, [128, 2, TT], BF16) for i in range(2)]
                    t_sl = [[Trk(), Trk()], [Trk(), Trk()]]
                    tl_ = [sb(st2, "tl%d" % i, [128, 2, TT], BF16) for i in range(2)]
                    t_tl = [[Trk(), Trk()], [Trk(), Trk()]]
                    gs_ = [sb(st2, "gs%d" % i, [128, TT], BF16) for i in range(2)]
                    t_gs = [Trk(), Trk()]
                    ab = [sb(st2, "ab%d" % i, [128, 2, TT], BF16) for i in range(2)]
                    t_ab = [Trk(), Trk()]
                    oi = [0]
                    munits = [(e_, tl) for e_ in range(NE) for tl in range(HT // TT)]

                    def emit_h(e_, tl, idx):
                        wb = e_ % 2
                        b = idx % 2
                        tsl = slice(tl * TT, (tl + 1) * TT)
                        for hc in range(2):
                            for k in range(NCH):
                                cx.op("pe", lambda e, k=k, hc=hc: e.matmul(
                                    ps[hc][:, :], lhsT=w1b[wb][:, k, hc * 128:(hc + 1) * 128], rhs=hn_all[:, k, tsl],
                                    start=(k == 0), stop=(k == NCH - 1)), reads=[t_wb[wb], t_hna[tl]], writes=[pst[hc]])
                        for hc in range(2):
                            for k in range(NCH):
                                cx.op("pe", lambda e, k=k, hc=hc: e.matmul(
                                    ps[2 + hc][:, :], lhsT=w3b[wb][:, k, hc * 128:(hc + 1) * 128], rhs=hn_all[:, k, tsl],
                                    start=(k == 0), stop=(k == NCH - 1)), reads=[t_wb[wb], t_hna[tl]], writes=[pst[2 + hc]])
                        cx.op("pe", lambda e: e.matmul(
                            ps[4][:, :], lhsT=selt[:, e_, :], rhs=gatesT[:, tsl], start=True, stop=True),
                            reads=[t_sel, t_gT[tl]], writes=[pst[4]])
                        cx.op("act", lambda e: e.copy(out=gs_[b][:], in_=ps[4][:, :]), reads=[pst[4]], writes=[t_gs[b]])
                        for hc in range(2):
                            cx.op("act", lambda e, hc=hc: e.activation(out=sl_[b][:, hc, :], in_=ps[hc][:, :], func=AF.Silu),
                                  reads=[pst[hc]], writes=[t_sl[b][hc]])
                            cx.op("act", lambda e, hc=hc: e.copy(out=tl_[b][:, hc, :], in_=ps[2 + hc][:, :]),
                                  reads=[pst[2 + hc]], writes=[t_tl[b][hc]])
                        for hc in range(2):
                            cx.op("pool", lambda e, hc=hc: e.tensor_tensor(out=sl_[b][:, hc, :], in0=sl_[b][:, hc, :],
                                                                        in1=tl_[b][:, hc, :], op=ALU.mult),
                                  reads=[t_tl[b][hc]], writes=[t_sl[b][hc]])
                            cx.op("pool", lambda e, hc=hc: e.tensor_tensor(out=ab[b][:, hc, :], in0=sl_[b][:, hc, :],
                                                                        in1=gs_[b][:], op=ALU.mult),
                                  reads=[t_sl[b][hc], t_gs[b]], writes=[t_ab[b]])

                    def emit_o(e_, tl, idx):
                        wb = e_ % 2
                        b = idx % 2
                        tsl = slice(tl * TT, (tl + 1) * TT)
                        for n in range(NCH):
                            po = 5 + (oi[0] % 3)
                            oi[0] += 1
                            for hc in range(2):
                                cx.op("pe", lambda e, n=n, hc=hc, po=po: e.matmul(
                                    ps[po][:, :], lhsT=w2b[wb][:, hc, n * 128:(n + 1) * 128], rhs=ab[b][:, hc, :],
                                    start=(hc == 0), stop=(hc == 1)), reads=[t_wb[wb], t_ab[b]], writes=[pst[po]])
                            if e_ == 0:
                                cx.op("dve", lambda e, n=n, po=po: e.tensor_copy(out=acc[:, n, tsl], in_=ps[po][:, :]),
                                      reads=[pst[po]], writes=[t_acc[tl][n]])
                            else:
                                cx.op("dve", lambda e, n=n, po=po: e.tensor_tensor(
                                    out=acc[:, n, tsl], in0=acc[:, n, tsl], in1=ps[po][:, :], op=ALU.add),
                                    reads=[pst[po], t_acc[tl][n]], writes=[t_acc[tl][n]])

                    def load_w(e_):
                        wb = e_ % 2
                        cx.dma("pool", w1b[wb][:], moe_w1[l, e_].rearrange("(c p) f -> p c f", p=128), writes=[t_wb[wb]])
                        cx.dma("pool", w3b[wb][:], moe_w3[l, e_].rearrange("(c p) f -> p c f", p=128), writes=[t_wb[wb]])
                        cx.dma("pool", w2b[wb][:], moe_w2[l, e_].rearrange("(c p) f -> p c f", p=128), writes=[t_wb[wb]])

                    load_w(0)
                    for idx in range(len(munits) + 1):
                        if idx < len(munits):
                            emit_h(munits[idx][0], munits[idx][1], idx)
                        if idx >= 1:
                            emit_o(munits[idx - 1][0], munits[idx - 1][1], idx - 1)
                        if idx < len(munits) and munits[idx][1] == 0 and munits[idx][0] + 1 < NE:
                            load_w(munits[idx][0] + 1)
                    cx.barrier()
                with ExitStack() as st2:
                    hts = [sb(st2, "m3h%d" % i, [128, NCH, TT]) for i in range(2)]
                    t_h3 = [Trk(), Trk()]
                    ho = [sb(st2, "m3o%d" % i, [128, NCH, TT]) for i in range(2)]
                    t_o3 = [Trk(), Trk()]
                    for tl in range(HT // TT):
                        b = tl % 2
                        t0 = hbase + tl * TT
                        tsl = slice(tl * TT, (tl + 1) * TT)
                        cx.dma("sp", hts[b][:], hin[:, :, t0:t0 + TT].rearrange("c p t -> p c t"), writes=[t_h3[b]])
                        for n in range(NCH):
                            cx.op("dve", lambda e, n=n, b=b, tsl=tsl: e.scalar_tensor_tensor(
                                out=ho[b][:, n, :], in0=acc[:, n, tsl], scalar=gatec(mi, n), in1=hts[b][:, n, :],
                                op0=ALU.mult, op1=ALU.add), reads=[t_acc[tl][n], t_mods, t_h3[b]], writes=[t_o3[b]])
                        cx.dma("sp", hdst[:, :, t0:t0 + TT].rearrange("c p t -> p c t"), ho[b][:], reads=[t_o3[b]])
                    cx.barrier()
            cx.barrier()

    moe_stage(0, 1, h1, h2)
    chk("m0")

    blkf = sb(es, "blkf", [128, 128])
    cx.dma("sp", blkf[:], blk_c[:, :], writes=[t_const])

    def head_norm(psi, ps2i, gcol, out_ap, sqk, t_sqk, rk, t_rk):
        cx.op("act", lambda e: e.activation(out=sqk[:], in_=ps[psi][:, :], func=AF.Square), reads=[pst[psi]], writes=[t_sqk])
        cx.op("pe", lambda e: e.matmul(ps[ps2i][:, :], lhsT=blkf[:], rhs=sqk[:], start=True, stop=True),
              reads=[t_sqk, t_const], writes=[pst[ps2i]])
        cx.op("act", lambda e: e.activation(out=rk[:], in_=ps[ps2i][:, :], func=AF.Sqrt, bias=epsc[:, 0:1], scale=1.0 / HD),
              reads=[pst[ps2i], t_const], writes=[t_rk])
        cx.op("dve", lambda e: e.reciprocal(out=rk[:], in_=rk[:]), reads=[t_rk], writes=[t_rk])
        return lambda wr_t: cx.op("dve", lambda e: e.scalar_tensor_tensor(
            out=out_ap, in0=ps[psi][:, :], scalar=gcol, in1=rk[:], op0=ALU.mult, op1=ALU.mult),
            reads=[pst[psi], t_rk, t_const], writes=[wr_t])

    def kv_stage():
        with ExitStack() as st:
            kvw = sb(st, "kvw", [128, NCH, 2 * D], BF16)
            t_kvw = Trk()
            ksrc = kv_w.rearrange("(c p) n -> p c n", p=128)
            for kc in range(NCH):
                cx.dma("pool", kvw[:, kc, :], ksrc[:, kc, 0:2 * D], writes=[t_kvw])
            fw = sb(st, "fw", [128, NCH, H])
            for kc in range(NCH):
                cx.dma("sp", fw[:, kc, :], ksrc[:, kc, 2 * D:2 * D + H], writes=[t_kvw])
            gk = sb(st, "gk", [128, 1])
            nfb = sb(st, "nfb", [H, 1])
            ones512 = sb(st, "ones512", [H, TT])
            cx.dma("sp", gk[:], gk_col[:, :], writes=[t_const])
            cx.dma("sp", nfb[:], fb_col[:, :], writes=[t_const])
            cx.op("dve", lambda e: e.tensor_scalar(out=nfb[:], in0=nfb[:], scalar1=-1.0, scalar2=None, op0=ALU.mult),
                  reads=[t_const], writes=[t_const])
            cx.op("dve", lambda e: e.memset(ones512[:], 1.0), writes=[t_const])
            Ft = sb(st, "Ft", [H, L])
            t_F = [Trk() for _ in range(NTT)]
            with ExitStack() as st2:
                hts = [sb(st2, "k_ht%d" % i, [128, NCH, TT]) for i in range(2)]
                t_ht = [Trk(), Trk()]
                sq = sb(st2, "k_sq", [128, NCH, TT])
                t_sq = Trk()
                rinv = sb(st2, "k_rinv", [128, TT])
                t_rinv = Trk()
                tmp = sb(st2, "k_tmp", [128, NCH, TT])
                t_tmp = Trk()
                hnf = sb(st2, "k_hnf", [128, NCH, TT])
                t_hnf = Trk()
                hnb = sb(st2, "k_hnb", [128, NCH, TT], BF16)
                t_hnb = Trk()
                kt = [sb(st2, "k_kt%d" % i, [128, NCH, TT], BF16) for i in range(2)]
                t_kt = [Trk(), Trk()]
                vt = [sb(st2, "k_vt%d" % i, [128, 4, D], BF16) for i in range(2)]
                t_vt = [Trk(), Trk()]
                sqk = sb(st2, "k_sqk", [128, TT])
                t_sqk = Trk()
                rk = sb(st2, "k_rk", [128, TT])
                t_rk = Trk()
                ef = sb(st2, "k_ef", [H, TT])
                t_ef = Trk()
                for tt in range(NTT):
                    b = tt % 2
                    t0 = tt * TT
                    cx.dma("sp", hts[b][:], h2[:, :, t0:t0 + TT].rearrange("c p t -> p c t"), writes=[t_ht[b]])
                    norm_mod(4, hts[b], t_ht[b], sq, t_sq, rinv, t_rinv, tmp, t_tmp, hnb, t_hnb, 0, hn_f=hnf, t_hnf=t_hnf)
                    for n in range(NCH):
                        pi = 1 + (n % 2)
                        for k in range(NCH):
                            cx.op("pe", lambda e, n=n, k=k, pi=pi: e.matmul(
                                ps[pi][:, :], lhsT=kvw[:, k, n * 128:(n + 1) * 128], rhs=hnb[:, k, :],
                                start=(k == 0), stop=(k == NCH - 1)), reads=[t_kvw, t_hnb], writes=[pst[pi]])
                        fin = head_norm(pi, 3, gk[:, 0:1], kt[b][:, n, :], sqk, t_sqk, rk, t_rk)
                        fin(t_kt[b])
                    cx.dma("sp", Kd[:, :, t0:t0 + TT].rearrange("c p t -> p c t"), kt[b][:], reads=[t_kt[b]])
                    for s_ in range(4):
                        for hf in range(2):
                            pi = 4 + ((s_ * 2 + hf) % 2)
                            for k in range(NCH):
                                cx.op("pe", lambda e, s_=s_, hf=hf, k=k, pi=pi: e.matmul(
                                    ps[pi][:, :], lhsT=hnb[:, k, s_ * 128:(s_ + 1) * 128],
                                    rhs=kvw[:, k, D + hf * 512:D + (hf + 1) * 512],
                                    start=(k == 0), stop=(k == NCH - 1)), reads=[t_kvw, t_hnb], writes=[pst[pi]])
                            if hf == 0:
                                cx.op("act", lambda e, s_=s_, hf=hf, pi=pi, b=b: e.copy(out=vt[b][:, s_, hf * 512:(hf + 1) * 512], in_=ps[pi][:, :]),
                                      reads=[pst[pi]], writes=[t_vt[b]])
                            else:
                                cx.op("dve", lambda e, s_=s_, hf=hf, pi=pi, b=b: e.tensor_copy(out=vt[b][:, s_, hf * 512:(hf + 1) * 512], in_=ps[pi][:, :]),
                                      reads=[pst[pi]], writes=[t_vt[b]])
                    cx.dma("sp", Vd[t0:t0 + TT, :].rearrange("(s p) d -> p s d", p=128), vt[b][:], reads=[t_vt[b]])
                    for k in range(NCH):
                        cx.op("pe", lambda e, k=k: e.matmul(ps[6][0:H, :], lhsT=fw[:, k, :], rhs=hnf[:, k, :],
                                                            start=(k == 0), stop=(k == NCH - 1)),
                              reads=[t_kvw, t_hnf], writes=[pst[6]])
                    cx.op("act", lambda e: e.activation(out=ef[:], in_=ps[6][0:H, :], func=AF.Exp, bias=nfb[:, 0:1], scale=-1.0),
                          reads=[pst[6], t_const], writes=[t_ef])
                    cx.op("act", lambda e: e.activation(out=ef[:], in_=ef[:], func=AF.Ln, bias=ones512[:, 0:1], scale=1.0),
                          reads=[t_ef, t_const], writes=[t_ef])
                    if tt == 0:
                        ini, rd = epsc[0:H, 2:3], [t_const]
                    else:
                        ini, rd = Ft[:, t0 - 1:t0], [t_F[tt - 1]]
                    cx.op("dve", lambda e, t0=t0, ini=ini: e.tensor_tensor_scan(
                        out=Ft[:, t0:t0 + TT], data0=ones512[:], data1=ef[:], initial=ini, op0=ALU.mult, op1=ALU.subtract),
                        reads=[t_ef, t_const] + rd, writes=[t_F[tt]])
                cx.barrier()
            with ExitStack() as st2:
                X = sb(st2, "f_X", [H, L])
                q3 = sb(st2, "f_q3", [H, 3, L], BF16)
                k3 = sb(st2, "f_k3", [H, 3, L], BF16)
                t_x = Trk()
                cx.op("dve", lambda e: e.tensor_scalar(out=X[:], in0=Ft[:], scalar1=8.0, scalar2=None, op0=ALU.mult),
                      reads=t_F, writes=[t_x])
                for i in range(3):
                    cx.op("dve", lambda e, i=i: e.tensor_copy(out=q3[:, i, :], in_=X[:]), reads=[t_x], writes=[t_x])
                    cx.op("dve", lambda e, i=i: e.tensor_scalar(out=k3[:, i, :], in0=q3[:, i, :], scalar1=-1.0, scalar2=None, op0=ALU.mult),
                          reads=[t_x], writes=[t_x])
                    if i < 2:
                        cx.op("dve", lambda e, i=i: e.tensor_tensor(out=X[:], in0=X[:], in1=q3[:, i, :], op=ALU.subtract),
                              reads=[t_x], writes=[t_x])
                cx.dma("sp", Fq[:, :, :], q3[:], reads=[t_x])
                cx.dma("sp", Fk[:, :, :], k3[:], reads=[t_x])
                cx.barrier()
            cx.barrier()

    def attn_stage():
        mi = 2
        with ExitStack() as st:
            wqg = sb(st, "wqg", [128, NCH, 2 * D], BF16)
            t_w = Trk()
            wsrc = fox_w_qg.rearrange("(c p) n -> p c n", p=128)
            for kc in range(NCH):
                cx.dma("pool", wqg[:, kc, :], wsrc[:, kc, :], writes=[t_w])
            gq = sb(st, "gq", [128, 1])
            cx.dma("sp", gq[:], gq_col[:, :], writes=[t_const])
            hts = [sb(st, "q_ht%d" % i, [128, NCH, TT]) for i in range(2)]
            t_ht = [Trk(), Trk()]
            sq = sb(st, "q_sq", [128, NCH, TT])
            t_sq = Trk()
            rinv = sb(st, "q_rinv", [128, TT])
            t_rinv = Trk()
            tmp = sb(st, "q_tmp", [128, NCH, TT])
            t_tmp = Trk()
            hnb = sb(st, "q_hnb", [128, NCH, TT], BF16)
            t_hnb = Trk()
            qt = [sb(st, "q_qt%d" % i, [128, NCH, TT], BF16) for i in range(2)]
            t_qt = [Trk(), Trk()]
            sgt = [sb(st, "q_sg%d" % i, [128, NCH, TT], BF16) for i in range(2)]
            t_sgt = [Trk(), Trk()]
            sqk = sb(st, "q_sqk", [128, TT])
            t_sqk = Trk()
            rk = sb(st, "q_rk", [128, TT])
            t_rk = Trk()
            for tt in range(NTT):
                b = tt % 2
                t0 = tt * TT
                cx.dma("sp", hts[b][:], h2[:, :, t0:t0 + TT].rearrange("c p t -> p c t"), writes=[t_ht[b]])
                norm_mod(mi, hts[b], t_ht[b], sq, t_sq, rinv, t_rinv, tmp, t_tmp, hnb, t_hnb, 0)
                for n in range(NCH):
                    pi = 1 + (n % 2)
                    for k in range(NCH):
                        cx.op("pe", lambda e, n=n, k=k, pi=pi: e.matmul(
                            ps[pi][:, :], lhsT=wqg[:, k, n * 128:(n + 1) * 128], rhs=hnb[:, k, :],
                            start=(k == 0), stop=(k == NCH - 1)), reads=[t_w, t_hnb], writes=[pst[pi]])
                    fin = head_norm(pi, 3, gq[:, 0:1], qt[b][:, n, :], sqk, t_sqk, rk, t_rk)
                    fin(t_qt[b])
                    pg = 4 + (n % 2)
                    for k in range(NCH):
                        cx.op("pe", lambda e, n=n, k=k, pg=pg: e.matmul(
                            ps[pg][:, :], lhsT=wqg[:, k, D + n * 128:D + (n + 1) * 128], rhs=hnb[:, k, :],
                            start=(k == 0), stop=(k == NCH - 1)), reads=[t_w, t_hnb], writes=[pst[pg]])
                    cx.op("act", lambda e, n=n, pg=pg, b=b: e.activation(out=sgt[b][:, n, :], in_=ps[pg][:, :], func=AF.Sigmoid),
                          reads=[pst[pg]], writes=[t_sgt[b]])
                cx.dma("sp", Qd[:, :, t0:t0 + TT].rearrange("c p t -> p c t"), qt[b][:], reads=[t_qt[b]])
                cx.dma("sp", SGd[:, :, t0:t0 + TT].rearrange("c p t -> p c t"), sgt[b][:], reads=[t_sgt[b]])
            cx.barrier()
        with ExitStack() as st:
            tri = sb(st, "tri", [128, 128], BF16)
            onesb = sb(st, "onesb", [128, 64], BF16)
            t_tri = Trk()
            cx.dma("pool", tri[:], tri_c[:, :], writes=[t_tri])
            identb = sb(st, "identb", [128, 128], BF16)
            cx.dma("pool", identb[:], ident[:, :], writes=[t_tri])
            cx.op("dve", lambda e: e.memset(onesb[:], 1.0), writes=[t_tri])
            Ka = [sb(st, "Ka%d" % i, [128, L], BF16) for i in range(2)]
            Qa = [sb(st, "Qa%d" % i, [128, L], BF16) for i in range(2)]
            Vh = [sb(st, "Vh%d" % i, [128, L // 128, HD + 1], BF16) for i in range(2)]
            SGh = [sb(st, "SGh%d" % i, [64, L], BF16) for i in range(2)]
            t_hd = [Trk(), Trk()]
            for i in range(2):
                cx.op("dve", lambda e, i=i: e.memset(Ka[i][64:128, :], 1.0), writes=[t_hd[i]])
                cx.op("dve", lambda e, i=i: e.memset(Qa[i][64:128, :], 1.0), writes=[t_hd[i]])
                cx.op("dve", lambda e, i=i: e.memset(Vh[i][:, :, HD:HD + 1], 1.0), writes=[t_hd[i]])
            NP = 3
            pt = [sb(st, "pt%d" % i, [128, TT], BF16) for i in range(NP)]
            t_pt = [Trk() for _ in range(NP)]
            rr = sb(st, "rr", [128, TT])
            t_rr = Trk()
            rb = sb(st, "rb", [64, TT])
            t_rb = Trk()
            ot = sb(st, "ot", [64, TT])
            t_ot = Trk()
            ob = [sb(st, "ob%d" % i, [64, TT], BF16) for i in range(2)]
            t_ob = [Trk(), Trk()]

            def load_head(h):
                hb = h % 2
                c, ro = h // 2, (h % 2) * 64
                cx.dma("sp", Ka[hb][0:64, :], Kd[c, ro:ro + 64, :], writes=[t_hd[hb]])
                cx.dma("sp", Ka[hb][67:70, :], Fk[h], writes=[t_hd[hb]])
                cx.dma("sp", Qa[hb][0:64, :], Qd[c, ro:ro + 64, :], writes=[t_hd[hb]])
                cx.dma("sp", Qa[hb][64:67, :], Fq[h], writes=[t_hd[hb]])
                cx.dma("sp", Vh[hb][:, :, 0:HD], Vd[:, h * HD:(h + 1) * HD].rearrange("(b p) d -> p b d", p=128),
                       writes=[t_hd[hb]])
                cx.dma("sp", SGh[hb][:], SGd[c, ro:ro + 64, :], writes=[t_hd[hb]])

            units = []
            ui = 0
            for h in range(H):
                for qc in range(NTT):
                    for kb in range(4 * qc + 4):
                        units.append((h, qc, kb, ui))
                    ui += 1

            def emit_s(u, idx):
                h, qc, kb, ui = u
                hb = h % 2
                i = kb - 4 * qc
                cs = max(0, i) * 128
                sbank = idx % 2
                pb = idx % NP
                cx.op("pe", lambda e: e.matmul(
                    ps[sbank][:, cs:TT], lhsT=Ka[hb][0:70, kb * 128:(kb + 1) * 128],
                    rhs=Qa[hb][0:70, qc * TT + cs:(qc + 1) * TT], start=True, stop=(i < 0)),
                    reads=[t_hd[hb]], writes=[pst[sbank]])
                if i >= 0:
                    cx.op("pe", lambda e: e.matmul(
                        ps[sbank][:, cs:cs + 128], lhsT=identb[:], rhs=tri[:], start=False, stop=True),
                        reads=[t_tri], writes=[pst[sbank]])
                cx.op("act", lambda e: e.activation(
                    out=pt[pb][:, cs:TT], in_=ps[sbank][:, cs:TT], func=AF.Exp, scale=0.125),
                    reads=[pst[sbank]], writes=[t_pt[pb]])

            def emit_pv(u, idx):
                h, qc, kb, ui = u
                hb = h % 2
                c, ro = h // 2, (h % 2) * 64
                i = kb - 4 * qc
                cs = max(0, i) * 128
                pb = idx % NP
                po = 2 + (ui % 2)
                pbb = 4 + (ui % 2)
                ub = ui % 2
                nkb = 4 * qc + 4
                cx.op("pe", lambda e: e.matmul(
                    ps[po][0:HD + 1, cs:TT], lhsT=Vh[hb][:, kb, :], rhs=pt[pb][:, cs:TT],
                    start=(kb == 0), stop=(kb == nkb - 1)), reads=[t_hd[hb], t_pt[pb]], writes=[pst[po]])
                if kb == nkb - 1:
                    cx.op("dve", lambda e: e.reciprocal(out=rr[64:65, :], in_=ps[po][64:65, :]), reads=[pst[po]], writes=[t_rr])
                    cx.op("pe", lambda e: e.matmul(ps[pbb][0:64, :], lhsT=onesf[64:65, 0:64], rhs=rr[64:65, :], start=True, stop=True),
                          reads=[t_rr, t_const], writes=[pst[pbb]])
                    cx.op("act", lambda e: e.copy(out=rb[:], in_=ps[pbb][0:64, :]), reads=[pst[pbb]], writes=[t_rb])
                    cx.op("dve", lambda e: e.tensor_tensor(out=ot[:], in0=rb[:], in1=ps[po][0:64, :], op=ALU.mult),
                          reads=[t_rb, pst[po]], writes=[t_ot])
                    cx.op("dve", lambda e: e.tensor_tensor(
                        out=ob[ub][:], in0=ot[:], in1=SGh[hb][:, qc * TT:(qc + 1) * TT], op=ALU.mult),
                        reads=[t_ot, t_hd[hb]], writes=[t_ob[ub]])
                    cx.dma("sp", Od[c, ro:ro + 64, qc * TT:(qc + 1) * TT], ob[ub][:], reads=[t_ob[ub]])

            load_head(0)
            for idx in range(len(units) + 1):
                if idx < len(units):
                    u = units[idx]
                    if u[1] == 0 and u[2] == 0 and u[0] + 1 < H and idx > 0:
                        pass
                    emit_s(u, idx)
                if idx >= 1:
                    emit_pv(units[idx - 1], idx - 1)
                    pu = units[idx - 1]
                    if idx < len(units) and units[idx][0] != pu[0]:
                        pass
                if idx < len(units):
                    u = units[idx]
                    if u[1] == 0 and u[2] == 1 - 1 and u[0] + 1 < H:
                        load_head(u[0] + 1)
            cx.barrier()
        with ExitStack() as st:
            wo = sb(st, "wo", [128, NCH, D], BF16)
            t_w = Trk()
            wsrc = fox_w_o.rearrange("(c p) n -> p c n", p=128)
            for kc in range(NCH):
                cx.dma("pool", wo[:, kc, :], wsrc[:, kc, :], writes=[t_w])
            otl = [sb(st, "o_ot%d" % i, [128, NCH, TT], BF16) for i in range(2)]
            t_otl = [Trk(), Trk()]
            hts = [sb(st, "o_ht%d" % i, [128, NCH, TT]) for i in range(2)]
            t_ht = [Trk(), Trk()]
            ho = [sb(st, "o_ho%d" % i, [128, NCH, TT]) for i in range(2)]
            t_ho = [Trk(), Trk()]
            it = 0
            for tt in range(NTT):
                b = tt % 2
                t0 = tt * TT
                cx.dma("sp", otl[b][:], Od[:, :, t0:t0 + TT].rearrange("c p t -> p c t"), writes=[t_otl[b]])
                cx.dma("sp", hts[b][:], h2[:, :, t0:t0 + TT].rearrange("c p t -> p c t"), writes=[t_ht[b]])
                for n in range(NCH):
                    pv = it % 4
                    it += 1
                    for k in range(NCH):
                        cx.op("pe", lambda e, n=n, k=k, pv=pv, b=b: e.matmul(
                            ps[pv][:, :], lhsT=wo[:, k, n * 128:(n + 1) * 128], rhs=otl[b][:, k, :],
                            start=(k == 0), stop=(k == NCH - 1)), reads=[t_w, t_otl[b]], writes=[pst[pv]])
                    cx.op("dve", lambda e, n=n, pv=pv, b=b: e.scalar_tensor_tensor(
                        out=ho[b][:, n, :], in0=ps[pv][:, :], scalar=gatec(mi, n), in1=hts[b][:, n, :],
                        op0=ALU.mult, op1=ALU.add), reads=[pst[pv], t_mods, t_ht[b]], writes=[t_ho[b]])
                cx.dma("sp", h3[:, :, t0:t0 + TT].rearrange("c p t -> p c t"), ho[b][:], reads=[t_ho[b]])
            cx.barrier()

    kv_stage()
    chk("kv")
    attn_stage()
    chk("at")
    moe_stage(1, 3, h3, hout)
    cx.barrier()
    return nc


def _state_layout(a):
    return np.ascontiguousarray(a.reshape(32, 2, 64).transpose(1, 2, 0).reshape(128, 32))


def _col_layout(v):
    return np.ascontiguousarray(v.reshape(-1, 128).T)


def make_inputs(inputs, b):
    f = np.float32
    m = {}
    m["xT"] = np.ascontiguousarray(inputs["x"][b].T).reshape(NCH, 128, L)
    m["c_col"] = _col_layout(inputs["c"][b])
    return m


def make_shared(inputs):
    f = np.float32
    m = {}
    m["ada_w"] = np.ascontiguousarray(inputs["ada_w"], dtype=f)
    m["ada_b"] = np.stack([_col_layout(inputs["ada_b"][i // 2, i % 2]) for i in range(4)])
    m["ln_g"] = np.stack([_col_layout(inputs["ln_g"][i // 2, i % 2]) for i in range(4)])
    m["kv_ada_w"] = np.ascontiguousarray(inputs["kv_ada_w"], dtype=f)
    m["kv_ada_b"] = _col_layout(inputs["kv_ada_b"])
    m["kv_g"] = _col_layout(inputs["kv_g"])
    m["s5_w_in"] = np.ascontiguousarray(inputs["s5_w_in"][0])
    m["s5_w_out"] = np.ascontiguousarray(inputs["s5_w_out"][0])
    ldt = np.repeat(inputs["s5_log_dt"][0][:, None], 64, axis=1)
    m["s5_par"] = np.stack([_state_layout(inputs["s5_lambda_re"][0]), _state_layout(inputs["s5_lambda_im"][0]),
                            _state_layout(ldt)])
    bp = np.zeros((2, 128, 32, 128), f)
    cp = np.zeros((2, 128, 32, 128), f)
    for k, (bn, cn) in enumerate([("s5_b_re", "s5_c_re"), ("s5_b_im", "s5_c_im")]):
        B_ = inputs[bn][0]
        C_ = inputs[cn][0]
        for j in range(32):
            o = (j % 4) * 32
            for gl in range(2):
                g = 2 * j + gl
                bp[k, gl * 64:(gl + 1) * 64, j, o + gl * 16:o + gl * 16 + 16] = B_[g]
                cp[k, gl * 64:(gl + 1) * 64, j, o + gl * 16:o + gl * 16 + 16] = C_[g].T
    m["s5_bpad"] = bp
    m["s5_cpad"] = cp
    m["s5_d"] = _col_layout(inputs["s5_d"][0])
    m["iota_t"] = np.ascontiguousarray(np.broadcast_to(np.arange(L, dtype=f)[None, :], (128, L)))
    m["ident"] = np.eye(128, dtype=f)
    m["moe_w1"] = np.ascontiguousarray(inputs["moe_w1"], dtype=f)
    m["moe_w3"] = np.ascontiguousarray(inputs["moe_w3"], dtype=f)
    m["moe_w2"] = np.ascontiguousarray(inputs["moe_w2"], dtype=f)
    m["moe_wr"] = np.ascontiguousarray(np.concatenate([inputs["moe_wg"], inputs["moe_we"]], axis=2), dtype=f)
    br = np.concatenate([inputs["moe_bg"], inputs["moe_be"]], axis=1).astype(f)
    m["moe_br"] = np.ascontiguousarray(np.broadcast_to(br[:, None, :], (2, 128, 36)))
    m["kv_w"] = np.ascontiguousarray(inputs["kv_w"], dtype=f)
    m["gk_col"] = np.ascontiguousarray(np.tile(inputs["k_norm_g"], 2)[:, None], dtype=f)
    m["gq_col"] = np.ascontiguousarray(np.tile(inputs["fox_q_norm_g"][0], 2)[:, None], dtype=f)
    m["fb_col"] = np.ascontiguousarray(inputs["kv_fb"][:, None], dtype=f)
    blk = np.zeros((128, 128), f)
    blk[0:64, 0:64] = 1.0
    blk[64:128, 64:128] = 1.0
    m["blk_c"] = blk
    m["tri_c"] = np.ascontiguousarray(np.tril(np.full((128, 128), -1.0e8, f), -1))
    m["fox_w_qg"] = np.ascontiguousarray(inputs["fox_w_qg"][0], dtype=f)
    m["fox_w_o"] = np.ascontiguousarray(inputs["fox_w_o"][0], dtype=f)
    sel = np.zeros((32, NE, 128), f)
    for e_ in range(NE):
        sel[e_, e_, :] = 1.0
    m["sel_c"] = sel
    return m


_NC_CACHE = {}


def kernel(**inputs):
    inputs = {k: np.asarray(v) for k, v in inputs.items()}
    if "nc" not in _NC_CACHE:
        _NC_CACHE["nc"] = build()
    nc = _NC_CACHE["nc"]
    shared = make_shared(inputs)
    in_maps = []
    for b in range(8):
        m = dict(shared)
        m.update(make_inputs(inputs, b))
        in_maps.append(m)
    res = run_bass_kernel_spmd(nc, in_maps, core_ids=list(range(8)))
    out = np.stack([np.ascontiguousarray(r["hout"].reshape(D, L).T) for r in res.results])
    return out.astype(np.float32)
```

```python
import math
from contextlib import ExitStack
import numpy as np
import concourse.bass as bass
import concourse.mybir as mybir
from concourse.bass_utils import run_bass_kernel_spmd

F32 = mybir.dt.float32
BF16 = mybir.dt.bfloat16
AF = mybir.ActivationFunctionType
ALU = mybir.AluOpType
AX = mybir.AxisListType

D = 1024
L = 4096
NCH = 8
TT = 512
NTT = L // TT
NG = 4
NE = 32
FE = 256
H = 16
HD = 64
EPS = 1e-6
MAGIC = 12582912.0
S2PI = 6.283180
HALFPI = 1.570795
GELU_C = 2.0 * math.sqrt(2.0 / math.pi)


class Trk:
    __slots__ = ("w", "r")

    def __init__(self):
        self.w = {}
        self.r = {}


class Ctx:
    def __init__(self, nc, es):
        self.nc = nc
        self.es = es
        self.engs = {"pe": nc.tensor, "act": nc.scalar, "dve": nc.vector, "pool": nc.gpsimd, "sp": nc.sync}
        self.sems = {}
        self.cnt = {}
        for k in ["pe", "act", "dve", "pool"]:
            self.sems[k] = es.enter_context(nc.semaphore("s_" + k))
            self.cnt[k] = 0
        self.seen = {k: {} for k in self.engs}
        self.dpool = {}
        self.dnext = {}
        for q, n in [("sp", 24), ("pool", 16), ("act", 6)]:
            keys = []
            for i in range(n):
                key = "d_%s%d" % (q, i)
                self.sems[key] = es.enter_context(nc.semaphore(key))
                self.cnt[key] = 0
                keys.append(key)
            self.dpool[q] = keys
            self.dnext[q] = 0

    def _wait(self, eng, deps):
        seen = self.seen[eng]
        for key, val in deps.items():
            if eng == "pe" and key == "pe":
                continue
            if seen.get(key, 0) < val:
                self.engs[eng].wait_ge(self.sems[key], val)
                seen[key] = val

    @staticmethod
    def _merge(dst, src):
        for k, v in src.items():
            if dst.get(k, 0) < v:
                dst[k] = v

    def _deps(self, reads, writes):
        deps = {}
        for t in reads:
            self._merge(deps, t.w)
        for t in writes:
            self._merge(deps, t.w)
            self._merge(deps, t.r)
        return deps

    def _record(self, ev, reads, writes):
        k, v = ev
        for t in reads:
            if t.r.get(k, 0) < v:
                t.r[k] = v
        for t in writes:
            t.w = {k: v}
            t.r = {}

    def op(self, eng, fn, reads=(), writes=()):
        self._wait(eng, self._deps(reads, writes))
        ins = fn(self.engs[eng])
        self.cnt[eng] += 1
        ins.then_inc(self.sems[eng], 1)
        self._record((eng, self.cnt[eng]), reads, writes)

    def dma(self, q, out, in_, reads=(), writes=(), **kw):
        keys = self.dpool[q]
        key = keys[self.dnext[q] % len(keys)]
        self.dnext[q] += 1
        deps = self._deps(reads, writes)
        if self.cnt[key] > 0:
            deps[key] = max(deps.get(key, 0), self.cnt[key])
        self._wait(q, deps)
        ins = self.engs[q].dma_start(out=out, in_=in_, **kw)
        self.cnt[key] += 16
        ins.then_inc(self.sems[key], 16)
        self._record((key, self.cnt[key]), reads, writes)

    def barrier(self, engines=("pe", "act", "dve", "pool", "sp")):
        allev = {k: v for k, v in self.cnt.items() if v > 0}
        for e in engines:
            self._wait(e, allev)


class _Stop(Exception):
    pass


def build(debug=False, stop=None):
    try:
        return _build(debug, stop)
    except _Stop as e:
        return e.args[0]


def _build(debug, stop):
    nc = bass.Bass("TRN2", target_bir_lowering=False)
    okind = "ExternalOutput" if debug else "Internal"

    def din(name, shape, dt=F32):
        return nc.dram_tensor(name, list(shape), dt, kind="ExternalInput").ap()

    def dscr(name, shape, dt=F32, out=False):
        return nc.dram_tensor(name, list(shape), dt, kind=("ExternalOutput" if out else okind)).ap()

    xT = din("xT", [NCH, 128, L])
    c_col = din("c_col", [128, NCH])
    ada_w = din("ada_w", [2, 2, D, 3 * D])
    ada_b = din("ada_b", [4, 128, 24])
    ln_g = din("ln_g", [4, 128, NCH])
    kv_ada_w = din("kv_ada_w", [D, 2 * D])
    kv_ada_b = din("kv_ada_b", [128, 16])
    kv_g = din("kv_g", [128, NCH])
    s5_w_in = din("s5_w_in", [D, D])
    s5_w_out = din("s5_w_out", [D, 2 * D])
    s5_par = din("s5_par", [3, 128, 32])
    s5_bpad = din("s5_bpad", [2, 128, 32, 128])
    s5_cpad = din("s5_cpad", [2, 128, 32, 128])
    s5_d = din("s5_d", [128, NCH])
    iota_t = din("iota_t", [128, L])
    ident = din("ident", [128, 128])
    hout = dscr("hout", [NCH, 128, L], out=True)
    moe_w1 = din("moe_w1", [2, NE, D, FE])
    moe_w3 = din("moe_w3", [2, NE, D, FE])
    moe_w2 = din("moe_w2", [2, NE, FE, D])
    moe_wr = din("moe_wr", [2, D, 36])
    moe_br = din("moe_br", [2, 128, 36])
    sel_c = din("sel_c", [32, NE, 128])
    h2 = dscr("h2", [NCH, 128, L])
    kv_w = din("kv_w", [D, 2 * D + H])
    gk_col = din("gk_col", [128, 1])
    gq_col = din("gq_col", [128, 1])
    fb_col = din("fb_col", [H, 1])
    blk_c = din("blk_c", [128, 128])
    tri_c = din("tri_c", [128, 128])
    fox_w_qg = din("fox_w_qg", [D, 2 * D])
    fox_w_o = din("fox_w_o", [D, D])
    Kd = dscr("Kd", [NCH, 128, L], BF16)
    Vd = dscr("Vd", [L, D], BF16)
    Fq = dscr("Fq", [H, 3, L], BF16)
    Fk = dscr("Fk", [H, 3, L], BF16)
    Qd = dscr("Qd", [NCH, 128, L], BF16)
    SGd = dscr("SGd", [NCH, 128, L], BF16)
    Od = dscr("Od", [NCH, 128, L], BF16)
    h3 = dscr("h3", [NCH, 128, L])

    h1 = dscr("h1", [NCH, 128, L])
    Gd = dscr("Gd", [NCH, 128, L], BF16)
    moddbg = dscr("moddbg", [128, 24 * 4 + 16])
    udbg = dscr("udbg", [NCH, 128, L], BF16) if debug else None

    es = ExitStack()
    cx = Ctx(nc, es)

    def chk(name):
        if stop == name:
            cx.barrier()
            raise _Stop(nc, es)

    _uid = [0]

    def sb(st, name, shape, dt=F32):
        _uid[0] += 1
        return st.enter_context(nc.sbuf_tensor("%s_%d" % (name, _uid[0]), list(shape), dt))

    ps = [es.enter_context(nc.psum_tensor("ps%d" % i, [128, 512], F32)) for i in range(8)]
    pst = [Trk() for _ in range(8)]

    mods = sb(es, "mods", [128, 24 * 4 + 16])
    modA = sb(es, "modA", [128, 5, NCH])
    t_mods = Trk()
    t_modA = Trk()
    identf = sb(es, "identf", [128, 128])
    onesf = sb(es, "onesf", [128, 128])
    onesbf = sb(es, "onesbf", [128, 128], BF16)
    t_const = Trk()
    cx.dma("sp", identf[:], ident[:, :], writes=[t_const])
    cx.op("dve", lambda e: e.memset(onesf[:], 1.0), writes=[t_const])
    cx.op("dve", lambda e: e.memset(onesbf[:], 1.0), writes=[t_const])

    with ExitStack() as st:
        ccol = sb(st, "ccol", [128, NCH])
        sc = sb(st, "sc", [128, NCH])
        bias_all = sb(st, "bias_all", [128, 24 * 4 + 16])
        g_all = sb(st, "g_all", [128, 5, NCH])
        wbuf = [sb(st, "wbuf%d" % i, [128, NCH, 1536]) for i in range(2)]
        t_w = [Trk(), Trk()]
        t_c = Trk()
        t_b = Trk()
        cx.dma("sp", ccol[:], c_col[:, :], writes=[t_c])
        for i in range(4):
            cx.dma("sp", bias_all[:, 24 * i:24 * (i + 1)], ada_b[i], writes=[t_b])
            cx.dma("sp", g_all[:, i, :], ln_g[i], writes=[t_b])
        cx.dma("sp", bias_all[:, 96:112], kv_ada_b[:, :], writes=[t_b])
        cx.dma("sp", g_all[:, 4, :], kv_g[:, :], writes=[t_b])
        cx.op("act", lambda e: e.activation(out=sc[:], in_=ccol[:], func=AF.Silu), reads=[t_c], writes=[t_c])
        units = []
        for i in range(4):
            for hf in range(2):
                units.append((ada_w[i // 2, i % 2], hf * 1536, 1536, 24 * i + 12 * hf))
        units.append((kv_ada_w, 0, 1024, 96))
        units.append((kv_ada_w, 1024, 1024, 104))
        for ui, (wap, c0, ncol, mcol) in enumerate(units):
            wb = wbuf[ui % 2]
            tw = t_w[ui % 2]
            src = wap.rearrange("(c p) n -> p c n", p=128)
            for kc in range(NCH):
                cx.dma("sp" if kc % 2 == 0 else "pool", wb[:, kc, 0:ncol], src[:, kc, c0:c0 + ncol], writes=[tw])
            pidx = ui % 2
            for n in range(ncol // 128):
                for kc in range(NCH):
                    cx.op("pe", lambda e, wb=wb, n=n, kc=kc, pidx=pidx: e.matmul(
                        ps[pidx][:, n:n + 1], lhsT=wb[:, kc, n * 128:(n + 1) * 128], rhs=sc[:, kc:kc + 1],
                        start=(kc == 0), stop=(kc == NCH - 1)), reads=[tw, t_c], writes=[pst[pidx]])
            nn = ncol // 128
            cx.op("dve", lambda e, pidx=pidx, nn=nn, mcol=mcol: e.tensor_tensor(
                out=mods[:, mcol:mcol + nn], in0=ps[pidx][:, 0:nn], in1=bias_all[:, mcol:mcol + nn], op=ALU.add),
                reads=[pst[pidx], t_b], writes=[t_mods])
        for i in range(5):
            sc0 = 24 * i + 8
            cx.op("dve", lambda e, i=i, sc0=sc0: e.scalar_tensor_tensor(
                out=modA[:, i, :], in0=mods[:, sc0:sc0 + 8], scalar=1.0, in1=g_all[:, i, :],
                op0=ALU.add, op1=ALU.mult), reads=[t_mods, t_b], writes=[t_modA])
        if debug:
            cx.dma("sp", moddbg[:, :], mods[:], reads=[t_mods])
        cx.barrier()

    chk("s0")

    def shiftc(i, c):
        return mods[:, 24 * i + c:24 * i + c + 1]

    def gatec(i, c):
        return mods[:, 24 * i + 16 + c:24 * i + 16 + c + 1]

    def scaleA(i, c):
        return modA[:, i, c:c + 1]

    _tmp_trk = {}

    def norm_mod(i, htile, t_h, sq, t_sq, rinv, t_rinv, tmp, t_tmp, hn_bf, t_hn, psi, hn_f=None, t_hnf=None):
        cx.op("act", lambda e: e.activation(out=sq[:], in_=htile[:], func=AF.Square), reads=[t_h], writes=[t_sq])
        for c in range(NCH):
            cx.op("pe", lambda e, c=c: e.matmul(ps[psi][:, :], lhsT=onesbf[:], rhs=sq[:, c, :],
                                                start=(c == 0), stop=(c == NCH - 1)),
                  reads=[t_sq, t_const], writes=[pst[psi]])
        cx.op("act", lambda e: e.activation(out=rinv[:], in_=ps[psi][:, :], func=AF.Sqrt, bias=epsc[:, 0:1],
                                            scale=1.0 / D), reads=[pst[psi], t_const], writes=[t_rinv])
        cx.op("dve", lambda e: e.reciprocal(out=rinv[:], in_=rinv[:]), reads=[t_rinv], writes=[t_rinv])
        tcs = _tmp_trk.setdefault(id(t_tmp), [Trk() for _ in range(NCH)])
        for c in range(NCH):
            cx.op("dve", lambda e, c=c: e.scalar_tensor_tensor(
                out=tmp[:, c, :], in0=htile[:, c, :], scalar=scaleA(i, c), in1=rinv[:],
                op0=ALU.mult, op1=ALU.mult), reads=[t_h, t_rinv, t_modA], writes=[tcs[c]])
            if hn_f is not None:
                cx.op("act", lambda e, c=c: e.activation(out=hn_f[:, c, :], in_=tmp[:, c, :], func=AF.Identity,
                                                        bias=shiftc(i, c), scale=1.0),
                      reads=[tcs[c], t_mods], writes=[t_hnf])
            cx.op("act", lambda e, c=c: e.activation(out=hn_bf[:, c, :], in_=tmp[:, c, :], func=AF.Identity,
                                                    bias=shiftc(i, c), scale=1.0),
                  reads=[tcs[c], t_mods], writes=[t_hn])

    epsc = sb(es, "epsc", [128, 4])
    cx.op("dve", lambda e: e.memset(epsc[:, 0:1], EPS), writes=[t_const])
    cx.op("dve", lambda e: e.memset(epsc[:, 1:2], HALFPI), writes=[t_const])
    cx.op("dve", lambda e: e.memset(epsc[:, 2:3], 0.0), writes=[t_const])
    cx.op("dve", lambda e: e.memset(epsc[:, 3:4], -MAGIC), writes=[t_const])

    with ExitStack() as st:
        u_bf = sb(st, "u_bf", [128, NCH, L], BF16)
        t_u = [Trk() for _ in range(NTT)]
        par = sb(st, "par", [128, 3, 32])
        t_par = Trk()
        for i in range(3):
            cx.dma("sp", par[:, i, :], s5_par[i], writes=[t_par])
        sp_ = sb(st, "s5small", [128, 22, 32])
        t_sp = Trk()

        def S(i):
            return sp_[:, i, :]
        lr, li, ldt = par[:, 0, :], par[:, 1, :], par[:, 2, :]
        DT, MAG, TH, THT, V, K_, FR, SIN, COS, ARE, AIM, DEN, CRE, CIM, T0, T1, C5, S5, U0, U1, U2, U3 = range(22)

        def dv(fn, eng="dve"):
            cx.op(eng, fn, reads=[t_par, t_sp, t_const], writes=[t_sp])
        dv(lambda e: e.activation(out=S(DT), in_=ldt, func=AF.Exp), "act")
        dv(lambda e: e.tensor_tensor(out=S(T0), in0=lr, in1=S(DT), op=ALU.mult))
        dv(lambda e: e.activation(out=S(MAG), in_=S(T0), func=AF.Exp), "act")
        dv(lambda e: e.tensor_tensor(out=S(TH), in0=li, in1=S(DT), op=ALU.mult))
        dv(lambda e: e.tensor_scalar(out=S(THT), in0=S(TH), scalar1=1.0 / (2 * math.pi), scalar2=None, op0=ALU.mult))
        dv(lambda e: e.tensor_scalar(out=S(V), in0=S(THT), scalar1=MAGIC, scalar2=None, op0=ALU.add))
        dv(lambda e: e.tensor_scalar(out=S(K_), in0=S(V), scalar1=-MAGIC, scalar2=None, op0=ALU.add))
        dv(lambda e: e.tensor_tensor(out=S(FR), in0=S(THT), in1=S(K_), op=ALU.subtract))
        dv(lambda e: e.activation(out=S(SIN), in_=S(FR), func=AF.Sin, scale=S2PI), "act")
        dv(lambda e: e.tensor_scalar(out=S(T0), in0=S(FR), scalar1=0.25, scalar2=-1.0, op0=ALU.is_gt, op1=ALU.mult))
        dv(lambda e: e.tensor_tensor(out=S(T0), in0=S(T0), in1=S(FR), op=ALU.add))
        dv(lambda e: e.activation(out=S(COS), in_=S(T0), func=AF.Sin, scale=S2PI, bias=epsc[:, 1:2]), "act")
        dv(lambda e: e.tensor_tensor(out=S(ARE), in0=S(MAG), in1=S(COS), op=ALU.mult))
        dv(lambda e: e.tensor_tensor(out=S(AIM), in0=S(MAG), in1=S(SIN), op=ALU.mult))
        dv(lambda e: e.tensor_tensor(out=S(T0), in0=lr, in1=lr, op=ALU.mult))
        dv(lambda e: e.tensor_tensor(out=S(T1), in0=li, in1=li, op=ALU.mult))
        dv(lambda e: e.tensor_tensor(out=S(DEN), in0=S(T0), in1=S(T1), op=ALU.add))
        dv(lambda e: e.reciprocal(out=S(DEN), in_=S(DEN)))
        dv(lambda e: e.tensor_scalar(out=S(T0), in0=S(ARE), scalar1=-1.0, scalar2=None, op0=ALU.add))
        dv(lambda e: e.tensor_tensor(out=S(CRE), in0=S(T0), in1=lr, op=ALU.mult))
        dv(lambda e: e.tensor_tensor(out=S(T1), in0=S(AIM), in1=li, op=ALU.mult))
        dv(lambda e: e.tensor_tensor(out=S(CRE), in0=S(CRE), in1=S(T1), op=ALU.add))
        dv(lambda e: e.tensor_tensor(out=S(CRE), in0=S(CRE), in1=S(DEN), op=ALU.mult))
        dv(lambda e: e.tensor_tensor(out=S(CIM), in0=S(AIM), in1=lr, op=ALU.mult))
        dv(lambda e: e.tensor_tensor(out=S(T1), in0=S(T0), in1=li, op=ALU.mult))
        dv(lambda e: e.tensor_tensor(out=S(CIM), in0=S(CIM), in1=S(T1), op=ALU.subtract))
        dv(lambda e: e.tensor_tensor(out=S(CIM), in0=S(CIM), in1=S(DEN), op=ALU.mult))
        dv(lambda e: e.tensor_copy(out=S(C5), in_=S(COS)))
        dv(lambda e: e.tensor_copy(out=S(S5), in_=S(SIN)))
        for _sq in range(9):
            dv(lambda e: e.tensor_tensor(out=S(U0), in0=S(C5), in1=S(C5), op=ALU.mult))
            dv(lambda e: e.tensor_tensor(out=S(U1), in0=S(S5), in1=S(S5), op=ALU.mult))
            dv(lambda e: e.scalar_tensor_tensor(out=S(U2), in0=S(C5), scalar=2.0, in1=S(S5), op0=ALU.mult, op1=ALU.mult))
            dv(lambda e: e.tensor_tensor(out=S(C5), in0=S(U0), in1=S(U1), op=ALU.subtract))
            dv(lambda e: e.tensor_copy(out=S(S5), in_=S(U2)))
        dv(lambda e: e.tensor_scalar(out=S(T1), in0=S(CIM), scalar1=-1.0, scalar2=None, op0=ALU.mult))

        Lre = sb(st, "Lre", [128, 32, 128], BF16)
        Lim = sb(st, "Lim", [128, 32, 128], BF16)
        Cre = sb(st, "Cre", [128, 32, 128], BF16)
        nCre = sb(st, "nCre", [128, 32, 128], BF16)
        nCim = sb(st, "nCim", [128, 32, 128], BF16)
        t_L = Trk()
        t_C = Trk()
        with ExitStack() as st2:
            bre = sb(st2, "bre", [128, 32, 128])
            bim = sb(st2, "bim", [128, 32, 128])
            t_bp = Trk()
            cx.dma("sp", bre[:], s5_bpad[0], writes=[t_bp])
            cx.dma("sp", bim[:], s5_bpad[1], writes=[t_bp])
            xa = [sb(st2, "xa%d" % i, [128, 128]) for i in range(2)]
            xb = [sb(st2, "xb%d" % i, [128, 128]) for i in range(2)]
            t_xa = [Trk(), Trk()]
            t_xb = [Trk(), Trk()]
            for j in range(32):
                b = j % 2
                cx.op("dve", lambda e, j=j, b=b: e.tensor_scalar(out=xa[b][:], in0=bim[:, j, :], scalar1=sp_[:, T1, j:j + 1],
                                                                 scalar2=None, op0=ALU.mult),
                      reads=[t_bp, t_sp], writes=[t_xa[b]])
                cx.op("dve", lambda e, j=j, b=b: e.scalar_tensor_tensor(out=xa[b][:], in0=bre[:, j, :], scalar=sp_[:, CRE, j:j + 1],
                                                                        in1=xa[b][:], op0=ALU.mult, op1=ALU.add),
                      reads=[t_bp, t_sp], writes=[t_xa[b]])
                cx.op("dve", lambda e, j=j, b=b: e.tensor_scalar(out=xb[b][:], in0=bre[:, j, :], scalar1=sp_[:, CIM, j:j + 1],
                                                                 scalar2=None, op0=ALU.mult),
                      reads=[t_bp, t_sp], writes=[t_xb[b]])
                cx.op("dve", lambda e, j=j, b=b: e.scalar_tensor_tensor(out=xb[b][:], in0=bim[:, j, :], scalar=sp_[:, CRE, j:j + 1],
                                                                        in1=xb[b][:], op0=ALU.mult, op1=ALU.add),
                      reads=[t_bp, t_sp], writes=[t_xb[b]])
                cx.op("pe", lambda e, b=b: e.transpose(out=ps[b][:, 0:128], in_=xa[b][:], identity=identf[:]),
                      reads=[t_xa[b], t_const], writes=[pst[b]])
                cx.op("pe", lambda e, b=b: e.transpose(out=ps[b][:, 128:256], in_=xb[b][:], identity=identf[:]),
                      reads=[t_xb[b], t_const], writes=[pst[b]])
                cx.op("act", lambda e, j=j, b=b: e.copy(out=Lre[:, j, :], in_=ps[b][:, 0:128]), reads=[pst[b]], writes=[t_L])
                cx.op("act", lambda e, j=j, b=b: e.copy(out=Lim[:, j, :], in_=ps[b][:, 128:256]), reads=[pst[b]], writes=[t_L])
            cx.dma("sp", bre[:], s5_cpad[0], reads=[], writes=[t_bp])
            cx.dma("sp", bim[:], s5_cpad[1], reads=[], writes=[t_bp])
            for q4 in range(4):
                sl = slice(q4 * 8, (q4 + 1) * 8)
                cx.op("act", lambda e, sl=sl: e.copy(out=Cre[:, sl, :], in_=bre[:, sl, :]), reads=[t_bp], writes=[t_C])
                cx.op("act", lambda e, sl=sl: e.mul(out=nCre[:, sl, :], in_=bre[:, sl, :], mul=-1.0), reads=[t_bp], writes=[t_C])
                cx.op("act", lambda e, sl=sl: e.mul(out=nCim[:, sl, :], in_=bim[:, sl, :], mul=-1.0), reads=[t_bp], writes=[t_C])
            cx.barrier()

        chk("a0")
        with ExitStack() as st2:
            win = sb(st2, "win", [128, NCH, D], BF16)
            t_win = Trk()
            wsrc = s5_w_in.rearrange("(c p) n -> p c n", p=128)
            for kc in range(NCH):
                cx.dma("pool", win[:, kc, :], wsrc[:, kc, :], writes=[t_win])
            hts = [sb(st2, "ht%d" % i, [128, NCH, TT]) for i in range(2)]
            t_ht = [Trk(), Trk()]
            sq = sb(st2, "sq", [128, NCH, TT], BF16)
            t_sq = Trk()
            rinv = sb(st2, "rinv", [128, TT])
            t_rinv = Trk()
            tmp = sb(st2, "tmpn", [128, NCH, TT])
            t_tmp = Trk()
            hnb = [sb(st2, "hnb%d" % i, [128, NCH, TT], BF16) for i in range(2)]
            t_hn = [Trk(), Trk()]
            for tt in range(NTT):
                b = tt % 2
                t0 = tt * TT
                cx.dma("sp", hts[b][:], xT[:, :, t0:t0 + TT].rearrange("c p t -> p c t"), writes=[t_ht[b]])
                norm_mod(0, hts[b], t_ht[b], sq, t_sq, rinv, t_rinv, tmp, t_tmp, hnb[b], t_hn[b], 0)
                for n in range(NCH):
                    pi = 1 + (n % 4)
                    for k in range(NCH):
                        cx.op("pe", lambda e, n=n, k=k, pi=pi, b=b: e.matmul(
                            ps[pi][:, :], lhsT=win[:, k, n * 128:(n + 1) * 128], rhs=hnb[b][:, k, :],
                            start=(k == 0), stop=(k == NCH - 1)), reads=[t_win, t_hn[b]], writes=[pst[pi]])
                    eng = "act" if n % 2 == 0 else "dve"
                    if eng == "act":
                        cx.op("act", lambda e, n=n, pi=pi, t0=t0: e.copy(out=u_bf[:, n, t0:t0 + TT], in_=ps[pi][:, :]),
                              reads=[pst[pi]], writes=[t_u[tt]])
                    else:
                        cx.op("dve", lambda e, n=n, pi=pi, t0=t0: e.tensor_copy(out=u_bf[:, n, t0:t0 + TT], in_=ps[pi][:, :]),
                              reads=[pst[pi]], writes=[t_u[tt]])
            cx.barrier()

        if debug:
            for n in range(NCH):
                cx.dma("sp", udbg[n], u_bf[:, n, :], reads=t_u)
        chk("a2")
        with ExitStack() as st2:
            iot = sb(st2, "iot", [128, TT])
            t_iot = Trk()
            cx.dma("sp", iot[:], iota_t[:, 0:TT], writes=[t_iot])
            dsk = sb(st2, "dsk", [128, NCH])
            cx.dma("sp", dsk[:], s5_d[:, :], writes=[t_iot])
            NB = 2

            def mk(name, dt=F32):
                return [sb(st2, "%s%d" % (name, i), [128, TT], dt) for i in range(NB)], [Trk() for _ in range(NB)]
            SNt, tSN = mk("SNt")
            CRt, tCR = mk("CRt")
            T1_, tT1 = mk("T1_")
            T2_, tT2 = mk("T2_")
            T3_, tT3 = mk("T3_")
            T4_, tT4 = mk("T4_")
            Vt, tVt = mk("Vt")
            Ft_, tFt = mk("Ftb")
            ini = [sb(st2, "ini%d" % i, [128, 4]) for i in range(NB)]
            t_ini = [Trk() for _ in range(NB)]
            XR, tXR = mk("XR")
            XI, tXI = mk("XI")
            SR, tSR = mk("SR")
            SI, tSI = mk("SI")
            P1, tP1 = mk("P1", BF16)
            P2, tP2 = mk("P2", BF16)
            P3, tP3 = mk("P3", BF16)
            P4, tP4 = mk("P4", BF16)
            ytmp = sb(st2, "ytmp", [128, L])
            t_y = [Trk() for _ in range(NTT)]
            gb = sb(st2, "gb", [128, L], BF16)
            t_gb = [Trk() for _ in range(NTT)]
            g1 = sb(st2, "g1", [128, TT])
            g2 = sb(st2, "g2", [128, TT])
            t_g1 = Trk()
            t_g2 = Trk()
            zero_init = epsc[:, 2:3]
            def gen_tables(j):
                thj = sp_[:, THT, j:j + 1]
                tb = j % 2
                cx.op("dve", lambda e: e.tensor_scalar(
                    out=Vt[tb][:], in0=iot[:, 0:TT], scalar1=thj, scalar2=MAGIC, op0=ALU.mult, op1=ALU.add),
                    reads=[t_iot, t_sp], writes=[tVt[tb]])
                cx.op("act", lambda e: e.activation(out=Vt[tb][:], in_=Vt[tb][:], func=AF.Identity,
                                                    bias=epsc[:, 3:4], scale=1.0),
                      reads=[tVt[tb], t_const], writes=[tVt[tb]])
                cx.op("dve", lambda e: e.scalar_tensor_tensor(
                    out=Ft_[tb][:], in0=iot[:, 0:TT], scalar=thj, in1=Vt[tb][:], op0=ALU.mult, op1=ALU.subtract),
                    reads=[t_iot, t_sp, tVt[tb]], writes=[tFt[tb]])
                cx.op("act", lambda e: e.activation(out=SNt[tb][:], in_=Ft_[tb][:], func=AF.Sin, scale=S2PI),
                      reads=[tFt[tb]], writes=[tSN[tb]])
                cx.op("dve", lambda e: e.tensor_scalar(
                    out=Vt[tb][:], in0=Ft_[tb][:], scalar1=0.25, scalar2=-1.0, op0=ALU.is_gt, op1=ALU.mult),
                    reads=[tFt[tb], tVt[tb]], writes=[tVt[tb]])
                cx.op("dve", lambda e: e.tensor_tensor(out=Vt[tb][:], in0=Vt[tb][:], in1=Ft_[tb][:], op=ALU.add),
                      reads=[tFt[tb], tVt[tb]], writes=[tVt[tb]])
                cx.op("act", lambda e: e.activation(out=CRt[tb][:], in_=Vt[tb][:], func=AF.Sin, scale=S2PI,
                                                    bias=epsc[:, 1:2]),
                      reads=[tVt[tb], t_const], writes=[tCR[tb]])

            class P_:
                pass
            pieces = []
            for c in range(NCH):
                for jj in range(4):
                    for tt in range(NTT):
                        p = P_()
                        p.c, p.jj, p.j, p.o, p.tt, p.s = c, jj, 4 * c + jj, jj * 32, tt, len(pieces)
                        p.b = p.s % NB
                        p.pb = 4 * (p.s % 2)
                        p.tb = p.j % 2
                        p.tsl = slice(tt * TT, (tt + 1) * TT)
                        pieces.append(p)

            def stg1(p):
                if p.tt == 0:
                    gen_tables(p.j)
                b, pb, tb, j, c, tsl, tt = p.b, p.pb, p.tb, p.j, p.c, p.tsl, p.tt
                cx.op("pe", lambda e: e.matmul(ps[pb][:, :], lhsT=Lre[:, j, :], rhs=u_bf[:, c, tsl], start=True, stop=True),
                      reads=[t_L, t_u[tt]], writes=[pst[pb]])
                cx.op("pe", lambda e: e.matmul(ps[pb + 1][:, :], lhsT=Lim[:, j, :], rhs=u_bf[:, c, tsl], start=True, stop=True),
                      reads=[t_L, t_u[tt]], writes=[pst[pb + 1]])
                cx.op("dve", lambda e: e.tensor_tensor(out=T1_[b][:], in0=CRt[tb][:], in1=ps[pb][:, :], op=ALU.mult),
                      reads=[tCR[tb], pst[pb]], writes=[tT1[b]])
                cx.op("dve", lambda e: e.tensor_tensor(out=T2_[b][:], in0=SNt[tb][:], in1=ps[pb + 1][:, :], op=ALU.mult),
                      reads=[tSN[tb], pst[pb + 1]], writes=[tT2[b]])
                cx.op("dve", lambda e: e.tensor_tensor(out=T3_[b][:], in0=CRt[tb][:], in1=ps[pb + 1][:, :], op=ALU.mult),
                      reads=[tCR[tb], pst[pb + 1]], writes=[tT3[b]])
                cx.op("dve", lambda e: e.tensor_tensor(out=T4_[b][:], in0=SNt[tb][:], in1=ps[pb][:, :], op=ALU.mult),
                      reads=[tSN[tb], pst[pb]], writes=[tT4[b]])

            def stg2(p):
                b = p.b
                cx.op("pool", lambda e: e.tensor_tensor(out=XR[b][:], in0=T1_[b][:], in1=T2_[b][:], op=ALU.add),
                      reads=[tT1[b], tT2[b]], writes=[tXR[b]])
                cx.op("pool", lambda e: e.tensor_tensor(out=XI[b][:], in0=T3_[b][:], in1=T4_[b][:], op=ALU.subtract),
                      reads=[tT3[b], tT4[b]], writes=[tXI[b]])

            def stg3(p):
                b, j, tt = p.b, p.j, p.tt
                pbuf = (b - 1) % NB
                magb = sp_[:, MAG, j:j + 1].to_broadcast([128, TT])
                if tt == 0:
                    ini_r, ini_i, rd = zero_init, zero_init, [t_const]
                else:
                    sre, sie = SR[pbuf][:, TT - 1:TT], SI[pbuf][:, TT - 1:TT]
                    c5, s5 = sp_[:, C5, j:j + 1], sp_[:, S5, j:j + 1]
                    rdp = [tSR[pbuf], tSI[pbuf], t_sp]
                    cx.op("dve", lambda e: e.tensor_tensor(out=ini[b][:, 2:3], in0=sie, in1=s5, op=ALU.mult),
                          reads=rdp, writes=[t_ini[b]])
                    cx.op("dve", lambda e: e.scalar_tensor_tensor(
                        out=ini[b][:, 0:1], in0=sre, scalar=c5, in1=ini[b][:, 2:3], op0=ALU.mult, op1=ALU.subtract),
                        reads=rdp + [t_ini[b]], writes=[t_ini[b]])
                    cx.op("dve", lambda e: e.tensor_tensor(out=ini[b][:, 3:4], in0=sie, in1=c5, op=ALU.mult),
                          reads=rdp + [t_ini[b]], writes=[t_ini[b]])
                    cx.op("dve", lambda e: e.scalar_tensor_tensor(
                        out=ini[b][:, 1:2], in0=sre, scalar=s5, in1=ini[b][:, 3:4], op0=ALU.mult, op1=ALU.add),
                        reads=rdp + [t_ini[b]], writes=[t_ini[b]])
                    ini_r, ini_i, rd = ini[b][:, 0:1], ini[b][:, 1:2], [t_ini[b]]
                cx.op("dve", lambda e: e.tensor_tensor_scan(
                    out=SR[b][:], data0=magb, data1=XR[b][:], initial=ini_r, op0=ALU.mult, op1=ALU.add),
                    reads=[tXR[b], t_sp] + rd, writes=[tSR[b]])
                cx.op("dve", lambda e: e.tensor_tensor_scan(
                    out=SI[b][:], data0=magb, data1=XI[b][:], initial=ini_i, op0=ALU.mult, op1=ALU.add),
                    reads=[tXI[b], t_sp] + rd, writes=[tSI[b]])

            def stg4(p):
                b, tb = p.b, p.tb
                cx.op("pool", lambda e: e.tensor_tensor(out=P1[b][:], in0=CRt[tb][:], in1=SR[b][:], op=ALU.mult),
                      reads=[tCR[tb], tSR[b]], writes=[tP1[b]])
                cx.op("pool", lambda e: e.tensor_tensor(out=P2[b][:], in0=SNt[tb][:], in1=SI[b][:], op=ALU.mult),
                      reads=[tSN[tb], tSI[b]], writes=[tP2[b]])
                cx.op("pool", lambda e: e.tensor_tensor(out=P3[b][:], in0=SNt[tb][:], in1=SR[b][:], op=ALU.mult),
                      reads=[tSN[tb], tSR[b]], writes=[tP3[b]])
                cx.op("pool", lambda e: e.tensor_tensor(out=P4[b][:], in0=CRt[tb][:], in1=SI[b][:], op=ALU.mult),
                      reads=[tCR[tb], tSI[b]], writes=[tP4[b]])

            def stg5(p):
                b, pb, j, c, o, tsl, tt = p.b, p.pb, p.j, p.c, p.o, p.tsl, p.tt
                py = pb + 2
                for idx, (cm, pp, tp) in enumerate([(Cre, P1, tP1), (nCre, P2, tP2), (nCim, P3, tP3), (nCim, P4, tP4)]):
                    cx.op("pe", lambda e, cm=cm, pp=pp, idx=idx: e.matmul(
                        ps[py][:, :], lhsT=cm[:, j, :], rhs=pp[b][:], start=(idx == 0), stop=(idx == 3)),
                        reads=[t_C, tp[b]], writes=[pst[py]])
                cx.op("dve", lambda e: e.scalar_tensor_tensor(
                    out=ytmp[o:o + 32, tsl], in0=u_bf[o:o + 32, c, tsl], scalar=dsk[o:o + 32, c:c + 1],
                    in1=ps[py][o:o + 32, :], op0=ALU.mult, op1=ALU.add),
                    reads=[t_u[tt], t_iot, pst[py]], writes=[t_y[tt]])
                if p.jj == 3 and tt == NTT - 1:
                    for t2 in range(NTT):
                        ts2 = slice(t2 * TT, (t2 + 1) * TT)
                        cx.op("act", lambda e, ts2=ts2: e.activation(out=g1[:], in_=ytmp[:, ts2], func=AF.Square),
                              reads=[t_y[t2]], writes=[t_g1])
                        cx.op("dve", lambda e: e.tensor_scalar(out=g1[:], in0=g1[:], scalar1=0.044715, scalar2=1.0,
                                                               op0=ALU.mult, op1=ALU.add), reads=[t_g1], writes=[t_g1])
                        cx.op("dve", lambda e, ts2=ts2: e.tensor_tensor(out=g2[:], in0=g1[:], in1=ytmp[:, ts2], op=ALU.mult),
                              reads=[t_g1, t_y[t2]], writes=[t_g2])
                        cx.op("act", lambda e: e.activation(out=g2[:], in_=g2[:], func=AF.Sigmoid, scale=GELU_C),
                              reads=[t_g2], writes=[t_g2])
                        cx.op("dve", lambda e, ts2=ts2: e.tensor_tensor(out=gb[:, ts2], in0=g2[:], in1=ytmp[:, ts2], op=ALU.mult),
                              reads=[t_g2, t_y[t2]], writes=[t_gb[t2]])
                    cx.dma("sp", Gd[c], gb[:], reads=t_gb, writes=[t_gd])
                    chk("a3g%d" % c)

            t_gd = Trk()
            npc = len(pieces)
            for s_ in range(npc + 2):
                if s_ < npc:
                    stg1(pieces[s_])
                    stg2(pieces[s_])
                if 1 <= s_ <= npc:
                    stg3(pieces[s_ - 1])
                    stg4(pieces[s_ - 1])
                if s_ >= 2:
                    stg5(pieces[s_ - 2])
            cx.barrier()
    cx.barrier()

    with ExitStack() as st:
        wout = sb(st, "wout", [128, NCH, 2 * D], BF16)
        t_wout = Trk()
        wsrc = s5_w_out.rearrange("(c p) n -> p c n", p=128)
        for kc in range(NCH):
            cx.dma("pool", wout[:, kc, :], wsrc[:, kc, :], writes=[t_wout])
        gt = [sb(st, "gt%d" % i, [128, NCH, TT], BF16) for i in range(2)]
        t_gt = [Trk(), Trk()]
        hts = [sb(st, "hto%d" % i, [128, NCH, TT]) for i in range(2)]
        t_ht = [Trk(), Trk()]
        ho = [sb(st, "ho%d" % i, [128, NCH, TT]) for i in range(2)]
        t_ho = [Trk(), Trk()]
        sg = [sb(st, "sg%d" % i, [128, TT]) for i in range(2)]
        t_sg = [Trk(), Trk()]
        mx = [sb(st, "mx%d" % i, [128, TT]) for i in range(2)]
        t_mx = [Trk(), Trk()]
        it = 0
        for tt in range(NTT):
            b = tt % 2
            t0 = tt * TT
            cx.dma("sp", gt[b][:], Gd[:, :, t0:t0 + TT].rearrange("c p t -> p c t"), writes=[t_gt[b]])
            cx.dma("sp", hts[b][:], xT[:, :, t0:t0 + TT].rearrange("c p t -> p c t"), writes=[t_ht[b]])
            for n in range(NCH):
                bb = it % 2
                pv = 2 * (it % 4)
                pg = pv + 1
                it += 1
                for k in range(NCH):
                    cx.op("pe", lambda e, n=n, k=k, pv=pv, b=b: e.matmul(
                        ps[pv][:, :], lhsT=wout[:, k, n * 128:(n + 1) * 128], rhs=gt[b][:, k, :],
                        start=(k == 0), stop=(k == NCH - 1)), reads=[t_wout, t_gt[b]], writes=[pst[pv]])
                for k in range(NCH):
                    cx.op("pe", lambda e, n=n, k=k, pg=pg, b=b: e.matmul(
                        ps[pg][:, :], lhsT=wout[:, k, D + n * 128:D + (n + 1) * 128], rhs=gt[b][:, k, :],
                        start=(k == 0), stop=(k == NCH - 1)), reads=[t_wout, t_gt[b]], writes=[pst[pg]])
                cx.op("act", lambda e, bb=bb, pg=pg: e.activation(out=sg[bb][:], in_=ps[pg][:, :], func=AF.Sigmoid),
                      reads=[pst[pg]], writes=[t_sg[bb]])
                cx.op("dve", lambda e, bb=bb, pv=pv: e.tensor_tensor(out=mx[bb][:], in0=sg[bb][:], in1=ps[pv][:, :], op=ALU.mult),
                      reads=[t_sg[bb], pst[pv]], writes=[t_mx[bb]])
                cx.op("dve", lambda e, bb=bb, n=n, b=b: e.scalar_tensor_tensor(
                    out=ho[b][:, n, :], in0=mx[bb][:], scalar=gatec(0, n), in1=hts[b][:, n, :], op0=ALU.mult, op1=ALU.add),
                    reads=[t_mx[bb], t_mods, t_ht[b]], writes=[t_ho[b]])
            cx.dma("sp", h1[:, :, t0:t0 + TT].rearrange("c p t -> p c t"), ho[b][:], reads=[t_ho[b]])
        cx.barrier()

    chk("a5")

    BIG = 1.0e30
    HT = 2048

    def moe_stage(l, mi, hin, hdst):
        with ExitStack() as st:
            acc = sb(st, "acc", [128, NCH, HT])
            t_acc = [[Trk() for _ in range(NCH)] for _ in range(4)]
            hn_all = sb(st, "hn_all", [128, NCH, HT], BF16)
            t_hna = [Trk() for _ in range(4)]
            gatesT = sb(st, "gatesT", [32, HT])
            t_gT = [Trk() for _ in range(4)]
            selt = sb(st, "selt", [32, NE, 128])
            t_sel = Trk()
            cx.dma("sp", selt[:], sel_c[:, :, :], writes=[t_sel])
            wr = sb(st, "wr", [128, NCH, 36])
            brt = sb(st, "brt", [128, 36])
            t_wr = Trk()
            cx.dma("sp", wr[:], moe_wr[l].rearrange("(c p) n -> p c n", p=128), writes=[t_wr])
            cx.dma("sp", brt[:], moe_br[l], writes=[t_wr])
            for half in range(L // HT):
                hbase = half * HT
                with ExitStack() as st2:
                    hts_ = [sb(st2, "m_ht%d" % i, [128, NCH, TT]) for i in range(2)]
                    t_hts = [Trk(), Trk()]
                    sq = sb(st2, "m_sq", [128, NCH, TT], BF16)
                    t_sq = Trk()
                    rinv = sb(st2, "m_rinv", [128, TT])
                    t_rinv = Trk()
                    tmp = sb(st2, "m_tmp", [128, NCH, TT])
                    t_tmp = Trk()
                    hnf = sb(st2, "m_hnf", [128, NCH, TT])
                    t_hn = Trk()
                    rt = sb(st2, "m_rt", [128, 8])
                    rb_lg = sb(st2, "rb_lg", [128, 4, 36])
                    rb_s = sb(st2, "rb_s", [128, 8, 4])
                    rb_g = sb(st2, "rb_g", [128, 3, 4, 4])
                    rb_m = sb(st2, "rb_m", [128, 4, 4, 32])
                    t_rt = Trk()
                    LG, GM, GMASK, GE, GS, PEN, MK, M1, MASK1, MK2, M2, MASK2, ED, W1, W2, GATES = (
                        slice(0, 36), slice(36, 37), slice(40, 44), slice(44, 48), slice(48, 49), slice(52, 56),
                        slice(56, 88), slice(88, 89), slice(96, 128), slice(128, 160), slice(160, 161),
                        slice(168, 200), slice(200, 201), slice(201, 202), slice(202, 203), slice(208, 240))
                    for tl in range(HT // TT):
                        t0 = hbase + tl * TT
                        ht, t_ht = hts_[tl % 2], t_hts[tl % 2]
                        cx.dma("sp", ht[:], hin[:, :, t0:t0 + TT].rearrange("c p t -> p c t"), writes=[t_ht])
                        norm_mod(mi, ht, t_ht, sq, t_sq, rinv, t_rinv, tmp, t_tmp,
                                 hn_all[:, :, tl * TT:(tl + 1) * TT], t_hna[tl], 7, hn_f=hnf, t_hnf=t_hn)
                        for sc_ in range(4):
                            for k in range(NCH):
                                cx.op("pe", lambda e, k=k, sc_=sc_: e.matmul(
                                    ps[6][:, sc_ * 36:(sc_ + 1) * 36], lhsT=hnf[:, k, sc_ * 128:(sc_ + 1) * 128], rhs=wr[:, k, :],
                                    start=(k == 0), stop=(k == NCH - 1)), reads=[t_hn, t_wr], writes=[pst[6]])

                        def dv(fn, eng="dve", extra=()):
                            cx.op(eng, fn, reads=[t_rt] + list(extra), writes=[t_rt])

                        def bc(ap2, n):
                            return ap2.unsqueeze(2).to_broadcast([128, 4, n])
                        LGv = rb_lg[:]
                        LGg = rb_lg[:, :, 0:4]
                        LGe = rb_lg[:, :, 4:36]
                        dv(lambda e: e.tensor_tensor(out=LGv, in0=ps[6][:, 0:144].rearrange("p (s n) -> p s n", s=4),
                                                     in1=brt[:].unsqueeze(1).to_broadcast([128, 4, 36]), op=ALU.add),
                           extra=[pst[6], t_wr])
                        dv(lambda e: e.reduce_max(out=rb_s[:, 0, :], in_=LGg, axis=AX.X))
                        dv(lambda e: e.tensor_tensor(out=rb_g[:, 0, :, :], in0=LGg, in1=bc(rb_s[:, 0, :], 4), op=ALU.is_ge))
                        dv(lambda e: e.tensor_tensor(out=rb_g[:, 1, :, :], in0=LGg, in1=bc(rb_s[:, 0, :], 4), op=ALU.subtract))
                        dv(lambda e: e.activation(out=rb_g[:, 1, :, :], in_=rb_g[:, 1, :, :], func=AF.Exp), "act")
                        dv(lambda e: e.reduce_sum(out=rb_s[:, 1, :], in_=rb_g[:, 1, :, :], axis=AX.X))
                        dv(lambda e: e.reciprocal(out=rb_s[:, 1, :], in_=rb_s[:, 1, :]))
                        dv(lambda e: e.tensor_scalar(out=rb_g[:, 2, :, :], in0=rb_g[:, 0, :, :], scalar1=-1.0, scalar2=BIG,
                                                     op0=ALU.add, op1=ALU.mult))
                        for s4 in range(4):
                            dv(lambda e, s4=s4: e.tensor_tensor(
                                out=rb_m[:, 0, s4, :].rearrange("p (g e) -> p g e", g=4),
                                in0=rb_lg[:, s4, 4:36].rearrange("p (g e) -> p g e", g=4),
                                in1=rb_g[:, 2, s4, :].unsqueeze(2).to_broadcast([128, 4, 8]), op=ALU.add))
                        dv(lambda e: e.reduce_max(out=rb_s[:, 2, :], in_=rb_m[:, 0, :, :], axis=AX.X))
                        dv(lambda e: e.tensor_tensor(out=rb_m[:, 1, :, :], in0=rb_m[:, 0, :, :], in1=bc(rb_s[:, 2, :], 32), op=ALU.is_ge))
                        dv(lambda e: e.scalar_tensor_tensor(out=rb_m[:, 2, :, :], in0=rb_m[:, 1, :, :], scalar=-BIG, in1=rb_m[:, 0, :, :],
                                                            op0=ALU.mult, op1=ALU.add))
                        dv(lambda e: e.reduce_max(out=rb_s[:, 3, :], in_=rb_m[:, 2, :, :], axis=AX.X))
                        dv(lambda e: e.tensor_tensor(out=rb_m[:, 3, :, :], in0=rb_m[:, 2, :, :], in1=bc(rb_s[:, 3, :], 32), op=ALU.is_ge))
                        dv(lambda e: e.tensor_tensor(out=rb_s[:, 4, :], in0=rb_s[:, 3, :], in1=rb_s[:, 2, :], op=ALU.subtract))
                        dv(lambda e: e.activation(out=rb_s[:, 4, :], in_=rb_s[:, 4, :], func=AF.Exp), "act")
                        dv(lambda e: e.tensor_scalar(out=rb_s[:, 5, :], in0=rb_s[:, 4, :], scalar1=1.0, scalar2=None, op0=ALU.add))
                        dv(lambda e: e.reciprocal(out=rb_s[:, 5, :], in_=rb_s[:, 5, :]))
                        dv(lambda e: e.tensor_tensor(out=rb_s[:, 5, :], in0=rb_s[:, 5, :], in1=rb_s[:, 1, :], op=ALU.mult))
                        dv(lambda e: e.tensor_tensor(out=rb_s[:, 6, :], in0=rb_s[:, 5, :], in1=rb_s[:, 4, :], op=ALU.mult))
                        dv(lambda e: e.tensor_tensor(out=rb_m[:, 1, :, :], in0=rb_m[:, 1, :, :], in1=bc(rb_s[:, 5, :], 32), op=ALU.mult))
                        dv(lambda e: e.tensor_tensor(out=rb_m[:, 3, :, :], in0=rb_m[:, 3, :, :], in1=bc(rb_s[:, 6, :], 32), op=ALU.mult))
                        dv(lambda e: e.tensor_tensor(out=rb_m[:, 0, :, :], in0=rb_m[:, 1, :, :], in1=rb_m[:, 3, :, :], op=ALU.add))
                        for s4 in range(4):
                            cx.op("pe", lambda e, s4=s4: e.transpose(out=ps[5][0:32, s4 * 128:(s4 + 1) * 128], in_=rb_m[:, 0, s4, :],
                                                                     identity=identf[:]),
                                  reads=[t_rt, t_const], writes=[pst[5]])
                        c0 = tl * TT
                        cx.op("act", lambda e, c0=c0: e.copy(out=gatesT[:, c0:c0 + TT], in_=ps[5][0:32, :]),
                              reads=[pst[5]], writes=[t_gT[tl]])
                    cx.barrier()
                with ExitStack() as st2:
                    w1b = [sb(st2, "w1b%d" % i, [128, NCH, FE], BF16) for i in range(2)]
                    w3b = [sb(st2, "w3b%d" % i, [128, NCH, FE], BF16) for i in range(2)]
                    w2b = [sb(st2, "w2b%d" % i, [128, 2, D], BF16) for i in range(2)]
                    t_wb = [Trk(), Trk()]
                    sl_ = [sb(st2, "sl%d" % i, [128, 2, TT], BF16) for i in range(2)]
                    t_sl = [[Trk(), Trk()], [Trk(), Trk()]]
                    tl_ = [sb(st2, "tl%d" % i, [128, 2, TT], BF16) for i in range(2)]
                    t_tl = [[Trk(), Trk()], [Trk(), Trk()]]
                    gs_ = [sb(st2, "gs%d" % i, [128, TT], BF16) for i in range(2)]
                    t_gs = [Trk(), Trk()]
                    ab = [sb(st2, "ab%d" % i, [128, 2, TT], BF16) for i in range(2)]
                    t_ab = [Trk(), Trk()]
                    oi = [0]
                    munits = [(e_, tl) for e_ in range(NE) for tl in range(HT // TT)]

                    def emit_h(e_, tl, idx):
                        wb = e_ % 2
                        b = idx % 2
                        tsl = slice(tl * TT, (tl + 1) * TT)
                        for hc in range(2):
                            for k in range(NCH):
                                cx.op("pe", lambda e, k=k, hc=hc: e.matmul(
                                    ps[hc][:, :], lhsT=w1b[wb][:, k, hc * 128:(hc + 1) * 128], rhs=hn_all[:, k, tsl],
                                    start=(k == 0), stop=(k == NCH - 1)), reads=[t_wb[wb], t_hna[tl]], writes=[pst[hc]])
                        for hc in range(2):
                            for k in range(NCH):
                                cx.op("pe", lambda e, k=k, hc=hc: e.matmul(
                                    ps[2 + hc][:, :], lhsT=w3b[wb][:, k, hc * 128:(hc + 1) * 128], rhs=hn_all[:, k, tsl],
                                    start=(k == 0), stop=(k == NCH - 1)), reads=[t_wb[wb], t_hna[tl]], writes=[pst[2 + hc]])
                        cx.op("pe", lambda e: e.matmul(
                            ps[4][:, :], lhsT=selt[:, e_, :], rhs=gatesT[:, tsl], start=True, stop=True),
                            reads=[t_sel, t_gT[tl]], writes=[pst[4]])
                        cx.op("act", lambda e: e.copy(out=gs_[b][:], in_=ps[4][:, :]), reads=[pst[4]], writes=[t_gs[b]])
                        for hc in range(2):
                            cx.op("act", lambda e, hc=hc: e.activation(out=sl_[b][:, hc, :], in_=ps[hc][:, :], func=AF.Silu),
                                  reads=[pst[hc]], writes=[t_sl[b][hc]])
                            cx.op("act", lambda e, hc=hc: e.copy(out=tl_[b][:, hc, :], in_=ps[2 + hc][:, :]),
                                  reads=[pst[2 + hc]], writes=[t_tl[b][hc]])
                        for hc in range(2):
                            cx.op("pool", lambda e, hc=hc: e.tensor_tensor(out=sl_[b][:, hc, :], in0=sl_[b][:, hc, :],
                                                                        in1=tl_[b][:, hc, :], op=ALU.mult),
                                  reads=[t_tl[b][hc]], writes=[t_sl[b][hc]])
                            cx.op("pool", lambda e, hc=hc: e.tensor_tensor(out=ab[b][:, hc, :], in0=sl_[b][:, hc, :],
                                                                        in1=gs_[b][:], op=ALU.mult),
                                  reads=[t_sl[b][hc], t_gs[b]], writes=[t_ab[b]])

                    def emit_o(e_, tl, idx):
                        wb = e_ % 2
                        b = idx % 2
                        tsl = slice(tl * TT, (tl + 1) * TT)
                        for n in range(NCH):
                            po = 5 + (oi[0] % 3)
                            oi[0] += 1
                            for hc in range(2):
                                cx.op("pe", lambda e, n=n, hc=hc, po=po: e.matmul(
                                    ps[po][:, :], lhsT=w2b[wb][:, hc, n * 128:(n + 1) * 128], rhs=ab[b][:, hc, :],
                                    start=(hc == 0), stop=(hc == 1)), reads=[t_wb[wb], t_ab[b]], writes=[pst[po]])
                            if e_ == 0:
                                cx.op("dve", lambda e, n=n, po=po: e.tensor_copy(out=acc[:, n, tsl], in_=ps[po][:, :]),
                                      reads=[pst[po]], writes=[t_acc[tl][n]])
                            else:
                                cx.op("dve", lambda e, n=n, po=po: e.tensor_tensor(
                                    out=acc[:, n, tsl], in0=acc[:, n, tsl], in1=ps[po][:, :], op=ALU.add),
                                    reads=[pst[po], t_acc[tl][n]], writes=[t_acc[tl][n]])

                    def load_w(e_):
                        wb = e_ % 2
                        cx.dma("pool", w1b[wb][:], moe_w1[l, e_].rearrange("(c p) f -> p c f", p=128), writes=[t_wb[wb]])
                        cx.dma("pool", w3b[wb][:], moe_w3[l, e_].rearrange("(c p) f -> p c f", p=128), writes=[t_wb[wb]])
                        cx.dma("pool", w2b[wb][:], moe_w2[l, e_].rearrange("(c p) f -> p c f", p=128), writes=[t_wb[wb]])

                    load_w(0)
                    for idx in range(len(munits) + 1):
                        if idx < len(munits):
                            emit_h(munits[idx][0], munits[idx][1], idx)
                        if idx >= 1:
                            emit_o(munits[idx - 1][0], munits[idx - 1][1], idx - 1)
                        if idx < len(munits) and munits[idx][1] == 0 and munits[idx][0] + 1 < NE:
                            load_w(munits[idx][0] + 1)
                    cx.barrier()
                with ExitStack() as st2:
                    hts = [sb(st2, "m3h%d" % i, [128, NCH, TT]) for i in range(2)]
                    t_h3 = [Trk(), Trk()]
                    ho = [sb(st2, "m3o%d" % i, [128, NCH, TT]) for i in range(2)]
                    t_o3 = [Trk(), Trk()]
                    for tl in range(HT // TT):
                        b = tl % 2
                        t0 = hbase + tl * TT
                        tsl = slice(tl * TT, (tl + 1) * TT)
                        cx.dma("sp", hts[b][:], hin[:, :, t0:t0 + TT].rearrange("c p t -> p c t"), writes=[t_h3[b]])
                        for n in range(NCH):
                            cx.op("dve", lambda e, n=n, b=b, tsl=tsl: e.scalar_tensor_tensor(
                                out=ho[b][:, n, :], in0=acc[:, n, tsl], scalar=gatec(mi, n), in1=hts[b][:, n, :],
                                op0=ALU.mult, op1=ALU.add), reads=[t_acc[tl][n], t_mods, t_h3[b]], writes=[t_o3[b]])
                        cx.dma("sp", hdst[:, :, t0:t0 + TT].rearrange("c p t -> p c t"), ho[b][:], reads=[t_o3[b]])
                    cx.barrier()
            cx.barrier()

    moe_stage(0, 1, h1, h2)
    chk("m0")

    blkf = sb(es, "blkf", [128, 128])
    cx.dma("sp", blkf[:], blk_c[:, :], writes=[t_const])

    def head_norm(psi, ps2i, gcol, out_ap, sqk, t_sqk, rk, t_rk):
        cx.op("act", lambda e: e.activation(out=sqk[:], in_=ps[psi][:, :], func=AF.Square), reads=[pst[psi]], writes=[t_sqk])
        cx.op("pe", lambda e: e.matmul(ps[ps2i][:, :], lhsT=blkf[:], rhs=sqk[:], start=True, stop=True),
              reads=[t_sqk, t_const], writes=[pst[ps2i]])
        cx.op("act", lambda e: e.activation(out=rk[:], in_=ps[ps2i][:, :], func=AF.Sqrt, bias=epsc[:, 0:1], scale=1.0 / HD),
              reads=[pst[ps2i], t_const], writes=[t_rk])
        cx.op("dve", lambda e: e.reciprocal(out=rk[:], in_=rk[:]), reads=[t_rk], writes=[t_rk])
        return lambda wr_t: cx.op("dve", lambda e: e.scalar_tensor_tensor(
            out=out_ap, in0=ps[psi][:, :], scalar=gcol, in1=rk[:], op0=ALU.mult, op1=ALU.mult),
            reads=[pst[psi], t_rk, t_const], writes=[wr_t])

    def kv_stage():
        with ExitStack() as st:
            kvw = sb(st, "kvw", [128, NCH, 2 * D], BF16)
            t_kvw = Trk()
            ksrc = kv_w.rearrange("(c p) n -> p c n", p=128)
            for kc in range(NCH):
                cx.dma("pool", kvw[:, kc, :], ksrc[:, kc, 0:2 * D], writes=[t_kvw])
            fw = sb(st, "fw", [128, NCH, H])
            for kc in range(NCH):
                cx.dma("sp", fw[:, kc, :], ksrc[:, kc, 2 * D:2 * D + H], writes=[t_kvw])
            gk = sb(st, "gk", [128, 1])
            nfb = sb(st, "nfb", [H, 1])
            ones512 = sb(st, "ones512", [H, TT])
            cx.dma("sp", gk[:], gk_col[:, :], writes=[t_const])
            cx.dma("sp", nfb[:], fb_col[:, :], writes=[t_const])
            cx.op("dve", lambda e: e.tensor_scalar(out=nfb[:], in0=nfb[:], scalar1=-1.0, scalar2=None, op0=ALU.mult),
                  reads=[t_const], writes=[t_const])
            cx.op("dve", lambda e: e.memset(ones512[:], 1.0), writes=[t_const])
            Ft = sb(st, "Ft", [H, L])
            t_F = [Trk() for _ in range(NTT)]
            with ExitStack() as st2:
                hts = [sb(st2, "k_ht%d" % i, [128, NCH, TT]) for i in range(2)]
                t_ht = [Trk(), Trk()]
                sq = sb(st2, "k_sq", [128, NCH, TT], BF16)
                t_sq = Trk()
                rinv = sb(st2, "k_rinv", [128, TT])
                t_rinv = Trk()
                tmp = sb(st2, "k_tmp", [128, NCH, TT])
                t_tmp = Trk()
                hnf = sb(st2, "k_hnf", [128, NCH, TT])
                t_hnf = Trk()
                hnb = sb(st2, "k_hnb", [128, NCH, TT], BF16)
                t_hnb = Trk()
                kt = [sb(st2, "k_kt%d" % i, [128, NCH, TT], BF16) for i in range(2)]
                t_kt = [Trk(), Trk()]
                vt = [sb(st2, "k_vt%d" % i, [128, 4, D], BF16) for i in range(2)]
                t_vt = [Trk(), Trk()]
                sqk = sb(st2, "k_sqk", [128, TT])
                t_sqk = Trk()
                rk = sb(st2, "k_rk", [128, TT])
                t_rk = Trk()
                ef = sb(st2, "k_ef", [H, TT])
                t_ef = Trk()
                for tt in range(NTT):
                    b = tt % 2
                    t0 = tt * TT
                    cx.dma("sp", hts[b][:], h2[:, :, t0:t0 + TT].rearrange("c p t -> p c t"), writes=[t_ht[b]])
                    norm_mod(4, hts[b], t_ht[b], sq, t_sq, rinv, t_rinv, tmp, t_tmp, hnb, t_hnb, 0, hn_f=hnf, t_hnf=t_hnf)
                    for n in range(NCH):
                        pi = 1 + (n % 2)
                        for k in range(NCH):
                            cx.op("pe", lambda e, n=n, k=k, pi=pi: e.matmul(
                                ps[pi][:, :], lhsT=kvw[:, k, n * 128:(n + 1) * 128], rhs=hnb[:, k, :],
                                start=(k == 0), stop=(k == NCH - 1)), reads=[t_kvw, t_hnb], writes=[pst[pi]])
                        fin = head_norm(pi, 3, gk[:, 0:1], kt[b][:, n, :], sqk, t_sqk, rk, t_rk)
                        fin(t_kt[b])
                    cx.dma("sp", Kd[:, :, t0:t0 + TT].rearrange("c p t -> p c t"), kt[b][:], reads=[t_kt[b]])
                    for s_ in range(4):
                        for hf in range(2):
                            pi = 4 + ((s_ * 2 + hf) % 2)
                            for k in range(NCH):
                                cx.op("pe", lambda e, s_=s_, hf=hf, k=k, pi=pi: e.matmul(
                                    ps[pi][:, :], lhsT=hnb[:, k, s_ * 128:(s_ + 1) * 128],
                                    rhs=kvw[:, k, D + hf * 512:D + (hf + 1) * 512],
                                    start=(k == 0), stop=(k == NCH - 1)), reads=[t_kvw, t_hnb], writes=[pst[pi]])
                            if hf == 0:
                                cx.op("act", lambda e, s_=s_, hf=hf, pi=pi, b=b: e.copy(out=vt[b][:, s_, hf * 512:(hf + 1) * 512], in_=ps[pi][:, :]),
                                      reads=[pst[pi]], writes=[t_vt[b]])
                            else:
                                cx.op("dve", lambda e, s_=s_, hf=hf, pi=pi, b=b: e.tensor_copy(out=vt[b][:, s_, hf * 512:(hf + 1) * 512], in_=ps[pi][:, :]),
                                      reads=[pst[pi]], writes=[t_vt[b]])
                    cx.dma("sp", Vd[t0:t0 + TT, :].rearrange("(s p) d -> p s d", p=128), vt[b][:], reads=[t_vt[b]])
                    for k in range(NCH):
                        cx.op("pe", lambda e, k=k: e.matmul(ps[6][0:H, :], lhsT=fw[:, k, :], rhs=hnf[:, k, :],
                                                            start=(k == 0), stop=(k == NCH - 1)),
                              reads=[t_kvw, t_hnf], writes=[pst[6]])
                    cx.op("act", lambda e: e.activation(out=ef[:], in_=ps[6][0:H, :], func=AF.Exp, bias=nfb[:, 0:1], scale=-1.0),
                          reads=[pst[6], t_const], writes=[t_ef])
                    cx.op("act", lambda e: e.activation(out=ef[:], in_=ef[:], func=AF.Ln, bias=ones512[:, 0:1], scale=1.0),
                          reads=[t_ef, t_const], writes=[t_ef])
                    if tt == 0:
                        ini, rd = epsc[0:H, 2:3], [t_const]
                    else:
                        ini, rd = Ft[:, t0 - 1:t0], [t_F[tt - 1]]
                    cx.op("dve", lambda e, t0=t0, ini=ini: e.tensor_tensor_scan(
                        out=Ft[:, t0:t0 + TT], data0=ones512[:], data1=ef[:], initial=ini, op0=ALU.mult, op1=ALU.subtract),
                        reads=[t_ef, t_const] + rd, writes=[t_F[tt]])
                cx.barrier()
            with ExitStack() as st2:
                X = sb(st2, "f_X", [H, L])
                q3 = sb(st2, "f_q3", [H, 3, L], BF16)
                k3 = sb(st2, "f_k3", [H, 3, L], BF16)
                t_x = Trk()
                cx.op("dve", lambda e: e.tensor_scalar(out=X[:], in0=Ft[:], scalar1=8.0, scalar2=None, op0=ALU.mult),
                      reads=t_F, writes=[t_x])
                for i in range(3):
                    cx.op("dve", lambda e, i=i: e.tensor_copy(out=q3[:, i, :], in_=X[:]), reads=[t_x], writes=[t_x])
                    cx.op("dve", lambda e, i=i: e.tensor_scalar(out=k3[:, i, :], in0=q3[:, i, :], scalar1=-1.0, scalar2=None, op0=ALU.mult),
                          reads=[t_x], writes=[t_x])
                    if i < 2:
                        cx.op("dve", lambda e, i=i: e.tensor_tensor(out=X[:], in0=X[:], in1=q3[:, i, :], op=ALU.subtract),
                              reads=[t_x], writes=[t_x])
                cx.dma("sp", Fq[:, :, :], q3[:], reads=[t_x])
                cx.dma("sp", Fk[:, :, :], k3[:], reads=[t_x])
                cx.barrier()
            cx.barrier()

    def attn_stage():
        mi = 2
        with ExitStack() as st:
            wqg = sb(st, "wqg", [128, NCH, 2 * D], BF16)
            t_w = Trk()
            wsrc = fox_w_qg.rearrange("(c p) n -> p c n", p=128)
            for kc in range(NCH):
                cx.dma("pool", wqg[:, kc, :], wsrc[:, kc, :], writes=[t_w])
            gq = sb(st, "gq", [128, 1])
            cx.dma("sp", gq[:], gq_col[:, :], writes=[t_const])
            hts = [sb(st, "q_ht%d" % i, [128, NCH, TT]) for i in range(2)]
            t_ht = [Trk(), Trk()]
            sq = sb(st, "q_sq", [128, NCH, TT], BF16)
            t_sq = Trk()
            rinv = sb(st, "q_rinv", [128, TT])
            t_rinv = Trk()
            tmp = sb(st, "q_tmp", [128, NCH, TT])
            t_tmp = Trk()
            hnb = sb(st, "q_hnb", [128, NCH, TT], BF16)
            t_hnb = Trk()
            qt = [sb(st, "q_qt%d" % i, [128, NCH, TT], BF16) for i in range(2)]
            t_qt = [Trk(), Trk()]
            sgt = [sb(st, "q_sg%d" % i, [128, NCH, TT], BF16) for i in range(2)]
            t_sgt = [Trk(), Trk()]
            sqk = sb(st, "q_sqk", [128, TT])
            t_sqk = Trk()
            rk = sb(st, "q_rk", [128, TT])
            t_rk = Trk()
            for tt in range(NTT):
                b = tt % 2
                t0 = tt * TT
                cx.dma("sp", hts[b][:], h2[:, :, t0:t0 + TT].rearrange("c p t -> p c t"), writes=[t_ht[b]])
                norm_mod(mi, hts[b], t_ht[b], sq, t_sq, rinv, t_rinv, tmp, t_tmp, hnb, t_hnb, 0)
                for n in range(NCH):
                    pi = 1 + (n % 2)
                    for k in range(NCH):
                        cx.op("pe", lambda e, n=n, k=k, pi=pi: e.matmul(
                            ps[pi][:, :], lhsT=wqg[:, k, n * 128:(n + 1) * 128], rhs=hnb[:, k, :],
                            start=(k == 0), stop=(k == NCH - 1)), reads=[t_w, t_hnb], writes=[pst[pi]])
                    fin = head_norm(pi, 3, gq[:, 0:1], qt[b][:, n, :], sqk, t_sqk, rk, t_rk)
                    fin(t_qt[b])
                    pg = 4 + (n % 2)
                    for k in range(NCH):
                        cx.op("pe", lambda e, n=n, k=k, pg=pg: e.matmul(
                            ps[pg][:, :], lhsT=wqg[:, k, D + n * 128:D + (n + 1) * 128], rhs=hnb[:, k, :],
                            start=(k == 0), stop=(k == NCH - 1)), reads=[t_w, t_hnb], writes=[pst[pg]])
                    cx.op("act", lambda e, n=n, pg=pg, b=b: e.activation(out=sgt[b][:, n, :], in_=ps[pg][:, :], func=AF.Sigmoid),
                          reads=[pst[pg]], writes=[t_sgt[b]])
                cx.dma("sp", Qd[:, :, t0:t0 + TT].rearrange("c p t -> p c t"), qt[b][:], reads=[t_qt[b]])
                cx.dma("sp", SGd[:, :, t0:t0 + TT].rearrange("c p t -> p c t"), sgt[b][:], reads=[t_sgt[b]])
            cx.barrier()
        with ExitStack() as st:
            tri = sb(st, "tri", [128, 128], BF16)
            onesb = sb(st, "onesb", [128, 64], BF16)
            t_tri = Trk()
            cx.dma("pool", tri[:], tri_c[:, :], writes=[t_tri])
            identb = sb(st, "identb", [128, 128], BF16)
            cx.dma("pool", identb[:], ident[:, :], writes=[t_tri])
            cx.op("dve", lambda e: e.memset(onesb[:], 1.0), writes=[t_tri])
            Ka = [sb(st, "Ka%d" % i, [128, L], BF16) for i in range(2)]
            Qa = [sb(st, "Qa%d" % i, [128, L], BF16) for i in range(2)]
            Vh = [sb(st, "Vh%d" % i, [128, L // 128, HD + 1], BF16) for i in range(2)]
            SGh = [sb(st, "SGh%d" % i, [64, L], BF16) for i in range(2)]
            t_hd = [Trk(), Trk()]
            for i in range(2):
                cx.op("dve", lambda e, i=i: e.memset(Ka[i][64:128, :], 1.0), writes=[t_hd[i]])
                cx.op("dve", lambda e, i=i: e.memset(Qa[i][64:128, :], 1.0), writes=[t_hd[i]])
                cx.op("dve", lambda e, i=i: e.memset(Vh[i][:, :, HD:HD + 1], 1.0), writes=[t_hd[i]])
            NP = 3
            pt = [sb(st, "pt%d" % i, [128, TT], BF16) for i in range(NP)]
            t_pt = [Trk() for _ in range(NP)]
            rr = sb(st, "rr", [128, TT])
            t_rr = Trk()
            rb = sb(st, "rb", [64, TT])
            t_rb = Trk()
            ot = sb(st, "ot", [64, TT])
            t_ot = Trk()
            ob = [sb(st, "ob%d" % i, [64, TT], BF16) for i in range(2)]
            t_ob = [Trk(), Trk()]

            def load_head(h):
                hb = h % 2
                c, ro = h // 2, (h % 2) * 64
                cx.dma("sp", Ka[hb][0:64, :], Kd[c, ro:ro + 64, :], writes=[t_hd[hb]])
                cx.dma("sp", Ka[hb][67:70, :], Fk[h], writes=[t_hd[hb]])
                cx.dma("sp", Qa[hb][0:64, :], Qd[c, ro:ro + 64, :], writes=[t_hd[hb]])
                cx.dma("sp", Qa[hb][64:67, :], Fq[h], writes=[t_hd[hb]])
                cx.dma("sp", Vh[hb][:, :, 0:HD], Vd[:, h * HD:(h + 1) * HD].rearrange("(b p) d -> p b d", p=128),
                       writes=[t_hd[hb]])
                cx.dma("sp", SGh[hb][:], SGd[c, ro:ro + 64, :], writes=[t_hd[hb]])

            units = []
            ui = 0
            for h in range(H):
                for qc in range(NTT):
                    for kb in range(4 * qc + 4):
                        units.append((h, qc, kb, ui))
                    ui += 1

            def emit_s(u, idx):
                h, qc, kb, ui = u
                hb = h % 2
                i = kb - 4 * qc
                cs = max(0, i) * 128
                sbank = idx % 2
                pb = idx % NP
                cx.op("pe", lambda e: e.matmul(
                    ps[sbank][:, cs:TT], lhsT=Ka[hb][0:70, kb * 128:(kb + 1) * 128],
                    rhs=Qa[hb][0:70, qc * TT + cs:(qc + 1) * TT], start=True, stop=(i < 0)),
                    reads=[t_hd[hb]], writes=[pst[sbank]])
                if i >= 0:
                    cx.op("pe", lambda e: e.matmul(
                        ps[sbank][:, cs:cs + 128], lhsT=identb[:], rhs=tri[:], start=False, stop=True),
                        reads=[t_tri], writes=[pst[sbank]])
                cx.op("act", lambda e: e.activation(
                    out=pt[pb][:, cs:TT], in_=ps[sbank][:, cs:TT], func=AF.Exp, scale=0.125),
                    reads=[pst[sbank]], writes=[t_pt[pb]])

            def emit_pv(u, idx):
                h, qc, kb, ui = u
                hb = h % 2
                c, ro = h // 2, (h % 2) * 64
                i = kb - 4 * qc
                cs = max(0, i) * 128
                pb = idx % NP
                po = 2 + (ui % 2)
                pbb = 4 + (ui % 2)
                ub = ui % 2
                nkb = 4 * qc + 4
                cx.op("pe", lambda e: e.matmul(
                    ps[po][0:HD + 1, cs:TT], lhsT=Vh[hb][:, kb, :], rhs=pt[pb][:, cs:TT],
                    start=(kb == 0), stop=(kb == nkb - 1)), reads=[t_hd[hb], t_pt[pb]], writes=[pst[po]])
                if kb == nkb - 1:
                    cx.op("dve", lambda e: e.reciprocal(out=rr[64:65, :], in_=ps[po][64:65, :]), reads=[pst[po]], writes=[t_rr])
                    cx.op("pe", lambda e: e.matmul(ps[pbb][0:64, :], lhsT=onesf[64:65, 0:64], rhs=rr[64:65, :], start=True, stop=True),
                          reads=[t_rr, t_const], writes=[pst[pbb]])
                    cx.op("act", lambda e: e.copy(out=rb[:], in_=ps[pbb][0:64, :]), reads=[pst[pbb]], writes=[t_rb])
                    cx.op("dve", lambda e: e.tensor_tensor(out=ot[:], in0=rb[:], in1=ps[po][0:64, :], op=ALU.mult),
                          reads=[t_rb, pst[po]], writes=[t_ot])
                    cx.op("dve", lambda e: e.tensor_tensor(
                        out=ob[ub][:], in0=ot[:], in1=SGh[hb][:, qc * TT:(qc + 1) * TT], op=ALU.mult),
                        reads=[t_ot, t_hd[hb]], writes=[t_ob[ub]])
                    cx.dma("sp", Od[c, ro:ro + 64, qc * TT:(qc + 1) * TT], ob[ub][:], reads=[t_ob[ub]])

            load_head(0)
            for idx in range(len(units) + 1):
                if idx < len(units):
                    u = units[idx]
                    if u[1] == 0 and u[2] == 0 and u[0] + 1 < H and idx > 0:
                        pass
                    emit_s(u, idx)
                if idx >= 1:
                    emit_pv(units[idx - 1], idx - 1)
                    pu = units[idx - 1]
                    if idx < len(units) and units[idx][0] != pu[0]:
                        pass
                if idx < len(units):
                    u = units[idx]
                    if u[1] == 0 and u[2] == 1 - 1 and u[0] + 1 < H:
                        load_head(u[0] + 1)
            cx.barrier()
        with ExitStack() as st:
            wo = sb(st, "wo", [128, NCH, D], BF16)
            t_w = Trk()
            wsrc = fox_w_o.rearrange("(c p) n -> p c n", p=128)
            for kc in range(NCH):
                cx.dma("pool", wo[:, kc, :], wsrc[:, kc, :], writes=[t_w])
            otl = [sb(st, "o_ot%d" % i, [128, NCH, TT], BF16) for i in range(2)]
            t_otl = [Trk(), Trk()]
            hts = [sb(st, "o_ht%d" % i, [128, NCH, TT]) for i in range(2)]
            t_ht = [Trk(), Trk()]
            ho = [sb(st, "o_ho%d" % i, [128, NCH, TT]) for i in range(2)]
            t_ho = [Trk(), Trk()]
            it = 0
            for tt in range(NTT):
                b = tt % 2
                t0 = tt * TT
                cx.dma("sp", otl[b][:], Od[:, :, t0:t0 + TT].rearrange("c p t -> p c t"), writes=[t_otl[b]])
                cx.dma("sp", hts[b][:], h2[:, :, t0:t0 + TT].rearrange("c p t -> p c t"), writes=[t_ht[b]])
                for n in range(NCH):
                    pv = it % 4
                    it += 1
                    for k in range(NCH):
                        cx.op("pe", lambda e, n=n, k=k, pv=pv, b=b: e.matmul(
                            ps[pv][:, :], lhsT=wo[:, k, n * 128:(n + 1) * 128], rhs=otl[b][:, k, :],
                            start=(k == 0), stop=(k == NCH - 1)), reads=[t_w, t_otl[b]], writes=[pst[pv]])
                    cx.op("dve", lambda e, n=n, pv=pv, b=b: e.scalar_tensor_tensor(
                        out=ho[b][:, n, :], in0=ps[pv][:, :], scalar=gatec(mi, n), in1=hts[b][:, n, :],
                        op0=ALU.mult, op1=ALU.add), reads=[pst[pv], t_mods, t_ht[b]], writes=[t_ho[b]])
                cx.dma("sp", h3[:, :, t0:t0 + TT].rearrange("c p t -> p c t"), ho[b][:], reads=[t_ho[b]])
            cx.barrier()

    kv_stage()
    chk("kv")
    attn_stage()
    chk("at")
    moe_stage(1, 3, h3, hout)
    cx.barrier()
    return nc


def _state_layout(a):
    return np.ascontiguousarray(a.reshape(32, 2, 64).transpose(1, 2, 0).reshape(128, 32))


def _col_layout(v):
    return np.ascontiguousarray(v.reshape(-1, 128).T)


def make_inputs(inputs, b):
    f = np.float32
    m = {}
    m["xT"] = np.ascontiguousarray(inputs["x"][b].T).reshape(NCH, 128, L)
    m["c_col"] = _col_layout(inputs["c"][b])
    return m


def make_shared(inputs):
    f = np.float32
    m = {}
    m["ada_w"] = np.ascontiguousarray(inputs["ada_w"], dtype=f)
    m["ada_b"] = np.stack([_col_layout(inputs["ada_b"][i // 2, i % 2]) for i in range(4)])
    m["ln_g"] = np.stack([_col_layout(inputs["ln_g"][i // 2, i % 2]) for i in range(4)])
    m["kv_ada_w"] = np.ascontiguousarray(inputs["kv_ada_w"], dtype=f)
    m["kv_ada_b"] = _col_layout(inputs["kv_ada_b"])
    m["kv_g"] = _col_layout(inputs["kv_g"])
    m["s5_w_in"] = np.ascontiguousarray(inputs["s5_w_in"][0])
    m["s5_w_out"] = np.ascontiguousarray(inputs["s5_w_out"][0])
    ldt = np.repeat(inputs["s5_log_dt"][0][:, None], 64, axis=1)
    m["s5_par"] = np.stack([_state_layout(inputs["s5_lambda_re"][0]), _state_layout(inputs["s5_lambda_im"][0]),
                            _state_layout(ldt)])
    bp = np.zeros((2, 128, 32, 128), f)
    cp = np.zeros((2, 128, 32, 128), f)
    for k, (bn, cn) in enumerate([("s5_b_re", "s5_c_re"), ("s5_b_im", "s5_c_im")]):
        B_ = inputs[bn][0]
        C_ = inputs[cn][0]
        for j in range(32):
            o = (j % 4) * 32
            for gl in range(2):
                g = 2 * j + gl
                bp[k, gl * 64:(gl + 1) * 64, j, o + gl * 16:o + gl * 16 + 16] = B_[g]
                cp[k, gl * 64:(gl + 1) * 64, j, o + gl * 16:o + gl * 16 + 16] = C_[g].T
    m["s5_bpad"] = bp
    m["s5_cpad"] = cp
    m["s5_d"] = _col_layout(inputs["s5_d"][0])
    m["iota_t"] = np.ascontiguousarray(np.broadcast_to(np.arange(L, dtype=f)[None, :], (128, L)))
    m["ident"] = np.eye(128, dtype=f)
    m["moe_w1"] = np.ascontiguousarray(inputs["moe_w1"], dtype=f)
    m["moe_w3"] = np.ascontiguousarray(inputs["moe_w3"], dtype=f)
    m["moe_w2"] = np.ascontiguousarray(inputs["moe_w2"], dtype=f)
    m["moe_wr"] = np.ascontiguousarray(np.concatenate([inputs["moe_wg"], inputs["moe_we"]], axis=2), dtype=f)
    br = np.concatenate([inputs["moe_bg"], inputs["moe_be"]], axis=1).astype(f)
    m["moe_br"] = np.ascontiguousarray(np.broadcast_to(br[:, None, :], (2, 128, 36)))
    m["kv_w"] = np.ascontiguousarray(inputs["kv_w"], dtype=f)
    m["gk_col"] = np.ascontiguousarray(np.tile(inputs["k_norm_g"], 2)[:, None], dtype=f)
    m["gq_col"] = np.ascontiguousarray(np.tile(inputs["fox_q_norm_g"][0], 2)[:, None], dtype=f)
    m["fb_col"] = np.ascontiguousarray(inputs["kv_fb"][:, None], dtype=f)
    blk = np.zeros((128, 128), f)
    blk[0:64, 0:64] = 1.0
    blk[64:128, 64:128] = 1.0
    m["blk_c"] = blk
    m["tri_c"] = np.ascontiguousarray(np.tril(np.full((128, 128), -1.0e8, f), -1))
    m["fox_w_qg"] = np.ascontiguousarray(inputs["fox_w_qg"][0], dtype=f)
    m["fox_w_o"] = np.ascontiguousarray(inputs["fox_w_o"][0], dtype=f)
    sel = np.zeros((32, NE, 128), f)
    for e_ in range(NE):
        sel[e_, e_, :] = 1.0
    m["sel_c"] = sel
    return m


_NC_CACHE = {}


def kernel(**inputs):
    inputs = {k: np.asarray(v) for k, v in inputs.items()}
    if "nc" not in _NC_CACHE:
        _NC_CACHE["nc"] = build()
    nc = _NC_CACHE["nc"]
    shared = make_shared(inputs)
    in_maps = []
    for b in range(8):
        m = dict(shared)
        m.update(make_inputs(inputs, b))
        in_maps.append(m)
    res = run_bass_kernel_spmd(nc, in_maps, core_ids=list(range(8)))
    out = np.stack([np.ascontiguousarray(r["hout"].reshape(D, L).T) for r in res.results])
    return out.astype(np.float32)
```

```python
import math
from contextlib import ExitStack
import numpy as np
import concourse.bass as bass
import concourse.mybir as mybir
from concourse.bass_utils import run_bass_kernel_spmd

F32 = mybir.dt.float32
BF16 = mybir.dt.bfloat16
AF = mybir.ActivationFunctionType
ALU = mybir.AluOpType
AX = mybir.AxisListType

D = 1024
L = 4096
NCH = 8
TT = 512
NTT = L // TT
NG = 4
NE = 32
FE = 256
H = 16
HD = 64
EPS = 1e-6
MAGIC = 12582912.0
S2PI = 6.283180
HALFPI = 1.570795
GELU_C = 2.0 * math.sqrt(2.0 / math.pi)


class Trk:
    __slots__ = ("w", "r")

    def __init__(self):
        self.w = {}
        self.r = {}


class Ctx:
    def __init__(self, nc, es):
        self.nc = nc
        self.es = es
        self.engs = {"pe": nc.tensor, "act": nc.scalar, "dve": nc.vector, "pool": nc.gpsimd, "sp": nc.sync}
        self.sems = {}
        self.cnt = {}
        for k in ["pe", "act", "dve", "pool"]:
            self.sems[k] = es.enter_context(nc.semaphore("s_" + k))
            self.cnt[k] = 0
        self.seen = {k: {} for k in self.engs}
        self.dpool = {}
        self.dnext = {}
        for q, n in [("sp", 24), ("pool", 16), ("act", 6)]:
            keys = []
            for i in range(n):
                key = "d_%s%d" % (q, i)
                self.sems[key] = es.enter_context(nc.semaphore(key))
                self.cnt[key] = 0
                keys.append(key)
            self.dpool[q] = keys
            self.dnext[q] = 0

    def _wait(self, eng, deps):
        seen = self.seen[eng]
        for key, val in deps.items():
            if eng == "pe" and key == "pe":
                continue
            if seen.get(key, 0) < val:
                self.engs[eng].wait_ge(self.sems[key], val)
                seen[key] = val

    @staticmethod
    def _merge(dst, src):
        for k, v in src.items():
            if dst.get(k, 0) < v:
                dst[k] = v

    def _deps(self, reads, writes):
        deps = {}
        for t in reads:
            self._merge(deps, t.w)
        for t in writes:
            self._merge(deps, t.w)
            self._merge(deps, t.r)
        return deps

    def _record(self, ev, reads, writes):
        k, v = ev
        for t in reads:
            if t.r.get(k, 0) < v:
                t.r[k] = v
        for t in writes:
            t.w = {k: v}
            t.r = {}

    def op(self, eng, fn, reads=(), writes=()):
        self._wait(eng, self._deps(reads, writes))
        ins = fn(self.engs[eng])
        self.cnt[eng] += 1
        ins.then_inc(self.sems[eng], 1)
        self._record((eng, self.cnt[eng]), reads, writes)

    def dma(self, q, out, in_, reads=(), writes=(), **kw):
        keys = self.dpool[q]
        key = keys[self.dnext[q] % len(keys)]
        self.dnext[q] += 1
        deps = self._deps(reads, writes)
        if self.cnt[key] > 0:
            deps[key] = max(deps.get(key, 0), self.cnt[key])
        self._wait(q, deps)
        ins = self.engs[q].dma_start(out=out, in_=in_, **kw)
        self.cnt[key] += 16
        ins.then_inc(self.sems[key], 16)
        self._record((key, self.cnt[key]), reads, writes)

    def barrier(self, engines=("pe", "act", "dve", "pool", "sp")):
        allev = {k: v for k, v in self.cnt.items() if v > 0}
        for e in engines:
            self._wait(e, allev)


class _Stop(Exception):
    pass


def build(debug=False, stop=None):
    try:
        return _build(debug, stop)
    except _Stop as e:
        return e.args[0]


def _build(debug, stop):
    nc = bass.Bass("TRN2", target_bir_lowering=False)
    okind = "ExternalOutput" if debug else "Internal"

    def din(name, shape, dt=F32):
        return nc.dram_tensor(name, list(shape), dt, kind="ExternalInput").ap()

    def dscr(name, shape, dt=F32, out=False):
        return nc.dram_tensor(name, list(shape), dt, kind=("ExternalOutput" if out else okind)).ap()

    xT = din("xT", [NCH, 128, L])
    c_col = din("c_col", [128, NCH])
    ada_w = din("ada_w", [2, 2, D, 3 * D])
    ada_b = din("ada_b", [4, 128, 24])
    ln_g = din("ln_g", [4, 128, NCH])
    kv_ada_w = din("kv_ada_w", [D, 2 * D])
    kv_ada_b = din("kv_ada_b", [128, 16])
    kv_g = din("kv_g", [128, NCH])
    s5_w_in = din("s5_w_in", [D, D])
    s5_w_out = din("s5_w_out", [D, 2 * D])
    s5_par = din("s5_par", [3, 128, 32])
    s5_bpad = din("s5_bpad", [2, 128, 32, 128])
    s5_cpad = din("s5_cpad", [2, 128, 32, 128])
    s5_d = din("s5_d", [128, NCH])
    iota_t = din("iota_t", [128, L])
    ident = din("ident", [128, 128])
    hout = dscr("hout", [NCH, 128, L], out=True)
    moe_w1 = din("moe_w1", [2, NE, D, FE])
    moe_w3 = din("moe_w3", [2, NE, D, FE])
    moe_w2 = din("moe_w2", [2, NE, FE, D])
    moe_wr = din("moe_wr", [2, D, 36])
    moe_br = din("moe_br", [2, 128, 36])
    sel_c = din("sel_c", [32, NE, 128])
    h2 = dscr("h2", [NCH, 128, L])
    kv_w = din("kv_w", [D, 2 * D + H])
    gk_col = din("gk_col", [128, 1])
    gq_col = din("gq_col", [128, 1])
    fb_col = din("fb_col", [H, 1])
    blk_c = din("blk_c", [128, 128])
    tri_c = din("tri_c", [128, 128])
    fox_w_qg = din("fox_w_qg", [D, 2 * D])
    fox_w_o = din("fox_w_o", [D, D])
    Kd = dscr("Kd", [NCH, 128, L], BF16)
    Vd = dscr("Vd", [L, D], BF16)
    Fq = dscr("Fq", [H, 3, L], BF16)
    Fk = dscr("Fk", [H, 3, L], BF16)
    Qd = dscr("Qd", [NCH, 128, L], BF16)
    SGd = dscr("SGd", [NCH, 128, L], BF16)
    Od = dscr("Od", [NCH, 128, L], BF16)
    h3 = dscr("h3", [NCH, 128, L])

    h1 = dscr("h1", [NCH, 128, L])
    Gd = dscr("Gd", [NCH, 128, L], BF16)
    moddbg = dscr("moddbg", [128, 24 * 4 + 16])
    udbg = dscr("udbg", [NCH, 128, L], BF16) if debug else None

    es = ExitStack()
    cx = Ctx(nc, es)

    def chk(name):
        if stop == name:
            cx.barrier()
            raise _Stop(nc, es)

    _uid = [0]

    def sb(st, name, shape, dt=F32):
        _uid[0] += 1
        return st.enter_context(nc.sbuf_tensor("%s_%d" % (name, _uid[0]), list(shape), dt))

    ps = [es.enter_context(nc.psum_tensor("ps%d" % i, [128, 512], F32)) for i in range(8)]
    pst = [Trk() for _ in range(8)]

    mods = sb(es, "mods", [128, 24 * 4 + 16])
    modA = sb(es, "modA", [128, 5, NCH])
    t_mods = Trk()
    t_modA = Trk()
    identf = sb(es, "identf", [128, 128])
    onesf = sb(es, "onesf", [128, 128])
    onesbf = sb(es, "onesbf", [128, 128], BF16)
    t_const = Trk()
    cx.dma("sp", identf[:], ident[:, :], writes=[t_const])
    cx.op("dve", lambda e: e.memset(onesf[:], 1.0), writes=[t_const])
    cx.op("dve", lambda e: e.memset(onesbf[:], 1.0), writes=[t_const])

    with ExitStack() as st:
        ccol = sb(st, "ccol", [128, NCH])
        sc = sb(st, "sc", [128, NCH])
        bias_all = sb(st, "bias_all", [128, 24 * 4 + 16])
        g_all = sb(st, "g_all", [128, 5, NCH])
        wbuf = [sb(st, "wbuf%d" % i, [128, NCH, 1536]) for i in range(2)]
        t_w = [Trk(), Trk()]
        t_c = Trk()
        t_b = Trk()
        cx.dma("sp", ccol[:], c_col[:, :], writes=[t_c])
        for i in range(4):
            cx.dma("sp", bias_all[:, 24 * i:24 * (i + 1)], ada_b[i], writes=[t_b])
            cx.dma("sp", g_all[:, i, :], ln_g[i], writes=[t_b])
        cx.dma("sp", bias_all[:, 96:112], kv_ada_b[:, :], writes=[t_b])
        cx.dma("sp", g_all[:, 4, :], kv_g[:, :], writes=[t_b])
        cx.op("act", lambda e: e.activation(out=sc[:], in_=ccol[:], func=AF.Silu), reads=[t_c], writes=[t_c])
        units = []
        for i in range(4):
            for hf in range(2):
                units.append((ada_w[i // 2, i % 2], hf * 1536, 1536, 24 * i + 12 * hf))
        units.append((kv_ada_w, 0, 1024, 96))
        units.append((kv_ada_w, 1024, 1024, 104))
        for ui, (wap, c0, ncol, mcol) in enumerate(units):
            wb = wbuf[ui % 2]
            tw = t_w[ui % 2]
            src = wap.rearrange("(c p) n -> p c n", p=128)
            for kc in range(NCH):
                cx.dma("sp" if kc % 2 == 0 else "pool", wb[:, kc, 0:ncol], src[:, kc, c0:c0 + ncol], writes=[tw])
            pidx = ui % 2
            for n in range(ncol // 128):
                for kc in range(NCH):
                    cx.op("pe", lambda e, wb=wb, n=n, kc=kc, pidx=pidx: e.matmul(
                        ps[pidx][:, n:n + 1], lhsT=wb[:, kc, n * 128:(n + 1) * 128], rhs=sc[:, kc:kc + 1],
                        start=(kc == 0), stop=(kc == NCH - 1)), reads=[tw, t_c], writes=[pst[pidx]])
            nn = ncol // 128
            cx.op("dve", lambda e, pidx=pidx, nn=nn, mcol=mcol: e.tensor_tensor(
                out=mods[:, mcol:mcol + nn], in0=ps[pidx][:, 0:nn], in1=bias_all[:, mcol:mcol + nn], op=ALU.add),
                reads=[pst[pidx], t_b], writes=[t_mods])
        for i in range(5):
            sc0 = 24 * i + 8
            cx.op("dve", lambda e, i=i, sc0=sc0: e.scalar_tensor_tensor(
                out=modA[:, i, :], in0=mods[:, sc0:sc0 + 8], scalar=1.0, in1=g_all[:, i, :],
                op0=ALU.add, op1=ALU.mult), reads=[t_mods, t_b], writes=[t_modA])
        if debug:
            cx.dma("sp", moddbg[:, :], mods[:], reads=[t_mods])
        cx.barrier()

    chk("s0")

    def shiftc(i, c):
        return mods[:, 24 * i + c:24 * i + c + 1]

    def gatec(i, c):
        return mods[:, 24 * i + 16 + c:24 * i + 16 + c + 1]

    def scaleA(i, c):
        return modA[:, i, c:c + 1]

    _tmp_trk = {}

    def norm_mod(i, htile, t_h, sq, t_sq, rinv, t_rinv, tmp, t_tmp, hn_bf, t_hn, psi, hn_f=None, t_hnf=None):
        cx.op("act", lambda e: e.activation(out=sq[:], in_=htile[:], func=AF.Square), reads=[t_h], writes=[t_sq])
        for c in range(NCH):
            cx.op("pe", lambda e, c=c: e.matmul(ps[psi][:, :], lhsT=onesbf[:], rhs=sq[:, c, :],
                                                start=(c == 0), stop=(c == NCH - 1)),
                  reads=[t_sq, t_const], writes=[pst[psi]])
        cx.op("act", lambda e: e.activation(out=rinv[:], in_=ps[psi][:, :], func=AF.Sqrt, bias=epsc[:, 0:1],
                                            scale=1.0 / D), reads=[pst[psi], t_const], writes=[t_rinv])
        cx.op("dve", lambda e: e.reciprocal(out=rinv[:], in_=rinv[:]), reads=[t_rinv], writes=[t_rinv])
        tcs = _tmp_trk.setdefault(id(t_tmp), [Trk() for _ in range(NCH)])
        for c in range(NCH):
            cx.op("dve", lambda e, c=c: e.scalar_tensor_tensor(
                out=tmp[:, c, :], in0=htile[:, c, :], scalar=scaleA(i, c), in1=rinv[:],
                op0=ALU.mult, op1=ALU.mult), reads=[t_h, t_rinv, t_modA], writes=[tcs[c]])
            if hn_f is not None:
                cx.op("act", lambda e, c=c: e.activation(out=hn_f[:, c, :], in_=tmp[:, c, :], func=AF.Identity,
                                                        bias=shiftc(i, c), scale=1.0),
                      reads=[tcs[c], t_mods], writes=[t_hnf])
            cx.op("act", lambda e, c=c: e.activation(out=hn_bf[:, c, :], in_=tmp[:, c, :], func=AF.Identity,
                                                    bias=shiftc(i, c), scale=1.0),
                  reads=[tcs[c], t_mods], writes=[t_hn])

    epsc = sb(es, "epsc", [128, 4])
    cx.op("dve", lambda e: e.memset(epsc[:, 0:1], EPS), writes=[t_const])
    cx.op("dve", lambda e: e.memset(epsc[:, 1:2], HALFPI), writes=[t_const])
    cx.op("dve", lambda e: e.memset(epsc[:, 2:3], 0.0), writes=[t_const])
    cx.op("dve", lambda e: e.memset(epsc[:, 3:4], -MAGIC), writes=[t_const])

    with ExitStack() as st:
        u_bf = sb(st, "u_bf", [128, NCH, L], BF16)
        t_u = [Trk() for _ in range(NTT)]
        par = sb(st, "par", [128, 3, 32])
        t_par = Trk()
        for i in range(3):
            cx.dma("sp", par[:, i, :], s5_par[i], writes=[t_par])
        sp_ = sb(st, "s5small", [128, 22, 32])
        t_sp = Trk()

        def S(i):
            return sp_[:, i, :]
        lr, li, ldt = par[:, 0, :], par[:, 1, :], par[:, 2, :]
        DT, MAG, TH, THT, V, K_, FR, SIN, COS, ARE, AIM, DEN, CRE, CIM, T0, T1, C5, S5, U0, U1, U2, U3 = range(22)

        def dv(fn, eng="dve"):
            cx.op(eng, fn, reads=[t_par, t_sp, t_const], writes=[t_sp])
        dv(lambda e: e.activation(out=S(DT), in_=ldt, func=AF.Exp), "act")
        dv(lambda e: e.tensor_tensor(out=S(T0), in0=lr, in1=S(DT), op=ALU.mult))
        dv(lambda e: e.activation(out=S(MAG), in_=S(T0), func=AF.Exp), "act")
        dv(lambda e: e.tensor_tensor(out=S(TH), in0=li, in1=S(DT), op=ALU.mult))
        dv(lambda e: e.tensor_scalar(out=S(THT), in0=S(TH), scalar1=1.0 / (2 * math.pi), scalar2=None, op0=ALU.mult))
        dv(lambda e: e.tensor_scalar(out=S(V), in0=S(THT), scalar1=MAGIC, scalar2=None, op0=ALU.add))
        dv(lambda e: e.tensor_scalar(out=S(K_), in0=S(V), scalar1=-MAGIC, scalar2=None, op0=ALU.add))
        dv(lambda e: e.tensor_tensor(out=S(FR), in0=S(THT), in1=S(K_), op=ALU.subtract))
        dv(lambda e: e.activation(out=S(SIN), in_=S(FR), func=AF.Sin, scale=S2PI), "act")
        dv(lambda e: e.tensor_scalar(out=S(T0), in0=S(FR), scalar1=0.25, scalar2=-1.0, op0=ALU.is_gt, op1=ALU.mult))
        dv(lambda e: e.tensor_tensor(out=S(T0), in0=S(T0), in1=S(FR), op=ALU.add))
        dv(lambda e: e.activation(out=S(COS), in_=S(T0), func=AF.Sin, scale=S2PI, bias=epsc[:, 1:2]), "act")
        dv(lambda e: e.tensor_tensor(out=S(ARE), in0=S(MAG), in1=S(COS), op=ALU.mult))
        dv(lambda e: e.tensor_tensor(out=S(AIM), in0=S(MAG), in1=S(SIN), op=ALU.mult))
        dv(lambda e: e.tensor_tensor(out=S(T0), in0=lr, in1=lr, op=ALU.mult))
        dv(lambda e: e.tensor_tensor(out=S(T1), in0=li, in1=li, op=ALU.mult))
        dv(lambda e: e.tensor_tensor(out=S(DEN), in0=S(T0), in1=S(T1), op=ALU.add))
        dv(lambda e: e.reciprocal(out=S(DEN), in_=S(DEN)))
        dv(lambda e: e.tensor_scalar(out=S(T0), in0=S(ARE), scalar1=-1.0, scalar2=None, op0=ALU.add))
        dv(lambda e: e.tensor_tensor(out=S(CRE), in0=S(T0), in1=lr, op=ALU.mult))
        dv(lambda e: e.tensor_tensor(out=S(T1), in0=S(AIM), in1=li, op=ALU.mult))
        dv(lambda e: e.tensor_tensor(out=S(CRE), in0=S(CRE), in1=S(T1), op=ALU.add))
        dv(lambda e: e.tensor_tensor(out=S(CRE), in0=S(CRE), in1=S(DEN), op=ALU.mult))
        dv(lambda e: e.tensor_tensor(out=S(CIM), in0=S(AIM), in1=lr, op=ALU.mult))
        dv(lambda e: e.tensor_tensor(out=S(T1), in0=S(T0), in1=li, op=ALU.mult))
        dv(lambda e: e.tensor_tensor(out=S(CIM), in0=S(CIM), in1=S(T1), op=ALU.subtract))
        dv(lambda e: e.tensor_tensor(out=S(CIM), in0=S(CIM), in1=S(DEN), op=ALU.mult))
        dv(lambda e: e.tensor_copy(out=S(C5), in_=S(COS)))
        dv(lambda e: e.tensor_copy(out=S(S5), in_=S(SIN)))
        for _sq in range(9):
            dv(lambda e: e.tensor_tensor(out=S(U0), in0=S(C5), in1=S(C5), op=ALU.mult))
            dv(lambda e: e.tensor_tensor(out=S(U1), in0=S(S5), in1=S(S5), op=ALU.mult))
            dv(lambda e: e.scalar_tensor_tensor(out=S(U2), in0=S(C5), scalar=2.0, in1=S(S5), op0=ALU.mult, op1=ALU.mult))
            dv(lambda e: e.tensor_tensor(out=S(C5), in0=S(U0), in1=S(U1), op=ALU.subtract))
            dv(lambda e: e.tensor_copy(out=S(S5), in_=S(U2)))
        dv(lambda e: e.tensor_scalar(out=S(U3), in0=S(S5), scalar1=-1.0, scalar2=None, op0=ALU.mult))
        dv(lambda e: e.tensor_scalar(out=S(T1), in0=S(CIM), scalar1=-1.0, scalar2=None, op0=ALU.mult))

        Lre = sb(st, "Lre", [128, 32, 128], BF16)
        Lim = sb(st, "Lim", [128, 32, 128], BF16)
        Cre = sb(st, "Cre", [128, 32, 128], BF16)
        nCre = sb(st, "nCre", [128, 32, 128], BF16)
        nCim = sb(st, "nCim", [128, 32, 128], BF16)
        t_L = Trk()
        t_C = Trk()
        with ExitStack() as st2:
            bre = sb(st2, "bre", [128, 32, 128])
            bim = sb(st2, "bim", [128, 32, 128])
            t_bp = Trk()
            cx.dma("sp", bre[:], s5_bpad[0], writes=[t_bp])
            cx.dma("sp", bim[:], s5_bpad[1], writes=[t_bp])
            xa = [sb(st2, "xa%d" % i, [128, 128]) for i in range(2)]
            xb = [sb(st2, "xb%d" % i, [128, 128]) for i in range(2)]
            t_xa = [Trk(), Trk()]
            t_xb = [Trk(), Trk()]
            for j in range(32):
                b = j % 2
                cx.op("dve", lambda e, j=j, b=b: e.tensor_scalar(out=xa[b][:], in0=bim[:, j, :], scalar1=sp_[:, T1, j:j + 1],
                                                                 scalar2=None, op0=ALU.mult),
                      reads=[t_bp, t_sp], writes=[t_xa[b]])
                cx.op("dve", lambda e, j=j, b=b: e.scalar_tensor_tensor(out=xa[b][:], in0=bre[:, j, :], scalar=sp_[:, CRE, j:j + 1],
                                                                        in1=xa[b][:], op0=ALU.mult, op1=ALU.add),
                      reads=[t_bp, t_sp], writes=[t_xa[b]])
                cx.op("dve", lambda e, j=j, b=b: e.tensor_scalar(out=xb[b][:], in0=bre[:, j, :], scalar1=sp_[:, CIM, j:j + 1],
                                                                 scalar2=None, op0=ALU.mult),
                      reads=[t_bp, t_sp], writes=[t_xb[b]])
                cx.op("dve", lambda e, j=j, b=b: e.scalar_tensor_tensor(out=xb[b][:], in0=bim[:, j, :], scalar=sp_[:, CRE, j:j + 1],
                                                                        in1=xb[b][:], op0=ALU.mult, op1=ALU.add),
                      reads=[t_bp, t_sp], writes=[t_xb[b]])
                cx.op("pe", lambda e, b=b: e.transpose(out=ps[b][:, 0:128], in_=xa[b][:], identity=identf[:]),
                      reads=[t_xa[b], t_const], writes=[pst[b]])
                cx.op("pe", lambda e, b=b: e.transpose(out=ps[b][:, 128:256], in_=xb[b][:], identity=identf[:]),
                      reads=[t_xb[b], t_const], writes=[pst[b]])
                cx.op("act", lambda e, j=j, b=b: e.copy(out=Lre[:, j, :], in_=ps[b][:, 0:128]), reads=[pst[b]], writes=[t_L])
                cx.op("act", lambda e, j=j, b=b: e.copy(out=Lim[:, j, :], in_=ps[b][:, 128:256]), reads=[pst[b]], writes=[t_L])
            cx.dma("sp", bre[:], s5_cpad[0], reads=[], writes=[t_bp])
            cx.dma("sp", bim[:], s5_cpad[1], reads=[], writes=[t_bp])
            for q4 in range(4):
                sl = slice(q4 * 8, (q4 + 1) * 8)
                cx.op("act", lambda e, sl=sl: e.copy(out=Cre[:, sl, :], in_=bre[:, sl, :]), reads=[t_bp], writes=[t_C])
                cx.op("act", lambda e, sl=sl: e.mul(out=nCre[:, sl, :], in_=bre[:, sl, :], mul=-1.0), reads=[t_bp], writes=[t_C])
                cx.op("act", lambda e, sl=sl: e.mul(out=nCim[:, sl, :], in_=bim[:, sl, :], mul=-1.0), reads=[t_bp], writes=[t_C])
            cx.barrier()

        chk("a0")
        with ExitStack() as st2:
            win = sb(st2, "win", [128, NCH, D], BF16)
            t_win = Trk()
            wsrc = s5_w_in.rearrange("(c p) n -> p c n", p=128)
            for kc in range(NCH):
                cx.dma("pool", win[:, kc, :], wsrc[:, kc, :], writes=[t_win])
            hts = [sb(st2, "ht%d" % i, [128, NCH, TT]) for i in range(2)]
            t_ht = [Trk(), Trk()]
            sq = sb(st2, "sq", [128, NCH, TT], BF16)
            t_sq = Trk()
            rinv = sb(st2, "rinv", [128, TT])
            t_rinv = Trk()
            tmp = sb(st2, "tmpn", [128, NCH, TT])
            t_tmp = Trk()
            hnb = [sb(st2, "hnb%d" % i, [128, NCH, TT], BF16) for i in range(2)]
            t_hn = [Trk(), Trk()]
            for tt in range(NTT):
                b = tt % 2
                t0 = tt * TT
                cx.dma("sp", hts[b][:], xT[:, :, t0:t0 + TT].rearrange("c p t -> p c t"), writes=[t_ht[b]])
                norm_mod(0, hts[b], t_ht[b], sq, t_sq, rinv, t_rinv, tmp, t_tmp, hnb[b], t_hn[b], 0)
                for n in range(NCH):
                    pi = 1 + (n % 4)
                    for k in range(NCH):
                        cx.op("pe", lambda e, n=n, k=k, pi=pi, b=b: e.matmul(
                            ps[pi][:, :], lhsT=win[:, k, n * 128:(n + 1) * 128], rhs=hnb[b][:, k, :],
                            start=(k == 0), stop=(k == NCH - 1)), reads=[t_win, t_hn[b]], writes=[pst[pi]])
                    eng = "act" if n % 2 == 0 else "dve"
                    if eng == "act":
                        cx.op("act", lambda e, n=n, pi=pi, t0=t0: e.copy(out=u_bf[:, n, t0:t0 + TT], in_=ps[pi][:, :]),
                              reads=[pst[pi]], writes=[t_u[tt]])
                    else:
                        cx.op("dve", lambda e, n=n, pi=pi, t0=t0: e.tensor_copy(out=u_bf[:, n, t0:t0 + TT], in_=ps[pi][:, :]),
                              reads=[pst[pi]], writes=[t_u[tt]])
            cx.barrier()

        if debug:
            for n in range(NCH):
                cx.dma("sp", udbg[n], u_bf[:, n, :], reads=t_u)
        chk("a2")
        with ExitStack() as st2:
            iot = sb(st2, "iot", [128, TT])
            t_iot = Trk()
            cx.dma("sp", iot[:], iota_t[:, 0:TT], writes=[t_iot])
            dsk = sb(st2, "dsk", [128, NCH])
            cx.dma("sp", dsk[:], s5_d[:, :], writes=[t_iot])
            NB = 2

            def mk(name, dt=F32):
                return [sb(st2, "%s%d" % (name, i), [128, TT], dt) for i in range(NB)], [Trk() for _ in range(NB)]
            SNt, tSN = mk("SNt")
            CRt, tCR = mk("CRt")
            T1_, tT1 = mk("T1_")
            T2_, tT2 = mk("T2_")
            T3_, tT3 = mk("T3_")
            T4_, tT4 = mk("T4_")
            Vt, tVt = mk("Vt")
            Ft_, tFt = mk("Ftb")
            ini = [sb(st2, "ini%d" % i, [128, 4]) for i in range(NB)]
            t_ini = [Trk() for _ in range(NB)]
            XR, tXR = mk("XR")
            XI, tXI = mk("XI")
            SR, tSR = mk("SR")
            SI, tSI = mk("SI")
            P1, tP1 = mk("P1", BF16)
            P2, tP2 = mk("P2", BF16)
            P3, tP3 = mk("P3", BF16)
            P4, tP4 = mk("P4", BF16)
            ytmp = sb(st2, "ytmp", [128, L])
            t_y = [Trk() for _ in range(NTT)]
            gb = sb(st2, "gb", [128, L], BF16)
            t_gb = [Trk() for _ in range(NTT)]
            g1 = sb(st2, "g1", [128, TT])
            g2 = sb(st2, "g2", [128, TT])
            t_g1 = Trk()
            t_g2 = Trk()
            zero_init = epsc[:, 2:3]
            def gen_tables(j):
                thj = sp_[:, THT, j:j + 1]
                tb = j % 2
                cx.op("dve", lambda e: e.tensor_scalar(
                    out=Vt[tb][:], in0=iot[:, 0:TT], scalar1=thj, scalar2=MAGIC, op0=ALU.mult, op1=ALU.add),
                    reads=[t_iot, t_sp], writes=[tVt[tb]])
                cx.op("act", lambda e: e.activation(out=Vt[tb][:], in_=Vt[tb][:], func=AF.Identity,
                                                    bias=epsc[:, 3:4], scale=1.0),
                      reads=[tVt[tb], t_const], writes=[tVt[tb]])
                cx.op("dve", lambda e: e.scalar_tensor_tensor(
                    out=Ft_[tb][:], in0=iot[:, 0:TT], scalar=thj, in1=Vt[tb][:], op0=ALU.mult, op1=ALU.subtract),
                    reads=[t_iot, t_sp, tVt[tb]], writes=[tFt[tb]])
                cx.op("act", lambda e: e.activation(out=SNt[tb][:], in_=Ft_[tb][:], func=AF.Sin, scale=S2PI),
                      reads=[tFt[tb]], writes=[tSN[tb]])
                cx.op("dve", lambda e: e.tensor_scalar(
                    out=Vt[tb][:], in0=Ft_[tb][:], scalar1=0.25, scalar2=-1.0, op0=ALU.is_gt, op1=ALU.mult),
                    reads=[tFt[tb], tVt[tb]], writes=[tVt[tb]])
                cx.op("dve", lambda e: e.tensor_tensor(out=Vt[tb][:], in0=Vt[tb][:], in1=Ft_[tb][:], op=ALU.add),
                      reads=[tFt[tb], tVt[tb]], writes=[tVt[tb]])
                cx.op("act", lambda e: e.activation(out=CRt[tb][:], in_=Vt[tb][:], func=AF.Sin, scale=S2PI,
                                                    bias=epsc[:, 1:2]),
                      reads=[tVt[tb], t_const], writes=[tCR[tb]])

            class P_:
                pass
            pieces = []
            for c in range(NCH):
                for jj in range(4):
                    for tt in range(NTT):
                        p = P_()
                        p.c, p.jj, p.j, p.o, p.tt, p.s = c, jj, 4 * c + jj, jj * 32, tt, len(pieces)
                        p.b = p.s % NB
                        p.pb = 4 * (p.s % 2)
                        p.tb = p.j % 2
                        p.tsl = slice(tt * TT, (tt + 1) * TT)
                        pieces.append(p)

            def stg1(p):
                if p.tt == 0:
                    gen_tables(p.j)
                b, pb, tb, j, c, tsl, tt = p.b, p.pb, p.tb, p.j, p.c, p.tsl, p.tt
                cx.op("pe", lambda e: e.matmul(ps[pb][:, :], lhsT=Lre[:, j, :], rhs=u_bf[:, c, tsl], start=True, stop=True),
                      reads=[t_L, t_u[tt]], writes=[pst[pb]])
                cx.op("pe", lambda e: e.matmul(ps[pb + 1][:, :], lhsT=Lim[:, j, :], rhs=u_bf[:, c, tsl], start=True, stop=True),
                      reads=[t_L, t_u[tt]], writes=[pst[pb + 1]])
                cx.op("dve", lambda e: e.tensor_tensor(out=T1_[b][:], in0=CRt[tb][:], in1=ps[pb][:, :], op=ALU.mult),
                      reads=[tCR[tb], pst[pb]], writes=[tT1[b]])
                cx.op("dve", lambda e: e.tensor_tensor(out=T2_[b][:], in0=SNt[tb][:], in1=ps[pb + 1][:, :], op=ALU.mult),
                      reads=[tSN[tb], pst[pb + 1]], writes=[tT2[b]])
                cx.op("dve", lambda e: e.tensor_tensor(out=T3_[b][:], in0=CRt[tb][:], in1=ps[pb + 1][:, :], op=ALU.mult),
                      reads=[tCR[tb], pst[pb + 1]], writes=[tT3[b]])
                cx.op("dve", lambda e: e.tensor_tensor(out=T4_[b][:], in0=SNt[tb][:], in1=ps[pb][:, :], op=ALU.mult),
                      reads=[tSN[tb], pst[pb]], writes=[tT4[b]])

            def stg2(p):
                b = p.b
                cx.op("pool", lambda e: e.tensor_tensor(out=XR[b][:], in0=T1_[b][:], in1=T2_[b][:], op=ALU.add),
                      reads=[tT1[b], tT2[b]], writes=[tXR[b]])
                cx.op("pool", lambda e: e.tensor_tensor(out=XI[b][:], in0=T3_[b][:], in1=T4_[b][:], op=ALU.subtract),
                      reads=[tT3[b], tT4[b]], writes=[tXI[b]])

            def stg3(p):
                b, j, tt = p.b, p.j, p.tt
                pbuf = (b - 1) % NB
                magb = sp_[:, MAG, j:j + 1].to_broadcast([128, TT])
                if tt == 0:
                    ini_r, ini_i, rd = zero_init, zero_init, [t_const]
                else:
                    sre, sie = SR[pbuf][:, TT - 1:TT], SI[pbuf][:, TT - 1:TT]
                    c5, s5 = sp_[:, C5, j:j + 1], sp_[:, S5, j:j + 1]
                    rdp = [tSR[pbuf], tSI[pbuf], t_sp]
                    ns5 = sp_[:, U3, j:j + 1]
                    cx.op("act", lambda e: e.activation(out=ini[b][:, 2:3], in_=sie, func=AF.Identity, scale=ns5, bias=epsc[:, 2:3]),
                          reads=rdp + [t_const], writes=[t_ini[b]])
                    cx.op("act", lambda e: e.activation(out=ini[b][:, 0:1], in_=sre, func=AF.Identity, scale=c5, bias=ini[b][:, 2:3]),
                          reads=rdp + [t_ini[b]], writes=[t_ini[b]])
                    cx.op("act", lambda e: e.activation(out=ini[b][:, 3:4], in_=sie, func=AF.Identity, scale=c5, bias=epsc[:, 2:3]),
                          reads=rdp + [t_ini[b], t_const], writes=[t_ini[b]])
                    cx.op("act", lambda e: e.activation(out=ini[b][:, 1:2], in_=sre, func=AF.Identity, scale=s5, bias=ini[b][:, 3:4]),
                          reads=rdp + [t_ini[b]], writes=[t_ini[b]])
                    ini_r, ini_i, rd = ini[b][:, 0:1], ini[b][:, 1:2], [t_ini[b]]
                cx.op("dve", lambda e: e.tensor_tensor_scan(
                    out=SR[b][:], data0=magb, data1=XR[b][:], initial=ini_r, op0=ALU.mult, op1=ALU.add),
                    reads=[tXR[b], t_sp] + rd, writes=[tSR[b]])
                cx.op("dve", lambda e: e.tensor_tensor_scan(
                    out=SI[b][:], data0=magb, data1=XI[b][:], initial=ini_i, op0=ALU.mult, op1=ALU.add),
                    reads=[tXI[b], t_sp] + rd, writes=[tSI[b]])

            def stg4(p):
                b, tb = p.b, p.tb
                cx.op("pool", lambda e: e.tensor_tensor(out=P1[b][:], in0=CRt[tb][:], in1=SR[b][:], op=ALU.mult),
                      reads=[tCR[tb], tSR[b]], writes=[tP1[b]])
                cx.op("pool", lambda e: e.tensor_tensor(out=P2[b][:], in0=SNt[tb][:], in1=SI[b][:], op=ALU.mult),
                      reads=[tSN[tb], tSI[b]], writes=[tP2[b]])
                cx.op("pool", lambda e: e.tensor_tensor(out=P3[b][:], in0=SNt[tb][:], in1=SR[b][:], op=ALU.mult),
                      reads=[tSN[tb], tSR[b]], writes=[tP3[b]])
                cx.op("pool", lambda e: e.tensor_tensor(out=P4[b][:], in0=CRt[tb][:], in1=SI[b][:], op=ALU.mult),
                      reads=[tCR[tb], tSI[b]], writes=[tP4[b]])

            def stg5(p):
                b, pb, j, c, o, tsl, tt = p.b, p.pb, p.j, p.c, p.o, p.tsl, p.tt
                py = pb + 2
                for idx, (cm, pp, tp) in enumerate([(Cre, P1, tP1), (nCre, P2, tP2), (nCim, P3, tP3), (nCim, P4, tP4)]):
                    cx.op("pe", lambda e, cm=cm, pp=pp, idx=idx: e.matmul(
                        ps[py][:, :], lhsT=cm[:, j, :], rhs=pp[b][:], start=(idx == 0), stop=(idx == 3)),
                        reads=[t_C, tp[b]], writes=[pst[py]])
                cx.op("dve", lambda e: e.scalar_tensor_tensor(
                    out=ytmp[o:o + 32, tsl], in0=u_bf[o:o + 32, c, tsl], scalar=dsk[o:o + 32, c:c + 1],
                    in1=ps[py][o:o + 32, :], op0=ALU.mult, op1=ALU.add),
                    reads=[t_u[tt], t_iot, pst[py]], writes=[t_y[tt]])
                if p.jj == 3 and tt == NTT - 1:
                    for t2 in range(NTT):
                        ts2 = slice(t2 * TT, (t2 + 1) * TT)
                        cx.op("act", lambda e, ts2=ts2: e.activation(out=g1[:], in_=ytmp[:, ts2], func=AF.Square),
                              reads=[t_y[t2]], writes=[t_g1])
                        cx.op("dve", lambda e: e.tensor_scalar(out=g1[:], in0=g1[:], scalar1=0.044715, scalar2=1.0,
                                                               op0=ALU.mult, op1=ALU.add), reads=[t_g1], writes=[t_g1])
                        cx.op("dve", lambda e, ts2=ts2: e.tensor_tensor(out=g2[:], in0=g1[:], in1=ytmp[:, ts2], op=ALU.mult),
                              reads=[t_g1, t_y[t2]], writes=[t_g2])
                        cx.op("act", lambda e: e.activation(out=g2[:], in_=g2[:], func=AF.Sigmoid, scale=GELU_C),
                              reads=[t_g2], writes=[t_g2])
                        cx.op("dve", lambda e, ts2=ts2: e.tensor_tensor(out=gb[:, ts2], in0=g2[:], in1=ytmp[:, ts2], op=ALU.mult),
                              reads=[t_g2, t_y[t2]], writes=[t_gb[t2]])
                    cx.dma("sp", Gd[c], gb[:], reads=t_gb, writes=[t_gd])
                    chk("a3g%d" % c)

            t_gd = Trk()
            npc = len(pieces)
            for s_ in range(npc + 2):
                if s_ < npc:
                    stg1(pieces[s_])
                    stg2(pieces[s_])
                if 1 <= s_ <= npc:
                    stg3(pieces[s_ - 1])
                    stg4(pieces[s_ - 1])
                if s_ >= 2:
                    stg5(pieces[s_ - 2])
            cx.barrier()
    cx.barrier()

    with ExitStack() as st:
        wout = sb(st, "wout", [128, NCH, 2 * D], BF16)
        t_wout = Trk()
        wsrc = s5_w_out.rearrange("(c p) n -> p c n", p=128)
        for kc in range(NCH):
            cx.dma("pool", wout[:, kc, :], wsrc[:, kc, :], writes=[t_wout])
        gt = [sb(st, "gt%d" % i, [128, NCH, TT], BF16) for i in range(2)]
        t_gt = [Trk(), Trk()]
        hts = [sb(st, "hto%d" % i, [128, NCH, TT]) for i in range(2)]
        t_ht = [Trk(), Trk()]
        ho = [sb(st, "ho%d" % i, [128, NCH, TT]) for i in range(2)]
        t_ho = [Trk(), Trk()]
        sg = [sb(st, "sg%d" % i, [128, TT]) for i in range(2)]
        t_sg = [Trk(), Trk()]
        mx = [sb(st, "mx%d" % i, [128, TT]) for i in range(2)]
        t_mx = [Trk(), Trk()]
        it = 0
        for tt in range(NTT):
            b = tt % 2
            t0 = tt * TT
            cx.dma("sp", gt[b][:], Gd[:, :, t0:t0 + TT].rearrange("c p t -> p c t"), writes=[t_gt[b]])
            cx.dma("sp", hts[b][:], xT[:, :, t0:t0 + TT].rearrange("c p t -> p c t"), writes=[t_ht[b]])
            for n in range(NCH):
                bb = it % 2
                pv = 2 * (it % 4)
                pg = pv + 1
                it += 1
                for k in range(NCH):
                    cx.op("pe", lambda e, n=n, k=k, pv=pv, b=b: e.matmul(
                        ps[pv][:, :], lhsT=wout[:, k, n * 128:(n + 1) * 128], rhs=gt[b][:, k, :],
                        start=(k == 0), stop=(k == NCH - 1)), reads=[t_wout, t_gt[b]], writes=[pst[pv]])
                for k in range(NCH):
                    cx.op("pe", lambda e, n=n, k=k, pg=pg, b=b: e.matmul(
                        ps[pg][:, :], lhsT=wout[:, k, D + n * 128:D + (n + 1) * 128], rhs=gt[b][:, k, :],
                        start=(k == 0), stop=(k == NCH - 1)), reads=[t_wout, t_gt[b]], writes=[pst[pg]])
                cx.op("act", lambda e, bb=bb, pg=pg: e.activation(out=sg[bb][:], in_=ps[pg][:, :], func=AF.Sigmoid),
                      reads=[pst[pg]], writes=[t_sg[bb]])
                cx.op("dve", lambda e, bb=bb, pv=pv: e.tensor_tensor(out=mx[bb][:], in0=sg[bb][:], in1=ps[pv][:, :], op=ALU.mult),
                      reads=[t_sg[bb], pst[pv]], writes=[t_mx[bb]])
                cx.op("dve", lambda e, bb=bb, n=n, b=b: e.scalar_tensor_tensor(
                    out=ho[b][:, n, :], in0=mx[bb][:], scalar=gatec(0, n), in1=hts[b][:, n, :], op0=ALU.mult, op1=ALU.add),
                    reads=[t_mx[bb], t_mods, t_ht[b]], writes=[t_ho[b]])
            cx.dma("sp", h1[:, :, t0:t0 + TT].rearrange("c p t -> p c t"), ho[b][:], reads=[t_ho[b]])
        cx.barrier()

    chk("a5")

    BIG = 1.0e30
    HT = 2048

    def moe_stage(l, mi, hin, hdst):
        with ExitStack() as st:
            acc = sb(st, "acc", [128, NCH, HT])
            t_acc = [[Trk() for _ in range(NCH)] for _ in range(4)]
            hn_all = sb(st, "hn_all", [128, NCH, HT], BF16)
            t_hna = [Trk() for _ in range(4)]
            gatesT = sb(st, "gatesT", [32, HT])
            t_gT = [Trk() for _ in range(4)]
            selt = sb(st, "selt", [32, NE, 128])
            t_sel = Trk()
            cx.dma("sp", selt[:], sel_c[:, :, :], writes=[t_sel])
            wr = sb(st, "wr", [128, NCH, 36])
            brt = sb(st, "brt", [128, 36])
            t_wr = Trk()
            cx.dma("sp", wr[:], moe_wr[l].rearrange("(c p) n -> p c n", p=128), writes=[t_wr])
            cx.dma("sp", brt[:], moe_br[l], writes=[t_wr])
            for half in range(L // HT):
                hbase = half * HT
                with ExitStack() as st2:
                    hts_ = [sb(st2, "m_ht%d" % i, [128, NCH, TT]) for i in range(2)]
                    t_hts = [Trk(), Trk()]
                    sq = sb(st2, "m_sq", [128, NCH, TT], BF16)
                    t_sq = Trk()
                    rinv = sb(st2, "m_rinv", [128, TT])
                    t_rinv = Trk()
                    tmp = sb(st2, "m_tmp", [128, NCH, TT])
                    t_tmp = Trk()
                    hnf = sb(st2, "m_hnf", [128, NCH, TT])
                    t_hn = Trk()
                    rt = sb(st2, "m_rt", [128, 8])
                    rb_lg = sb(st2, "rb_lg", [128, 4, 36])
                    rb_s = sb(st2, "rb_s", [128, 8, 4])
                    rb_g = sb(st2, "rb_g", [128, 3, 4, 4])
                    rb_m = sb(st2, "rb_m", [128, 4, 4, 32])
                    t_rt = Trk()
                    LG, GM, GMASK, GE, GS, PEN, MK, M1, MASK1, MK2, M2, MASK2, ED, W1, W2, GATES = (
                        slice(0, 36), slice(36, 37), slice(40, 44), slice(44, 48), slice(48, 49), slice(52, 56),
                        slice(56, 88), slice(88, 89), slice(96, 128), slice(128, 160), slice(160, 161),
                        slice(168, 200), slice(200, 201), slice(201, 202), slice(202, 203), slice(208, 240))
                    for tl in range(HT // TT):
                        t0 = hbase + tl * TT
                        ht, t_ht = hts_[tl % 2], t_hts[tl % 2]
                        cx.dma("sp", ht[:], hin[:, :, t0:t0 + TT].rearrange("c p t -> p c t"), writes=[t_ht])
                        norm_mod(mi, ht, t_ht, sq, t_sq, rinv, t_rinv, tmp, t_tmp,
                                 hn_all[:, :, tl * TT:(tl + 1) * TT], t_hna[tl], 7, hn_f=hnf, t_hnf=t_hn)
                        for sc_ in range(4):
                            for k in range(NCH):
                                cx.op("pe", lambda e, k=k, sc_=sc_: e.matmul(
                                    ps[6][:, sc_ * 36:(sc_ + 1) * 36], lhsT=hnf[:, k, sc_ * 128:(sc_ + 1) * 128], rhs=wr[:, k, :],
                                    start=(k == 0), stop=(k == NCH - 1)), reads=[t_hn, t_wr], writes=[pst[6]])

                        def dv(fn, eng="dve", extra=()):
                            cx.op(eng, fn, reads=[t_rt] + list(extra), writes=[t_rt])

                        def bc(ap2, n):
                            return ap2.unsqueeze(2).to_broadcast([128, 4, n])
                        LGv = rb_lg[:]
                        LGg = rb_lg[:, :, 0:4]
                        LGe = rb_lg[:, :, 4:36]
                        dv(lambda e: e.tensor_tensor(out=LGv, in0=ps[6][:, 0:144].rearrange("p (s n) -> p s n", s=4),
                                                     in1=brt[:].unsqueeze(1).to_broadcast([128, 4, 36]), op=ALU.add),
                           extra=[pst[6], t_wr])
                        dv(lambda e: e.reduce_max(out=rb_s[:, 0, :], in_=LGg, axis=AX.X))
                        dv(lambda e: e.tensor_tensor(out=rb_g[:, 0, :, :], in0=LGg, in1=bc(rb_s[:, 0, :], 4), op=ALU.is_ge))
                        dv(lambda e: e.tensor_tensor(out=rb_g[:, 1, :, :], in0=LGg, in1=bc(rb_s[:, 0, :], 4), op=ALU.subtract))
                        dv(lambda e: e.activation(out=rb_g[:, 1, :, :], in_=rb_g[:, 1, :, :], func=AF.Exp), "act")
                        dv(lambda e: e.reduce_sum(out=rb_s[:, 1, :], in_=rb_g[:, 1, :, :], axis=AX.X))
                        dv(lambda e: e.reciprocal(out=rb_s[:, 1, :], in_=rb_s[:, 1, :]))
                        dv(lambda e: e.tensor_scalar(out=rb_g[:, 2, :, :], in0=rb_g[:, 0, :, :], scalar1=-1.0, scalar2=BIG,
                                                     op0=ALU.add, op1=ALU.mult))
                        for s4 in range(4):
                            dv(lambda e, s4=s4: e.tensor_tensor(
                                out=rb_m[:, 0, s4, :].rearrange("p (g e) -> p g e", g=4),
                                in0=rb_lg[:, s4, 4:36].rearrange("p (g e) -> p g e", g=4),
                                in1=rb_g[:, 2, s4, :].unsqueeze(2).to_broadcast([128, 4, 8]), op=ALU.add))
                        dv(lambda e: e.reduce_max(out=rb_s[:, 2, :], in_=rb_m[:, 0, :, :], axis=AX.X))
                        dv(lambda e: e.tensor_tensor(out=rb_m[:, 1, :, :], in0=rb_m[:, 0, :, :], in1=bc(rb_s[:, 2, :], 32), op=ALU.is_ge))
                        dv(lambda e: e.scalar_tensor_tensor(out=rb_m[:, 2, :, :], in0=rb_m[:, 1, :, :], scalar=-BIG, in1=rb_m[:, 0, :, :],
                                                            op0=ALU.mult, op1=ALU.add))
                        dv(lambda e: e.reduce_max(out=rb_s[:, 3, :], in_=rb_m[:, 2, :, :], axis=AX.X))
                        dv(lambda e: e.tensor_tensor(out=rb_m[:, 3, :, :], in0=rb_m[:, 2, :, :], in1=bc(rb_s[:, 3, :], 32), op=ALU.is_ge))
                        dv(lambda e: e.tensor_tensor(out=rb_s[:, 4, :], in0=rb_s[:, 3, :], in1=rb_s[:, 2, :], op=ALU.subtract))
                        dv(lambda e: e.activation(out=rb_s[:, 4, :], in_=rb_s[:, 4, :], func=AF.Exp), "act")
                        dv(lambda e: e.tensor_scalar(out=rb_s[:, 5, :], in0=rb_s[:, 4, :], scalar1=1.0, scalar2=None, op0=ALU.add))
                        dv(lambda e: e.reciprocal(out=rb_s[:, 5, :], in_=rb_s[:, 5, :]))
                        dv(lambda e: e.tensor_tensor(out=rb_s[:, 5, :], in0=rb_s[:, 5, :], in1=rb_s[:, 1, :], op=ALU.mult))
                        dv(lambda e: e.tensor_tensor(out=rb_s[:, 6, :], in0=rb_s[:, 5, :], in1=rb_s[:, 4, :], op=ALU.mult))
                        dv(lambda e: e.tensor_tensor(out=rb_m[:, 1, :, :], in0=rb_m[:, 1, :, :], in1=bc(rb_s[:, 5, :], 32), op=ALU.mult))
                        dv(lambda e: e.tensor_tensor(out=rb_m[:, 3, :, :], in0=rb_m[:, 3, :, :], in1=bc(rb_s[:, 6, :], 32), op=ALU.mult))
                        dv(lambda e: e.tensor_tensor(out=rb_m[:, 0, :, :], in0=rb_m[:, 1, :, :], in1=rb_m[:, 3, :, :], op=ALU.add))
                        for s4 in range(4):
                            cx.op("pe", lambda e, s4=s4: e.transpose(out=ps[5][0:32, s4 * 128:(s4 + 1) * 128], in_=rb_m[:, 0, s4, :],
                                                                     identity=identf[:]),
                                  reads=[t_rt, t_const], writes=[pst[5]])
                        c0 = tl * TT
                        cx.op("act", lambda e, c0=c0: e.copy(out=gatesT[:, c0:c0 + TT], in_=ps[5][0:32, :]),
                              reads=[pst[5]], writes=[t_gT[tl]])
                    cx.barrier()
                with ExitStack() as st2:
                    w1b = [sb(st2, "w1b%d" % i, [128, NCH, FE], BF16) for i in range(2)]
                    w3b = [sb(st2, "w3b%d" % i, [128, NCH, FE], BF16) for i in range(2)]
                    w2b = [sb(st2, "w2b%d" % i, [128, 2, D], BF16) for i in range(2)]
                    t_wb = [Trk(), Trk()]
                    sl_ = [sb(st2, "sl%d" % i, [128, 2, TT], BF16) for i in range(2)]
                    t_sl = [[Trk(), Trk()], [Trk(), Trk()]]
                    tl_ = [sb(st2, "tl%d" % i, [128, 2, TT], BF16) for i in range(2)]
                    t_tl = [[Trk(), Trk()], [Trk(), Trk()]]
                    gs_ = [sb(st2, "gs%d" % i, [128, TT], BF16) for i in range(2)]
                    t_gs = [Trk(), Trk()]
                    ab = [sb(st2, "ab%d" % i, [128, 2, TT], BF16) for i in range(2)]
                    t_ab = [Trk(), Trk()]
                    oi = [0]
                    munits = [(e_, tl) for e_ in range(NE) for tl in range(HT // TT)]

                    def emit_h(e_, tl, idx):
                        wb = e_ % 2
                        b = idx % 2
                        tsl = slice(tl * TT, (tl + 1) * TT)
                        for hc in range(2):
                            for k in range(NCH):
                                cx.op("pe", lambda e, k=k, hc=hc: e.matmul(
                                    ps[hc][:, :], lhsT=w1b[wb][:, k, hc * 128:(hc + 1) * 128], rhs=hn_all[:, k, tsl],
                                    start=(k == 0), stop=(k == NCH - 1)), reads=[t_wb[wb], t_hna[tl]], writes=[pst[hc]])
                        for hc in range(2):
                            for k in range(NCH):
                                cx.op("pe", lambda e, k=k, hc=hc: e.matmul(
                                    ps[2 + hc][:, :], lhsT=w3b[wb][:, k, hc * 128:(hc + 1) * 128], rhs=hn_all[:, k, tsl],
                                    start=(k == 0), stop=(k == NCH - 1)), reads=[t_wb[wb], t_hna[tl]], writes=[pst[2 + hc]])
                        cx.op("pe", lambda e: e.matmul(
                            ps[4][:, :], lhsT=selt[:, e_, :], rhs=gatesT[:, tsl], start=True, stop=True),
                            reads=[t_sel, t_gT[tl]], writes=[pst[4]])
                        cx.op("act", lambda e: e.copy(out=gs_[b][:], in_=ps[4][:, :]), reads=[pst[4]], writes=[t_gs[b]])
                        for hc in range(2):
                            cx.op("act", lambda e, hc=hc: e.activation(out=sl_[b][:, hc, :], in_=ps[hc][:, :], func=AF.Silu),
                                  reads=[pst[hc]], writes=[t_sl[b][hc]])
                            cx.op("act", lambda e, hc=hc: e.copy(out=tl_[b][:, hc, :], in_=ps[2 + hc][:, :]),
                                  reads=[pst[2 + hc]], writes=[t_tl[b][hc]])
                        for hc in range(2):
                            cx.op("pool", lambda e, hc=hc: e.tensor_tensor(out=sl_[b][:, hc, :], in0=sl_[b][:, hc, :],
                                                                        in1=tl_[b][:, hc, :], op=ALU.mult),
                                  reads=[t_tl[b][hc]], writes=[t_sl[b][hc]])
                            cx.op("pool", lambda e, hc=hc: e.tensor_tensor(out=ab[b][:, hc, :], in0=sl_[b][:, hc, :],
                                                                        in1=gs_[b][:], op=ALU.mult),
                                  reads=[t_sl[b][hc], t_gs[b]], writes=[t_ab[b]])

                    def emit_o(e_, tl, idx):
                        wb = e_ % 2
                        b = idx % 2
                        tsl = slice(tl * TT, (tl + 1) * TT)
                        for n in range(NCH):
                            po = 5 + (oi[0] % 3)
                            oi[0] += 1
                            for hc in range(2):
                                cx.op("pe", lambda e, n=n, hc=hc, po=po: e.matmul(
                                    ps[po][:, :], lhsT=w2b[wb][:, hc, n * 128:(n + 1) * 128], rhs=ab[b][:, hc, :],
                                    start=(hc == 0), stop=(hc == 1)), reads=[t_wb[wb], t_ab[b]], writes=[pst[po]])
                            if e_ == 0:
                                cx.op("dve", lambda e, n=n, po=po: e.tensor_copy(out=acc[:, n, tsl], in_=ps[po][:, :]),
                                      reads=[pst[po]], writes=[t_acc[tl][n]])
                            else:
                                cx.op("dve", lambda e, n=n, po=po: e.tensor_tensor(
                                    out=acc[:, n, tsl], in0=acc[:, n, tsl], in1=ps[po][:, :], op=ALU.add),
                                    reads=[pst[po], t_acc[tl][n]], writes=[t_acc[tl][n]])

                    def load_w(e_):
                        wb = e_ % 2
                        cx.dma("pool", w1b[wb][:], moe_w1[l, e_].rearrange("(c p) f -> p c f", p=128), writes=[t_wb[wb]])
                        cx.dma("pool", w3b[wb][:], moe_w3[l, e_].rearrange("(c p) f -> p c f", p=128), writes=[t_wb[wb]])
                        cx.dma("pool", w2b[wb][:], moe_w2[l, e_].rearrange("(c p) f -> p c f", p=128), writes=[t_wb[wb]])

                    load_w(0)
                    for idx in range(len(munits) + 1):
                        if idx < len(munits):
                            emit_h(munits[idx][0], munits[idx][1], idx)
                        if idx >= 1:
                            emit_o(munits[idx - 1][0], munits[idx - 1][1], idx - 1)
                        if idx < len(munits) and munits[idx][1] == 0 and munits[idx][0] + 1 < NE:
                            load_w(munits[idx][0] + 1)
                    cx.barrier()
                with ExitStack() as st2:
                    hts = [sb(st2, "m3h%d" % i, [128, NCH, TT]) for i in range(2)]
                    t_h3 = [Trk(), Trk()]
                    ho = [sb(st2, "m3o%d" % i, [128, NCH, TT]) for i in range(2)]
                    t_o3 = [Trk(), Trk()]
                    for tl in range(HT // TT):
                        b = tl % 2
                        t0 = hbase + tl * TT
                        tsl = slice(tl * TT, (tl + 1) * TT)
                        cx.dma("sp", hts[b][:], hin[:, :, t0:t0 + TT].rearrange("c p t -> p c t"), writes=[t_h3[b]])
                        for n in range(NCH):
                            cx.op("dve", lambda e, n=n, b=b, tsl=tsl: e.scalar_tensor_tensor(
                                out=ho[b][:, n, :], in0=acc[:, n, tsl], scalar=gatec(mi, n), in1=hts[b][:, n, :],
                                op0=ALU.mult, op1=ALU.add), reads=[t_acc[tl][n], t_mods, t_h3[b]], writes=[t_o3[b]])
                        cx.dma("sp", hdst[:, :, t0:t0 + TT].rearrange("c p t -> p c t"), ho[b][:], reads=[t_o3[b]])
                    cx.barrier()
            cx.barrier()

    moe_stage(0, 1, h1, h2)
    chk("m0")

    blkf = sb(es, "blkf", [128, 128])
    cx.dma("sp", blkf[:], blk_c[:, :], writes=[t_const])

    def head_norm(psi, ps2i, gcol, out_ap, sqk, t_sqk, rk, t_rk):
        cx.op("act", lambda e: e.activation(out=sqk[:], in_=ps[psi][:, :], func=AF.Square), reads=[pst[psi]], writes=[t_sqk])
        cx.op("pe", lambda e: e.matmul(ps[ps2i][:, :], lhsT=blkf[:], rhs=sqk[:], start=True, stop=True),
              reads=[t_sqk, t_const], writes=[pst[ps2i]])
        cx.op("act", lambda e: e.activation(out=rk[:], in_=ps[ps2i][:, :], func=AF.Sqrt, bias=epsc[:, 0:1], scale=1.0 / HD),
              reads=[pst[ps2i], t_const], writes=[t_rk])
        cx.op("dve", lambda e: e.reciprocal(out=rk[:], in_=rk[:]), reads=[t_rk], writes=[t_rk])
        return lambda wr_t: cx.op("dve", lambda e: e.scalar_tensor_tensor(
            out=out_ap, in0=ps[psi][:, :], scalar=gcol, in1=rk[:], op0=ALU.mult, op1=ALU.mult),
            reads=[pst[psi], t_rk, t_const], writes=[wr_t])

    def kv_stage():
        with ExitStack() as st:
            kvw = sb(st, "kvw", [128, NCH, 2 * D], BF16)
            t_kvw = Trk()
            ksrc = kv_w.rearrange("(c p) n -> p c n", p=128)
            for kc in range(NCH):
                cx.dma("pool", kvw[:, kc, :], ksrc[:, kc, 0:2 * D], writes=[t_kvw])
            fw = sb(st, "fw", [128, NCH, H])
            for kc in range(NCH):
                cx.dma("sp", fw[:, kc, :], ksrc[:, kc, 2 * D:2 * D + H], writes=[t_kvw])
            gk = sb(st, "gk", [128, 1])
            nfb = sb(st, "nfb", [H, 1])
            ones512 = sb(st, "ones512", [H, TT])
            cx.dma("sp", gk[:], gk_col[:, :], writes=[t_const])
            cx.dma("sp", nfb[:], fb_col[:, :], writes=[t_const])
            cx.op("dve", lambda e: e.tensor_scalar(out=nfb[:], in0=nfb[:], scalar1=-1.0, scalar2=None, op0=ALU.mult),
                  reads=[t_const], writes=[t_const])
            cx.op("dve", lambda e: e.memset(ones512[:], 1.0), writes=[t_const])
            Ft = sb(st, "Ft", [H, L])
            t_F = [Trk() for _ in range(NTT)]
            with ExitStack() as st2:
                hts = [sb(st2, "k_ht%d" % i, [128, NCH, TT]) for i in range(2)]
                t_ht = [Trk(), Trk()]
                sq = sb(st2, "k_sq", [128, NCH, TT], BF16)
                t_sq = Trk()
                rinv = sb(st2, "k_rinv", [128, TT])
                t_rinv = Trk()
                tmp = sb(st2, "k_tmp", [128, NCH, TT])
                t_tmp = Trk()
                hnf = sb(st2, "k_hnf", [128, NCH, TT])
                t_hnf = Trk()
                hnb = sb(st2, "k_hnb", [128, NCH, TT], BF16)
                t_hnb = Trk()
                kt = [sb(st2, "k_kt%d" % i, [128, NCH, TT], BF16) for i in range(2)]
                t_kt = [Trk(), Trk()]
                vt = [sb(st2, "k_vt%d" % i, [128, 4, D], BF16) for i in range(2)]
                t_vt = [Trk(), Trk()]
                sqk = sb(st2, "k_sqk", [128, TT])
                t_sqk = Trk()
                rk = sb(st2, "k_rk", [128, TT])
                t_rk = Trk()
                ef = sb(st2, "k_ef", [H, TT])
                t_ef = Trk()
                for tt in range(NTT):
                    b = tt % 2
                    t0 = tt * TT
                    cx.dma("sp", hts[b][:], h2[:, :, t0:t0 + TT].rearrange("c p t -> p c t"), writes=[t_ht[b]])
                    norm_mod(4, hts[b], t_ht[b], sq, t_sq, rinv, t_rinv, tmp, t_tmp, hnb, t_hnb, 0, hn_f=hnf, t_hnf=t_hnf)
                    for n in range(NCH):
                        pi = 1 + (n % 2)
                        for k in range(NCH):
                            cx.op("pe", lambda e, n=n, k=k, pi=pi: e.matmul(
                                ps[pi][:, :], lhsT=kvw[:, k, n * 128:(n + 1) * 128], rhs=hnb[:, k, :],
                                start=(k == 0), stop=(k == NCH - 1)), reads=[t_kvw, t_hnb], writes=[pst[pi]])
                        fin = head_norm(pi, 3, gk[:, 0:1], kt[b][:, n, :], sqk, t_sqk, rk, t_rk)
                        fin(t_kt[b])
                    cx.dma("sp", Kd[:, :, t0:t0 + TT].rearrange("c p t -> p c t"), kt[b][:], reads=[t_kt[b]])
                    for s_ in range(4):
                        for hf in range(2):
                            pi = 4 + ((s_ * 2 + hf) % 2)
                            for k in range(NCH):
                                cx.op("pe", lambda e, s_=s_, hf=hf, k=k, pi=pi: e.matmul(
                                    ps[pi][:, :], lhsT=hnb[:, k, s_ * 128:(s_ + 1) * 128],
                                    rhs=kvw[:, k, D + hf * 512:D + (hf + 1) * 512],
                                    start=(k == 0), stop=(k == NCH - 1)), reads=[t_kvw, t_hnb], writes=[pst[pi]])
                            if hf == 0:
                                cx.op("act", lambda e, s_=s_, hf=hf, pi=pi, b=b: e.copy(out=vt[b][:, s_, hf * 512:(hf + 1) * 512], in_=ps[pi][:, :]),
                                      reads=[pst[pi]], writes=[t_vt[b]])
                            else:
                                cx.op("dve", lambda e, s_=s_, hf=hf, pi=pi, b=b: e.tensor_copy(out=vt[b][:, s_, hf * 512:(hf + 1) * 512], in_=ps[pi][:, :]),
                                      reads=[pst[pi]], writes=[t_vt[b]])
                    cx.dma("sp", Vd[t0:t0 + TT, :].rearrange("(s p) d -> p s d", p=128), vt[b][:], reads=[t_vt[b]])
                    for k in range(NCH):
                        cx.op("pe", lambda e, k=k: e.matmul(ps[6][0:H, :], lhsT=fw[:, k, :], rhs=hnf[:, k, :],
                                                            start=(k == 0), stop=(k == NCH - 1)),
                              reads=[t_kvw, t_hnf], writes=[pst[6]])
                    cx.op("act", lambda e: e.activation(out=ef[:], in_=ps[6][0:H, :], func=AF.Exp, bias=nfb[:, 0:1], scale=-1.0),
                          reads=[pst[6], t_const], writes=[t_ef])
                    cx.op("act", lambda e: e.activation(out=ef[:], in_=ef[:], func=AF.Ln, bias=ones512[:, 0:1], scale=1.0),
                          reads=[t_ef, t_const], writes=[t_ef])
                    if tt == 0:
                        ini, rd = epsc[0:H, 2:3], [t_const]
                    else:
                        ini, rd = Ft[:, t0 - 1:t0], [t_F[tt - 1]]
                    cx.op("dve", lambda e, t0=t0, ini=ini: e.tensor_tensor_scan(
                        out=Ft[:, t0:t0 + TT], data0=ones512[:], data1=ef[:], initial=ini, op0=ALU.mult, op1=ALU.subtract),
                        reads=[t_ef, t_const] + rd, writes=[t_F[tt]])
                cx.barrier()
            with ExitStack() as st2:
                X = sb(st2, "f_X", [H, L])
                q3 = sb(st2, "f_q3", [H, 3, L], BF16)
                k3 = sb(st2, "f_k3", [H, 3, L], BF16)
                t_x = Trk()
                cx.op("dve", lambda e: e.tensor_scalar(out=X[:], in0=Ft[:], scalar1=8.0, scalar2=None, op0=ALU.mult),
                      reads=t_F, writes=[t_x])
                for i in range(3):
                    cx.op("dve", lambda e, i=i: e.tensor_copy(out=q3[:, i, :], in_=X[:]), reads=[t_x], writes=[t_x])
                    cx.op("dve", lambda e, i=i: e.tensor_scalar(out=k3[:, i, :], in0=q3[:, i, :], scalar1=-1.0, scalar2=None, op0=ALU.mult),
                          reads=[t_x], writes=[t_x])
                    if i < 2:
                        cx.op("dve", lambda e, i=i: e.tensor_tensor(out=X[:], in0=X[:], in1=q3[:, i, :], op=ALU.subtract),
                              reads=[t_x], writes=[t_x])
                cx.dma("sp", Fq[:, :, :], q3[:], reads=[t_x])
                cx.dma("sp", Fk[:, :, :], k3[:], reads=[t_x])
                cx.barrier()
            cx.barrier()

    def attn_stage():
        mi = 2
        with ExitStack() as st:
            wqg = sb(st, "wqg", [128, NCH, 2 * D], BF16)
            t_w = Trk()
            wsrc = fox_w_qg.rearrange("(c p) n -> p c n", p=128)
            for kc in range(NCH):
                cx.dma("pool", wqg[:, kc, :], wsrc[:, kc, :], writes=[t_w])
            gq = sb(st, "gq", [128, 1])
            cx.dma("sp", gq[:], gq_col[:, :], writes=[t_const])
            hts = [sb(st, "q_ht%d" % i, [128, NCH, TT]) for i in range(2)]
            t_ht = [Trk(), Trk()]
            sq = sb(st, "q_sq", [128, NCH, TT], BF16)
            t_sq = Trk()
            rinv = sb(st, "q_rinv", [128, TT])
            t_rinv = Trk()
            tmp = sb(st, "q_tmp", [128, NCH, TT])
            t_tmp = Trk()
            hnb = sb(st, "q_hnb", [128, NCH, TT], BF16)
            t_hnb = Trk()
            qt = [sb(st, "q_qt%d" % i, [128, NCH, TT], BF16) for i in range(2)]
            t_qt = [Trk(), Trk()]
            sgt = [sb(st, "q_sg%d" % i, [128, NCH, TT], BF16) for i in range(2)]
            t_sgt = [Trk(), Trk()]
            sqk = sb(st, "q_sqk", [128, TT])
            t_sqk = Trk()
            rk = sb(st, "q_rk", [128, TT])
            t_rk = Trk()
            for tt in range(NTT):
                b = tt % 2
                t0 = tt * TT
                cx.dma("sp", hts[b][:], h2[:, :, t0:t0 + TT].rearrange("c p t -> p c t"), writes=[t_ht[b]])
                norm_mod(mi, hts[b], t_ht[b], sq, t_sq, rinv, t_rinv, tmp, t_tmp, hnb, t_hnb, 0)
                for n in range(NCH):
                    pi = 1 + (n % 2)
                    for k in range(NCH):
                        cx.op("pe", lambda e, n=n, k=k, pi=pi: e.matmul(
                            ps[pi][:, :], lhsT=wqg[:, k, n * 128:(n + 1) * 128], rhs=hnb[:, k, :],
                            start=(k == 0), stop=(k == NCH - 1)), reads=[t_w, t_hnb], writes=[pst[pi]])
                    fin = head_norm(pi, 3, gq[:, 0:1], qt[b][:, n, :], sqk, t_sqk, rk, t_rk)
                    fin(t_qt[b])
                    pg = 4 + (n % 2)
                    for k in range(NCH):
                        cx.op("pe", lambda e, n=n, k=k, pg=pg: e.matmul(
                            ps[pg][:, :], lhsT=wqg[:, k, D + n * 128:D + (n + 1) * 128], rhs=hnb[:, k, :],
                            start=(k == 0), stop=(k == NCH - 1)), reads=[t_w, t_hnb], writes=[pst[pg]])
                    cx.op("act", lambda e, n=n, pg=pg, b=b: e.activation(out=sgt[b][:, n, :], in_=ps[pg][:, :], func=AF.Sigmoid),
                          reads=[pst[pg]], writes=[t_sgt[b]])
                cx.dma("sp", Qd[:, :, t0:t0 + TT].rearrange("c p t -> p c t"), qt[b][:], reads=[t_qt[b]])
                cx.dma("sp", SGd[:, :, t0:t0 + TT].rearrange("c p t -> p c t"), sgt[b][:], reads=[t_sgt[b]])
            cx.barrier()
        with ExitStack() as st:
            tri = sb(st, "tri", [128, 128], BF16)
            onesb = sb(st, "onesb", [128, 64], BF16)
            t_tri = Trk()
            cx.dma("pool", tri[:], tri_c[:, :], writes=[t_tri])
            identb = sb(st, "identb", [128, 128], BF16)
            cx.dma("pool", identb[:], ident[:, :], writes=[t_tri])
            cx.op("dve", lambda e: e.memset(onesb[:], 1.0), writes=[t_tri])
            Ka = [sb(st, "Ka%d" % i, [128, L], BF16) for i in range(2)]
            Qa = [sb(st, "Qa%d" % i, [128, L], BF16) for i in range(2)]
            Vh = [sb(st, "Vh%d" % i, [128, L // 128, HD + 1], BF16) for i in range(2)]
            SGh = [sb(st, "SGh%d" % i, [64, L], BF16) for i in range(2)]
            t_hd = [Trk(), Trk()]
            for i in range(2):
                cx.op("dve", lambda e, i=i: e.memset(Ka[i][64:128, :], 1.0), writes=[t_hd[i]])
                cx.op("dve", lambda e, i=i: e.memset(Qa[i][64:128, :], 1.0), writes=[t_hd[i]])
                cx.op("dve", lambda e, i=i: e.memset(Vh[i][:, :, HD:HD + 1], 1.0), writes=[t_hd[i]])
            NP = 3
            pt = [sb(st, "pt%d" % i, [128, TT], BF16) for i in range(NP)]
            t_pt = [Trk() for _ in range(NP)]
            rr = sb(st, "rr", [128, TT])
            t_rr = Trk()
            rb = sb(st, "rb", [64, TT])
            t_rb = Trk()
            ot = sb(st, "ot", [64, TT])
            t_ot = Trk()
            ob = [sb(st, "ob%d" % i, [64, TT], BF16) for i in range(2)]
            t_ob = [Trk(), Trk()]

            def load_head(h):
                hb = h % 2
                c, ro = h // 2, (h % 2) * 64
                cx.dma("sp", Ka[hb][0:64, :], Kd[c, ro:ro + 64, :], writes=[t_hd[hb]])
                cx.dma("sp", Ka[hb][67:70, :], Fk[h], writes=[t_hd[hb]])
                cx.dma("sp", Qa[hb][0:64, :], Qd[c, ro:ro + 64, :], writes=[t_hd[hb]])
                cx.dma("sp", Qa[hb][64:67, :], Fq[h], writes=[t_hd[hb]])
                cx.dma("sp", Vh[hb][:, :, 0:HD], Vd[:, h * HD:(h + 1) * HD].rearrange("(b p) d -> p b d", p=128),
                       writes=[t_hd[hb]])
                cx.dma("sp", SGh[hb][:], SGd[c, ro:ro + 64, :], writes=[t_hd[hb]])

            units = []
            ui = 0
            for h in range(H):
                for qc in range(NTT):
                    for kb in range(4 * qc + 4):
                        units.append((h, qc, kb, ui))
                    ui += 1

            def emit_s(u, idx):
                h, qc, kb, ui = u
                hb = h % 2
                i = kb - 4 * qc
                cs = max(0, i) * 128
                sbank = idx % 2
                pb = idx % NP
                cx.op("pe", lambda e: e.matmul(
                    ps[sbank][:, cs:TT], lhsT=Ka[hb][0:70, kb * 128:(kb + 1) * 128],
                    rhs=Qa[hb][0:70, qc * TT + cs:(qc + 1) * TT], start=True, stop=(i < 0)),
                    reads=[t_hd[hb]], writes=[pst[sbank]])
                if i >= 0:
                    cx.op("pe", lambda e: e.matmul(
                        ps[sbank][:, cs:cs + 128], lhsT=identb[:], rhs=tri[:], start=False, stop=True),
                        reads=[t_tri], writes=[pst[sbank]])
                cx.op("act", lambda e: e.activation(
                    out=pt[pb][:, cs:TT], in_=ps[sbank][:, cs:TT], func=AF.Exp, scale=0.125),
                    reads=[pst[sbank]], writes=[t_pt[pb]])

            def emit_pv(u, idx):
                h, qc, kb, ui = u
                hb = h % 2
                c, ro = h // 2, (h % 2) * 64
                i = kb - 4 * qc
                cs = max(0, i) * 128
                pb = idx % NP
                po = 2 + (ui % 2)
                pbb = 4 + (ui % 2)
                ub = ui % 2
                nkb = 4 * qc + 4
                cx.op("pe", lambda e: e.matmul(
                    ps[po][0:HD + 1, cs:TT], lhsT=Vh[hb][:, kb, :], rhs=pt[pb][:, cs:TT],
                    start=(kb == 0), stop=(kb == nkb - 1)), reads=[t_hd[hb], t_pt[pb]], writes=[pst[po]])
                if kb == nkb - 1:
                    cx.op("dve", lambda e: e.reciprocal(out=rr[64:65, :], in_=ps[po][64:65, :]), reads=[pst[po]], writes=[t_rr])
                    cx.op("pe", lambda e: e.matmul(ps[pbb][0:64, :], lhsT=onesf[64:65, 0:64], rhs=rr[64:65, :], start=True, stop=True),
                          reads=[t_rr, t_const], writes=[pst[pbb]])
                    cx.op("act", lambda e: e.copy(out=rb[:], in_=ps[pbb][0:64, :]), reads=[pst[pbb]], writes=[t_rb])
                    cx.op("dve", lambda e: e.tensor_tensor(out=ot[:], in0=rb[:], in1=ps[po][0:64, :], op=ALU.mult),
                          reads=[t_rb, pst[po]], writes=[t_ot])
                    cx.op("dve", lambda e: e.tensor_tensor(
                        out=ob[ub][:], in0=ot[:], in1=SGh[hb][:, qc * TT:(qc + 1) * TT], op=ALU.mult),
                        reads=[t_ot, t_hd[hb]], writes=[t_ob[ub]])
                    cx.dma("sp", Od[c, ro:ro + 64, qc * TT:(qc + 1) * TT], ob[ub][:], reads=[t_ob[ub]])

            load_head(0)
            for idx in range(len(units) + 1):
                if idx < len(units):
                    u = units[idx]
                    if u[1] == 0 and u[2] == 0 and u[0] + 1 < H and idx > 0:
                        pass
                    emit_s(u, idx)
                if idx >= 1:
                    emit_pv(units[idx - 1], idx - 1)
                    pu = units[idx - 1]
                    if idx < len(units) and units[idx][0] != pu[0]:
                        pass
                if idx < len(units):
                    u = units[idx]
                    if u[1] == 0 and u[2] == 1 - 1 and u[0] + 1 < H:
                        load_head(u[0] + 1)
            cx.barrier()
        with ExitStack() as st:
            wo = sb(st, "wo", [128, NCH, D], BF16)
            t_w = Trk()
            wsrc = fox_w_o.rearrange("(c p) n -> p c n", p=128)
            for kc in range(NCH):
                cx.dma("pool", wo[:, kc, :], wsrc[:, kc, :], writes=[t_w])
            otl = [sb(st, "o_ot%d" % i, [128, NCH, TT], BF16) for i in range(2)]
            t_otl = [Trk(), Trk()]
            hts = [sb(st, "o_ht%d" % i, [128, NCH, TT]) for i in range(2)]
            t_ht = [Trk(), Trk()]
            ho = [sb(st, "o_ho%d" % i, [128, NCH, TT]) for i in range(2)]
            t_ho = [Trk(), Trk()]
            it = 0
            for tt in range(NTT):
                b = tt % 2
                t0 = tt * TT
                cx.dma("sp", otl[b][:], Od[:, :, t0:t0 + TT].rearrange("c p t -> p c t"), writes=[t_otl[b]])
                cx.dma("sp", hts[b][:], h2[:, :, t0:t0 + TT].rearrange("c p t -> p c t"), writes=[t_ht[b]])
                for n in range(NCH):
                    pv = it % 4
                    it += 1
                    for k in range(NCH):
                        cx.op("pe", lambda e, n=n, k=k, pv=pv, b=b: e.matmul(
                            ps[pv][:, :], lhsT=wo[:, k, n * 128:(n + 1) * 128], rhs=otl[b][:, k, :],
                            start=(k == 0), stop=(k == NCH - 1)), reads=[t_w, t_otl[b]], writes=[pst[pv]])
                    cx.op("dve", lambda e, n=n, pv=pv, b=b: e.scalar_tensor_tensor(
                        out=ho[b][:, n, :], in0=ps[pv][:, :], scalar=gatec(mi, n), in1=hts[b][:, n, :],
                        op0=ALU.mult, op1=ALU.add), reads=[pst[pv], t_mods, t_ht[b]], writes=[t_ho[b]])
                cx.dma("sp", h3[:, :, t0:t0 + TT].rearrange("c p t -> p c t"), ho[b][:], reads=[t_ho[b]])
            cx.barrier()

    kv_stage()
    chk("kv")
    attn_stage()
    chk("at")
    moe_stage(1, 3, h3, hout)
    cx.barrier()
    return nc


def _state_layout(a):
    return np.ascontiguousarray(a.reshape(32, 2, 64).transpose(1, 2, 0).reshape(128, 32))


def _col_layout(v):
    return np.ascontiguousarray(v.reshape(-1, 128).T)


def make_inputs(inputs, b):
    f = np.float32
    m = {}
    m["xT"] = np.ascontiguousarray(inputs["x"][b].T).reshape(NCH, 128, L)
    m["c_col"] = _col_layout(inputs["c"][b])
    return m


def make_shared(inputs):
    f = np.float32
    m = {}
    m["ada_w"] = np.ascontiguousarray(inputs["ada_w"], dtype=f)
    m["ada_b"] = np.stack([_col_layout(inputs["ada_b"][i // 2, i % 2]) for i in range(4)])
    m["ln_g"] = np.stack([_col_layout(inputs["ln_g"][i // 2, i % 2]) for i in range(4)])
    m["kv_ada_w"] = np.ascontiguousarray(inputs["kv_ada_w"], dtype=f)
    m["kv_ada_b"] = _col_layout(inputs["kv_ada_b"])
    m["kv_g"] = _col_layout(inputs["kv_g"])
    m["s5_w_in"] = np.ascontiguousarray(inputs["s5_w_in"][0])
    m["s5_w_out"] = np.ascontiguousarray(inputs["s5_w_out"][0])
    ldt = np.repeat(inputs["s5_log_dt"][0][:, None], 64, axis=1)
    m["s5_par"] = np.stack([_state_layout(inputs["s5_lambda_re"][0]), _state_layout(inputs["s5_lambda_im"][0]),
                            _state_layout(ldt)])
    bp = np.zeros((2, 128, 32, 128), f)
    cp = np.zeros((2, 128, 32, 128), f)
    for k, (bn, cn) in enumerate([("s5_b_re", "s5_c_re"), ("s5_b_im", "s5_c_im")]):
        B_ = inputs[bn][0]
        C_ = inputs[cn][0]
        for j in range(32):
            o = (j % 4) * 32
            for gl in range(2):
                g = 2 * j + gl
                bp[k, gl * 64:(gl + 1) * 64, j, o + gl * 16:o + gl * 16 + 16] = B_[g]
                cp[k, gl * 64:(gl + 1) * 64, j, o + gl * 16:o + gl * 16 + 16] = C_[g].T
    m["s5_bpad"] = bp
    m["s5_cpad"] = cp
    m["s5_d"] = _col_layout(inputs["s5_d"][0])
    m["iota_t"] = np.ascontiguousarray(np.broadcast_to(np.arange(L, dtype=f)[None, :], (128, L)))
    m["ident"] = np.eye(128, dtype=f)
    m["moe_w1"] = np.ascontiguousarray(inputs["moe_w1"], dtype=f)
    m["moe_w3"] = np.ascontiguousarray(inputs["moe_w3"], dtype=f)
    m["moe_w2"] = np.ascontiguousarray(inputs["moe_w2"], dtype=f)
    m["moe_wr"] = np.ascontiguousarray(np.concatenate([inputs["moe_wg"], inputs["moe_we"]], axis=2), dtype=f)
    br = np.concatenate([inputs["moe_bg"], inputs["moe_be"]], axis=1).astype(f)
    m["moe_br"] = np.ascontiguousarray(np.broadcast_to(br[:, None, :], (2, 128, 36)))
    m["kv_w"] = np.ascontiguousarray(inputs["kv_w"], dtype=f)
    m["gk_col"] = np.ascontiguousarray(np.tile(inputs["k_norm_g"], 2)[:, None], dtype=f)
    m["gq_col"] = np.ascontiguousarray(np.tile(inputs["fox_q_norm_g"][0], 2)[:, None], dtype=f)
    m["fb_col"] = np.ascontiguousarray(inputs["kv_fb"][:, None], dtype=f)
    blk = np.zeros((128, 128), f)
    blk[0:64, 0:64] = 1.0
    blk[64:128, 64:128] = 1.0
    m["blk_c"] = blk
    m["tri_c"] = np.ascontiguousarray(np.tril(np.full((128, 128), -1.0e8, f), -1))
    m["fox_w_qg"] = np.ascontiguousarray(inputs["fox_w_qg"][0], dtype=f)
    m["fox_w_o"] = np.ascontiguousarray(inputs["fox_w_o"][0], dtype=f)
    sel = np.zeros((32, NE, 128), f)
    for e_ in range(NE):
        sel[e_, e_, :] = 1.0
    m["sel_c"] = sel
    return m


_NC_CACHE = {}


def kernel(**inputs):
    inputs = {k: np.asarray(v) for k, v in inputs.items()}
    if "nc" not in _NC_CACHE:
        _NC_CACHE["nc"] = build()
    nc = _NC_CACHE["nc"]
    shared = make_shared(inputs)
    in_maps = []
    for b in range(8):
        m = dict(shared)
        m.update(make_inputs(inputs, b))
        in_maps.append(m)
    res = run_bass_kernel_spmd(nc, in_maps, core_ids=list(range(8)))
    out = np.stack([np.ascontiguousarray(r["hout"].reshape(D, L).T) for r in res.results])
    return out.astype(np.float32)
```

```python
import math
from contextlib import ExitStack
import numpy as np
import concourse.bass as bass
import concourse.mybir as mybir
from concourse.bass_utils import run_bass_kernel_spmd

F32 = mybir.dt.float32
BF16 = mybir.dt.bfloat16
AF = mybir.ActivationFunctionType
ALU = mybir.AluOpType
AX = mybir.AxisListType

D = 1024
L = 4096
NCH = 8
TT = 512
NTT = L // TT
NG = 4
NE = 32
FE = 256
H = 16
HD = 64
EPS = 1e-6
MAGIC = 12582912.0
S2PI = 6.283180
HALFPI = 1.570795
GELU_C = 2.0 * math.sqrt(2.0 / math.pi)


class Trk:
    __slots__ = ("w", "r")

    def __init__(self):
        self.w = {}
        self.r = {}


class Ctx:
    def __init__(self, nc, es):
        self.nc = nc
        self.es = es
        self.engs = {"pe": nc.tensor, "act": nc.scalar, "dve": nc.vector, "pool": nc.gpsimd, "sp": nc.sync}
        self.sems = {}
        self.cnt = {}
        for k in ["pe", "act", "dve", "pool"]:
            self.sems[k] = es.enter_context(nc.semaphore("s_" + k))
            self.cnt[k] = 0
        self.seen = {k: {} for k in self.engs}
        self.dpool = {}
        self.dnext = {}
        for q, n in [("sp", 24), ("pool", 16), ("act", 6)]:
            keys = []
            for i in range(n):
                key = "d_%s%d" % (q, i)
                self.sems[key] = es.enter_context(nc.semaphore(key))
                self.cnt[key] = 0
                keys.append(key)
            self.dpool[q] = keys
            self.dnext[q] = 0

    def _wait(self, eng, deps):
        seen = self.seen[eng]
        for key, val in deps.items():
            if eng == "pe" and key == "pe":
                continue
            if seen.get(key, 0) < val:
                self.engs[eng].wait_ge(self.sems[key], val)
                seen[key] = val

    @staticmethod
    def _merge(dst, src):
        for k, v in src.items():
            if dst.get(k, 0) < v:
                dst[k] = v

    def _deps(self, reads, writes):
        deps = {}
        for t in reads:
            self._merge(deps, t.w)
        for t in writes:
            self._merge(deps, t.w)
            self._merge(deps, t.r)
        return deps

    def _record(self, ev, reads, writes):
        k, v = ev
        for t in reads:
            if t.r.get(k, 0) < v:
                t.r[k] = v
        for t in writes:
            t.w = {k: v}
            t.r = {}

    def op(self, eng, fn, reads=(), writes=()):
        self._wait(eng, self._deps(reads, writes))
        ins = fn(self.engs[eng])
        self.cnt[eng] += 1
        ins.then_inc(self.sems[eng], 1)
        self._record((eng, self.cnt[eng]), reads, writes)

    def dma(self, q, out, in_, reads=(), writes=(), **kw):
        keys = self.dpool[q]
        key = keys[self.dnext[q] % len(keys)]
        self.dnext[q] += 1
        deps = self._deps(reads, writes)
        if self.cnt[key] > 0:
            deps[key] = max(deps.get(key, 0), self.cnt[key])
        self._wait(q, deps)
        ins = self.engs[q].dma_start(out=out, in_=in_, **kw)
        self.cnt[key] += 16
        ins.then_inc(self.sems[key], 16)
        self._record((key, self.cnt[key]), reads, writes)

    def barrier(self, engines=("pe", "act", "dve", "pool", "sp")):
        allev = {k: v for k, v in self.cnt.items() if v > 0}
        for e in engines:
            self._wait(e, allev)


class _Stop(Exception):
    pass


def build(debug=False, stop=None):
    try:
        return _build(debug, stop)
    except _Stop as e:
        return e.args[0]


def _build(debug, stop):
    nc = bass.Bass("TRN2", target_bir_lowering=False)
    okind = "ExternalOutput" if debug else "Internal"

    def din(name, shape, dt=F32):
        return nc.dram_tensor(name, list(shape), dt, kind="ExternalInput").ap()

    def dscr(name, shape, dt=F32, out=False):
        return nc.dram_tensor(name, list(shape), dt, kind=("ExternalOutput" if out else okind)).ap()

    xT = din("xT", [NCH, 128, L])
    c_col = din("c_col", [128, NCH])
    ada_w = din("ada_w", [2, 2, D, 3 * D])
    ada_b = din("ada_b", [4, 128, 24])
    ln_g = din("ln_g", [4, 128, NCH])
    kv_ada_w = din("kv_ada_w", [D, 2 * D])
    kv_ada_b = din("kv_ada_b", [128, 16])
    kv_g = din("kv_g", [128, NCH])
    s5_w_in = din("s5_w_in", [D, D])
    s5_w_out = din("s5_w_out", [D, 2 * D])
    s5_par = din("s5_par", [3, 128, 32])
    s5_bpad = din("s5_bpad", [2, 128, 32, 128])
    s5_cpad = din("s5_cpad", [2, 128, 32, 128])
    s5_d = din("s5_d", [128, NCH])
    iota_t = din("iota_t", [128, L])
    ident = din("ident", [128, 128])
    hout = dscr("hout", [NCH, 128, L], out=True)
    moe_w1 = din("moe_w1", [2, NE, D, FE])
    moe_w3 = din("moe_w3", [2, NE, D, FE])
    moe_w2 = din("moe_w2", [2, NE, FE, D])
    moe_wr = din("moe_wr", [2, D, 36])
    moe_br = din("moe_br", [2, 128, 36])
    sel_c = din("sel_c", [32, NE, 128])
    h2 = dscr("h2", [NCH, 128, L])
    kv_w = din("kv_w", [D, 2 * D + H])
    gk_col = din("gk_col", [128, 1])
    gq_col = din("gq_col", [128, 1])
    fb_col = din("fb_col", [H, 1])
    blk_c = din("blk_c", [128, 128])
    tri_c = din("tri_c", [128, 128])
    fox_w_qg = din("fox_w_qg", [D, 2 * D])
    fox_w_o = din("fox_w_o", [D, D])
    Kd = dscr("Kd", [NCH, 128, L], BF16)
    Vd = dscr("Vd", [L, D], BF16)
    Fq = dscr("Fq", [H, 3, L], BF16)
    Fk = dscr("Fk", [H, 3, L], BF16)
    Qd = dscr("Qd", [NCH, 128, L], BF16)
    SGd = dscr("SGd", [NCH, 128, L], BF16)
    Od = dscr("Od", [NCH, 128, L], BF16)
    h3 = dscr("h3", [NCH, 128, L])

    h1 = dscr("h1", [NCH, 128, L])
    Gd = dscr("Gd", [NCH, 128, L], BF16)
    moddbg = dscr("moddbg", [128, 24 * 4 + 16])
    udbg = dscr("udbg", [NCH, 128, L], BF16) if debug else None

    es = ExitStack()
    cx = Ctx(nc, es)

    def chk(name):
        if stop == name:
            cx.barrier()
            raise _Stop(nc, es)

    _uid = [0]

    def sb(st, name, shape, dt=F32):
        _uid[0] += 1
        return st.enter_context(nc.sbuf_tensor("%s_%d" % (name, _uid[0]), list(shape), dt))

    ps = [es.enter_context(nc.psum_tensor("ps%d" % i, [128, 512], F32)) for i in range(8)]
    pst = [Trk() for _ in range(8)]

    mods = sb(es, "mods", [128, 24 * 4 + 16])
    modA = sb(es, "modA", [128, 5, NCH])
    t_mods = Trk()
    t_modA = Trk()
    identf = sb(es, "identf", [128, 128])
    onesf = sb(es, "onesf", [128, 128])
    onesbf = sb(es, "onesbf", [128, 128], BF16)
    t_const = Trk()
    cx.dma("sp", identf[:], ident[:, :], writes=[t_const])
    cx.op("dve", lambda e: e.memset(onesf[:], 1.0), writes=[t_const])
    cx.op("dve", lambda e: e.memset(onesbf[:], 1.0), writes=[t_const])

    with ExitStack() as st:
        ccol = sb(st, "ccol", [128, NCH])
        sc = sb(st, "sc", [128, NCH])
        bias_all = sb(st, "bias_all", [128, 24 * 4 + 16])
        g_all = sb(st, "g_all", [128, 5, NCH])
        wbuf = [sb(st, "wbuf%d" % i, [128, NCH, 1536]) for i in range(2)]
        t_w = [Trk(), Trk()]
        t_c = Trk()
        t_b = Trk()
        cx.dma("sp", ccol[:], c_col[:, :], writes=[t_c])
        for i in range(4):
            cx.dma("sp", bias_all[:, 24 * i:24 * (i + 1)], ada_b[i], writes=[t_b])
            cx.dma("sp", g_all[:, i, :], ln_g[i], writes=[t_b])
        cx.dma("sp", bias_all[:, 96:112], kv_ada_b[:, :], writes=[t_b])
        cx.dma("sp", g_all[:, 4, :], kv_g[:, :], writes=[t_b])
        cx.op("act", lambda e: e.activation(out=sc[:], in_=ccol[:], func=AF.Silu), reads=[t_c], writes=[t_c])
        units = []
        for i in range(4):
            for hf in range(2):
                units.append((ada_w[i // 2, i % 2], hf * 1536, 1536, 24 * i + 12 * hf))
        units.append((kv_ada_w, 0, 1024, 96))
        units.append((kv_ada_w, 1024, 1024, 104))
        for ui, (wap, c0, ncol, mcol) in enumerate(units):
            wb = wbuf[ui % 2]
            tw = t_w[ui % 2]
            src = wap.rearrange("(c p) n -> p c n", p=128)
            for kc in range(NCH):
                cx.dma("sp" if kc % 2 == 0 else "pool", wb[:, kc, 0:ncol], src[:, kc, c0:c0 + ncol], writes=[tw])
            pidx = ui % 2
            for n in range(ncol // 128):
                for kc in range(NCH):
                    cx.op("pe", lambda e, wb=wb, n=n, kc=kc, pidx=pidx: e.matmul(
                        ps[pidx][:, n:n + 1], lhsT=wb[:, kc, n * 128:(n + 1) * 128], rhs=sc[:, kc:kc + 1],
                        start=(kc == 0), stop=(kc == NCH - 1)), reads=[tw, t_c], writes=[pst[pidx]])
            nn = ncol // 128
            cx.op("dve", lambda e, pidx=pidx, nn=nn, mcol=mcol: e.tensor_tensor(
                out=mods[:, mcol:mcol + nn], in0=ps[pidx][:, 0:nn], in1=bias_all[:, mcol:mcol + nn], op=ALU.add),
                reads=[pst[pidx], t_b], writes=[t_mods])
        for i in range(5):
            sc0 = 24 * i + 8
            cx.op("dve", lambda e, i=i, sc0=sc0: e.scalar_tensor_tensor(
                out=modA[:, i, :], in0=mods[:, sc0:sc0 + 8], scalar=1.0, in1=g_all[:, i, :],
                op0=ALU.add, op1=ALU.mult), reads=[t_mods, t_b], writes=[t_modA])
        if debug:
            cx.dma("sp", moddbg[:, :], mods[:], reads=[t_mods])
        cx.barrier()

    chk("s0")

    def shiftc(i, c):
        return mods[:, 24 * i + c:24 * i + c + 1]

    def gatec(i, c):
        return mods[:, 24 * i + 16 + c:24 * i + 16 + c + 1]

    def scaleA(i, c):
        return modA[:, i, c:c + 1]

    _tmp_trk = {}

    def norm_mod(i, htile, t_h, sq, t_sq, rinv, t_rinv, tmp, t_tmp, hn_bf, t_hn, psi, hn_f=None, t_hnf=None):
        cx.op("act", lambda e: e.activation(out=sq[:], in_=htile[:], func=AF.Square), reads=[t_h], writes=[t_sq])
        for c in range(NCH):
            cx.op("pe", lambda e, c=c: e.matmul(ps[psi][:, :], lhsT=onesbf[:], rhs=sq[:, c, :],
                                                start=(c == 0), stop=(c == NCH - 1)),
                  reads=[t_sq, t_const], writes=[pst[psi]])
        cx.op("act", lambda e: e.activation(out=rinv[:], in_=ps[psi][:, :], func=AF.Sqrt, bias=epsc[:, 0:1],
                                            scale=1.0 / D), reads=[pst[psi], t_const], writes=[t_rinv])
        cx.op("dve", lambda e: e.reciprocal(out=rinv[:], in_=rinv[:]), reads=[t_rinv], writes=[t_rinv])
        tcs = _tmp_trk.setdefault(id(t_tmp), [Trk() for _ in range(NCH)])
        for c in range(NCH):
            cx.op("dve", lambda e, c=c: e.scalar_tensor_tensor(
                out=tmp[:, c, :], in0=htile[:, c, :], scalar=scaleA(i, c), in1=rinv[:],
                op0=ALU.mult, op1=ALU.mult), reads=[t_h, t_rinv, t_modA], writes=[tcs[c]])
            if hn_f is not None:
                cx.op("act", lambda e, c=c: e.activation(out=hn_f[:, c, :], in_=tmp[:, c, :], func=AF.Identity,
                                                        bias=shiftc(i, c), scale=1.0),
                      reads=[tcs[c], t_mods], writes=[t_hnf])
            cx.op("act", lambda e, c=c: e.activation(out=hn_bf[:, c, :], in_=tmp[:, c, :], func=AF.Identity,
                                                    bias=shiftc(i, c), scale=1.0),
                  reads=[tcs[c], t_mods], writes=[t_hn])

    epsc = sb(es, "epsc", [128, 4])
    cx.op("dve", lambda e: e.memset(epsc[:, 0:1], EPS), writes=[t_const])
    cx.op("dve", lambda e: e.memset(epsc[:, 1:2], HALFPI), writes=[t_const])
    cx.op("dve", lambda e: e.memset(epsc[:, 2:3], 0.0), writes=[t_const])
    cx.op("dve", lambda e: e.memset(epsc[:, 3:4], -MAGIC), writes=[t_const])

    with ExitStack() as st:
        u_bf = sb(st, "u_bf", [128, NCH, L], BF16)
        t_u = [Trk() for _ in range(NTT)]
        par = sb(st, "par", [128, 3, 32])
        t_par = Trk()
        for i in range(3):
            cx.dma("sp", par[:, i, :], s5_par[i], writes=[t_par])
        sp_ = sb(st, "s5small", [128, 22, 32])
        t_sp = Trk()

        def S(i):
            return sp_[:, i, :]
        lr, li, ldt = par[:, 0, :], par[:, 1, :], par[:, 2, :]
        DT, MAG, TH, THT, V, K_, FR, SIN, COS, ARE, AIM, DEN, CRE, CIM, T0, T1, C5, S5, U0, U1, U2, U3 = range(22)

        def dv(fn, eng="dve"):
            cx.op(eng, fn, reads=[t_par, t_sp, t_const], writes=[t_sp])
        dv(lambda e: e.activation(out=S(DT), in_=ldt, func=AF.Exp), "act")
        dv(lambda e: e.tensor_tensor(out=S(T0), in0=lr, in1=S(DT), op=ALU.mult))
        dv(lambda e: e.activation(out=S(MAG), in_=S(T0), func=AF.Exp), "act")
        dv(lambda e: e.tensor_tensor(out=S(TH), in0=li, in1=S(DT), op=ALU.mult))
        dv(lambda e: e.tensor_scalar(out=S(THT), in0=S(TH), scalar1=1.0 / (2 * math.pi), scalar2=None, op0=ALU.mult))
        dv(lambda e: e.tensor_scalar(out=S(V), in0=S(THT), scalar1=MAGIC, scalar2=None, op0=ALU.add))
        dv(lambda e: e.tensor_scalar(out=S(K_), in0=S(V), scalar1=-MAGIC, scalar2=None, op0=ALU.add))
        dv(lambda e: e.tensor_tensor(out=S(FR), in0=S(THT), in1=S(K_), op=ALU.subtract))
        dv(lambda e: e.activation(out=S(SIN), in_=S(FR), func=AF.Sin, scale=S2PI), "act")
        dv(lambda e: e.tensor_scalar(out=S(T0), in0=S(FR), scalar1=0.25, scalar2=-1.0, op0=ALU.is_gt, op1=ALU.mult))
        dv(lambda e: e.tensor_tensor(out=S(T0), in0=S(T0), in1=S(FR), op=ALU.add))
        dv(lambda e: e.activation(out=S(COS), in_=S(T0), func=AF.Sin, scale=S2PI, bias=epsc[:, 1:2]), "act")
        dv(lambda e: e.tensor_tensor(out=S(ARE), in0=S(MAG), in1=S(COS), op=ALU.mult))
        dv(lambda e: e.tensor_tensor(out=S(AIM), in0=S(MAG), in1=S(SIN), op=ALU.mult))
        dv(lambda e: e.tensor_tensor(out=S(T0), in0=lr, in1=lr, op=ALU.mult))
        dv(lambda e: e.tensor_tensor(out=S(T1), in0=li, in1=li, op=ALU.mult))
        dv(lambda e: e.tensor_tensor(out=S(DEN), in0=S(T0), in1=S(T1), op=ALU.add))
        dv(lambda e: e.reciprocal(out=S(DEN), in_=S(DEN)))
        dv(lambda e: e.tensor_scalar(out=S(T0), in0=S(ARE), scalar1=-1.0, scalar2=None, op0=ALU.add))
        dv(lambda e: e.tensor_tensor(out=S(CRE), in0=S(T0), in1=lr, op=ALU.mult))
        dv(lambda e: e.tensor_tensor(out=S(T1), in0=S(AIM), in1=li, op=ALU.mult))
        dv(lambda e: e.tensor_tensor(out=S(CRE), in0=S(CRE), in1=S(T1), op=ALU.add))
        dv(lambda e: e.tensor_tensor(out=S(CRE), in0=S(CRE), in1=S(DEN), op=ALU.mult))
        dv(lambda e: e.tensor_tensor(out=S(CIM), in0=S(AIM), in1=lr, op=ALU.mult))
        dv(lambda e: e.tensor_tensor(out=S(T1), in0=S(T0), in1=li, op=ALU.mult))
        dv(lambda e: e.tensor_tensor(out=S(CIM), in0=S(CIM), in1=S(T1), op=ALU.subtract))
        dv(lambda e: e.tensor_tensor(out=S(CIM), in0=S(CIM), in1=S(DEN), op=ALU.mult))
        dv(lambda e: e.tensor_copy(out=S(C5), in_=S(COS)))
        dv(lambda e: e.tensor_copy(out=S(S5), in_=S(SIN)))
        for _sq in range(9):
            dv(lambda e: e.tensor_tensor(out=S(U0), in0=S(C5), in1=S(C5), op=ALU.mult))
            dv(lambda e: e.tensor_tensor(out=S(U1), in0=S(S5), in1=S(S5), op=ALU.mult))
            dv(lambda e: e.scalar_tensor_tensor(out=S(U2), in0=S(C5), scalar=2.0, in1=S(S5), op0=ALU.mult, op1=ALU.mult))
            dv(lambda e: e.tensor_tensor(out=S(C5), in0=S(U0), in1=S(U1), op=ALU.subtract))
            dv(lambda e: e.tensor_copy(out=S(S5), in_=S(U2)))
        dv(lambda e: e.tensor_scalar(out=S(U3), in0=S(S5), scalar1=-1.0, scalar2=None, op0=ALU.mult))
        dv(lambda e: e.tensor_scalar(out=S(T1), in0=S(CIM), scalar1=-1.0, scalar2=None, op0=ALU.mult))

        Lre = sb(st, "Lre", [128, 32, 128], BF16)
        Lim = sb(st, "Lim", [128, 32, 128], BF16)
        Cre = sb(st, "Cre", [128, 32, 128], BF16)
        nCre = sb(st, "nCre", [128, 32, 128], BF16)
        nCim = sb(st, "nCim", [128, 32, 128], BF16)
        t_L = Trk()
        t_C = Trk()
        with ExitStack() as st2:
            bre = sb(st2, "bre", [128, 32, 128])
            bim = sb(st2, "bim", [128, 32, 128])
            t_bp = Trk()
            cx.dma("sp", bre[:], s5_bpad[0], writes=[t_bp])
            cx.dma("sp", bim[:], s5_bpad[1], writes=[t_bp])
            xa = [sb(st2, "xa%d" % i, [128, 128]) for i in range(2)]
            xb = [sb(st2, "xb%d" % i, [128, 128]) for i in range(2)]
            t_xa = [Trk(), Trk()]
            t_xb = [Trk(), Trk()]
            for j in range(32):
                b = j % 2
                cx.op("dve", lambda e, j=j, b=b: e.tensor_scalar(out=xa[b][:], in0=bim[:, j, :], scalar1=sp_[:, T1, j:j + 1],
                                                                 scalar2=None, op0=ALU.mult),
                      reads=[t_bp, t_sp], writes=[t_xa[b]])
                cx.op("dve", lambda e, j=j, b=b: e.scalar_tensor_tensor(out=xa[b][:], in0=bre[:, j, :], scalar=sp_[:, CRE, j:j + 1],
                                                                        in1=xa[b][:], op0=ALU.mult, op1=ALU.add),
                      reads=[t_bp, t_sp], writes=[t_xa[b]])
                cx.op("dve", lambda e, j=j, b=b: e.tensor_scalar(out=xb[b][:], in0=bre[:, j, :], scalar1=sp_[:, CIM, j:j + 1],
                                                                 scalar2=None, op0=ALU.mult),
                      reads=[t_bp, t_sp], writes=[t_xb[b]])
                cx.op("dve", lambda e, j=j, b=b: e.scalar_tensor_tensor(out=xb[b][:], in0=bim[:, j, :], scalar=sp_[:, CRE, j:j + 1],
                                                                        in1=xb[b][:], op0=ALU.mult, op1=ALU.add),
                      reads=[t_bp, t_sp], writes=[t_xb[b]])
                cx.op("pe", lambda e, b=b: e.transpose(out=ps[b][:, 0:128], in_=xa[b][:], identity=identf[:]),
                      reads=[t_xa[b], t_const], writes=[pst[b]])
                cx.op("pe", lambda e, b=b: e.transpose(out=ps[b][:, 128:256], in_=xb[b][:], identity=identf[:]),
                      reads=[t_xb[b], t_const], writes=[pst[b]])
                cx.op("act", lambda e, j=j, b=b: e.copy(out=Lre[:, j, :], in_=ps[b][:, 0:128]), reads=[pst[b]], writes=[t_L])
                cx.op("act", lambda e, j=j, b=b: e.copy(out=Lim[:, j, :], in_=ps[b][:, 128:256]), reads=[pst[b]], writes=[t_L])
            cx.dma("sp", bre[:], s5_cpad[0], reads=[], writes=[t_bp])
            cx.dma("sp", bim[:], s5_cpad[1], reads=[], writes=[t_bp])
            for q4 in range(4):
                sl = slice(q4 * 8, (q4 + 1) * 8)
                cx.op("act", lambda e, sl=sl: e.copy(out=Cre[:, sl, :], in_=bre[:, sl, :]), reads=[t_bp], writes=[t_C])
                cx.op("act", lambda e, sl=sl: e.mul(out=nCre[:, sl, :], in_=bre[:, sl, :], mul=-1.0), reads=[t_bp], writes=[t_C])
                cx.op("act", lambda e, sl=sl: e.mul(out=nCim[:, sl, :], in_=bim[:, sl, :], mul=-1.0), reads=[t_bp], writes=[t_C])
            cx.barrier()

        chk("a0")
        with ExitStack() as st2:
            win = sb(st2, "win", [128, NCH, D], BF16)
            t_win = Trk()
            wsrc = s5_w_in.rearrange("(c p) n -> p c n", p=128)
            for kc in range(NCH):
                cx.dma("pool", win[:, kc, :], wsrc[:, kc, :], writes=[t_win])
            hts = [sb(st2, "ht%d" % i, [128, NCH, TT]) for i in range(2)]
            t_ht = [Trk(), Trk()]
            sq = sb(st2, "sq", [128, NCH, TT], BF16)
            t_sq = Trk()
            rinv = sb(st2, "rinv", [128, TT])
            t_rinv = Trk()
            tmp = sb(st2, "tmpn", [128, NCH, TT])
            t_tmp = Trk()
            hnb = [sb(st2, "hnb%d" % i, [128, NCH, TT], BF16) for i in range(2)]
            t_hn = [Trk(), Trk()]
            for tt in range(NTT):
                b = tt % 2
                t0 = tt * TT
                cx.dma("sp", hts[b][:], xT[:, :, t0:t0 + TT].rearrange("c p t -> p c t"), writes=[t_ht[b]])
                norm_mod(0, hts[b], t_ht[b], sq, t_sq, rinv, t_rinv, tmp, t_tmp, hnb[b], t_hn[b], 0)
                for n in range(NCH):
                    pi = 1 + (n % 4)
                    for k in range(NCH):
                        cx.op("pe", lambda e, n=n, k=k, pi=pi, b=b: e.matmul(
                            ps[pi][:, :], lhsT=win[:, k, n * 128:(n + 1) * 128], rhs=hnb[b][:, k, :],
                            start=(k == 0), stop=(k == NCH - 1)), reads=[t_win, t_hn[b]], writes=[pst[pi]])
                    eng = "act" if n % 2 == 0 else "dve"
                    if eng == "act":
                        cx.op("act", lambda e, n=n, pi=pi, t0=t0: e.copy(out=u_bf[:, n, t0:t0 + TT], in_=ps[pi][:, :]),
                              reads=[pst[pi]], writes=[t_u[tt]])
                    else:
                        cx.op("dve", lambda e, n=n, pi=pi, t0=t0: e.tensor_copy(out=u_bf[:, n, t0:t0 + TT], in_=ps[pi][:, :]),
                              reads=[pst[pi]], writes=[t_u[tt]])
            cx.barrier()

        if debug:
            for n in range(NCH):
                cx.dma("sp", udbg[n], u_bf[:, n, :], reads=t_u)
        chk("a2")
        with ExitStack() as st2:
            iot = sb(st2, "iot", [128, TT])
            t_iot = Trk()
            cx.dma("sp", iot[:], iota_t[:, 0:TT], writes=[t_iot])
            dsk = sb(st2, "dsk", [128, NCH])
            cx.dma("sp", dsk[:], s5_d[:, :], writes=[t_iot])
            NB = 2

            def mk(name, dt=F32):
                return [sb(st2, "%s%d" % (name, i), [128, TT], dt) for i in range(NB)], [Trk() for _ in range(NB)]
            SNt, tSN = mk("SNt")
            CRt, tCR = mk("CRt")
            T1_, tT1 = mk("T1_")
            T2_, tT2 = mk("T2_")
            T3_, tT3 = mk("T3_")
            T4_, tT4 = mk("T4_")
            Vt, tVt = mk("Vt")
            Ft_, tFt = mk("Ftb")
            ini = [sb(st2, "ini%d" % i, [128, 4]) for i in range(NB)]
            t_ini = [Trk() for _ in range(NB)]
            XR, tXR = mk("XR")
            XI, tXI = mk("XI")
            SR, tSR = mk("SR")
            SI, tSI = mk("SI")
            P1, tP1 = mk("P1", BF16)
            P2, tP2 = mk("P2", BF16)
            P3, tP3 = mk("P3", BF16)
            P4, tP4 = mk("P4", BF16)
            ytmp = sb(st2, "ytmp", [128, L])
            t_y = [Trk() for _ in range(NTT)]
            gb = sb(st2, "gb", [128, L], BF16)
            t_gb = [Trk() for _ in range(NTT)]
            g1 = sb(st2, "g1", [128, TT])
            g2 = sb(st2, "g2", [128, TT])
            t_g1 = Trk()
            t_g2 = Trk()
            zero_init = epsc[:, 2:3]
            def gen_tables(j):
                thj = sp_[:, THT, j:j + 1]
                tb = j % 2
                cx.op("dve", lambda e: e.tensor_scalar(
                    out=Vt[tb][:], in0=iot[:, 0:TT], scalar1=thj, scalar2=MAGIC, op0=ALU.mult, op1=ALU.add),
                    reads=[t_iot, t_sp], writes=[tVt[tb]])
                cx.op("act", lambda e: e.activation(out=Vt[tb][:], in_=Vt[tb][:], func=AF.Identity,
                                                    bias=epsc[:, 3:4], scale=1.0),
                      reads=[tVt[tb], t_const], writes=[tVt[tb]])
                cx.op("dve", lambda e: e.scalar_tensor_tensor(
                    out=Ft_[tb][:], in0=iot[:, 0:TT], scalar=thj, in1=Vt[tb][:], op0=ALU.mult, op1=ALU.subtract),
                    reads=[t_iot, t_sp, tVt[tb]], writes=[tFt[tb]])
                cx.op("act", lambda e: e.activation(out=SNt[tb][:], in_=Ft_[tb][:], func=AF.Sin, scale=S2PI),
                      reads=[tFt[tb]], writes=[tSN[tb]])
                cx.op("dve", lambda e: e.tensor_scalar(
                    out=Vt[tb][:], in0=Ft_[tb][:], scalar1=0.25, scalar2=-1.0, op0=ALU.is_gt, op1=ALU.mult),
                    reads=[tFt[tb], tVt[tb]], writes=[tVt[tb]])
                cx.op("dve", lambda e: e.tensor_tensor(out=Vt[tb][:], in0=Vt[tb][:], in1=Ft_[tb][:], op=ALU.add),
                      reads=[tFt[tb], tVt[tb]], writes=[tVt[tb]])
                cx.op("act", lambda e: e.activation(out=CRt[tb][:], in_=Vt[tb][:], func=AF.Sin, scale=S2PI,
                                                    bias=epsc[:, 1:2]),
                      reads=[tVt[tb], t_const], writes=[tCR[tb]])

            class P_:
                pass
            pieces = []
            for c in range(NCH):
                for jj in range(4):
                    for tt in range(NTT):
                        p = P_()
                        p.c, p.jj, p.j, p.o, p.tt, p.s = c, jj, 4 * c + jj, jj * 32, tt, len(pieces)
                        p.b = p.s % NB
                        p.pb = 4 * (p.s % 2)
                        p.tb = p.j % 2
                        p.tsl = slice(tt * TT, (tt + 1) * TT)
                        pieces.append(p)

            def stg1(p):
                if p.tt == 0:
                    gen_tables(p.j)
                b, pb, tb, j, c, tsl, tt = p.b, p.pb, p.tb, p.j, p.c, p.tsl, p.tt
                cx.op("pe", lambda e: e.matmul(ps[pb][:, :], lhsT=Lre[:, j, :], rhs=u_bf[:, c, tsl], start=True, stop=True),
                      reads=[t_L, t_u[tt]], writes=[pst[pb]])
                cx.op("pe", lambda e: e.matmul(ps[pb + 1][:, :], lhsT=Lim[:, j, :], rhs=u_bf[:, c, tsl], start=True, stop=True),
                      reads=[t_L, t_u[tt]], writes=[pst[pb + 1]])
                cx.op("dve", lambda e: e.tensor_tensor(out=T1_[b][:], in0=CRt[tb][:], in1=ps[pb][:, :], op=ALU.mult),
                      reads=[tCR[tb], pst[pb]], writes=[tT1[b]])
                cx.op("dve", lambda e: e.tensor_tensor(out=T2_[b][:], in0=SNt[tb][:], in1=ps[pb + 1][:, :], op=ALU.mult),
                      reads=[tSN[tb], pst[pb + 1]], writes=[tT2[b]])
                cx.op("dve", lambda e: e.tensor_tensor(out=T3_[b][:], in0=CRt[tb][:], in1=ps[pb + 1][:, :], op=ALU.mult),
                      reads=[tCR[tb], pst[pb + 1]], writes=[tT3[b]])
                cx.op("dve", lambda e: e.tensor_tensor(out=T4_[b][:], in0=SNt[tb][:], in1=ps[pb][:, :], op=ALU.mult),
                      reads=[tSN[tb], pst[pb]], writes=[tT4[b]])

            def stg2(p):
                b = p.b
                cx.op("pool", lambda e: e.tensor_tensor(out=XR[b][:], in0=T1_[b][:], in1=T2_[b][:], op=ALU.add),
                      reads=[tT1[b], tT2[b]], writes=[tXR[b]])
                cx.op("pool", lambda e: e.tensor_tensor(out=XI[b][:], in0=T3_[b][:], in1=T4_[b][:], op=ALU.subtract),
                      reads=[tT3[b], tT4[b]], writes=[tXI[b]])

            def stg3(p):
                b, j, tt = p.b, p.j, p.tt
                pbuf = (b - 1) % NB
                magb = sp_[:, MAG, j:j + 1].to_broadcast([128, TT])
                if tt == 0:
                    ini_r, ini_i, rd = zero_init, zero_init, [t_const]
                else:
                    sre, sie = SR[pbuf][:, TT - 1:TT], SI[pbuf][:, TT - 1:TT]
                    c5, s5 = sp_[:, C5, j:j + 1], sp_[:, S5, j:j + 1]
                    rdp = [tSR[pbuf], tSI[pbuf], t_sp]
                    ns5 = sp_[:, U3, j:j + 1]
                    cx.op("act", lambda e: e.activation(out=ini[b][:, 2:3], in_=sie, func=AF.Identity, scale=ns5, bias=epsc[:, 2:3]),
                          reads=rdp + [t_const], writes=[t_ini[b]])
                    cx.op("act", lambda e: e.activation(out=ini[b][:, 0:1], in_=sre, func=AF.Identity, scale=c5, bias=ini[b][:, 2:3]),
                          reads=rdp + [t_ini[b]], writes=[t_ini[b]])
                    cx.op("act", lambda e: e.activation(out=ini[b][:, 3:4], in_=sie, func=AF.Identity, scale=c5, bias=epsc[:, 2:3]),
                          reads=rdp + [t_ini[b], t_const], writes=[t_ini[b]])
                    cx.op("act", lambda e: e.activation(out=ini[b][:, 1:2], in_=sre, func=AF.Identity, scale=s5, bias=ini[b][:, 3:4]),
                          reads=rdp + [t_ini[b]], writes=[t_ini[b]])
                    ini_r, ini_i, rd = ini[b][:, 0:1], ini[b][:, 1:2], [t_ini[b]]
                cx.op("dve", lambda e: e.tensor_tensor_scan(
                    out=SR[b][:], data0=magb, data1=XR[b][:], initial=ini_r, op0=ALU.mult, op1=ALU.add),
                    reads=[tXR[b], t_sp] + rd, writes=[tSR[b]])
                cx.op("dve", lambda e: e.tensor_tensor_scan(
                    out=SI[b][:], data0=magb, data1=XI[b][:], initial=ini_i, op0=ALU.mult, op1=ALU.add),
                    reads=[tXI[b], t_sp] + rd, writes=[tSI[b]])

            def stg4(p):
                b, tb = p.b, p.tb
                cx.op("pool", lambda e: e.tensor_tensor(out=P1[b][:], in0=CRt[tb][:], in1=SR[b][:], op=ALU.mult),
                      reads=[tCR[tb], tSR[b]], writes=[tP1[b]])
                cx.op("pool", lambda e: e.tensor_tensor(out=P2[b][:], in0=SNt[tb][:], in1=SI[b][:], op=ALU.mult),
                      reads=[tSN[tb], tSI[b]], writes=[tP2[b]])
                cx.op("pool", lambda e: e.tensor_tensor(out=P3[b][:], in0=SNt[tb][:], in1=SR[b][:], op=ALU.mult),
                      reads=[tSN[tb], tSR[b]], writes=[tP3[b]])
                cx.op("pool", lambda e: e.tensor_tensor(out=P4[b][:], in0=CRt[tb][:], in1=SI[b][:], op=ALU.mult),
                      reads=[tCR[tb], tSI[b]], writes=[tP4[b]])

            def stg5(p):
                b, pb, j, c, o, tsl, tt = p.b, p.pb, p.j, p.c, p.o, p.tsl, p.tt
                py = pb + 2
                for idx, (cm, pp, tp) in enumerate([(Cre, P1, tP1), (nCre, P2, tP2), (nCim, P3, tP3), (nCim, P4, tP4)]):
                    cx.op("pe", lambda e, cm=cm, pp=pp, idx=idx: e.matmul(
                        ps[py][:, :], lhsT=cm[:, j, :], rhs=pp[b][:], start=(idx == 0), stop=(idx == 3)),
                        reads=[t_C, tp[b]], writes=[pst[py]])
                cx.op("dve", lambda e: e.scalar_tensor_tensor(
                    out=ytmp[o:o + 32, tsl], in0=u_bf[o:o + 32, c, tsl], scalar=dsk[o:o + 32, c:c + 1],
                    in1=ps[py][o:o + 32, :], op0=ALU.mult, op1=ALU.add),
                    reads=[t_u[tt], t_iot, pst[py]], writes=[t_y[tt]])
                if p.jj == 3 and tt == NTT - 1:
                    for t2 in range(NTT):
                        ts2 = slice(t2 * TT, (t2 + 1) * TT)
                        cx.op("act", lambda e, ts2=ts2: e.activation(out=g1[:], in_=ytmp[:, ts2], func=AF.Square),
                              reads=[t_y[t2]], writes=[t_g1])
                        cx.op("dve", lambda e: e.tensor_scalar(out=g1[:], in0=g1[:], scalar1=0.044715, scalar2=1.0,
                                                               op0=ALU.mult, op1=ALU.add), reads=[t_g1], writes=[t_g1])
                        cx.op("dve", lambda e, ts2=ts2: e.tensor_tensor(out=g2[:], in0=g1[:], in1=ytmp[:, ts2], op=ALU.mult),
                              reads=[t_g1, t_y[t2]], writes=[t_g2])
                        cx.op("act", lambda e: e.activation(out=g2[:], in_=g2[:], func=AF.Sigmoid, scale=GELU_C),
                              reads=[t_g2], writes=[t_g2])
                        cx.op("dve", lambda e, ts2=ts2: e.tensor_tensor(out=gb[:, ts2], in0=g2[:], in1=ytmp[:, ts2], op=ALU.mult),
                              reads=[t_g2, t_y[t2]], writes=[t_gb[t2]])
                    cx.dma("sp", Gd[c], gb[:], reads=t_gb, writes=[t_gd])
                    chk("a3g%d" % c)

            t_gd = Trk()
            npc = len(pieces)
            for s_ in range(npc + 2):
                if s_ < npc:
                    stg1(pieces[s_])
                    stg2(pieces[s_])
                if 1 <= s_ <= npc:
                    stg3(pieces[s_ - 1])
                    stg4(pieces[s_ - 1])
                if s_ >= 2:
                    stg5(pieces[s_ - 2])
            cx.barrier()
    cx.barrier()

    with ExitStack() as st:
        wout = sb(st, "wout", [128, NCH, 2 * D], BF16)
        t_wout = Trk()
        wsrc = s5_w_out.rearrange("(c p) n -> p c n", p=128)
        for kc in range(NCH):
            cx.dma("pool", wout[:, kc, :], wsrc[:, kc, :], writes=[t_wout])
        gt = [sb(st, "gt%d" % i, [128, NCH, TT], BF16) for i in range(2)]
        t_gt = [Trk(), Trk()]
        hts = [sb(st, "hto%d" % i, [128, NCH, TT]) for i in range(2)]
        t_ht = [Trk(), Trk()]
        ho = [sb(st, "ho%d" % i, [128, NCH, TT]) for i in range(2)]
        t_ho = [Trk(), Trk()]
        sg = [sb(st, "sg%d" % i, [128, TT]) for i in range(2)]
        t_sg = [Trk(), Trk()]
        mx = [sb(st, "mx%d" % i, [128, TT]) for i in range(2)]
        t_mx = [Trk(), Trk()]
        it = 0
        for tt in range(NTT):
            b = tt % 2
            t0 = tt * TT
            cx.dma("sp", gt[b][:], Gd[:, :, t0:t0 + TT].rearrange("c p t -> p c t"), writes=[t_gt[b]])
            cx.dma("sp", hts[b][:], xT[:, :, t0:t0 + TT].rearrange("c p t -> p c t"), writes=[t_ht[b]])
            for n in range(NCH):
                bb = it % 2
                pv = 2 * (it % 4)
                pg = pv + 1
                it += 1
                for k in range(NCH):
                    cx.op("pe", lambda e, n=n, k=k, pv=pv, b=b: e.matmul(
                        ps[pv][:, :], lhsT=wout[:, k, n * 128:(n + 1) * 128], rhs=gt[b][:, k, :],
                        start=(k == 0), stop=(k == NCH - 1)), reads=[t_wout, t_gt[b]], writes=[pst[pv]])
                for k in range(NCH):
                    cx.op("pe", lambda e, n=n, k=k, pg=pg, b=b: e.matmul(
                        ps[pg][:, :], lhsT=wout[:, k, D + n * 128:D + (n + 1) * 128], rhs=gt[b][:, k, :],
                        start=(k == 0), stop=(k == NCH - 1)), reads=[t_wout, t_gt[b]], writes=[pst[pg]])
                cx.op("act", lambda e, bb=bb, pg=pg: e.activation(out=sg[bb][:], in_=ps[pg][:, :], func=AF.Sigmoid),
                      reads=[pst[pg]], writes=[t_sg[bb]])
                cx.op("dve", lambda e, bb=bb, pv=pv: e.tensor_tensor(out=mx[bb][:], in0=sg[bb][:], in1=ps[pv][:, :], op=ALU.mult),
                      reads=[t_sg[bb], pst[pv]], writes=[t_mx[bb]])
                cx.op("dve", lambda e, bb=bb, n=n, b=b: e.scalar_tensor_tensor(
                    out=ho[b][:, n, :], in0=mx[bb][:], scalar=gatec(0, n), in1=hts[b][:, n, :], op0=ALU.mult, op1=ALU.add),
                    reads=[t_mx[bb], t_mods, t_ht[b]], writes=[t_ho[b]])
            cx.dma("sp", h1[:, :, t0:t0 + TT].rearrange("c p t -> p c t"), ho[b][:], reads=[t_ho[b]])
        cx.barrier()

    chk("a5")

    BIG = 1.0e30
    HT = 2048

    def moe_stage(l, mi, hin, hdst):
        with ExitStack() as st:
            acc = sb(st, "acc", [128, NCH, HT])
            t_acc = [[Trk() for _ in range(NCH)] for _ in range(4)]
            hn_all = sb(st, "hn_all", [128, NCH, HT], BF16)
            t_hna = [Trk() for _ in range(4)]
            gatesT = sb(st, "gatesT", [32, HT])
            t_gT = [Trk() for _ in range(4)]
            selt = sb(st, "selt", [32, NE, 128])
            t_sel = Trk()
            cx.dma("sp", selt[:], sel_c[:, :, :], writes=[t_sel])
            wr = sb(st, "wr", [128, NCH, 36])
            brt = sb(st, "brt", [128, 36])
            t_wr = Trk()
            cx.dma("sp", wr[:], moe_wr[l].rearrange("(c p) n -> p c n", p=128), writes=[t_wr])
            cx.dma("sp", brt[:], moe_br[l], writes=[t_wr])
            for half in range(L // HT):
                hbase = half * HT
                with ExitStack() as st2:
                    hts_ = [sb(st2, "m_ht%d" % i, [128, NCH, TT]) for i in range(2)]
                    t_hts = [Trk(), Trk()]
                    sq = sb(st2, "m_sq", [128, NCH, TT], BF16)
                    t_sq = Trk()
                    rinv = sb(st2, "m_rinv", [128, TT])
                    t_rinv = Trk()
                    tmp = sb(st2, "m_tmp", [128, NCH, TT])
                    t_tmp = Trk()
                    hnf = sb(st2, "m_hnf", [128, NCH, TT])
                    t_hn = Trk()
                    rt = sb(st2, "m_rt", [128, 8])
                    rb_lg = sb(st2, "rb_lg", [128, 4, 36])
                    rb_s = sb(st2, "rb_s", [128, 8, 4])
                    rb_g = sb(st2, "rb_g", [128, 3, 4, 4])
                    rb_m = sb(st2, "rb_m", [128, 4, 4, 32])
                    t_rt = Trk()
                    LG, GM, GMASK, GE, GS, PEN, MK, M1, MASK1, MK2, M2, MASK2, ED, W1, W2, GATES = (
                        slice(0, 36), slice(36, 37), slice(40, 44), slice(44, 48), slice(48, 49), slice(52, 56),
                        slice(56, 88), slice(88, 89), slice(96, 128), slice(128, 160), slice(160, 161),
                        slice(168, 200), slice(200, 201), slice(201, 202), slice(202, 203), slice(208, 240))
                    for tl in range(HT // TT):
                        t0 = hbase + tl * TT
                        ht, t_ht = hts_[tl % 2], t_hts[tl % 2]
                        cx.dma("sp", ht[:], hin[:, :, t0:t0 + TT].rearrange("c p t -> p c t"), writes=[t_ht])
                        norm_mod(mi, ht, t_ht, sq, t_sq, rinv, t_rinv, tmp, t_tmp,
                                 hn_all[:, :, tl * TT:(tl + 1) * TT], t_hna[tl], 7, hn_f=hnf, t_hnf=t_hn)
                        for sc_ in range(4):
                            for k in range(NCH):
                                cx.op("pe", lambda e, k=k, sc_=sc_: e.matmul(
                                    ps[6][:, sc_ * 36:(sc_ + 1) * 36], lhsT=hnf[:, k, sc_ * 128:(sc_ + 1) * 128], rhs=wr[:, k, :],
                                    start=(k == 0), stop=(k == NCH - 1)), reads=[t_hn, t_wr], writes=[pst[6]])

                        def dv(fn, eng="dve", extra=()):
                            cx.op(eng, fn, reads=[t_rt] + list(extra), writes=[t_rt])

                        def bc(ap2, n):
                            return ap2.unsqueeze(2).to_broadcast([128, 4, n])
                        LGv = rb_lg[:]
                        LGg = rb_lg[:, :, 0:4]
                        LGe = rb_lg[:, :, 4:36]
                        dv(lambda e: e.tensor_tensor(out=LGv, in0=ps[6][:, 0:144].rearrange("p (s n) -> p s n", s=4),
                                                     in1=brt[:].unsqueeze(1).to_broadcast([128, 4, 36]), op=ALU.add),
                           extra=[pst[6], t_wr])
                        dv(lambda e: e.reduce_max(out=rb_s[:, 0, :], in_=LGg, axis=AX.X))
                        dv(lambda e: e.tensor_tensor(out=rb_g[:, 0, :, :], in0=LGg, in1=bc(rb_s[:, 0, :], 4), op=ALU.is_ge))
                        dv(lambda e: e.tensor_tensor(out=rb_g[:, 1, :, :], in0=LGg, in1=bc(rb_s[:, 0, :], 4), op=ALU.subtract))
                        dv(lambda e: e.activation(out=rb_g[:, 1, :, :], in_=rb_g[:, 1, :, :], func=AF.Exp), "act")
                        dv(lambda e: e.reduce_sum(out=rb_s[:, 1, :], in_=rb_g[:, 1, :, :], axis=AX.X))
                        dv(lambda e: e.reciprocal(out=rb_s[:, 1, :], in_=rb_s[:, 1, :]))
                        dv(lambda e: e.tensor_scalar(out=rb_g[:, 2, :, :], in0=rb_g[:, 0, :, :], scalar1=-1.0, scalar2=BIG,
                                                     op0=ALU.add, op1=ALU.mult))
                        for s4 in range(4):
                            dv(lambda e, s4=s4: e.tensor_tensor(
                                out=rb_m[:, 0, s4, :].rearrange("p (g e) -> p g e", g=4),
                                in0=rb_lg[:, s4, 4:36].rearrange("p (g e) -> p g e", g=4),
                                in1=rb_g[:, 2, s4, :].unsqueeze(2).to_broadcast([128, 4, 8]), op=ALU.add))
                        dv(lambda e: e.reduce_max(out=rb_s[:, 2, :], in_=rb_m[:, 0, :, :], axis=AX.X))
                        dv(lambda e: e.tensor_tensor(out=rb_m[:, 1, :, :], in0=rb_m[:, 0, :, :], in1=bc(rb_s[:, 2, :], 32), op=ALU.is_ge))
                        dv(lambda e: e.scalar_tensor_tensor(out=rb_m[:, 2, :, :], in0=rb_m[:, 1, :, :], scalar=-BIG, in1=rb_m[:, 0, :, :],
                                                            op0=ALU.mult, op1=ALU.add))
                        dv(lambda e: e.reduce_max(out=rb_s[:, 3, :], in_=rb_m[:, 2, :, :], axis=AX.X))
                        dv(lambda e: e.tensor_tensor(out=rb_m[:, 3, :, :], in0=rb_m[:, 2, :, :], in1=bc(rb_s[:, 3, :], 32), op=ALU.is_ge))
                        dv(lambda e: e.tensor_tensor(out=rb_s[:, 4, :], in0=rb_s[:, 3, :], in1=rb_s[:, 2, :], op=ALU.subtract))
                        dv(lambda e: e.activation(out=rb_s[:, 4, :], in_=rb_s[:, 4, :], func=AF.Exp), "act")
                        dv(lambda e: e.tensor_scalar(out=rb_s[:, 5, :], in0=rb_s[:, 4, :], scalar1=1.0, scalar2=None, op0=ALU.add))
                        dv(lambda e: e.reciprocal(out=rb_s[:, 5, :], in_=rb_s[:, 5, :]))
                        dv(lambda e: e.tensor_tensor(out=rb_s[:, 5, :], in0=rb_s[:, 5, :], in1=rb_s[:, 1, :], op=ALU.mult))
                        dv(lambda e: e.tensor_tensor(out=rb_s[:, 6, :], in0=rb_s[:, 5, :], in1=rb_s[:, 4, :], op=ALU.mult))
                        dv(lambda e: e.tensor_tensor(out=rb_m[:, 1, :, :], in0=rb_m[:, 1, :, :], in1=bc(rb_s[:, 5, :], 32), op=ALU.mult))
                        dv(lambda e: e.tensor_tensor(out=rb_m[:, 3, :, :], in0=rb_m[:, 3, :, :], in1=bc(rb_s[:, 6, :], 32), op=ALU.mult))
                        dv(lambda e: e.tensor_tensor(out=rb_m[:, 0, :, :], in0=rb_m[:, 1, :, :], in1=rb_m[:, 3, :, :], op=ALU.add))
                        for s4 in range(4):
                            cx.op("pe", lambda e, s4=s4: e.transpose(out=ps[5][0:32, s4 * 128:(s4 + 1) * 128], in_=rb_m[:, 0, s4, :],
                                                                     identity=identf[:]),
                                  reads=[t_rt, t_const], writes=[pst[5]])
                        c0 = tl * TT
                        cx.op("act", lambda e, c0=c0: e.copy(out=gatesT[:, c0:c0 + TT], in_=ps[5][0:32, :]),
                              reads=[pst[5]], writes=[t_gT[tl]])
                    cx.barrier()
                with ExitStack() as st2:
                    w1b = [sb(st2, "w1b%d" % i, [128, NCH, FE], BF16) for i in range(2)]
                    w3b = [sb(st2, "w3b%d" % i, [128, NCH, FE], BF16) for i in range(2)]
                    w2b = [sb(st2, "w2b%d" % i, [128, 2, D], BF16) for i in range(2)]
                    t_wb = [Trk(), Trk()]
                    sl_ = [sb(st2, "sl%d" % i, [128, 2, TT], BF16) for i in range(2)]
                    t_sl = [[Trk(), Trk()], [Trk(), Trk()]]
                    tl_ = [sb(st2, "tl%d" % i, [128, 2, TT], BF16) for i in range(2)]
                    t_tl = [[Trk(), Trk()], [Trk(), Trk()]]
                    gs_ = [sb(st2, "gs%d" % i, [128, TT], BF16) for i in range(2)]
                    t_gs = [Trk(), Trk()]
                    ab = [sb(st2, "ab%d" % i, [128, 2, TT], BF16) for i in range(2)]
                    t_ab = [Trk(), Trk()]
                    oi = [0]
                    munits = [(e_, tl) for e_ in range(NE) for tl in range(HT // TT)]

                    def emit_h(e_, tl, idx):
                        wb = e_ % 2
                        b = idx % 2
                        tsl = slice(tl * TT, (tl + 1) * TT)
                        for hc in range(2):
                            for k in range(NCH):
                                cx.op("pe", lambda e, k=k, hc=hc: e.matmul(
                                    ps[hc][:, :], lhsT=w1b[wb][:, k, hc * 128:(hc + 1) * 128], rhs=hn_all[:, k, tsl],
                                    start=(k == 0), stop=(k == NCH - 1)), reads=[t_wb[wb], t_hna[tl]], writes=[pst[hc]])
                        for hc in range(2):
                            for k in range(NCH):
                                cx.op("pe", lambda e, k=k, hc=hc: e.matmul(
                                    ps[2 + hc][:, :], lhsT=w3b[wb][:, k, hc * 128:(hc + 1) * 128], rhs=hn_all[:, k, tsl],
                                    start=(k == 0), stop=(k == NCH - 1)), reads=[t_wb[wb], t_hna[tl]], writes=[pst[2 + hc]])
                        cx.op("pe", lambda e: e.matmul(
                            ps[4][:, :], lhsT=selt[:, e_, :], rhs=gatesT[:, tsl], start=True, stop=True),
                            reads=[t_sel, t_gT[tl]], writes=[pst[4]])
                        cx.op("act", lambda e: e.copy(out=gs_[b][:], in_=ps[4][:, :]), reads=[pst[4]], writes=[t_gs[b]])
                        for hc in range(2):
                            cx.op("act", lambda e, hc=hc: e.activation(out=sl_[b][:, hc, :], in_=ps[hc][:, :], func=AF.Silu),
                                  reads=[pst[hc]], writes=[t_sl[b][hc]])
                            cx.op("act", lambda e, hc=hc: e.copy(out=tl_[b][:, hc, :], in_=ps[2 + hc][:, :]),
                                  reads=[pst[2 + hc]], writes=[t_tl[b][hc]])
                        for hc in range(2):
                            cx.op("pool", lambda e, hc=hc: e.tensor_tensor(out=sl_[b][:, hc, :], in0=sl_[b][:, hc, :],
                                                                        in1=tl_[b][:, hc, :], op=ALU.mult),
                                  reads=[t_tl[b][hc]], writes=[t_sl[b][hc]])
                            cx.op("pool", lambda e, hc=hc: e.tensor_tensor(out=ab[b][:, hc, :], in0=sl_[b][:, hc, :],
                                                                        in1=gs_[b][:], op=ALU.mult),
                                  reads=[t_sl[b][hc], t_gs[b]], writes=[t_ab[b]])

                    def emit_o(e_, tl, idx):
                        wb = e_ % 2
                        b = idx % 2
                        tsl = slice(tl * TT, (tl + 1) * TT)
                        for n in range(NCH):
                            po = 5 + (oi[0] % 3)
                            oi[0] += 1
                            for hc in range(2):
                                cx.op("pe", lambda e, n=n, hc=hc, po=po: e.matmul(
                                    ps[po][:, :], lhsT=w2b[wb][:, hc, n * 128:(n + 1) * 128], rhs=ab[b][:, hc, :],
                                    start=(hc == 0), stop=(hc == 1)), reads=[t_wb[wb], t_ab[b]], writes=[pst[po]])
                            if e_ == 0:
                                cx.op("dve", lambda e, n=n, po=po: e.tensor_copy(out=acc[:, n, tsl], in_=ps[po][:, :]),
                                      reads=[pst[po]], writes=[t_acc[tl][n]])
                            else:
                                cx.op("dve", lambda e, n=n, po=po: e.tensor_tensor(
                                    out=acc[:, n, tsl], in0=acc[:, n, tsl], in1=ps[po][:, :], op=ALU.add),
                                    reads=[pst[po], t_acc[tl][n]], writes=[t_acc[tl][n]])

                    def load_w(e_):
                        wb = e_ % 2
                        cx.dma("pool", w1b[wb][:], moe_w1[l, e_].rearrange("(c p) f -> p c f", p=128), writes=[t_wb[wb]])
                        cx.dma("pool", w3b[wb][:], moe_w3[l, e_].rearrange("(c p) f -> p c f", p=128), writes=[t_wb[wb]])
                        cx.dma("pool", w2b[wb][:], moe_w2[l, e_].rearrange("(c p) f -> p c f", p=128), writes=[t_wb[wb]])

                    load_w(0)
                    for idx in range(len(munits) + 1):
                        if idx < len(munits):
                            emit_h(munits[idx][0], munits[idx][1], idx)
                        if idx >= 1:
                            emit_o(munits[idx - 1][0], munits[idx - 1][1], idx - 1)
                        if idx < len(munits) and munits[idx][1] == 0 and munits[idx][0] + 1 < NE:
                            load_w(munits[idx][0] + 1)
                    cx.barrier()
                with ExitStack() as st2:
                    hts = [sb(st2, "m3h%d" % i, [128, NCH, TT]) for i in range(2)]
                    t_h3 = [Trk(), Trk()]
                    ho = [sb(st2, "m3o%d" % i, [128, NCH, TT]) for i in range(2)]
                    t_o3 = [Trk(), Trk()]
                    for tl in range(HT // TT):
                        b = tl % 2
                        t0 = hbase + tl * TT
                        tsl = slice(tl * TT, (tl + 1) * TT)
                        cx.dma("sp", hts[b][:], hin[:, :, t0:t0 + TT].rearrange("c p t -> p c t"), writes=[t_h3[b]])
                        for n in range(NCH):
                            cx.op("dve", lambda e, n=n, b=b, tsl=tsl: e.scalar_tensor_tensor(
                                out=ho[b][:, n, :], in0=acc[:, n, tsl], scalar=gatec(mi, n), in1=hts[b][:, n, :],
                                op0=ALU.mult, op1=ALU.add), reads=[t_acc[tl][n], t_mods, t_h3[b]], writes=[t_o3[b]])
                        cx.dma("sp", hdst[:, :, t0:t0 + TT].rearrange("c p t -> p c t"), ho[b][:], reads=[t_o3[b]])
                    cx.barrier()
            cx.barrier()

    moe_stage(0, 1, h1, h2)
    chk("m0")

    blkf = sb(es, "blkf", [128, 128])
    cx.dma("sp", blkf[:], blk_c[:, :], writes=[t_const])

    def head_norm(psi, ps2i, gcol, out_ap, sqk, t_sqk, rk, t_rk):
        cx.op("act", lambda e: e.activation(out=sqk[:], in_=ps[psi][:, :], func=AF.Square), reads=[pst[psi]], writes=[t_sqk])
        cx.op("pe", lambda e: e.matmul(ps[ps2i][:, :], lhsT=blkf[:], rhs=sqk[:], start=True, stop=True),
              reads=[t_sqk, t_const], writes=[pst[ps2i]])
        cx.op("act", lambda e: e.activation(out=rk[:], in_=ps[ps2i][:, :], func=AF.Sqrt, bias=epsc[:, 0:1], scale=1.0 / HD),
              reads=[pst[ps2i], t_const], writes=[t_rk])
        cx.op("dve", lambda e: e.reciprocal(out=rk[:], in_=rk[:]), reads=[t_rk], writes=[t_rk])
        return lambda wr_t: cx.op("dve", lambda e: e.scalar_tensor_tensor(
            out=out_ap, in0=ps[psi][:, :], scalar=gcol, in1=rk[:], op0=ALU.mult, op1=ALU.mult),
            reads=[pst[psi], t_rk, t_const], writes=[wr_t])

    def kv_stage():
        with ExitStack() as st:
            kvw = sb(st, "kvw", [128, NCH, 2 * D], BF16)
            t_kvw = Trk()
            ksrc = kv_w.rearrange("(c p) n -> p c n", p=128)
            for kc in range(NCH):
                cx.dma("pool", kvw[:, kc, :], ksrc[:, kc, 0:2 * D], writes=[t_kvw])
            fw = sb(st, "fw", [128, NCH, H])
            for kc in range(NCH):
                cx.dma("sp", fw[:, kc, :], ksrc[:, kc, 2 * D:2 * D + H], writes=[t_kvw])
            gk = sb(st, "gk", [128, 1])
            nfb = sb(st, "nfb", [H, 1])
            ones512 = sb(st, "ones512", [H, TT])
            cx.dma("sp", gk[:], gk_col[:, :], writes=[t_const])
            cx.dma("sp", nfb[:], fb_col[:, :], writes=[t_const])
            cx.op("dve", lambda e: e.tensor_scalar(out=nfb[:], in0=nfb[:], scalar1=-1.0, scalar2=None, op0=ALU.mult),
                  reads=[t_const], writes=[t_const])
            cx.op("dve", lambda e: e.memset(ones512[:], 1.0), writes=[t_const])
            Ft = sb(st, "Ft", [H, L])
            t_F = [Trk() for _ in range(NTT)]
            with ExitStack() as st2:
                hts = [sb(st2, "k_ht%d" % i, [128, NCH, TT]) for i in range(2)]
                t_ht = [Trk(), Trk()]
                sq = sb(st2, "k_sq", [128, NCH, TT], BF16)
                t_sq = Trk()
                rinv = sb(st2, "k_rinv", [128, TT])
                t_rinv = Trk()
                tmp = sb(st2, "k_tmp", [128, NCH, TT])
                t_tmp = Trk()
                hnf = sb(st2, "k_hnf", [128, NCH, TT])
                t_hnf = Trk()
                hnb = sb(st2, "k_hnb", [128, NCH, TT], BF16)
                t_hnb = Trk()
                kt = [sb(st2, "k_kt%d" % i, [128, NCH, TT], BF16) for i in range(2)]
                t_kt = [Trk(), Trk()]
                vt = [sb(st2, "k_vt%d" % i, [128, 4, D], BF16) for i in range(2)]
                t_vt = [Trk(), Trk()]
                sqk = sb(st2, "k_sqk", [128, TT])
                t_sqk = Trk()
                rk = sb(st2, "k_rk", [128, TT])
                t_rk = Trk()
                ef = sb(st2, "k_ef", [H, TT])
                t_ef = Trk()
                for tt in range(NTT):
                    b = tt % 2
                    t0 = tt * TT
                    cx.dma("sp", hts[b][:], h2[:, :, t0:t0 + TT].rearrange("c p t -> p c t"), writes=[t_ht[b]])
                    norm_mod(4, hts[b], t_ht[b], sq, t_sq, rinv, t_rinv, tmp, t_tmp, hnb, t_hnb, 0, hn_f=hnf, t_hnf=t_hnf)
                    for n in range(NCH):
                        pi = 1 + (n % 2)
                        for k in range(NCH):
                            cx.op("pe", lambda e, n=n, k=k, pi=pi: e.matmul(
                                ps[pi][:, :], lhsT=kvw[:, k, n * 128:(n + 1) * 128], rhs=hnb[:, k, :],
                                start=(k == 0), stop=(k == NCH - 1)), reads=[t_kvw, t_hnb], writes=[pst[pi]])
                        fin = head_norm(pi, 3, gk[:, 0:1], kt[b][:, n, :], sqk, t_sqk, rk, t_rk)
                        fin(t_kt[b])
                    cx.dma("sp", Kd[:, :, t0:t0 + TT].rearrange("c p t -> p c t"), kt[b][:], reads=[t_kt[b]])
                    for s_ in range(4):
                        for hf in range(2):
                            pi = 4 + ((s_ * 2 + hf) % 2)
                            for k in range(NCH):
                                cx.op("pe", lambda e, s_=s_, hf=hf, k=k, pi=pi: e.matmul(
                                    ps[pi][:, :], lhsT=hnb[:, k, s_ * 128:(s_ + 1) * 128],
                                    rhs=kvw[:, k, D + hf * 512:D + (hf + 1) * 512],
                                    start=(k == 0), stop=(k == NCH - 1)), reads=[t_kvw, t_hnb], writes=[pst[pi]])
                            if hf == 0:
                                cx.op("act", lambda e, s_=s_, hf=hf, pi=pi, b=b: e.copy(out=vt[b][:, s_, hf * 512:(hf + 1) * 512], in_=ps[pi][:, :]),
                                      reads=[pst[pi]], writes=[t_vt[b]])
                            else:
                                cx.op("dve", lambda e, s_=s_, hf=hf, pi=pi, b=b: e.tensor_copy(out=vt[b][:, s_, hf * 512:(hf + 1) * 512], in_=ps[pi][:, :]),
                                      reads=[pst[pi]], writes=[t_vt[b]])
                    cx.dma("sp", Vd[t0:t0 + TT, :].rearrange("(s p) d -> p s d", p=128), vt[b][:], reads=[t_vt[b]])
                    for k in range(NCH):
                        cx.op("pe", lambda e, k=k: e.matmul(ps[6][0:H, :], lhsT=fw[:, k, :], rhs=hnf[:, k, :],
                                                            start=(k == 0), stop=(k == NCH - 1)),
                              reads=[t_kvw, t_hnf], writes=[pst[6]])
                    cx.op("act", lambda e: e.activation(out=ef[:], in_=ps[6][0:H, :], func=AF.Exp, bias=nfb[:, 0:1], scale=-1.0),
                          reads=[pst[6], t_const], writes=[t_ef])
                    cx.op("act", lambda e: e.activation(out=ef[:], in_=ef[:], func=AF.Ln, bias=ones512[:, 0:1], scale=1.0),
                          reads=[t_ef, t_const], writes=[t_ef])
                    if tt == 0:
                        ini, rd = epsc[0:H, 2:3], [t_const]
                    else:
                        ini, rd = Ft[:, t0 - 1:t0], [t_F[tt - 1]]
                    cx.op("dve", lambda e, t0=t0, ini=ini: e.tensor_tensor_scan(
                        out=Ft[:, t0:t0 + TT], data0=ones512[:], data1=ef[:], initial=ini, op0=ALU.mult, op1=ALU.subtract),
                        reads=[t_ef, t_const] + rd, writes=[t_F[tt]])
                cx.barrier()
            with ExitStack() as st2:
                X = sb(st2, "f_X", [H, L])
                q3 = sb(st2, "f_q3", [H, 3, L], BF16)
                k3 = sb(st2, "f_k3", [H, 3, L], BF16)
                t_x = Trk()
                cx.op("dve", lambda e: e.tensor_scalar(out=X[:], in0=Ft[:], scalar1=8.0, scalar2=None, op0=ALU.mult),
                      reads=t_F, writes=[t_x])
                for i in range(3):
                    cx.op("dve", lambda e, i=i: e.tensor_copy(out=q3[:, i, :], in_=X[:]), reads=[t_x], writes=[t_x])
                    cx.op("dve", lambda e, i=i: e.tensor_scalar(out=k3[:, i, :], in0=q3[:, i, :], scalar1=-1.0, scalar2=None, op0=ALU.mult),
                          reads=[t_x], writes=[t_x])
                    if i < 2:
                        cx.op("dve", lambda e, i=i: e.tensor_tensor(out=X[:], in0=X[:], in1=q3[:, i, :], op=ALU.subtract),
                              reads=[t_x], writes=[t_x])
                cx.dma("sp", Fq[:, :, :], q3[:], reads=[t_x])
                cx.dma("sp", Fk[:, :, :], k3[:], reads=[t_x])
                cx.barrier()
            cx.barrier()

    def attn_stage():
        mi = 2
        with ExitStack() as st:
            wqg = sb(st, "wqg", [128, NCH, 2 * D], BF16)
            t_w = Trk()
            wsrc = fox_w_qg.rearrange("(c p) n -> p c n", p=128)
            for kc in range(NCH):
                cx.dma("pool", wqg[:, kc, :], wsrc[:, kc, :], writes=[t_w])
            gq = sb(st, "gq", [128, 1])
            cx.dma("sp", gq[:], gq_col[:, :], writes=[t_const])
            hts = [sb(st, "q_ht%d" % i, [128, NCH, TT]) for i in range(2)]
            t_ht = [Trk(), Trk()]
            sq = sb(st, "q_sq", [128, NCH, TT], BF16)
            t_sq = Trk()
            rinv = sb(st, "q_rinv", [128, TT])
            t_rinv = Trk()
            tmp = sb(st, "q_tmp", [128, NCH, TT])
            t_tmp = Trk()
            hnb = sb(st, "q_hnb", [128, NCH, TT], BF16)
            t_hnb = Trk()
            qt = [sb(st, "q_qt%d" % i, [128, NCH, TT], BF16) for i in range(2)]
            t_qt = [Trk(), Trk()]
            sgt = [sb(st, "q_sg%d" % i, [128, NCH, TT], BF16) for i in range(2)]
            t_sgt = [Trk(), Trk()]
            sqk = sb(st, "q_sqk", [128, TT])
            t_sqk = Trk()
            rk = sb(st, "q_rk", [128, TT])
            t_rk = Trk()
            for tt in range(NTT):
                b = tt % 2
                t0 = tt * TT
                cx.dma("sp", hts[b][:], h2[:, :, t0:t0 + TT].rearrange("c p t -> p c t"), writes=[t_ht[b]])
                norm_mod(mi, hts[b], t_ht[b], sq, t_sq, rinv, t_rinv, tmp, t_tmp, hnb, t_hnb, 0)
                for n in range(NCH):
                    pi = 1 + (n % 2)
                    for k in range(NCH):
                        cx.op("pe", lambda e, n=n, k=k, pi=pi: e.matmul(
                            ps[pi][:, :], lhsT=wqg[:, k, n * 128:(n + 1) * 128], rhs=hnb[:, k, :],
                            start=(k == 0), stop=(k == NCH - 1)), reads=[t_w, t_hnb], writes=[pst[pi]])
                    fin = head_norm(pi, 3, gq[:, 0:1], qt[b][:, n, :], sqk, t_sqk, rk, t_rk)
                    fin(t_qt[b])
                    pg = 4 + (n % 2)
                    for k in range(NCH):
                        cx.op("pe", lambda e, n=n, k=k, pg=pg: e.matmul(
                            ps[pg][:, :], lhsT=wqg[:, k, D + n * 128:D + (n + 1) * 128], rhs=hnb[:, k, :],
                            start=(k == 0), stop=(k == NCH - 1)), reads=[t_w, t_hnb], writes=[pst[pg]])
                    cx.op("act", lambda e, n=n, pg=pg, b=b: e.activation(out=sgt[b][:, n, :], in_=ps[pg][:, :], func=AF.Sigmoid),
                          reads=[pst[pg]], writes=[t_sgt[b]])
                cx.dma("sp", Qd[:, :, t0:t0 + TT].rearrange("c p t -> p c t"), qt[b][:], reads=[t_qt[b]])
                cx.dma("sp", SGd[:, :, t0:t0 + TT].rearrange("c p t -> p c t"), sgt[b][:], reads=[t_sgt[b]])
            cx.barrier()
        with ExitStack() as st:
            tri = sb(st, "tri", [128, 128], BF16)
            onesb = sb(st, "onesb", [128, 64], BF16)
            t_tri = Trk()
            cx.dma("pool", tri[:], tri_c[:, :], writes=[t_tri])
            identb = sb(st, "identb", [128, 128], BF16)
            cx.dma("pool", identb[:], ident[:, :], writes=[t_tri])
            cx.op("dve", lambda e: e.memset(onesb[:], 1.0), writes=[t_tri])
            Ka = [sb(st, "Ka%d" % i, [128, L], BF16) for i in range(2)]
            Qa = [sb(st, "Qa%d" % i, [128, L], BF16) for i in range(2)]
            Vh = [sb(st, "Vh%d" % i, [128, L // 128, 128], BF16) for i in range(2)]
            SGh = [sb(st, "SGh%d" % i, [64, L], BF16) for i in range(2)]
            t_hd = [Trk(), Trk()]
            for i in range(2):
                cx.op("dve", lambda e, i=i: e.memset(Ka[i][64:128, :], 1.0), writes=[t_hd[i]])
                cx.op("dve", lambda e, i=i: e.memset(Qa[i][64:128, :], 0.0), writes=[t_hd[i]])
                cx.op("dve", lambda e, i=i: e.memset(Vh[i][:, :, HD:128], 0.0), writes=[t_hd[i]])
                cx.op("dve", lambda e, i=i: e.memset(Vh[i][:, :, HD:HD + 1], 1.0), writes=[t_hd[i]])
                cx.dma("sp", Qa[i][67:70, :], Ka[i][96:99, :], reads=[t_hd[i]], writes=[t_hd[i]])
            NP = 4
            SB3 = [0, 1, 6]
            pt = [sb(st, "pt%d" % i, [128, TT], BF16) for i in range(NP)]
            t_pt = [Trk() for _ in range(NP)]
            rr = sb(st, "rr", [128, TT])
            t_rr = Trk()
            rb = sb(st, "rb", [64, TT])
            t_rb = Trk()
            ot = sb(st, "ot", [64, TT])
            t_ot = Trk()
            ob = [sb(st, "ob%d" % i, [64, TT], BF16) for i in range(2)]
            t_ob = [Trk(), Trk()]

            def load_head(h):
                hb = h % 2
                c, ro = h // 2, (h % 2) * 64
                cx.dma("sp", Ka[hb][0:64, :], Kd[c, ro:ro + 64, :], writes=[t_hd[hb]])
                cx.dma("sp", Ka[hb][67:70, :], Fk[h], writes=[t_hd[hb]])
                cx.dma("sp", Qa[hb][0:64, :], Qd[c, ro:ro + 64, :], writes=[t_hd[hb]])
                cx.dma("sp", Qa[hb][64:67, :], Fq[h], writes=[t_hd[hb]])
                cx.dma("sp", Vh[hb][:, :, 0:HD], Vd[:, h * HD:(h + 1) * HD].rearrange("(b p) d -> p b d", p=128),
                       writes=[t_hd[hb]])
                cx.dma("sp", SGh[hb][:], SGd[c, ro:ro + 64, :], writes=[t_hd[hb]])

            units = []
            ui = 0
            for h in range(H):
                for qc in range(NTT):
                    for kb in range(4 * qc + 4):
                        units.append((h, qc, kb, ui))
                    ui += 1

            def emit_s(u, idx):
                h, qc, kb, ui = u
                hb = h % 2
                i = kb - 4 * qc
                cs = max(0, i) * 128
                sbank = SB3[idx % 3]
                pb = idx % NP
                cx.op("pe", lambda e: e.matmul(
                    ps[sbank][:, cs:TT], lhsT=Ka[hb][:, kb * 128:(kb + 1) * 128],
                    rhs=Qa[hb][:, qc * TT + cs:(qc + 1) * TT], start=True, stop=(i < 0)),
                    reads=[t_hd[hb]], writes=[pst[sbank]])
                if i >= 0:
                    cx.op("pe", lambda e: e.matmul(
                        ps[sbank][:, cs:cs + 128], lhsT=identb[:], rhs=tri[:], start=False, stop=True),
                        reads=[t_tri], writes=[pst[sbank]])
                cx.op("act", lambda e: e.activation(
                    out=pt[pb][:, cs:TT], in_=ps[sbank][:, cs:TT], func=AF.Exp, scale=0.125),
                    reads=[pst[sbank]], writes=[t_pt[pb]])

            def emit_pv(u, idx):
                h, qc, kb, ui = u
                hb = h % 2
                c, ro = h // 2, (h % 2) * 64
                i = kb - 4 * qc
                cs = max(0, i) * 128
                pb = idx % NP
                po = 2 + (ui % 2)
                pbb = 4 + (ui % 2)
                ub = ui % 2
                nkb = 4 * qc + 4
                cx.op("pe", lambda e: e.matmul(
                    ps[po][:, cs:TT], lhsT=Vh[hb][:, kb, :], rhs=pt[pb][:, cs:TT],
                    start=(kb == 0), stop=(kb == nkb - 1)), reads=[t_hd[hb], t_pt[pb]], writes=[pst[po]])
                if kb == nkb - 1:
                    cx.op("dve", lambda e: e.reciprocal(out=rr[64:65, :], in_=ps[po][64:65, :]), reads=[pst[po]], writes=[t_rr])
                    cx.op("pe", lambda e: e.matmul(ps[pbb][0:64, :], lhsT=onesf[64:65, 0:64], rhs=rr[64:65, :], start=True, stop=True),
                          reads=[t_rr, t_const], writes=[pst[pbb]])
                    cx.op("act", lambda e: e.copy(out=rb[:], in_=ps[pbb][0:64, :]), reads=[pst[pbb]], writes=[t_rb])
                    cx.op("dve", lambda e: e.tensor_tensor(out=ot[:], in0=rb[:], in1=ps[po][0:64, :], op=ALU.mult),
                          reads=[t_rb, pst[po]], writes=[t_ot])
                    cx.op("dve", lambda e: e.tensor_tensor(
                        out=ob[ub][:], in0=ot[:], in1=SGh[hb][:, qc * TT:(qc + 1) * TT], op=ALU.mult),
                        reads=[t_ot, t_hd[hb]], writes=[t_ob[ub]])
                    cx.dma("sp", Od[c, ro:ro + 64, qc * TT:(qc + 1) * TT], ob[ub][:], reads=[t_ob[ub]])

            load_head(0)
            LAG = 2
            for idx in range(len(units) + LAG):
                if idx < len(units):
                    emit_s(units[idx], idx)
                if idx >= LAG:
                    emit_pv(units[idx - LAG], idx - LAG)
                if idx < len(units):
                    u = units[idx]
                    if u[1] == 0 and u[2] == LAG - 1 and u[0] + 1 < H:
                        load_head(u[0] + 1)
            cx.barrier()
        with ExitStack() as st:
            wo = sb(st, "wo", [128, NCH, D], BF16)
            t_w = Trk()
            wsrc = fox_w_o.rearrange("(c p) n -> p c n", p=128)
            for kc in range(NCH):
                cx.dma("pool", wo[:, kc, :], wsrc[:, kc, :], writes=[t_w])
            otl = [sb(st, "o_ot%d" % i, [128, NCH, TT], BF16) for i in range(2)]
            t_otl = [Trk(), Trk()]
            hts = [sb(st, "o_ht%d" % i, [128, NCH, TT]) for i in range(2)]
            t_ht = [Trk(), Trk()]
            ho = [sb(st, "o_ho%d" % i, [128, NCH, TT]) for i in range(2)]
            t_ho = [Trk(), Trk()]
            it = 0
            for tt in range(NTT):
                b = tt % 2
                t0 = tt * TT
                cx.dma("sp", otl[b][:], Od[:, :, t0:t0 + TT].rearrange("c p t -> p c t"), writes=[t_otl[b]])
                cx.dma("sp", hts[b][:], h2[:, :, t0:t0 + TT].rearrange("c p t -> p c t"), writes=[t_ht[b]])
                for n in range(NCH):
                    pv = it % 4
                    it += 1
                    for k in range(NCH):
                        cx.op("pe", lambda e, n=n, k=k, pv=pv, b=b: e.matmul(
                            ps[pv][:, :], lhsT=wo[:, k, n * 128:(n + 1) * 128], rhs=otl[b][:, k, :],
                            start=(k == 0), stop=(k == NCH - 1)), reads=[t_w, t_otl[b]], writes=[pst[pv]])
                    cx.op("dve", lambda e, n=n, pv=pv, b=b: e.scalar_tensor_tensor(
                        out=ho[b][:, n, :], in0=ps[pv][:, :], scalar=gatec(mi, n), in1=hts[b][:, n, :],
                        op0=ALU.mult, op1=ALU.add), reads=[pst[pv], t_mods, t_ht[b]], writes=[t_ho[b]])
                cx.dma("sp", h3[:, :, t0:t0 + TT].rearrange("c p t -> p c t"), ho[b][:], reads=[t_ho[b]])
            cx.barrier()

    kv_stage()
    chk("kv")
    attn_stage()
    chk("at")
    moe_stage(1, 3, h3, hout)
    cx.barrier()
    return nc


def _state_layout(a):
    return np.ascontiguousarray(a.reshape(32, 2, 64).transpose(1, 2, 0).reshape(128, 32))


def _col_layout(v):
    return np.ascontiguousarray(v.reshape(-1, 128).T)


def make_inputs(inputs, b):
    f = np.float32
    m = {}
    m["xT"] = np.ascontiguousarray(inputs["x"][b].T).reshape(NCH, 128, L)
    m["c_col"] = _col_layout(inputs["c"][b])
    return m


def make_shared(inputs):
    f = np.float32
    m = {}
    m["ada_w"] = np.ascontiguousarray(inputs["ada_w"], dtype=f)
    m["ada_b"] = np.stack([_col_layout(inputs["ada_b"][i // 2, i % 2]) for i in range(4)])
    m["ln_g"] = np.stack([_col_layout(inputs["ln_g"][i // 2, i % 2]) for i in range(4)])
    m["kv_ada_w"] = np.ascontiguousarray(inputs["kv_ada_w"], dtype=f)
    m["kv_ada_b"] = _col_layout(inputs["kv_ada_b"])
    m["kv_g"] = _col_layout(inputs["kv_g"])
    m["s5_w_in"] = np.ascontiguousarray(inputs["s5_w_in"][0])
    m["s5_w_out"] = np.ascontiguousarray(inputs["s5_w_out"][0])
    ldt = np.repeat(inputs["s5_log_dt"][0][:, None], 64, axis=1)
    m["s5_par"] = np.stack([_state_layout(inputs["s5_lambda_re"][0]), _state_layout(inputs["s5_lambda_im"][0]),
                            _state_layout(ldt)])
    bp = np.zeros((2, 128, 32, 128), f)
    cp = np.zeros((2, 128, 32, 128), f)
    for k, (bn, cn) in enumerate([("s5_b_re", "s5_c_re"), ("s5_b_im", "s5_c_im")]):
        B_ = inputs[bn][0]
        C_ = inputs[cn][0]
        for j in range(32):
            o = (j % 4) * 32
            for gl in range(2):
                g = 2 * j + gl
                bp[k, gl * 64:(gl + 1) * 64, j, o + gl * 16:o + gl * 16 + 16] = B_[g]
                cp[k, gl * 64:(gl + 1) * 64, j, o + gl * 16:o + gl * 16 + 16] = C_[g].T
    m["s5_bpad"] = bp
    m["s5_cpad"] = cp
    m["s5_d"] = _col_layout(inputs["s5_d"][0])
    m["iota_t"] = np.ascontiguousarray(np.broadcast_to(np.arange(L, dtype=f)[None, :], (128, L)))
    m["ident"] = np.eye(128, dtype=f)
    m["moe_w1"] = np.ascontiguousarray(inputs["moe_w1"], dtype=f)
    m["moe_w3"] = np.ascontiguousarray(inputs["moe_w3"], dtype=f)
    m["moe_w2"] = np.ascontiguousarray(inputs["moe_w2"], dtype=f)
    m["moe_wr"] = np.ascontiguousarray(np.concatenate([inputs["moe_wg"], inputs["moe_we"]], axis=2), dtype=f)
    br = np.concatenate([inputs["moe_bg"], inputs["moe_be"]], axis=1).astype(f)
    m["moe_br"] = np.ascontiguousarray(np.broadcast_to(br[:, None, :], (2, 128, 36)))
    m["kv_w"] = np.ascontiguousarray(inputs["kv_w"], dtype=f)
    m["gk_col"] = np.ascontiguousarray(np.tile(inputs["k_norm_g"], 2)[:, None], dtype=f)
    m["gq_col"] = np.ascontiguousarray(np.tile(inputs["fox_q_norm_g"][0], 2)[:, None], dtype=f)
    m["fb_col"] = np.ascontiguousarray(inputs["kv_fb"][:, None], dtype=f)
    blk = np.zeros((128, 128), f)
    blk[0:64, 0:64] = 1.0
    blk[64:128, 64:128] = 1.0
    m["blk_c"] = blk
    m["tri_c"] = np.ascontiguousarray(np.tril(np.full((128, 128), -1.0e8, f), -1))
    m["fox_w_qg"] = np.ascontiguousarray(inputs["fox_w_qg"][0], dtype=f)
    m["fox_w_o"] = np.ascontiguousarray(inputs["fox_w_o"][0], dtype=f)
    sel = np.zeros((32, NE, 128), f)
    for e_ in range(NE):
        sel[e_, e_, :] = 1.0
    m["sel_c"] = sel
    return m


_NC_CACHE = {}


def kernel(**inputs):
    inputs = {k: np.asarray(v) for k, v in inputs.items()}
    if "nc" not in _NC_CACHE:
        _NC_CACHE["nc"] = build()
    nc = _NC_CACHE["nc"]
    shared = make_shared(inputs)
    in_maps = []
    for b in range(8):
        m = dict(shared)
        m.update(make_inputs(inputs, b))
        in_maps.append(m)
    res = run_bass_kernel_spmd(nc, in_maps, core_ids=list(range(8)))
    out = np.stack([np.ascontiguousarray(r["hout"].reshape(D, L).T) for r in res.results])
    return out.astype(np.float32)
```
